# Optimizing a Trainium2 kernel written in Bass

```python
import jax, jax.numpy as jnp
from jax import lax
import numpy as np

D_MODEL = 1024
BATCH = 8
SEQ = 2048
DEPTH = 2

CHUNK = 64
Q_BLOCK = 128
ROPE_THETA = 10000.0
LN_EPS = 1e-5
RMS_EPS = 1e-6
NEG = -1e30

A_HEADS = 8
A_NOPE = 64
A_ROPE = 32
A_VDIM = 64
A_Q_RANK = 384
A_KV_RANK = 256

B_HEADS = 8
B_KV_HEADS = 2
B_HDIM = 64
IDX_HEADS = 4
IDX_DIM = 64
TOPK_MAX = 256

C_HEADS = 8
C_KV_HEADS = 2
C_HDIM = 64
WINDOW = 128
WIN_CHUNKS = WINDOW // CHUNK

N_BRANCH = 3
BRANCH_WIDTH = 512

N_EXPERTS = 16
N_GROUPS = 4
EXPERTS_PER_GROUP = N_EXPERTS // N_GROUPS
TOP_K = 2
D_EXPERT = 512

ALPHA = (2.0 * DEPTH) ** 0.25
BETA = (8.0 * DEPTH) ** -0.25

SPLIT_SIZES = (
    A_Q_RANK, A_KV_RANK, A_ROPE,
    B_HEADS * B_HDIM, B_KV_HEADS * B_HDIM, B_KV_HEADS * B_HDIM,
    IDX_HEADS * IDX_DIM, IDX_DIM, IDX_HEADS,
    C_HEADS * C_HDIM, C_KV_HEADS * C_HDIM, C_KV_HEADS * C_HDIM,
    N_BRANCH * D_MODEL,
)
IN_COLS = sum(SPLIT_SIZES)

kernel_name = "hybrid_mla_dsa_swa_grouped_moe_deepnorm"


def layer_norm(x, g, b):
    xf = x.astype(jnp.float32)
    mu = jnp.mean(xf, axis=-1, keepdims=True)
    var = jnp.mean(jnp.square(xf - mu), axis=-1, keepdims=True)
    return ((xf - mu) * lax.rsqrt(var + LN_EPS) * g + b).astype(x.dtype)


def rms_norm(x, g):
    xf = x.astype(jnp.float32)
    return (xf * lax.rsqrt(jnp.mean(jnp.square(xf), axis=-1, keepdims=True) + RMS_EPS) * g).astype(x.dtype)


def rope(x, pos):
    half = x.shape[-1] // 2
    inv = ROPE_THETA ** (-jnp.arange(half, dtype=jnp.float32) / half)
    ang = pos.astype(jnp.float32)[:, None] * inv[None, :]
    cos = jnp.cos(ang)[:, None, :].astype(x.dtype)
    sin = jnp.sin(ang)[:, None, :].astype(x.dtype)
    x1, x2 = x[..., :half], x[..., half:]
    return jnp.concatenate([x1 * cos - x2 * sin, x2 * cos + x1 * sin], axis=-1)


def to_blocks(a):
    b, t = a.shape[:2]
    return jnp.moveaxis(a.reshape((b, t // Q_BLOCK, Q_BLOCK) + a.shape[2:]), 1, 0)


def from_blocks(a):
    nb, b, qb = a.shape[:3]
    return jnp.moveaxis(a, 0, 1).reshape((b, nb * qb) + a.shape[3:])


def mla_branch(q_lat, kv_lat, k_pe, q_ln_g, kv_ln_g, w_uq, w_ukv, pos):
    B, T, _ = q_lat.shape
    q = (rms_norm(q_lat, q_ln_g) @ w_uq).reshape(B, T, A_HEADS, A_NOPE + A_ROPE)
    q_nope, q_pe = q[..., :A_NOPE], rope(q[..., A_NOPE:], pos)
    kv = (rms_norm(kv_lat, kv_ln_g) @ w_ukv).reshape(B, T, A_HEADS, A_NOPE + A_VDIM)
    k_nope, v = kv[..., :A_NOPE], kv[..., A_NOPE:]
    k_pe = rope(k_pe[:, :, None, :], pos)[:, :, 0, :]
    scale = (A_NOPE + A_ROPE) ** -0.5
    key_chunk = jnp.arange(T) // CHUNK

    def block(args):
        qn, qp, qpos = args
        s = (jnp.einsum('bqhd,bshd->bhqs', qn, k_nope)
             + jnp.einsum('bqhd,bsd->bhqs', qp, k_pe)).astype(jnp.float32) * scale
        mask = key_chunk[None, :] <= (qpos // CHUNK)[:, None]
        p = jax.nn.softmax(jnp.where(mask, s, NEG), axis=-1).astype(v.dtype)
        return jnp.einsum('bhqs,bshd->bqhd', p, v)

    out = lax.map(block, (to_blocks(q_nope), to_blocks(q_pe), pos.reshape(-1, Q_BLOCK)))
    return from_blocks(out).reshape(B, T, A_HEADS * A_VDIM)


def dsa_branch(q, k, v, q_idx, k_idx, w_idx, pos):
    B, T, _ = q.shape
    rep = B_HEADS // B_KV_HEADS
    q = rope(q.reshape(B, T, B_HEADS, B_HDIM), pos)
    k = rope(k.reshape(B, T, B_KV_HEADS, B_HDIM), pos)
    v = v.reshape(B, T, B_KV_HEADS, B_HDIM)
    q_idx = rope(q_idx.reshape(B, T, IDX_HEADS, IDX_DIM), pos)
    k_idx = rope(k_idx[:, :, None, :], pos)[:, :, 0, :]
    w_idx = w_idx * (IDX_HEADS * IDX_DIM) ** -0.5
    n_sel = min(TOPK_MAX, T // 4)
    scale = B_HDIM ** -0.5
    key_chunk = jnp.arange(T) // CHUNK
    gather = jax.vmap(lambda a, i: a[i])

    def block(args):
        qb, qib, wb, qpos = args
        qchunk = qpos // CHUNK
        rel = jax.nn.relu(jnp.einsum('bqhd,bsd->bqhs', qib, k_idx).astype(jnp.float32))
        score = jnp.einsum('bqh,bqhs->bqs', wb.astype(jnp.float32), rel)
        score = jnp.where(key_chunk[None, None, :] <= qchunk[None, :, None], score, NEG)
        _, idx = lax.top_k(score, n_sel)
        ks, vs = gather(k, idx), gather(v, idx)
        valid = (idx // CHUNK) <= qchunk[None, :, None]
        qg = qb.reshape(B, Q_BLOCK, B_KV_HEADS, rep, B_HDIM)
        s = jnp.einsum('bqgrd,bqkgd->bgrqk', qg, ks).astype(jnp.float32) * scale
        s = jnp.where(valid[:, None, None], s, NEG)
        p = jax.nn.softmax(s, axis=-1).astype(vs.dtype)
        o = jnp.einsum('bgrqk,bqkgd->bqgrd', p, vs)
        return o.reshape(B, Q_BLOCK, B_HEADS * B_HDIM)

    out = lax.map(block, (to_blocks(q), to_blocks(q_idx), to_blocks(w_idx), pos.reshape(-1, Q_BLOCK)))
    return from_blocks(out)


def swa_branch(q, k, v, sinks, pos):
    B, T, _ = q.shape
    nC = T // CHUNK
    rep = C_HEADS // C_KV_HEADS
    q = rope(q.reshape(B, T, C_HEADS, C_HDIM), pos).reshape(B, nC, CHUNK, C_KV_HEADS, rep, C_HDIM)
    k = rope(k.reshape(B, T, C_KV_HEADS, C_HDIM), pos)
    v = v.reshape(B, T, C_KV_HEADS, C_HDIM)
    pad = WIN_CHUNKS * CHUNK
    band_len = (WIN_CHUNKS + 1) * CHUNK

    def band(a):
        ap = jnp.pad(a, ((0, 0), (pad, 0), (0, 0), (0, 0)))
        ap = ap.reshape(B, nC + WIN_CHUNKS, CHUNK, C_KV_HEADS, C_HDIM)
        return jnp.concatenate([ap[:, j:j + nC] for j in range(WIN_CHUNKS + 1)], axis=2)

    kb, vb = band(k), band(v)
    key_pos = jnp.arange(nC)[:, None] * CHUNK - pad + jnp.arange(band_len)[None, :]
    valid = (key_pos >= 0)[:, None, :]
    s = jnp.einsum('bcqgrd,bckgd->bgrcqk', q, kb).astype(jnp.float32) * C_HDIM ** -0.5
    s = jnp.where(valid, s, NEG)
    sink = jnp.broadcast_to(sinks.astype(jnp.float32).reshape(C_KV_HEADS, rep, 1, 1, 1), s.shape[:-1] + (1,))
    p = jax.nn.softmax(jnp.concatenate([s, sink], axis=-1), axis=-1)[..., :-1].astype(vb.dtype)
    o = jnp.einsum('bgrcqk,bckgd->bcqgrd', p, vb)
    return o.reshape(B, T, C_HEADS * C_HDIM)


def mixer(h, w_in, a_q_ln_g, a_kv_ln_g, a_w_uq, a_w_ukv, c_sinks, w_br_a, w_br_b, w_br_c, w_out, pos):
    B, T, D = h.shape
    points = [int(p) for p in np.cumsum(SPLIT_SIZES)[:-1]]
    (a_q, a_kv, a_kpe, b_q, b_k, b_v, b_qi, b_ki, b_wi,
     c_q, c_k, c_v, gates) = jnp.split(h @ w_in, points, axis=-1)
    y_a = mla_branch(a_q, a_kv, a_kpe, a_q_ln_g, a_kv_ln_g, a_w_uq, a_w_ukv, pos) @ w_br_a
    y_b = dsa_branch(b_q, b_k, b_v, b_qi, b_ki, b_wi, pos) @ w_br_b
    y_c = swa_branch(c_q, c_k, c_v, c_sinks, pos) @ w_br_c
    g = jax.nn.sigmoid(gates).reshape(B, T, N_BRANCH, D)
    merged = g[:, :, 0] * y_a + g[:, :, 1] * y_b + g[:, :, 2] * y_c
    return merged @ w_out


def moe(h, w_router, router_bias, w_gate, w_up, w_down):
    B, T, D = h.shape
    hf = h.reshape(B * T, D)
    scores = jax.nn.sigmoid((hf @ w_router).astype(jnp.float32))
    biased = scores + router_bias.astype(jnp.float32)
    grp = biased.reshape(-1, N_GROUPS, EXPERTS_PER_GROUP)
    group_score = lax.top_k(grp, TOP_K)[0].sum(-1)
    g_sel = jnp.argmax(group_score, axis=-1)
    in_group = (jnp.arange(N_EXPERTS) // EXPERTS_PER_GROUP)[None, :] == g_sel[:, None]
    _, top_idx = lax.top_k(jnp.where(in_group, biased, NEG), TOP_K)
    w = jnp.take_along_axis(scores, top_idx, axis=-1)
    w = w / jnp.sum(w, axis=-1, keepdims=True)
    gate = jnp.sum(jax.nn.one_hot(top_idx, N_EXPERTS, dtype=jnp.float32) * w[..., None], axis=1)
    act = jax.nn.silu(jnp.einsum('nd,edf->nef', hf, w_gate)) * jnp.einsum('nd,edf->nef', hf, w_up)
    act = act * gate.astype(act.dtype)[:, :, None]
    return jnp.einsum('nef,efd->nd', act, w_down).reshape(B, T, D)


def setup_inputs(seed: int = 0) -> dict:
    key = jax.random.key(seed)
    ks = jax.random.split(key, 24)
    f32 = jnp.float32
    L, D = DEPTH, D_MODEL

    def nrm(k, shape, fan_in, scale=1.0):
        return jax.random.normal(k, shape, f32) * (scale * fan_in ** -0.5)

    def gain(k, shape):
        return 1.0 + 0.05 * jax.random.normal(k, shape, f32)

    return {
        "x": jax.random.normal(ks[0], (BATCH, SEQ, D), f32),
        "ln_in_g": gain(ks[1], (D,)),
        "ln_in_b": 0.02 * jax.random.normal(ks[2], (D,), f32),
        "w_in": nrm(ks[3], (L, D, IN_COLS), D),
        "a_q_ln_g": gain(ks[4], (L, A_Q_RANK)),
        "a_kv_ln_g": gain(ks[5], (L, A_KV_RANK)),
        "a_w_uq": nrm(ks[6], (L, A_Q_RANK, A_HEADS * (A_NOPE + A_ROPE)), A_Q_RANK),
        "a_w_ukv": nrm(ks[7], (L, A_KV_RANK, A_HEADS * (A_NOPE + A_VDIM)), A_KV_RANK),
        "c_sinks": 0.5 * jax.random.normal(ks[8], (L, C_HEADS), f32),
        "w_br_a": nrm(ks[9], (L, A_HEADS * A_VDIM, D), A_HEADS * A_VDIM),
        "w_br_b": nrm(ks[10], (L, B_HEADS * B_HDIM, D), B_HEADS * B_HDIM),
        "w_br_c": nrm(ks[11], (L, C_HEADS * C_HDIM, D), C_HEADS * C_HDIM),
        "w_out": nrm(ks[12], (L, D, D), D, BETA),
        "ln1_g": gain(ks[13], (L, D)),
        "ln1_b": 0.02 * jax.random.normal(ks[14], (L, D), f32),
        "w_router": nrm(ks[15], (D, N_EXPERTS), D),
        "router_bias": 0.01 * jax.random.normal(ks[16], (N_EXPERTS,), f32),
        "w_exp_gate": nrm(ks[17], (L, N_EXPERTS, D, D_EXPERT), D),
        "w_exp_up": nrm(ks[18], (L, N_EXPERTS, D, D_EXPERT), D),
        "w_exp_down": nrm(ks[19], (L, N_EXPERTS, D_EXPERT, D), D_EXPERT, BETA),
        "ln2_g": gain(ks[20], (L, D)),
        "ln2_b": 0.02 * jax.random.normal(ks[21], (L, D), f32),
    }


def reference(x, ln_in_g, ln_in_b, w_in, a_q_ln_g, a_kv_ln_g, a_w_uq, a_w_ukv, c_sinks,
              w_br_a, w_br_b, w_br_c, w_out, ln1_g, ln1_b, w_router, router_bias,
              w_exp_gate, w_exp_up, w_exp_down, ln2_g, ln2_b):
    pos = jnp.arange(x.shape[1], dtype=jnp.int32)
    x = layer_norm(x, ln_in_g, ln_in_b)
    for l in range(DEPTH):
        mix = mixer(x, w_in[l], a_q_ln_g[l], a_kv_ln_g[l], a_w_uq[l], a_w_ukv[l], c_sinks[l],
                    w_br_a[l], w_br_b[l], w_br_c[l], w_out[l], pos)
        x = layer_norm(ALPHA * x + mix, ln1_g[l], ln1_b[l])
        ffn = moe(x, w_router, router_bias, w_exp_gate[l], w_exp_up[l], w_exp_down[l])
        x = layer_norm(ALPHA * x + ffn, ln2_g[l], ln2_b[l])
    return x
```

```python
import contextlib
import numpy as np
import concourse.bass as bass
import concourse.mybir as mybir
from concourse.bass_utils import run_bass_kernel_spmd

F32 = mybir.dt.float32
BF16 = mybir.dt.bfloat16
AF = mybir.ActivationFunctionType
ALU = mybir.AluOpType
AX = mybir.AxisListType


GUARD = True


class Buf:
    __slots__ = ("name", "w", "r", "sem_in", "cnt_in", "sem_out", "cnt_out", "psum")

    def __init__(self, name):
        self.name = name
        self.w = None
        self.r = []
        self.sem_in = None
        self.cnt_in = 0
        self.sem_out = None
        self.cnt_out = 0
        self.psum = False


class Prog:
    ENG = ("pe", "act", "dve", "pool", "sp")

    def __init__(self, nc, es):
        self.nc = nc
        self.es = es
        self.root = es
        self.eng = {"pe": nc.tensor, "act": nc.scalar, "dve": nc.vector,
                    "pool": nc.gpsimd, "sp": nc.sync}
        self.ops = {e: [] for e in self.ENG}
        self.idx = {e: 0 for e in self.ENG}
        self.sem = {e: es.enter_context(nc.semaphore("s_" + e)) for e in self.ENG if e != "sp"}
        self.waited = {e: {} for e in self.ENG}
        self.nsem = 0
        self.uid = 0
        self.last_out_events = []
        self.pending = {e: {} for e in self.ENG}
        self.dma_since = {}
        self.guard = {}
        self.guard_hist = {"act": [], "dve": []}
        self.guard_src = None

    def new_sem(self, name):
        self.nsem += 1
        return self.root.enter_context(self.nc.semaphore("%s_%d" % (name, self.nsem)))

    def sb(self, name, shape, dt):
        self.uid += 1
        return self.es.enter_context(self.nc.sbuf_tensor("%s_%d" % (name, self.uid), list(shape), dt))

    def ps(self, name, shape, dt):
        self.uid += 1
        return self.es.enter_context(self.nc.psum_tensor("%s_%d" % (name, self.uid), list(shape), dt))

    def _collect(self, e, reads, writes, skip_sem=None):
        waits = {}

        def need(ev, raw, waw=False):
            if ev is None:
                return
            sem, val, ee, ii = ev
            if waw and skip_sem is not None and sem is skip_sem:
                return
            if ee == e and ii is not None:
                if e == "pe":
                    return
            k = id(sem)
            if self.waited[e].get(k, 0) >= val:
                return
            if k not in waits or waits[k][1] < val:
                waits[k] = (sem, val)

        for b in reads:
            need(b.w, True)
            if b.psum and e in ("act", "dve"):
                for r in b.r:
                    if r[2] != e:
                        need(r, True)
        for b in writes:
            need(b.w, True, True)
            for r in b.r:
                if GUARD and b.psum and e == "pe" and r[3] is not None and r[2] in ("act", "dve"):
                    r = self._guarded(r)
                need(r, False)
        if self.pending[e]:
            for k, (sem, val) in self.pending[e].items():
                if self.waited[e].get(k, 0) >= val:
                    continue
                if k not in waits or waits[k][1] < val:
                    waits[k] = (sem, val)
            self.pending[e] = {}
        for k, (sem, val) in waits.items():
            self.waited[e][k] = val
        return list(waits.values())

    def _commit(self, ev, reads, writes):
        for b in reads:
            b.r.append(ev)
        for b in writes:
            b.w = ev
            b.r = []

    def _guarded(self, r):
        sem, val, E, ii = r
        if self.idx[E] > ii + 1:
            return (sem, ii + 2, E, ii + 1)
        hist = self.guard_hist[E]
        n = len(hist)
        g = self.guard[E][:, (n % 8):(n % 8) + 1]
        gw = []
        if n >= 8:
            k = id(self.sem[E])
            need = hist[n - 8] + 1
            if self.waited[E].get(k, 0) < need:
                gw.append((self.sem[E], need))
                self.waited[E][k] = need
        if E == "act":
            src, bsrc = self.guard_src
            sv = bsrc.w
            if sv is not None and self.waited[E].get(id(sv[0]), 0) < sv[1]:
                gw.append((sv[0], sv[1]))
                self.waited[E][id(sv[0])] = sv[1]
        i2 = self.idx[E]
        self.idx[E] = i2 + 1
        hist.append(i2)
        if E == "act":
            self.ops[E].append((gw, (lambda en, g=g, src=src: en.activation(out=g, in_=src, func=AF.Copy)), (self.sem[E], 1)))
        else:
            self.ops[E].append((gw, (lambda en, g=g: en.memset(g, 0.0)), (self.sem[E], 1)))
        return (self.sem[E], i2 + 1, E, i2)

    def op(self, e, fn, reads=(), writes=()):
        waits = self._collect(e, reads, writes)
        i = self.idx[e]
        self.idx[e] = i + 1
        ev = (self.sem[e], i + 1, e, i)
        self.ops[e].append((waits, fn, (self.sem[e], 1)))
        self._commit(ev, reads, writes)
        return ev

    def dma(self, e, fn, reads=(), writes=(), key=None):
        if key is None:
            key = writes[0] if writes else reads[0]
        if key.sem_in is None:
            key.sem_in = {}
        if e not in key.sem_in:
            key.sem_in[e] = [self.new_sem("d%s_%s" % (e, key.name)), 0]
        ent = key.sem_in[e]
        sem = ent[0]
        waits = self._collect(e, reads, writes, skip_sem=sem)
        ent[1] += 16
        val = ent[1]
        ev = (sem, val, e, None)
        self.dma_since[id(sem)] = (sem, val)
        self.ops[e].append((waits, fn, (sem, 16)))
        self._commit(ev, reads, writes)
        return ev

    def mm(self, out, lhsT, rhs, start, stop, reads, writes, sgc=False):
        if sgc:
            return self.op("pe", lambda e: e.matmul(out, lhsT=lhsT, rhs=rhs, start=start, stop=stop, skip_group_check=True), reads, writes)
        return self.op("pe", lambda e: e.matmul(out, lhsT=lhsT, rhs=rhs, start=start, stop=stop), reads, writes)

    def tr(self, out, in_, ident, reads, writes):
        return self.op("pe", lambda e: e.transpose(out, in_, ident), reads, writes)

    def act(self, out, in_, func, reads, writes, bias=None, scale=None, accum=None):
        kw = {}
        if bias is not None:
            kw["bias"] = bias
        if scale is not None:
            kw["scale"] = scale
        if accum is not None:
            kw["accum_out"] = accum
        return self.op("act", lambda e: e.activation(out=out, in_=in_, func=func, **kw), reads, writes)

    def ts(self, eng, out, in0, s1, s2, op0, reads, writes, op1=None, accum=None):
        kw = {}
        if op1 is not None:
            kw["op1"] = op1
        if accum is not None:
            kw["accum_out"] = accum
        return self.op(eng, lambda e: e.tensor_scalar(out=out, in0=in0, scalar1=s1, scalar2=s2, op0=op0, **kw), reads, writes)

    def tt(self, eng, out, in0, in1, op, reads, writes):
        return self.op(eng, lambda e: e.tensor_tensor(out=out, in0=in0, in1=in1, op=op), reads, writes)

    def stt(self, eng, out, in0, scalar, in1, op0, op1, reads, writes):
        return self.op(eng, lambda e: e.scalar_tensor_tensor(out=out, in0=in0, scalar=scalar, in1=in1, op0=op0, op1=op1), reads, writes)

    def cp(self, eng, out, in_, reads, writes):
        if eng == "act":
            return self.op("act", lambda e: e.activation(out=out, in_=in_, func=AF.Copy), reads, writes)
        return self.op(eng, lambda e: e.tensor_copy(out=out, in_=in_), reads, writes)

    def memset(self, eng, ap, val, writes):
        return self.op(eng, lambda e: e.memset(ap, val), (), writes)

    def recip(self, out, in_, reads, writes):
        return self.op("dve", lambda e: e.reciprocal(out=out, in_=in_), reads, writes)

    def ld(self, q, out, in_, reads, writes):
        return self.dma(q, lambda e: e.dma_start(out=out, in_=in_), reads, writes)

    def st(self, q, out, in_, reads, writes, key):
        return self.dma(q, lambda e: e.dma_start(out=out, in_=in_), reads, writes, key=key)

    def fence(self):
        for e in self.ENG:
            pend = self.pending[e]
            for f in self.ENG:
                if f == "sp" or f == e or self.idx[f] == 0:
                    continue
                k = id(self.sem[f])
                pend[k] = (self.sem[f], self.idx[f])
            for k, sv in self.dma_since.items():
                if k not in pend or pend[k][1] < sv[1]:
                    pend[k] = sv
        self.dma_since = {}

    def finish(self, final_events):
        nc = self.nc
        ops = self.ops
        with nc.Block() as block:
            def emit(engname, eng):
                for waits, fn, (sem, inc) in ops[engname]:
                    for (s, v) in waits:
                        eng.wait_ge(s, v)
                    ins = fn(eng)
                    ins.then_inc(sem, inc)

            @block.tensor
            def _(eng):
                emit("pe", eng)

            @block.scalar
            def _(eng):
                emit("act", eng)

            @block.vector
            def _(eng):
                emit("dve", eng)

            @block.gpsimd
            def _(eng):
                emit("pool", eng)

            @block.sync
            def _(eng):
                emit("sp", eng)
                for (s, v, _e, _i) in final_events:
                    eng.wait_ge(s, v)

T = 2048
D = 1024
NT = 16
NG = 4
KC = 8
LN_EPS = 1e-5
RMS_EPS = 1e-6
DEPTH = 2
ALPHA = (2.0 * DEPTH) ** 0.25
IN_COLS = 5604
VP = 68
NIT = 18
W0 = 64.0


def make_consts():
    f32 = np.float32
    c = {}
    c["c_ident"] = np.eye(128, dtype=f32)
    c["c_ones"] = np.ones((128, 128), f32)
    t = np.arange(T, dtype=f32)
    p = np.arange(128)
    i64 = ((p % 64) % 32).astype(f32)
    inv64 = np.power(f32(10000.0), -(i64 / f32(32.0))).astype(f32)
    ang = (t[None, :] * inv64[:, None]).astype(f32).astype(np.float64)
    c["c_cos64"] = np.cos(ang).astype(f32)
    c["c_sin64"] = np.sin(ang).astype(f32)
    iA = ((p - 64) % 16).astype(f32)
    invA = np.power(f32(10000.0), -(iA / f32(16.0))).astype(f32)
    angA = (t[None, :] * invA[:, None]).astype(f32).astype(np.float64)
    c["c_cosA"] = np.cos(angA).astype(f32)
    c["c_sinA"] = np.sin(angA).astype(f32)
    pm = np.zeros((128, 128), f32)
    for fp in range(128):
        d = fp % 64
        if d < 32:
            pm[fp + 32, fp] = -1.0
        else:
            pm[fp - 32, fp] = 1.0
    c["c_pm64"] = pm
    pa = np.zeros((128, 128), f32)
    for fp in range(64, 96):
        d = fp - 64
        if d < 16:
            pa[fp + 16, fp] = -1.0
        else:
            pa[fp - 16, fp] = 1.0
    c["c_pmA"] = pa
    s = np.arange(128)[:, None]
    q = np.arange(128)[None, :]
    c["c_diag"] = ((s < 64) | (q >= 64)).astype(f32)
    c["c_swaprev"] = (~((s < 64) & (q >= 64))).astype(f32)
    tt_ = np.arange(128)[:, None]
    ss_ = np.arange(128)[None, :]
    c["c_negdiag"] = np.where((tt_ < 64) & (ss_ >= 64), f32(-1e30), f32(0.0)).astype(f32)
    return c


CONST_SHAPES = {"c_ident": [128, 128], "c_ones": [128, 128], "c_cos64": [128, T], "c_sin64": [128, T],
                "c_cosA": [128, T], "c_sinA": [128, T], "c_pm64": [128, 128], "c_pmA": [128, 128],
                "c_diag": [128, 128], "c_swaprev": [128, 128], "c_negdiag": [128, 128]}

WEIGHT_SHAPES = {
    "ln_in_g": [D], "ln_in_b": [D], "w_in": [2, D, IN_COLS],
    "a_q_ln_g": [2, 128, 3], "a_kv_ln_g": [2, 128, 2],
    "a_w_uq": [2, 384, 768], "a_w_ukv": [2, 256, 1024], "c_sinks": [2, 8],
    "w_br_a": [2, 512, D], "w_br_b": [2, 512, D], "w_br_c": [2, 512, D], "w_out": [2, D, D],
    "ln1_g": [2, D], "ln1_b": [2, D], "w_router": [D, 16], "router_bias": [16],
    "w_exp_gate": [2, 16, D, 512], "w_exp_up": [2, 16, D, 512], "w_exp_down": [2, 16, 512, D],
    "ln2_g": [2, D], "ln2_b": [2, D],
}


class Scope:
    def __init__(self, P):
        self.P = P

    def __enter__(self):
        self.prev = self.P.es
        self.stack = contextlib.ExitStack()
        self.stack.__enter__()
        self.P.es = self.stack
        return self

    def __exit__(self, *a):
        self.P.es = self.prev
        self.P.fence()
        return self.stack.__exit__(*a)


def bufs(name, n):
    return [NB("%s%d" % (name, i)) for i in range(n)]


def build_program(layers, do_ln_in, final, dbg_names=()):
    nc = bass.Bass("TRN2", target_bir_lowering=False)
    dr = {}
    dr["x"] = nc.dram_tensor("x", [T, D], F32, kind="ExternalInput").ap()
    for k, shp in WEIGHT_SHAPES.items():
        dr[k] = nc.dram_tensor(k, shp, F32, kind="ExternalInput").ap()
    for k, shp in CONST_SHAPES.items():
        dr[k] = nc.dram_tensor(k, shp, F32, kind="ExternalInput").ap()
    y = nc.dram_tensor("y", [T, D], F32, kind="ExternalOutput").ap()
    xres = nc.dram_tensor("xres", [T, D], F32).ap()
    dbg = {}
    DBG_SHAPES = {"d_xT": ([128, KC, T], BF16), "d_oTa": ([128, 4, T], BF16), "d_oTb": ([128, 4, T], BF16),
                  "d_oTc": ([128, 4, T], BF16), "d_gate": ([128, NT, 16], F32)}
    for k in dbg_names:
        shp, dt_ = DBG_SHAPES[k]
        dbg[k] = nc.dram_tensor(k, shp, dt_, kind="ExternalOutput").ap()

    es = contextlib.ExitStack()
    with es:
        P = Prog(nc, es)
        final_events = []
        banks = [P.ps("bank%d" % i, [128, 512], F32) for i in range(8)]
        Bbank = bufs("bank", 8)
        for b_ in Bbank:
            b_.psum = True
        P.guard["act"] = P.sb("guard_act", [128, 8], F32)[:]
        P.guard["dve"] = P.sb("guard_dve", [128, 8], F32)[:]
        xT = P.sb("xT", [128, KC, T], BF16)
        BxT = bufs("xT", NT)
        Bxres = bufs("xres", NT)
        Bc = NB("consts")
        ident_bf = P.sb("ident_bf", [128, 128], BF16)
        ident_f = P.sb("ident_f", [128, 128], F32)
        ones_bf = P.sb("ones_bf", [128, 128], BF16)
        pm64 = P.sb("pm64", [128, 128], BF16)
        pmA = P.sb("pmA", [128, 128], BF16)
        diag_bf = P.sb("diag_bf", [128, 128], BF16)
        swaprev_bf = P.sb("swaprev_bf", [128, 128], BF16)
        negdiag = P.sb("negdiag", [128, 128], F32)
        P.ld("pool", ident_bf[:], dr["c_ident"], [], [Bc])
        P.ld("pool", ones_bf[:], dr["c_ones"], [], [Bc])
        P.ld("pool", pm64[:], dr["c_pm64"], [], [Bc])
        P.ld("pool", pmA[:], dr["c_pmA"], [], [Bc])
        P.ld("pool", diag_bf[:], dr["c_diag"], [], [Bc])
        P.ld("pool", swaprev_bf[:], dr["c_swaprev"], [], [Bc])
        P.ld("sp", ident_f[:], dr["c_ident"], [], [Bc])
        P.ld("sp", negdiag[:], dr["c_negdiag"], [], [Bc])
        P.guard_src = (ident_f[:, 0:1], Bc)

        def gsl(G):
            return slice(G * 512, (G + 1) * 512)

        def tsl(tt):
            return slice(tt * 128, (tt + 1) * 128)

        class Rot:
            def __init__(self, items):
                self.items = items
                self.i = 0

            def next(self):
                it = self.items[self.i % len(self.items)]
                self.i += 1
                return it

        def dump(name, ap_sb, rbufs):
            if name in dbg:
                final_events.append(P.st("sp", dbg[name], ap_sb, rbufs, [], key=NB("dbg_" + name)))

        def ln_inplace(t_ap, Bt, gB, bB, Bgb, st, Bst, junk, Bjunk):
            P.act(junk, t_ap, AF.Copy, [Bt], [Bjunk, Bst], accum=st[:, 0:1])
            P.act(junk, t_ap, AF.Square, [Bt], [Bjunk, Bst], accum=st[:, 1:2])
            P.ts("dve", st[:, 2:3], st[:, 0:1], 1.0 / D, None, ALU.mult, [Bst], [Bst])
            P.tt("dve", st[:, 3:4], st[:, 2:3], st[:, 2:3], ALU.mult, [Bst], [Bst])
            P.stt("dve", st[:, 4:5], st[:, 1:2], 1.0 / D, st[:, 3:4], ALU.mult, ALU.subtract, [Bst], [Bst])
            P.ts("dve", st[:, 4:5], st[:, 4:5], LN_EPS, None, ALU.add, [Bst], [Bst])
            P.act(st[:, 5:6], st[:, 4:5], AF.Sqrt, [Bst], [Bst])
            P.recip(st[:, 6:7], st[:, 5:6], [Bst], [Bst])
            P.stt("dve", st[:, 7:8], st[:, 2:3], -1.0, st[:, 6:7], ALU.mult, ALU.mult, [Bst], [Bst])
            P.act(t_ap, t_ap, AF.Identity, [Bt, Bst], [Bt], scale=st[:, 6:7], bias=st[:, 7:8])
            P.tt("dve", t_ap, t_ap, gB, ALU.mult, [Bt, Bgb], [Bt])
            P.tt("dve", t_ap, t_ap, bB, ALU.add, [Bt, Bgb], [Bt])

        def to_xT(t_ap, Bt, tt, xbf, Bxbf, bk):
            P.cp("act", xbf, t_ap, [Bt], [Bxbf])
            bv = banks[bk][:].bitcast(BF16)
            for kc in range(KC):
                P.tr(bv[:, kc * 128:(kc + 1) * 128], xbf[:, kc * 128:(kc + 1) * 128], ident_bf[:],
                     [Bxbf, Bc], [Bbank[bk]])
            P.cp("dve", xT[:, :, tsl(tt)], bv.rearrange("p (k t) -> p k t", t=128), [Bbank[bk]], [BxT[tt]])

        def ln_batch(items, gB, bB, Bgb, st, Bsum, Bsq, Bvec, junks, Bjunks):
            nb = len(items)
            for i, (t_ap, Bt) in enumerate(items):
                P.act(junks[i % 2], t_ap, AF.Copy, [Bt], [Bjunks[i % 2], Bsum[i]], accum=st[:, 0, i:i + 1])
            for i, (t_ap, Bt) in enumerate(items):
                P.act(junks[i % 2], t_ap, AF.Square, [Bt], [Bjunks[i % 2], Bsq[i]], accum=st[:, 1, i:i + 1])
            V = [Bvec]
            P.ts("dve", st[:, 2, 0:nb], st[:, 0, 0:nb], 1.0 / D, None, ALU.mult, Bsum[0:nb], V)
            P.tt("dve", st[:, 3, 0:nb], st[:, 2, 0:nb], st[:, 2, 0:nb], ALU.mult, V, V)
            P.stt("dve", st[:, 4, 0:nb], st[:, 1, 0:nb], 1.0 / D, st[:, 3, 0:nb], ALU.mult, ALU.subtract, Bsq[0:nb] + V, V)
            P.ts("dve", st[:, 4, 0:nb], st[:, 4, 0:nb], LN_EPS, None, ALU.add, V, V)
            P.act(st[:, 5, 0:nb], st[:, 4, 0:nb], AF.Sqrt, V, V)
            P.recip(st[:, 6, 0:nb], st[:, 5, 0:nb], V, V)
            P.stt("dve", st[:, 7, 0:nb], st[:, 2, 0:nb], -1.0, st[:, 6, 0:nb], ALU.mult, ALU.mult, V, V)
            for i, (t_ap, Bt) in enumerate(items):
                P.act(t_ap, t_ap, AF.Identity, [Bt, Bvec], [Bt], scale=st[:, 6, i:i + 1], bias=st[:, 7, i:i + 1])
            for i, (t_ap, Bt) in enumerate(items):
                P.tt("dve", t_ap, t_ap, gB, ALU.mult, [Bt, Bgb], [Bt])
            for i, (t_ap, Bt) in enumerate(items):
                P.tt("dve", t_ap, t_ap, bB, ALU.add, [Bt, Bgb], [Bt])

        def to_xT_batch(items, tts, xbfs, Bxbfs, bks):
            for i, (t_ap, Bt) in enumerate(items):
                P.cp("act", xbfs[i % len(xbfs)], t_ap, [Bt], [Bxbfs[i % len(xbfs)]])
                bk = bks[i % len(bks)]
                bv = banks[bk][:].bitcast(BF16)
                xb = xbfs[i % len(xbfs)]
                for kc in range(KC):
                    P.tr(bv[:, kc * 128:(kc + 1) * 128], xb[:, kc * 128:(kc + 1) * 128], ident_bf[:],
                         [Bxbfs[i % len(xbfs)], Bc], [Bbank[bk]])
                P.cp("dve", xT[:, :, tsl(tts[i])], bv.rearrange("p (k t) -> p k t", t=128), [Bbank[bk]], [BxT[tts[i]]])

        with Scope(P):
            gB = P.sb("ln0_g", [128, D], F32)
            bB = P.sb("ln0_b", [128, D], F32)
            Bgb = NB("ln0gb")
            if do_ln_in:
                P.ld("sp", gB[:], dr["ln_in_g"].partition_broadcast(128), [], [Bgb])
                P.ld("sp", bB[:], dr["ln_in_b"].partition_broadcast(128), [], [Bgb])
            xt = [P.sb("in_x%d" % i, [128, D], F32) for i in range(4)]
            Bxt = bufs("in_x", 4)
            stt_ = P.sb("in_st", [128, 8, 4], F32)
            Bsum = bufs("in_sum", 4)
            Bsq = bufs("in_sq", 4)
            Bvec = NB("in_vec")
            junks = [P.sb("in_junk%d" % i, [128, D], BF16)[:] for i in range(2)]
            Bjunks = bufs("in_junk", 2)
            xbf = [P.sb("in_xbf%d" % i, [128, D], BF16)[:] for i in range(2)]
            Bxbf = bufs("in_xbf", 2)
            for t0 in range(0, NT, 4):
                items = []
                for i in range(4):
                    P.ld("sp", xt[i][:], dr["x"][tsl(t0 + i), :], [], [Bxt[i]])
                    items.append((xt[i][:], Bxt[i]))
                if do_ln_in:
                    ln_batch(items, gB[:], bB[:], Bgb, stt_, Bsum, Bsq, Bvec, junks, Bjunks)
                for i in range(4):
                    P.st("sp", xres[tsl(t0 + i), :], xt[i][:], [Bxt[i]], [Bxres[t0 + i]], key=Bxt[i])
                to_xT_batch(items, [t0 + i for i in range(4)], xbf, Bxbf, [0, 1, 2, 3])

        def load_w(q, dst_ap, src_ap, Bw):
            P.ld(q, dst_ap, src_ap, [], [Bw])

        def proj_fm(lhsT_of_kc, M, G, bk, wr):
            for kc in range(KC):
                P.mm(banks[bk][0:M, :], lhsT_of_kc(kc), xT[:, kc, gsl(G)], kc == 0, kc == KC - 1,
                     [BxT[4 * G + j] for j in range(4)] + wr, [Bbank[bk]])

        def layer(l, last):
            w_in_v = dr["w_in"][l].rearrange("(kc p) c -> p kc c", p=128)
            logit = P.sb("logit", [128, NT, 16], F32)
            Blogit = bufs("logit", NT)
            with Scope(P):
                oT = [P.sb("oT%d" % i, [128, 4, T], BF16) for i in range(3)]
                BoT = [bufs("oT%d_" % i, NG) for i in range(3)]
                with Scope(P):
                    aqn = P.sb("aqn", [128, 3, T], BF16)
                    Baqn = bufs("aqn", NG)
                    akvn = P.sb("akvn", [128, 2, T], BF16)
                    Bakvn = bufs("akvn", NG)
                    kpe = P.sb("kpe", [96, T], BF16)
                    Bkpe = bufs("kpe", NG)
                    cosA = P.sb("cosA", [128, T], F32)
                    sinA = P.sb("sinA", [128, T], F32)
                    Btab = NB("tabA")
                    P.ld("sp", cosA[:], dr["c_cosA"], [], [Btab])
                    P.ld("sp", sinA[:], dr["c_sinA"], [], [Btab])
                    wuq = P.sb("wuq", [128, 3, 768], BF16)
                    wukv = P.sb("wukv", [128, 2, 1024], BF16)
                    Bwu = NB("wu")
                    P.ld("pool", wuq[:], dr["a_w_uq"][l].rearrange("(kc p) c -> p kc c", p=128), [], [Bwu])
                    P.ld("pool", wukv[:], dr["a_w_ukv"][l].rearrange("(kc p) c -> p kc c", p=128), [], [Bwu])
                    t1 = [P.sb("a_t1_%d" % i, [128, 512], F32) for i in range(2)]
                    t2 = [P.sb("a_t2_%d" % i, [128, 512], F32) for i in range(2)]
                    Bt1 = bufs("a_t1", 2)
                    Bt2 = bufs("a_t2", 2)
                    trot = Rot([0, 1])
                    with Scope(P):
                        Wa = P.sb("Wa", [128, KC, 672], BF16)
                        BWa = NB("Wa")
                        P.ld("pool", Wa[:], w_in_v[:, :, 0:672], [], [BWa])
                        gq = P.sb("gq", [128, 3], F32)
                        gkv = P.sb("gkv", [128, 2], F32)
                        Bg = NB("gqkv")
                        P.ld("sp", gq[:], dr["a_q_ln_g"][l], [], [Bg])
                        P.ld("sp", gkv[:], dr["a_kv_ln_g"][l], [], [Bg])
                        sq = P.sb("sq", [128, 3, 512], BF16)
                        Bsq = bufs("sq", 3)
                        rs = P.sb("rs", [128, 512], F32)
                        Brs = NB("rs")
                        kraw = P.sb("kraw", [96, 512], BF16)
                        Bkraw = NB("kraw")
                        P.memset("dve", kraw[:], 0.0, [Bkraw])
                        brot = Rot([0, 1, 2, 3])
                        for G in range(NG):
                            for (dst, Bdst, nch, col0, g_ap, nfeat) in ((aqn, Baqn, 3, 0, gq, 384.0), (akvn, Bakvn, 2, 384, gkv, 256.0)):
                                for c in range(nch):
                                    bk = brot.next()
                                    proj_fm(lambda kc, c=c, col0=col0: Wa[:, kc, col0 + c * 128: col0 + (c + 1) * 128], 128, G, bk, [BWa])
                                    P.cp("act", dst[:, c, gsl(G)], banks[bk][:], [Bbank[bk]], [Bdst[G]])
                                    P.act(sq[:, c, :], banks[bk][:], AF.Square, [Bbank[bk]], [Bsq[c]])
                                bk = brot.next()
                                for c in range(nch):
                                    P.mm(banks[bk][:], ones_bf[:], sq[:, c, :], c == 0, c == nch - 1, [Bc, Bsq[c]], [Bbank[bk]])
                                P.ts("dve", rs[:], banks[bk][:], 1.0 / nfeat, RMS_EPS, ALU.mult, [Bbank[bk]], [Brs], op1=ALU.add)
                                P.act(rs[:], rs[:], AF.Sqrt, [Brs], [Brs])
                                P.recip(rs[:], rs[:], [Brs], [Brs])
                                for c in range(nch):
                                    P.stt("dve", dst[:, c, gsl(G)], dst[:, c, gsl(G)], g_ap[:, c:c + 1], rs[:], ALU.mult, ALU.mult,
                                          [Bdst[G], Bg, Brs], [Bdst[G]])
                            bk = brot.next()
                            proj_fm(lambda kc: Wa[:, kc, 576:672], 96, G, bk, [BWa])
                            P.cp("act", kraw[64:96, :], banks[bk][64:96, :], [Bbank[bk]], [Bkraw])
                            bk2 = brot.next()
                            P.mm(banks[bk2][0:96, :], pmA[0:96, 0:96], kraw[0:96, :], True, True, [Bc, Bkraw], [Bbank[bk2]])
                            k = trot.next()
                            P.tt("dve", t1[k][64:96, :], kraw[64:96, :], cosA[64:96, gsl(G)], ALU.mult, [Bkraw, Btab], [Bt1[k]])
                            P.tt("dve", t2[k][64:96, :], banks[bk2][64:96, :], sinA[64:96, gsl(G)], ALU.mult, [Bbank[bk2], Btab], [Bt2[k]])
                            P.tt("dve", kpe[64:96, gsl(G)], t1[k][64:96, :], t2[k][64:96, :], ALU.add, [Bt1[k], Bt2[k]], [Bkpe[G]])
                    Va = P.sb("Va", [128, NT, 8, VP], BF16)
                    BVa = bufs("Va", NT)
                    BVa1 = NB("Va_ones")
                    P.memset("dve", Va[:, :, :, 64:VP], 1.0, [BVa1])
                    wukv_v = wukv[:].rearrange("p k (h d) -> p k h d", d=128)
                    brot = Rot([5, 6, 7])
                    for tt in range(NT):
                        bk = brot.next()
                        for kc in range(2):
                            P.mm(banks[bk][:].rearrange("p (h d) -> p h d", d=64), akvn[:, kc, tsl(tt)], wukv_v[:, kc, :, 64:128],
                                 kc == 0, kc == 1, [Bakvn[tt // 4], Bwu], [Bbank[bk]])
                        P.cp("act", Va[:, tt, :, 0:64], banks[bk][:].rearrange("p (h d) -> p h d", d=64), [Bbank[bk]], [BVa[tt]])
                    QT = [P.sb("QTa%d" % i, [96, T], BF16) for i in range(2)]
                    KT = [P.sb("KTa%d" % i, [96, T], BF16) for i in range(2)]
                    BQT = [bufs("QTa%d_" % i, NG) for i in range(2)]
                    BKT = [bufs("KTa%d_" % i, NG) for i in range(2)]
                    pt = [P.sb("a_pt%d" % i, [128, 512], BF16) for i in range(3)]
                    Bpt = bufs("a_pt", 3)
                    ptrot = Rot([0, 1, 2])
                    srot = Rot([0, 1, 2])
                    arot = Rot([3, 4])
                    otok = [P.sb("a_otok%d" % i, [128, NT, 128], BF16) for i in range(2)]
                    Botok = [bufs("a_otok%d_" % i, NT) for i in range(2)]
                    rec = [P.sb("a_rec%d" % i, [128, 4], F32) for i in range(2)]
                    Brec = bufs("a_rec", 2)
                    recrot = Rot([0, 1])
                    sc_a = 96.0 ** -0.5
                    def proj_head(h):
                        hp = h % 2
                        for G in range(NG):
                            bk = brot.next()
                            for kc in range(3):
                                P.mm(banks[bk][0:96, :], wuq[:, kc, h * 96:(h + 1) * 96], aqn[:, kc, gsl(G)], kc == 0, kc == 2,
                                     [Bwu, Baqn[G]], [Bbank[bk]])
                            P.cp("act", QT[hp][0:96, gsl(G)], banks[bk][0:96, :], [Bbank[bk]], [BQT[hp][G]])
                            bk2 = brot.next()
                            P.mm(banks[bk2][0:96, :], pmA[0:96, 0:96], QT[hp][0:96, gsl(G)], True, True, [Bc, BQT[hp][G]], [Bbank[bk2]])
                            k = trot.next()
                            P.tt("dve", t1[k][64:96, :], QT[hp][64:96, gsl(G)], cosA[64:96, gsl(G)], ALU.mult, [BQT[hp][G], Btab], [Bt1[k]])
                            P.tt("dve", t2[k][64:96, :], banks[bk2][64:96, :], sinA[64:96, gsl(G)], ALU.mult, [Bbank[bk2], Btab], [Bt2[k]])
                            P.tt("dve", QT[hp][64:96, gsl(G)], t1[k][64:96, :], t2[k][64:96, :], ALU.add, [Bt1[k], Bt2[k]], [BQT[hp][G]])
                            bk = brot.next()
                            for kc in range(2):
                                P.mm(banks[bk][0:64, :], wukv[:, kc, h * 128:h * 128 + 64], akvn[:, kc, gsl(G)], kc == 0, kc == 1,
                                     [Bwu, Bakvn[G]], [Bbank[bk]])
                            P.cp("act", KT[hp][0:64, gsl(G)], banks[bk][0:64, :], [Bbank[bk]], [BKT[hp][G]])
                            P.cp("dve", KT[hp][64:96, gsl(G)], kpe[64:96, gsl(G)], [Bkpe[G]], [BKT[hp][G]])

                    blocks = [(G, st_) for G in range(NG) for st_ in range(4 * G + 4)]
                    proj_head(0)
                    for h in range(8):
                        hp = h % 2
                        cpair = h // 2
                        if h + 1 < 8:
                            proj_head(h + 1)
                        info = {}

                        def qk(k):
                            G, st_ = blocks[k]
                            j0 = max(0, st_ - 4 * G)
                            ncol = (4 - j0) * 128
                            q0 = (4 * G + j0) * 128
                            sb_ = srot.next()
                            P.mm(banks[sb_][:, 0:ncol], KT[hp][0:96, tsl(st_)], QT[hp][0:96, q0:q0 + ncol], True, True,
                                 [BKT[hp][st_ // 4], BQT[hp][G]], [Bbank[sb_]])
                            info[k] = (sb_, j0, ncol)

                        qk(0)
                        ab = None
                        for k, (G, st_) in enumerate(blocks):
                            if k + 1 < len(blocks):
                                qk(k + 1)
                            sb_, j0, ncol = info[k]
                            if st_ == 0:
                                ab = arot.next()
                            accv = banks[ab][:, 0:4 * VP].rearrange("p (j d) -> p j d", d=VP)
                            pk = ptrot.next()
                            P.act(pt[pk][:, 0:ncol], banks[sb_][:, 0:ncol], AF.Exp, [Bbank[sb_]], [Bpt[pk]], scale=sc_a)
                            if st_ >= 4 * G:
                                P.tt("pool", pt[pk][:, 0:128], pt[pk][:, 0:128], diag_bf[:], ALU.mult, [Bpt[pk], Bc], [Bpt[pk]])
                            for j in range(j0, 4):
                                P.mm(accv[:, j, 0:65], pt[pk][:, (j - j0) * 128:(j - j0 + 1) * 128], Va[:, st_, h, 0:65],
                                     (st_ == 0 and j == 0), st_ == 4 * G + j, [Bpt[pk], BVa[st_], BVa1], [Bbank[ab]], sgc=True)
                            if st_ == 4 * G + 3:
                                rk = recrot.next()
                                P.recip(rec[rk][:], accv[:, :, 64], [Bbank[ab]], [Brec[rk]])
                                for j in range(4):
                                    tt = 4 * G + j
                                    P.ts("dve", otok[cpair % 2][:, tt, hp * 64:(hp + 1) * 64], accv[:, j, 0:64], rec[rk][:, j:j + 1], None, ALU.mult,
                                         [Bbank[ab], Brec[rk]], [Botok[cpair % 2][tt]])
                        if hp == 1:
                            for t0 in range(0, NT, 8):
                                bk = brot.next()
                                bv = banks[bk][:].bitcast(BF16)
                                for tt in range(t0, t0 + 8):
                                    P.tr(bv[:, (tt - t0) * 128:(tt - t0 + 1) * 128], otok[cpair % 2][:, tt, :], ident_bf[:],
                                         [Botok[cpair % 2][tt], Bc], [Bbank[bk]])
                                P.cp("act", oT[0][:, cpair, t0 * 128:(t0 + 8) * 128], bv[:], [Bbank[bk]], [BoT[0][t0 // 4], BoT[0][t0 // 4 + 1]])
                dump("d_oTa", oT[0][:], BoT[0])
                if STOP_AFTER == "mla":
                    return

                def hd64_proj(name, col_q, col_k, col_v, QTx, BQTx, KTx, BKTx, Vx, BVx, extra=None):
                    with Scope(P):
                        cos64 = P.sb(name + "cos", [128, T], F32)
                        sin64 = P.sb(name + "sin", [128, T], F32)
                        Btab = NB(name + "tab")
                        P.ld("sp", cos64[:], dr["c_cos64"], [], [Btab])
                        P.ld("sp", sin64[:], dr["c_sin64"], [], [Btab])
                        Wq = P.sb(name + "Wq", [128, KC, 512], BF16)
                        Wk2 = P.sb(name + "Wk2", [128, KC, 2, 128], BF16)
                        Wv = P.sb(name + "Wv", [128, KC, 128], BF16)
                        BW = NB(name + "W")
                        P.ld("pool", Wq[:], w_in_v[:, :, col_q:col_q + 512], [], [BW])
                        for g in range(2):
                            for i in range(2):
                                P.ld("pool", Wk2[:, :, g, i * 64:(i + 1) * 64], w_in_v[:, :, col_k + g * 64: col_k + (g + 1) * 64], [], [BW])
                        P.ld("pool", Wv[:], w_in_v[:, :, col_v:col_v + 128], [], [BW])
                        chunks = []
                        for c in range(4):
                            chunks.append((lambda kc, c=c: Wq[:, kc, c * 128:(c + 1) * 128], lambda G, c=c: QTx[:, c, gsl(G)], BQTx))
                        for g in range(2):
                            chunks.append((lambda kc, g=g: Wk2[:, kc, g, :], lambda G, g=g: KTx[:, g, gsl(G)], BKTx))
                        xw = None
                        if extra is not None:
                            xw = extra(BW, chunks)
                        raw = [P.sb(name + "raw%d" % i, [128, 512], BF16) for i in range(2)]
                        Braw = bufs(name + "raw", 2)
                        t1 = [P.sb(name + "t1_%d" % i, [128, 512], F32) for i in range(2)]
                        t2 = [P.sb(name + "t2_%d" % i, [128, 512], F32) for i in range(2)]
                        Bt1 = bufs(name + "t1", 2)
                        Bt2 = bufs(name + "t2", 2)
                        rrot = Rot([0, 1])
                        brot = Rot([0, 1, 2, 3, 4, 5, 6, 7])
                        for G in range(NG):
                            if STOP_AFTER == name + "w":
                                break
                            for (lf, df, Bd) in chunks:
                                bk = brot.next()
                                proj_fm(lf, 128, G, bk, [BW])
                                k = rrot.next()
                                P.cp("act", raw[k][:], banks[bk][:], [Bbank[bk]], [Braw[k]])
                                bk2 = brot.next()
                                P.mm(banks[bk2][:], pm64[:], raw[k][:], True, True, [Bc, Braw[k]], [Bbank[bk2]])
                                P.tt("dve", t1[k][:], raw[k][:], cos64[:, gsl(G)], ALU.mult, [Braw[k], Btab], [Bt1[k]])
                                P.tt("dve", t2[k][:], banks[bk2][:], sin64[:, gsl(G)], ALU.mult, [Bbank[bk2], Btab], [Bt2[k]])
                                P.tt("dve", df(G), t1[k][:], t2[k][:], ALU.add, [Bt1[k], Bt2[k]], [Bd[G]])
                        BV1 = NB(name + "V1")
                        if STOP_AFTER == name + "rope":
                            return BV1
                        P.memset("dve", Vx[:, :, :, 64:VP], 1.0, [BV1])
                        for tt in range(NT):
                            bk = brot.next()
                            for kc in range(KC):
                                P.mm(banks[bk][:, 0:128], xT[:, kc, tsl(tt)], Wv[:, kc, :], kc == 0, kc == KC - 1, [BxT[tt], BW], [Bbank[bk]])
                            if xw is None and EXPER == "A":
                                for kc in range(KC):
                                    P.mm(banks[bk][:, 128:132], xT[:, kc, tsl(tt)], Wv[:, kc, 0:4], kc == 0, kc == KC - 1, [BxT[tt], BW], [Bbank[bk]])
                            if xw is not None:
                                Wwi, widx, Bwidx = xw
                                for kc in range(KC):
                                    P.mm(banks[bk][:, 128:132], xT[:, kc, tsl(tt)], Wwi[:, kc, :], kc == 0, kc == KC - 1, [BxT[tt], BW], [Bbank[bk]])
                                P.act(widx[:, tt, :], banks[bk][:, 128:132], AF.Copy, [Bbank[bk]], [Bwidx[tt]], scale=1.0 / 16.0)
                            P.cp("act", Vx[:, tt, :, 0:64], banks[bk][:, 0:128].rearrange("p (g d) -> p g d", d=64), [Bbank[bk]], [BVx[tt]])
                        return BV1

                with Scope(P):
                    if SKIP_DSA:
                        raise_skip = True
                    QTb = P.sb("QTb", [128, 4, T], BF16)
                    BQTb = bufs("QTb", NG)
                    KTb = P.sb("KTb", [128, 2, T], BF16)
                    BKTb = bufs("KTb", NG)
                    Vb = P.sb("Vb", [128, NT, 2, VP], BF16)
                    BVb = bufs("Vb", NT)
                    QIT = P.sb("QIT", [128, 2, T], BF16)
                    BQIT = bufs("QIT", NG)
                    KIT = P.sb("KIT", [128, T], BF16)
                    BKIT = bufs("KIT", NG)
                    widx = P.sb("widx", [128, NT, 4], F32)
                    Bwidx = bufs("widx", NT)

                    def extra_b(BW, chunks):
                        Wqi = P.sb("Wqi", [128, KC, 256], BF16)
                        Wki2 = P.sb("Wki2", [128, KC, 128], BF16)
                        Wwi = P.sb("Wwi", [128, KC, 4], BF16)
                        P.ld("pool", Wqi[:], w_in_v[:, :, 1440:1696], [], [BW])
                        for i in range(2):
                            P.ld("pool", Wki2[:, :, i * 64:(i + 1) * 64], w_in_v[:, :, 1696:1760], [], [BW])
                        P.ld("pool", Wwi[:], w_in_v[:, :, 1760:1764], [], [BW])
                        for c in range(2):
                            chunks.append((lambda kc, c=c: Wqi[:, kc, c * 128:(c + 1) * 128], lambda G, c=c: QIT[:, c, gsl(G)], BQIT))
                        chunks.append((lambda kc: Wki2[:, kc, :], lambda G: KIT[:, gsl(G)], BKIT))
                        return (Wwi, widx, Bwidx)

                    if not SKIP_DSA:
                        BVb1 = hd64_proj("b_", 672, 1184, 1312, QTb, BQTb, KTb, BKTb, Vb, BVb, extra_b)
                    if not SKIP_DSA:
                        score32 = [P.sb("score32_%d" % i, [128, 512], F32) for i in range(2)]
                        Bs32 = bufs("score32_", 2)
                        s32rot = Rot([0, 1])
                        scorebf = [P.sb("scorebf%d" % i, [128, T], BF16) for i in range(4)]
                        Bsbf = bufs("scorebf", 4)
                        maskq = [P.sb("maskq%d" % i, [128, T], BF16) for i in range(1)]
                        Bmaskq = bufs("maskq", 1)
                        junkb = P.sb("junkb", [128, T], BF16)
                        Bjunkb = NB("junkb")
                        junka = P.sb("junka", [128, T], BF16)
                        Bjunka = NB("junka")
                        maskTs = [P.sb("maskT0", [128, 12, 512], BF16), P.sb("maskT1", [128, NT, 512], BF16)]
                        BmaskTs = [bufs("maskT0_", 4), bufs("maskT1_", 4)]
                        rr = [P.sb("rr%d" % i, [128, 512], F32) for i in range(2)]
                        Brr = bufs("rr", 2)
                        rrot = Rot([0, 1])
                        bis = P.sb("bis", [128, 16], F32)
                        Bmid = NB("bis_mid")
                        Bval = bufs("bis_val", 4)
                        Btmp = NB("bis_tmp")
                        Bthr = NB("bis_thr")
                        pt = [P.sb("b_pt%d" % i, [128, 512], BF16) for i in range(3)]
                        Bpt = bufs("b_pt", 3)
                        ptrot = Rot([0, 1, 2])
                        srot = Rot([0, 1, 2])
                        arot = Rot([3, 4])
                        irot = Rot([5, 6, 7])
                        otg = [P.sb("b_otg%d" % i, [128, 4, 512], BF16) for i in range(2)]
                        Botg = [bufs("b_otg%d_" % i, 4) for i in range(2)]
                        rec = [P.sb("b_rec%d" % i, [128, 4], F32) for i in range(2)]
                        Brec = bufs("b_rec", 2)
                        recrot = Rot([0, 1])

                        def topk_gen(G):
                            maskT = maskTs[G % 2]
                            BmaskT = BmaskTs[G % 2]
                            nSs = [(4 * G + j + 1) * 128 for j in range(4)]
                            for j in range(4):
                                qt = 4 * G + j
                                nS = nSs[j]
                                nsc = (nS + 511) // 512
                                for sc in range(nsc):
                                    ncol = min(512, nS - sc * 512)
                                    cols = slice(sc * 512, sc * 512 + ncol)
                                    sk = s32rot.next()
                                    s32 = score32[sk]
                                    for ih in range(4):
                                        c, i = ih // 2, ih % 2
                                        bk = irot.next()
                                        P.mm(banks[bk][:, 0:ncol], QIT[i * 64:(i + 1) * 64, c, tsl(qt)], KIT[i * 64:(i + 1) * 64, cols], True, True,
                                             [BQIT[G], BKIT[sc]], [Bbank[bk]])
                                        rk = rrot.next()
                                        r = rr[rk]
                                        P.act(r[:, 0:ncol], banks[bk][:, 0:ncol], AF.Relu, [Bbank[bk]], [Brr[rk]])
                                        if ih == 0:
                                            P.ts("dve", s32[:, 0:ncol], r[:, 0:ncol], widx[:, qt, 0:1], None, ALU.mult, [Brr[rk], Bwidx[qt]], [Bs32[sk]])
                                        elif ih < 3:
                                            P.stt("dve", s32[:, 0:ncol], r[:, 0:ncol], widx[:, qt, ih:ih + 1], s32[:, 0:ncol], ALU.mult, ALU.add,
                                                  [Brr[rk], Bwidx[qt], Bs32[sk]], [Bs32[sk]])
                                        else:
                                            if sc == nsc - 1:
                                                P.tt("dve", s32[:, ncol - 128:ncol], s32[:, ncol - 128:ncol], negdiag[:], ALU.add, [Bs32[sk], Bc], [Bs32[sk]])
                                            P.stt("dve", scorebf[j][:, cols], r[:, 0:ncol], widx[:, qt, 3:4], s32[:, 0:ncol], ALU.mult, ALU.add,
                                                  [Brr[rk], Bwidx[qt], Bs32[sk]], [Bsbf[j]])
                                    yield
                            P.memset("dve", bis[:, 0:4], 0.0, [Bmid])
                            w = W0
                            for it in range(NIT):
                                for j in (2, 3):
                                    P.act(junka[:, 0:nSs[j]], scorebf[j][:, 0:nSs[j]], AF.Sign, [Bsbf[j], Bmid], [Bjunka, Bval[j]],
                                          bias=bis[:, j:j + 1], scale=-1.0, accum=bis[:, 4 + j:5 + j])
                                for j in (0, 1):
                                    P.ts("dve", junkb[:, 0:nSs[j]], scorebf[j][:, 0:nSs[j]], bis[:, j:j + 1], None, ALU.is_ge, [Bsbf[j], Bmid], [Bjunkb, Bval[j]],
                                         op1=ALU.add, accum=bis[:, 4 + j:5 + j])
                                P.ts("dve", bis[:, 8:10], bis[:, 4:6], 256.0, w, ALU.is_ge, [Bval[0], Bval[1]], [Btmp], op1=ALU.mult)
                                for j in (2, 3):
                                    P.ts("dve", bis[:, 8 + j:9 + j], bis[:, 4 + j:5 + j], float(nSs[j] - 512), w, ALU.is_le, [Bval[j]], [Btmp], op1=ALU.mult)
                                P.stt("dve", bis[:, 0:4], bis[:, 8:12], -w / 2, bis[:, 0:4], ALU.add, ALU.add, [Btmp, Bmid], [Bmid])
                                w = w / 2
                                yield
                            P.ts("dve", bis[:, 12:16], bis[:, 0:4], -w, None, ALU.add, [Bmid], [Bthr])
                            for j in range(4):
                                qt = 4 * G + j
                                nS = nSs[j]
                                k2 = 0
                                P.ts("dve", maskq[k2][:, 0:nS], scorebf[j][:, 0:nS], bis[:, 12 + j:13 + j], None, ALU.is_ge, [Bsbf[j], Bthr], [Bmaskq[k2]])
                                for s0 in range(0, qt + 1, 8):
                                    n = min(8, qt + 1 - s0)
                                    bk = irot.next()
                                    bv = banks[bk][:].bitcast(BF16)
                                    for st_ in range(s0, s0 + n):
                                        P.tr(bv[:, (st_ - s0) * 128:(st_ - s0 + 1) * 128], maskq[k2][:, tsl(st_)], ident_bf[:], [Bmaskq[k2], Bc], [Bbank[bk]])
                                    P.cp("act", maskT[:, s0:s0 + n, j * 128:(j + 1) * 128], bv[:, 0:n * 128].rearrange("p (s t) -> p s t", t=128),
                                         [Bbank[bk]], [BmaskT[j]])
                                yield

                        def attn_gen(G):
                            maskT = maskTs[G % 2]
                            BmaskT = BmaskTs[G % 2]
                            og = otg[G % 2]
                            Bog = Botg[G % 2]
                            blocks = [(h, st_) for h in range(8) for st_ in range(4 * G + 4)]
                            info = {}

                            def qk(k):
                                h, st_ = blocks[k]
                                c, i, g = h // 2, h % 2, h // 4
                                j0 = max(0, st_ - 4 * G)
                                ncol = (4 - j0) * 128
                                q0 = (4 * G + j0) * 128
                                sb_ = srot.next()
                                P.mm(banks[sb_][:, 0:ncol], KTb[i * 64:(i + 1) * 64, g, tsl(st_)], QTb[i * 64:(i + 1) * 64, c, q0:q0 + ncol], True, True,
                                     [BKTb[st_ // 4], BQTb[G]], [Bbank[sb_]])
                                info[k] = (sb_, j0, ncol)

                            qk(0)
                            ab = None
                            for k, (h, st_) in enumerate(blocks):
                                g = h // 4
                                if k + 1 < len(blocks):
                                    qk(k + 1)
                                sb_, j0, ncol = info[k]
                                if st_ == 0:
                                    ab = arot.next()
                                accv = banks[ab][:, 0:4 * VP].rearrange("p (j d) -> p j d", d=VP)
                                pk = ptrot.next()
                                P.act(pt[pk][:, 0:ncol], banks[sb_][:, 0:ncol], AF.Exp, [Bbank[sb_]], [Bpt[pk]], scale=0.125)
                                P.tt("pool", pt[pk][:, 0:ncol], pt[pk][:, 0:ncol], maskT[:, st_, j0 * 128:512], ALU.mult,
                                     [Bpt[pk]] + BmaskT[j0:4], [Bpt[pk]])
                                for j in range(j0, 4):
                                    P.mm(accv[:, j, 0:65], pt[pk][:, (j - j0) * 128:(j - j0 + 1) * 128], Vb[:, st_, g, 0:65],
                                         (st_ == 0 and j == 0), st_ == 4 * G + j, [Bpt[pk], BVb[st_], BVb1], [Bbank[ab]], sgc=True)
                                if st_ == 4 * G + 3:
                                    rk = recrot.next()
                                    P.recip(rec[rk][:], accv[:, :, 64], [Bbank[ab]], [Brec[rk]])
                                    for j in range(4):
                                        P.ts("dve", og[:, j, h * 64:(h + 1) * 64], accv[:, j, 0:64], rec[rk][:, j:j + 1], None, ALU.mult,
                                             [Bbank[ab], Brec[rk]], [Bog[j]])
                                yield
                            for j in range(4):
                                tt = 4 * G + j
                                bk = irot.next()
                                bv = banks[bk][:].bitcast(BF16)
                                for c in range(4):
                                    P.tr(bv[:, c * 128:(c + 1) * 128], og[:, j, c * 128:(c + 1) * 128], ident_bf[:], [Bog[j], Bc], [Bbank[bk]])
                                P.cp("act", oT[1][:, :, tsl(tt)], bv[:, 0:512].rearrange("p (c t) -> p c t", t=128), [Bbank[bk]], [BoT[1][G]])
                            yield

                        for _ in topk_gen(0):
                            pass
                        for G in range(NG):
                            ag = list_len = None
                            a_units = 8 * (4 * G + 4) + 1
                            if G + 1 < NG:
                                t_units = sum(((4 * (G + 1) + j + 1) * 128 + 511) // 512 for j in range(4)) + NIT + 4
                                tg = topk_gen(G + 1)
                            else:
                                t_units = 0
                                tg = None
                            done_t = 0
                            for ai, _ in enumerate(attn_gen(G)):
                                if tg is not None:
                                    want = ((ai + 1) * t_units) // a_units
                                    while done_t < want:
                                        try:
                                            next(tg)
                                        except StopIteration:
                                            tg = None
                                            break
                                        done_t += 1
                            if tg is not None:
                                for _ in tg:
                                    pass
                dump("d_oTb", oT[1][:], BoT[1])
                if STOP_AFTER == "dsa":
                    return
                with Scope(P):
                    QTc = P.sb("QTc", [128, 4, T], BF16)
                    BQTc = bufs("QTc", NG)
                    KTc = P.sb("KTc", [128, 2, T], BF16)
                    BKTc = bufs("KTc", NG)
                    Vc = P.sb("Vc", [128, NT, 2, VP], BF16)
                    BVc = bufs("Vc", NT)
                    BVc1 = hd64_proj("c_", 1764, 2276, 2404, QTc, BQTc, KTc, BKTc, Vc, BVc, None)
                    if STOP_AFTER in ("swa_proj", "c_rope", "c_w"):
                        return
                    esink = P.sb("esink", [128, 8], F32)
                    Besink = NB("esink")
                    P.ld("sp", esink[:], dr["c_sinks"][l].partition_broadcast(128), [], [Besink])
                    P.act(esink[:], esink[:], AF.Exp, [Besink], [Besink])
                    smask = P.sb("smask", [128, 2, 2, 128], BF16)
                    Bsm = NB("smask")
                    for i in range(2):
                        P.cp("dve", smask[:, i, 0, :], swaprev_bf[:], [Bc], [Bsm])
                        P.cp("dve", smask[:, i, 1, :], diag_bf[:], [Bc], [Bsm])
                    ptc = [P.sb("c_pt%d" % i, [128, 2, 2, 128], BF16) for i in range(3)]
                    Bptc = bufs("c_pt", 3)
                    ptrot = Rot([0, 1, 2])
                    srot = Rot([0, 1, 2])
                    arot = Rot([3, 4])
                    irot = Rot([5, 6, 7])
                    otc = [P.sb("c_ot%d" % i, [128, 512], BF16) for i in range(2)]
                    Botc = bufs("c_ot", 2)
                    den = [P.sb("c_den%d" % i, [128, 2], F32) for i in range(2)]
                    Bden = bufs("c_den", 2)
                    drot = Rot([0, 1])
                    srot = Rot([0, 1, 2, 5])
                    irot = Rot([6, 7])
                    blocks = [(qt, c) for qt in range(NT) for c in range(4)]
                    info = {}

                    def qk_c(k):
                        qt, c = blocks[k]
                        g = c // 2
                        u0 = 0 if qt > 0 else 1
                        sbs = []
                        for i in range(2):
                            sb_ = srot.next()
                            sv = banks[sb_][:, 0:256].rearrange("p (u t) -> p u t", u=2)
                            for u in range(u0, 2):
                                st_ = qt - 1 + u
                                P.mm(sv[:, u, :], KTc[i * 64:(i + 1) * 64, g, tsl(st_)], QTc[i * 64:(i + 1) * 64, c, tsl(qt)], True, True,
                                     [BKTc[st_ // 4], BQTc[qt // 4]], [Bbank[sb_]])
                            sbs.append((sb_, sv))
                        info[k] = sbs

                    qk_c(0)
                    for k, (qt, c) in enumerate(blocks):
                        if k + 1 < len(blocks):
                            qk_c(k + 1)
                        g = c // 2
                        u0 = 0 if qt > 0 else 1
                        ok = qt % 2
                        pk = ptrot.next()
                        for i in range(2):
                            sb_, sv = info[k][i]
                            P.act(ptc[pk][:, i, u0:2, :], sv[:, u0:2, :], AF.Exp, [Bbank[sb_]], [Bptc[pk]], scale=0.125)
                        P.tt("pool", ptc[pk][:, :, u0:2, :], ptc[pk][:, :, u0:2, :], smask[:, :, u0:2, :], ALU.mult, [Bptc[pk], Bsm], [Bptc[pk]])
                        ab = arot.next()
                        accv = banks[ab][:, 0:2 * VP].rearrange("p (i d) -> p i d", d=VP)
                        for i in range(2):
                            for u in range(u0, 2):
                                st_ = qt - 1 + u
                                P.mm(accv[:, i, 0:65], ptc[pk][:, i, u, :], Vc[:, st_, g, 0:65], (u == u0 and i == 0), u == 1, [Bptc[pk], BVc[st_], BVc1], [Bbank[ab]], sgc=True)
                        dk = drot.next()
                        P.tt("dve", den[dk][:], accv[:, :, 64], esink[:, 2 * c:2 * c + 2], ALU.add, [Bbank[ab], Besink], [Bden[dk]])
                        P.recip(den[dk][:], den[dk][:], [Bden[dk]], [Bden[dk]])
                        for i in range(2):
                            h = 2 * c + i
                            P.ts("dve", otc[ok][:, h * 64:(h + 1) * 64], accv[:, i, 0:64], den[dk][:, i:i + 1], None, ALU.mult,
                                 [Bbank[ab], Bden[dk]], [Botc[ok]])
                        if c == 3:
                            bk = irot.next()
                            bv = banks[bk][:].bitcast(BF16)
                            for c2 in range(4):
                                P.tr(bv[:, c2 * 128:(c2 + 1) * 128], otc[ok][:, c2 * 128:(c2 + 1) * 128], ident_bf[:], [Botc[ok], Bc], [Bbank[bk]])
                            P.cp("act", oT[2][:, :, tsl(qt)], bv[:, 0:512].rearrange("p (c t) -> p c t", t=128), [Bbank[bk]], [BoT[2][qt // 4]])
                dump("d_oTc", oT[2][:], BoT[2])
                if STOP_AFTER == "swa":
                    return
                with Scope(P):
                    wout = P.sb("wout", [128, KC, D], BF16)
                    Bwout = NB("wout")
                    P.ld("pool", wout[:], dr["w_out"][l].rearrange("(kc p) c -> p kc c", p=128), [], [Bwout])
                    g1B = P.sb("g1B", [128, D], F32)
                    b1B = P.sb("b1B", [128, D], F32)
                    Bg1 = NB("g1b1")
                    P.ld("sp", g1B[:], dr["ln1_g"][l].partition_broadcast(128), [], [Bg1])
                    P.ld("sp", b1B[:], dr["ln1_b"][l].partition_broadcast(128), [], [Bg1])
                    wr = P.sb("wr", [128, KC, 16], F32)
                    Bwr = NB("wr")
                    P.ld("sp", wr[:], dr["w_router"].rearrange("(kc p) e -> p kc e", p=128), [], [Bwr])
                    wbr = [P.sb("wbr%d" % i, [128, 3, 4, 128], BF16) for i in range(2)]
                    wgt = [P.sb("wgt%d" % i, [128, 3, KC, 128], BF16) for i in range(2)]
                    Bwm = bufs("wm", 2)
                    mg = P.sb("mg", [128, KC, T], BF16)
                    Bmg = [bufs("mg%d_" % i, NG) for i in range(KC)]
                    sg = [P.sb("sg%d" % i, [128, 512], F32) for i in range(2)]
                    Bsg = bufs("sg", 2)
                    sgrot = Rot([0, 1])
                    macc = P.sb("macc", [128, 512], F32)
                    Bmacc = NB("macc")
                    pre = [P.sb("m_pre%d" % i, [128, D], F32) for i in range(4)]
                    Bpre = bufs("m_pre", 4)
                    st1 = P.sb("m_st", [128, 8, 4], F32)
                    Bsum1 = bufs("m_sum", 4)
                    Bsq1 = bufs("m_sq", 4)
                    Bvec1 = NB("m_vec")
                    junks = [P.sb("m_junk%d" % i, [128, D], BF16)[:] for i in range(2)]
                    Bjunks = bufs("m_junk", 2)
                    xbf = [P.sb("m_xbf%d" % i, [128, D], BF16)[:] for i in range(2)]
                    Bxbf = bufs("m_xbf", 2)
                    x1Tf = P.sb("x1Tf", [128, KC, 128], F32)
                    Bx1Tf = NB("x1Tf")
                    brot = Rot([0, 1, 2, 3, 4, 5, 6, 7])
                    wbr_src = [dr[n][l].rearrange("(kc p) c -> p kc c", p=128) for n in ("w_br_a", "w_br_b", "w_br_c")]
                    def ld_merge(dc):
                        k = dc % 2
                        for i in range(3):
                            P.ld("pool", wbr[k][:, i, :, :], wbr_src[i][:, :, dc * 128:(dc + 1) * 128], [], [Bwm[k]])
                            P.ld("pool", wgt[k][:, i, :, :], w_in_v[:, :, 2532 + i * 1024 + dc * 128: 2532 + i * 1024 + (dc + 1) * 128], [], [Bwm[k]])

                    ld_merge(0)
                    for dc in range(KC):
                        k = dc % 2
                        if dc + 1 < KC:
                            ld_merge(dc + 1)
                        for G in range(NG):
                            for i in range(3):
                                yb = brot.next()
                                for kc in range(4):
                                    P.mm(banks[yb][:], wbr[k][:, i, kc, :], oT[i][:, kc, gsl(G)], kc == 0, kc == 3, [Bwm[k], BoT[i][G]], [Bbank[yb]])
                                gb = brot.next()
                                for kc in range(KC):
                                    P.mm(banks[gb][:], wgt[k][:, i, kc, :], xT[:, kc, gsl(G)], kc == 0, kc == KC - 1,
                                         [Bwm[k]] + [BxT[4 * G + j] for j in range(4)], [Bbank[gb]])
                                sk = sgrot.next()
                                P.act(sg[sk][:], banks[gb][:], AF.Sigmoid, [Bbank[gb]], [Bsg[sk]])
                                if i == 0:
                                    P.tt("dve", macc[:], sg[sk][:], banks[yb][:], ALU.mult, [Bsg[sk], Bbank[yb]], [Bmacc])
                                elif i == 1:
                                    P.tt("dve", sg[sk][:], sg[sk][:], banks[yb][:], ALU.mult, [Bsg[sk], Bbank[yb]], [Bsg[sk]])
                                    P.tt("dve", macc[:], macc[:], sg[sk][:], ALU.add, [Bmacc, Bsg[sk]], [Bmacc])
                                else:
                                    P.tt("dve", sg[sk][:], sg[sk][:], banks[yb][:], ALU.mult, [Bsg[sk], Bbank[yb]], [Bsg[sk]])
                                    P.tt("dve", mg[:, dc, gsl(G)], macc[:], sg[sk][:], ALU.add, [Bmacc, Bsg[sk]], [Bmg[dc][G]])
                    for G in range(NG):
                        items = []
                        for j in range(4):
                            tt = 4 * G + j
                            P.ld("sp", pre[j][:], xres[tsl(tt), :], [Bxres[tt]], [Bpre[j]])
                            items.append((pre[j][:], Bpre[j]))
                        for j in range(4):
                            tt = 4 * G + j
                            for half in range(2):
                                ob = brot.next()
                                for kc in range(KC):
                                    P.mm(banks[ob][:], mg[:, kc, tsl(tt)], wout[:, kc, half * 512:(half + 1) * 512], kc == 0, kc == KC - 1,
                                         [Bmg[kc][G], Bwout], [Bbank[ob]])
                                hs = slice(half * 512, (half + 1) * 512)
                                P.stt("dve", pre[j][:, hs], pre[j][:, hs], ALPHA, banks[ob][:], ALU.mult, ALU.add, [Bpre[j], Bbank[ob]], [Bpre[j]])
                        ln_batch(items, g1B[:], b1B[:], Bg1, st1, Bsum1, Bsq1, Bvec1, junks, Bjunks)
                        for j in range(4):
                            tt = 4 * G + j
                            P.st("sp", xres[tsl(tt), :], pre[j][:], [Bpre[j]], [Bxres[tt]], key=Bpre[j])
                        to_xT_batch(items, [4 * G + j for j in range(4)], xbf, Bxbf, [brot.next() for _ in range(4)])
                        for j in range(4):
                            tt = 4 * G + j
                            tb0 = brot.next()
                            tb1 = brot.next()
                            for kc in range(KC):
                                tb = tb0 if kc < 4 else tb1
                                P.tr(banks[tb][:, (kc % 4) * 128:(kc % 4 + 1) * 128], pre[j][:, kc * 128:(kc + 1) * 128], ident_f[:], [Bpre[j], Bc], [Bbank[tb]])
                            P.cp("act", x1Tf[:, 0:4, :], banks[tb0][:].rearrange("p (k t) -> p k t", t=128), [Bbank[tb0]], [Bx1Tf])
                            P.cp("act", x1Tf[:, 4:8, :], banks[tb1][:].rearrange("p (k t) -> p k t", t=128), [Bbank[tb1]], [Bx1Tf])
                            lb = brot.next()
                            for kc in range(KC):
                                P.mm(banks[lb][:, 0:16], x1Tf[:, kc, :], wr[:, kc, :], kc == 0, kc == KC - 1, [Bx1Tf, Bwr], [Bbank[lb]])
                            P.cp("dve", logit[:, tt, :], banks[lb][:, 0:16], [Bbank[lb]], [Blogit[tt]])
            if STOP_AFTER == "ln1":
                return
            with Scope(P):
                gate = P.sb("gate", [128, NT, 16], F32)
                Bgate = NB("gate")
                with Scope(P):
                    sco = P.sb("r_sco", [128, NT, 16], F32)
                    bia = P.sb("r_bia", [128, NT, 16], F32)
                    mb = P.sb("r_mb", [128, NT, 16], F32)
                    rbias = P.sb("r_bias", [128, 16], F32)
                    ps6 = P.sb("r_ps6", [128, 6, NT, 4], F32)
                    gs = P.sb("r_gs", [128, NT, 4], F32)
                    gmax = P.sb("r_gmax", [128, NT], F32)
                    ing = P.sb("r_ing", [128, NT, 4], F32)
                    red = P.sb("r_red", [128, NT, 8], F32)
                    tmax = P.sb("r_tmax", [128, NT], F32)
                    e1 = P.sb("r_e1", [128, NT, 16], F32)
                    e2 = P.sb("r_e2", [128, NT, 16], F32)
                    Br = NB("routing")
                    R = [Br] + Blogit
                    P.ld("sp", rbias[:], dr["router_bias"].partition_broadcast(128), [], [Br])
                    P.act(sco[:], logit[:], AF.Sigmoid, R, [Br])
                    for tt in range(NT):
                        P.tt("dve", bia[:, tt, :], sco[:, tt, :], rbias[:], ALU.add, [Br], [Br])
                    bg = bia[:].rearrange("p t (g e) -> p t g e", e=4)
                    pairs = [(0, 1), (0, 2), (0, 3), (1, 2), (1, 3), (2, 3)]
                    for pi, (a, b_) in enumerate(pairs):
                        P.tt("dve", ps6[:, pi, :, :], bg[:, :, :, a], bg[:, :, :, b_], ALU.add, [Br], [Br])
                    P.tt("dve", gs[:], ps6[:, 0, :, :], ps6[:, 1, :, :], ALU.max, [Br], [Br])
                    for pi in range(2, 6):
                        P.tt("dve", gs[:], gs[:], ps6[:, pi, :, :], ALU.max, [Br], [Br])
                    P.tt("dve", gmax[:], gs[:, :, 0], gs[:, :, 1], ALU.max, [Br], [Br])
                    P.tt("dve", gmax[:], gmax[:], gs[:, :, 2], ALU.max, [Br], [Br])
                    P.tt("dve", gmax[:], gmax[:], gs[:, :, 3], ALU.max, [Br], [Br])
                    for g in range(4):
                        P.tt("dve", ing[:, :, g], gs[:, :, g], gmax[:], ALU.is_equal, [Br], [Br])
                    P.ts("dve", ing[:], ing[:], -1.0, 1e30, ALU.add, [Br], [Br], op1=ALU.mult)
                    mbg = mb[:].rearrange("p t (g e) -> p t g e", e=4)
                    for e_ in range(4):
                        P.tt("dve", mbg[:, :, :, e_], bg[:, :, :, e_], ing[:], ALU.add, [Br], [Br])

                    def max16(dst, src):
                        P.tt("dve", red[:, :, 0:8], src[:, :, 0:8], src[:, :, 8:16], ALU.max, [Br], [Br])
                        P.tt("dve", red[:, :, 0:4], red[:, :, 0:4], red[:, :, 4:8], ALU.max, [Br], [Br])
                        P.tt("dve", red[:, :, 0:2], red[:, :, 0:2], red[:, :, 2:4], ALU.max, [Br], [Br])
                        P.tt("dve", dst, red[:, :, 0], red[:, :, 1], ALU.max, [Br], [Br])

                    max16(tmax[:], mb)
                    for tt in range(NT):
                        P.ts("dve", e1[:, tt, :], mb[:, tt, :], tmax[:, tt:tt + 1], None, ALU.is_equal, [Br], [Br])
                    P.stt("dve", mb[:], e1[:], -1e30, mb[:], ALU.mult, ALU.add, [Br], [Br])
                    max16(tmax[:], mb)
                    for tt in range(NT):
                        P.ts("dve", e2[:, tt, :], mb[:, tt, :], tmax[:, tt:tt + 1], None, ALU.is_equal, [Br], [Br])
                    P.tt("dve", e1[:], e1[:], e2[:], ALU.add, [Br], [Br])
                    P.tt("dve", e1[:], e1[:], sco[:], ALU.mult, [Br], [Br])
                    P.tt("dve", red[:, :, 0:8], e1[:, :, 0:8], e1[:, :, 8:16], ALU.add, [Br], [Br])
                    P.tt("dve", red[:, :, 0:4], red[:, :, 0:4], red[:, :, 4:8], ALU.add, [Br], [Br])
                    P.tt("dve", red[:, :, 0:2], red[:, :, 0:2], red[:, :, 2:4], ALU.add, [Br], [Br])
                    P.tt("dve", tmax[:], red[:, :, 0], red[:, :, 1], ALU.add, [Br], [Br])
                    P.recip(tmax[:], tmax[:], [Br], [Br])
                    for tt in range(NT):
                        P.ts("dve", gate[:, tt, :], e1[:, tt, :], tmax[:, tt:tt + 1], None, ALU.mult, [Br], [Bgate])
                dump("d_gate", gate[:], [Bgate])
                if STOP_AFTER == "route":
                    return
                acc = P.sb("acc", [128, NT, D], F32)
                Bacc = bufs("acc", NT)
                with Scope(P):
                    Wg = [P.sb("Wg%d" % i, [128, KC, 512], BF16) for i in range(2)]
                    Wu = [P.sb("Wu%d" % i, [128, KC, 512], BF16) for i in range(2)]
                    Wd = [P.sb("Wd%d" % i, [128, 4, D], BF16) for i in range(2)]
                    BWe = bufs("We", 2)
                    actT = [P.sb("actT%d" % i, [128, 4, 512], BF16) for i in range(2)]
                    BactT = [bufs("actT%d_" % i, 4) for i in range(2)]
                    sgm = [P.sb("sgm%d" % i, [128, 512], BF16) for i in range(2)]
                    Bsgm = bufs("sgm", 2)
                    sgrot = Rot([0, 1])
                    hrot = Rot([0, 1, 2, 3])
                    orot = Rot([4, 5, 6, 7])
                    ai = 0
                    for e_ in range(16):
                        k = e_ % 2
                        P.ld("pool", Wg[k][:], dr["w_exp_gate"][l, e_].rearrange("(kc p) f -> p kc f", p=128), [], [BWe[k]])
                        P.ld("pool", Wu[k][:], dr["w_exp_up"][l, e_].rearrange("(kc p) f -> p kc f", p=128), [], [BWe[k]])
                        P.ld("pool", Wd[k][:], dr["w_exp_down"][l, e_].rearrange("(kc p) f -> p kc f", p=128), [], [BWe[k]])
                        for G in range(NG):
                            a = ai % 2
                            ai += 1
                            xr_ = [BxT[4 * G + j] for j in range(4)]
                            for fc in range(4):
                                hb = hrot.next()
                                for kc in range(KC):
                                    P.mm(banks[hb][:], Wg[k][:, kc, fc * 128:(fc + 1) * 128], xT[:, kc, gsl(G)], kc == 0, kc == KC - 1, [BWe[k]] + xr_, [Bbank[hb]])
                                ub = hrot.next()
                                for kc in range(KC):
                                    P.mm(banks[ub][:], Wu[k][:, kc, fc * 128:(fc + 1) * 128], xT[:, kc, gsl(G)], kc == 0, kc == KC - 1, [BWe[k]] + xr_, [Bbank[ub]])
                                sk = sgrot.next()
                                P.act(sgm[sk][:], banks[hb][:], AF.Silu, [Bbank[hb]], [Bsgm[sk]])
                                P.tt("dve", actT[a][:, fc, :], sgm[sk][:], banks[ub][:], ALU.mult, [Bsgm[sk], Bbank[ub]], [BactT[a][fc]])
                            for j in range(4):
                                tt = 4 * G + j
                                for half in range(2):
                                    ob = orot.next()
                                    for fc in range(4):
                                        P.mm(banks[ob][:], actT[a][:, fc, j * 128:(j + 1) * 128], Wd[k][:, fc, half * 512:(half + 1) * 512], fc == 0, fc == 3,
                                             [BactT[a][fc], BWe[k]], [Bbank[ob]])
                                    dst = acc[:, tt, half * 512:(half + 1) * 512]
                                    if e_ == 0:
                                        P.ts("dve", dst, banks[ob][:], gate[:, tt, e_:e_ + 1], None, ALU.mult, [Bbank[ob], Bgate], [Bacc[tt]])
                                    else:
                                        P.stt("dve", dst, banks[ob][:], gate[:, tt, e_:e_ + 1], dst, ALU.mult, ALU.add, [Bbank[ob], Bgate, Bacc[tt]], [Bacc[tt]])
                with Scope(P):
                    g2B = P.sb("g2B", [128, D], F32)
                    b2B = P.sb("b2B", [128, D], F32)
                    Bg2 = NB("g2b2")
                    P.ld("sp", g2B[:], dr["ln2_g"][l].partition_broadcast(128), [], [Bg2])
                    P.ld("sp", b2B[:], dr["ln2_b"][l].partition_broadcast(128), [], [Bg2])
                    xr = [P.sb("f_xr%d" % i, [128, D], F32) for i in range(4)]
                    Bxr = bufs("f_xr", 4)
                    st2 = P.sb("f_st", [128, 8, 4], F32)
                    Bsum2 = bufs("f_sum", 4)
                    Bsq2 = bufs("f_sq", 4)
                    Bvec2 = NB("f_vec")
                    junks = [P.sb("f_junk%d" % i, [128, D], BF16)[:] for i in range(2)]
                    Bjunks = bufs("f_junk", 2)
                    xbf = [P.sb("f_xbf%d" % i, [128, D], BF16)[:] for i in range(2)]
                    Bxbf = bufs("f_xbf", 2)
                    Bstk = bufs("f_stkey", 4)
                    for t0 in range(0, NT, 4):
                        items = []
                        for i in range(4):
                            tt = t0 + i
                            P.ld("sp", xr[i][:], xres[tsl(tt), :], [Bxres[tt]], [Bxr[i]])
                        for i in range(4):
                            tt = t0 + i
                            P.stt("dve", acc[:, tt, :], xr[i][:], ALPHA, acc[:, tt, :], ALU.mult, ALU.add, [Bxr[i], Bacc[tt]], [Bacc[tt]])
                            items.append((acc[:, tt, :], Bacc[tt]))
                        ln_batch(items, g2B[:], b2B[:], Bg2, st2, Bsum2, Bsq2, Bvec2, junks, Bjunks)
                        for i in range(4):
                            tt = t0 + i
                            kb = Bstk[i]
                            if last:
                                final_events.append(P.st("sp", y[tsl(tt), :], acc[:, tt, :], [Bacc[tt]], [kb], key=kb))
                            else:
                                P.st("sp", xres[tsl(tt), :], acc[:, tt, :], [Bacc[tt]], [Bxres[tt], kb], key=kb)
                        if not last:
                            to_xT_batch(items, [t0 + i for i in range(4)], xbf, Bxbf, [0, 1, 2, 3])

        for li, l in enumerate(layers):
            layer(l, li == len(layers) - 1)

        dump("d_xT", xT[:], BxT)
        P.finish(final_events)
        print("build: sems", P.nsem, "ops", {e: len(P.ops[e]) for e in P.ENG})
    return nc


STOP_AFTER = None
EXPER = None
SKIP_DSA = False
SKIP_MLA = False
_NB = {}


def NB(name):
    if name not in _NB:
        _NB[name] = Buf(name)
    return _NB[name]


def prep_inputs(inputs):
    f32 = np.float32
    common = {}
    for k in WEIGHT_SHAPES:
        a = np.ascontiguousarray(np.asarray(inputs[k], dtype=f32))
        if k == "a_q_ln_g":
            a = np.ascontiguousarray(a.reshape(2, 3, 128).transpose(0, 2, 1))
        if k == "a_kv_ln_g":
            a = np.ascontiguousarray(a.reshape(2, 2, 128).transpose(0, 2, 1))
        common[k] = a
    common.update(make_consts())
    return common


_PROG_CACHE = {}


def get_prog(key, *a, **kw):
    if key not in _PROG_CACHE:
        _NB.clear()
        _PROG_CACHE[key] = build_program(*a, **kw)
    return _PROG_CACHE[key]


FUSED = True


def kernel(**inputs):
    x = np.ascontiguousarray(np.asarray(inputs["x"], dtype=np.float32))
    B = x.shape[0]
    common = prep_inputs(inputs)
    cores = list(range(B))
    if FUSED:
        nc = get_prog("fused", [0, 1], True, True)
        maps = [dict(common, x=x[b]) for b in cores]
        res = run_bass_kernel_spmd(nc, maps, core_ids=cores)
        return np.stack([res.results[b]["y"] for b in cores], axis=0).astype(np.float32)
    nc0 = get_prog("l0", [0], True, True)
    maps = [dict(common, x=x[b]) for b in cores]
    res = run_bass_kernel_spmd(nc0, maps, core_ids=cores)
    mid = [res.results[b]["y"] for b in cores]
    nc1 = get_prog("l1", [1], False, True)
    maps = [dict(common, x=np.ascontiguousarray(mid[b])) for b in cores]
    res = run_bass_kernel_spmd(nc1, maps, core_ids=cores)
    return np.stack([res.results[b]["y"] for b in cores], axis=0).astype(np.float32)
```

```python
import contextlib
import numpy as np
import concourse.bass as bass
import concourse.mybir as mybir
from concourse.bass_utils import run_bass_kernel_spmd

F32 = mybir.dt.float32
BF16 = mybir.dt.bfloat16
AF = mybir.ActivationFunctionType
ALU = mybir.AluOpType
AX = mybir.AxisListType


GUARD = True


class Buf:
    __slots__ = ("name", "w", "r", "sem_in", "cnt_in", "sem_out", "cnt_out", "psum")

    def __init__(self, name):
        self.name = name
        self.w = None
        self.r = []
        self.sem_in = None
        self.cnt_in = 0
        self.sem_out = None
        self.cnt_out = 0
        self.psum = False


class Prog:
    ENG = ("pe", "act", "dve", "pool", "sp")

    def __init__(self, nc, es):
        self.nc = nc
        self.es = es
        self.root = es
        self.eng = {"pe": nc.tensor, "act": nc.scalar, "dve": nc.vector,
                    "pool": nc.gpsimd, "sp": nc.sync}
        self.ops = {e: [] for e in self.ENG}
        self.idx = {e: 0 for e in self.ENG}
        self.sem = {e: es.enter_context(nc.semaphore("s_" + e)) for e in self.ENG if e != "sp"}
        self.waited = {e: {} for e in self.ENG}
        self.nsem = 0
        self.uid = 0
        self.last_out_events = []
        self.pending = {e: {} for e in self.ENG}
        self.dma_since = {}
        self.guard = {}
        self.guard_hist = {"act": [], "dve": []}
        self.guard_src = None

    def new_sem(self, name):
        self.nsem += 1
        return self.root.enter_context(self.nc.semaphore("%s_%d" % (name, self.nsem)))

    def sb(self, name, shape, dt):
        self.uid += 1
        return self.es.enter_context(self.nc.sbuf_tensor("%s_%d" % (name, self.uid), list(shape), dt))

    def ps(self, name, shape, dt):
        self.uid += 1
        return self.es.enter_context(self.nc.psum_tensor("%s_%d" % (name, self.uid), list(shape), dt))

    def _collect(self, e, reads, writes, skip_sem=None):
        waits = {}

        def need(ev, raw, waw=False):
            if ev is None:
                return
            sem, val, ee, ii = ev
            if waw and skip_sem is not None and sem is skip_sem:
                return
            if ee == e and ii is not None:
                if e == "pe":
                    return
            k = id(sem)
            if self.waited[e].get(k, 0) >= val:
                return
            if k not in waits or waits[k][1] < val:
                waits[k] = (sem, val)

        for b in reads:
            need(b.w, True)
            if b.psum and e in ("act", "dve"):
                for r in b.r:
                    if r[2] != e:
                        need(r, True)
        for b in writes:
            need(b.w, True, True)
            for r in b.r:
                if GUARD and b.psum and e == "pe" and r[3] is not None and r[2] in ("act", "dve"):
                    r = self._guarded(r)
                need(r, False)
        if self.pending[e]:
            for k, (sem, val) in self.pending[e].items():
                if self.waited[e].get(k, 0) >= val:
                    continue
                if k not in waits or waits[k][1] < val:
                    waits[k] = (sem, val)
            self.pending[e] = {}
        for k, (sem, val) in waits.items():
            self.waited[e][k] = val
        return list(waits.values())

    def _commit(self, ev, reads, writes):
        for b in reads:
            b.r.append(ev)
        for b in writes:
            b.w = ev
            b.r = []

    def _guarded(self, r):
        sem, val, E, ii = r
        if self.idx[E] > ii + 1:
            return (sem, ii + 2, E, ii + 1)
        hist = self.guard_hist[E]
        n = len(hist)
        g = self.guard[E][:, (n % 8):(n % 8) + 1]
        gw = []
        if n >= 8:
            k = id(self.sem[E])
            need = hist[n - 8] + 1
            if self.waited[E].get(k, 0) < need:
                gw.append((self.sem[E], need))
                self.waited[E][k] = need
        if E == "act":
            src, bsrc = self.guard_src
            sv = bsrc.w
            if sv is not None and self.waited[E].get(id(sv[0]), 0) < sv[1]:
                gw.append((sv[0], sv[1]))
                self.waited[E][id(sv[0])] = sv[1]
        i2 = self.idx[E]
        self.idx[E] = i2 + 1
        hist.append(i2)
        if E == "act":
            self.ops[E].append((gw, (lambda en, g=g, src=src: en.activation(out=g, in_=src, func=AF.Copy)), (self.sem[E], 1)))
        else:
            self.ops[E].append((gw, (lambda en, g=g: en.memset(g, 0.0)), (self.sem[E], 1)))
        return (self.sem[E], i2 + 1, E, i2)

    def op(self, e, fn, reads=(), writes=()):
        waits = self._collect(e, reads, writes)
        i = self.idx[e]
        self.idx[e] = i + 1
        ev = (self.sem[e], i + 1, e, i)
        self.ops[e].append((waits, fn, (self.sem[e], 1)))
        self._commit(ev, reads, writes)
        return ev

    def dma(self, e, fn, reads=(), writes=(), key=None):
        if key is None:
            key = writes[0] if writes else reads[0]
        if key.sem_in is None:
            key.sem_in = {}
        if e not in key.sem_in:
            key.sem_in[e] = [self.new_sem("d%s_%s" % (e, key.name)), 0]
        ent = key.sem_in[e]
        sem = ent[0]
        waits = self._collect(e, reads, writes, skip_sem=sem)
        ent[1] += 16
        val = ent[1]
        ev = (sem, val, e, None)
        self.dma_since[id(sem)] = (sem, val)
        self.ops[e].append((waits, fn, (sem, 16)))
        self._commit(ev, reads, writes)
        return ev

    def mm(self, out, lhsT, rhs, start, stop, reads, writes, sgc=False):
        if sgc:
            return self.op("pe", lambda e: e.matmul(out, lhsT=lhsT, rhs=rhs, start=start, stop=stop, skip_group_check=True), reads, writes)
        return self.op("pe", lambda e: e.matmul(out, lhsT=lhsT, rhs=rhs, start=start, stop=stop), reads, writes)

    def tr(self, out, in_, ident, reads, writes):
        return self.op("pe", lambda e: e.transpose(out, in_, ident), reads, writes)

    def act(self, out, in_, func, reads, writes, bias=None, scale=None, accum=None):
        kw = {}
        if bias is not None:
            kw["bias"] = bias
        if scale is not None:
            kw["scale"] = scale
        if accum is not None:
            kw["accum_out"] = accum
        return self.op("act", lambda e: e.activation(out=out, in_=in_, func=func, **kw), reads, writes)

    def ts(self, eng, out, in0, s1, s2, op0, reads, writes, op1=None, accum=None):
        kw = {}
        if op1 is not None:
            kw["op1"] = op1
        if accum is not None:
            kw["accum_out"] = accum
        return self.op(eng, lambda e: e.tensor_scalar(out=out, in0=in0, scalar1=s1, scalar2=s2, op0=op0, **kw), reads, writes)

    def tt(self, eng, out, in0, in1, op, reads, writes):
        return self.op(eng, lambda e: e.tensor_tensor(out=out, in0=in0, in1=in1, op=op), reads, writes)

    def stt(self, eng, out, in0, scalar, in1, op0, op1, reads, writes):
        return self.op(eng, lambda e: e.scalar_tensor_tensor(out=out, in0=in0, scalar=scalar, in1=in1, op0=op0, op1=op1), reads, writes)

    def cp(self, eng, out, in_, reads, writes):
        if eng == "act":
            return self.op("act", lambda e: e.activation(out=out, in_=in_, func=AF.Copy), reads, writes)
        return self.op(eng, lambda e: e.tensor_copy(out=out, in_=in_), reads, writes)

    def memset(self, eng, ap, val, writes):
        return self.op(eng, lambda e: e.memset(ap, val), (), writes)

    def recip(self, out, in_, reads, writes):
        return self.op("dve", lambda e: e.reciprocal(out=out, in_=in_), reads, writes)

    def ld(self, q, out, in_, reads, writes):
        return self.dma(q, lambda e: e.dma_start(out=out, in_=in_), reads, writes)

    def st(self, q, out, in_, reads, writes, key):
        return self.dma(q, lambda e: e.dma_start(out=out, in_=in_), reads, writes, key=key)

    def fence(self):
        for e in self.ENG:
            pend = self.pending[e]
            for f in self.ENG:
                if f == "sp" or f == e or self.idx[f] == 0:
                    continue
                k = id(self.sem[f])
                pend[k] = (self.sem[f], self.idx[f])
            for k, sv in self.dma_since.items():
                if k not in pend or pend[k][1] < sv[1]:
                    pend[k] = sv
        self.dma_since = {}

    def finish(self, final_events):
        nc = self.nc
        ops = self.ops
        with nc.Block() as block:
            def emit(engname, eng):
                for waits, fn, (sem, inc) in ops[engname]:
                    for (s, v) in waits:
                        eng.wait_ge(s, v)
                    ins = fn(eng)
                    ins.then_inc(sem, inc)

            @block.tensor
            def _(eng):
                emit("pe", eng)

            @block.scalar
            def _(eng):
                emit("act", eng)

            @block.vector
            def _(eng):
                emit("dve", eng)

            @block.gpsimd
            def _(eng):
                emit("pool", eng)

            @block.sync
            def _(eng):
                emit("sp", eng)
                for (s, v, _e, _i) in final_events:
                    eng.wait_ge(s, v)

T = 2048
D = 1024
NT = 16
NG = 4
KC = 8
LN_EPS = 1e-5
RMS_EPS = 1e-6
DEPTH = 2
ALPHA = (2.0 * DEPTH) ** 0.25
IN_COLS = 5604
VP = 68
NIT = 18
W0 = 64.0


def make_consts():
    f32 = np.float32
    c = {}
    c["c_ident"] = np.eye(128, dtype=f32)
    c["c_ones"] = np.ones((128, 128), f32)
    t = np.arange(T, dtype=f32)
    p = np.arange(128)
    i64 = ((p % 64) % 32).astype(f32)
    inv64 = np.power(f32(10000.0), -(i64 / f32(32.0))).astype(f32)
    ang = (t[None, :] * inv64[:, None]).astype(f32).astype(np.float64)
    c["c_cos64"] = np.cos(ang).astype(f32)
    c["c_sin64"] = np.sin(ang).astype(f32)
    iA = ((p - 64) % 16).astype(f32)
    invA = np.power(f32(10000.0), -(iA / f32(16.0))).astype(f32)
    angA = (t[None, :] * invA[:, None]).astype(f32).astype(np.float64)
    c["c_cosA"] = np.cos(angA).astype(f32)
    c["c_sinA"] = np.sin(angA).astype(f32)
    pm = np.zeros((128, 128), f32)
    for fp in range(128):
        d = fp % 64
        if d < 32:
            pm[fp + 32, fp] = -1.0
        else:
            pm[fp - 32, fp] = 1.0
    c["c_pm64"] = pm
    pa = np.zeros((128, 128), f32)
    for fp in range(64, 96):
        d = fp - 64
        if d < 16:
            pa[fp + 16, fp] = -1.0
        else:
            pa[fp - 16, fp] = 1.0
    c["c_pmA"] = pa
    s = np.arange(128)[:, None]
    q = np.arange(128)[None, :]
    c["c_diag"] = ((s < 64) | (q >= 64)).astype(f32)
    c["c_swaprev"] = (~((s < 64) & (q >= 64))).astype(f32)
    tt_ = np.arange(128)[:, None]
    ss_ = np.arange(128)[None, :]
    c["c_negdiag"] = np.where((tt_ < 64) & (ss_ >= 64), f32(-1e30), f32(0.0)).astype(f32)
    return c


CONST_SHAPES = {"c_ident": [128, 128], "c_ones": [128, 128], "c_cos64": [128, T], "c_sin64": [128, T],
                "c_cosA": [128, T], "c_sinA": [128, T], "c_pm64": [128, 128], "c_pmA": [128, 128],
                "c_diag": [128, 128], "c_swaprev": [128, 128], "c_negdiag": [128, 128]}

WEIGHT_SHAPES = {
    "ln_in_g": [D], "ln_in_b": [D], "w_in": [2, D, IN_COLS],
    "a_q_ln_g": [2, 128, 3], "a_kv_ln_g": [2, 128, 2],
    "a_w_uq": [2, 384, 768], "a_w_ukv": [2, 256, 1024], "c_sinks": [2, 8],
    "w_br_a": [2, 512, D], "w_br_b": [2, 512, D], "w_br_c": [2, 512, D], "w_out": [2, D, D],
    "ln1_g": [2, D], "ln1_b": [2, D], "w_router": [D, 16], "router_bias": [16],
    "w_exp_gate": [2, 16, D, 512], "w_exp_up": [2, 16, D, 512], "w_exp_down": [2, 16, 512, D],
    "ln2_g": [2, D], "ln2_b": [2, D],
}


class Scope:
    def __init__(self, P):
        self.P = P

    def __enter__(self):
        self.prev = self.P.es
        self.stack = contextlib.ExitStack()
        self.stack.__enter__()
        self.P.es = self.stack
        return self

    def __exit__(self, *a):
        self.P.es = self.prev
        self.P.fence()
        return self.stack.__exit__(*a)


def bufs(name, n):
    return [NB("%s%d" % (name, i)) for i in range(n)]


def build_program(layers, do_ln_in, final, dbg_names=()):
    nc = bass.Bass("TRN2", target_bir_lowering=False)
    dr = {}
    dr["x"] = nc.dram_tensor("x", [T, D], F32, kind="ExternalInput").ap()
    for k, shp in WEIGHT_SHAPES.items():
        dr[k] = nc.dram_tensor(k, shp, F32, kind="ExternalInput").ap()
    for k, shp in CONST_SHAPES.items():
        dr[k] = nc.dram_tensor(k, shp, F32, kind="ExternalInput").ap()
    y = nc.dram_tensor("y", [T, D], F32, kind="ExternalOutput").ap()
    xres = nc.dram_tensor("xres", [T, D], F32).ap()
    dbg = {}
    DBG_SHAPES = {"d_xT": ([128, KC, T], BF16), "d_oTa": ([128, 4, T], BF16), "d_oTb": ([128, 4, T], BF16),
                  "d_oTc": ([128, 4, T], BF16), "d_gate": ([128, NT, 16], F32)}
    for k in dbg_names:
        shp, dt_ = DBG_SHAPES[k]
        dbg[k] = nc.dram_tensor(k, shp, dt_, kind="ExternalOutput").ap()

    es = contextlib.ExitStack()
    with es:
        P = Prog(nc, es)
        final_events = []
        banks = [P.ps("bank%d" % i, [128, 512], F32) for i in range(8)]
        Bbank = bufs("bank", 8)
        for b_ in Bbank:
            b_.psum = True
        P.guard["act"] = P.sb("guard_act", [128, 8], F32)[:]
        P.guard["dve"] = P.sb("guard_dve", [128, 8], F32)[:]
        xT = P.sb("xT", [128, KC, T], BF16)
        BxT = bufs("xT", NT)
        Bxres = bufs("xres", NT)
        Bc = NB("consts")
        ident_bf = P.sb("ident_bf", [128, 128], BF16)
        ident_f = P.sb("ident_f", [128, 128], F32)
        ones_bf = P.sb("ones_bf", [128, 128], BF16)
        pm64 = P.sb("pm64", [128, 128], BF16)
        pmA = P.sb("pmA", [128, 128], BF16)
        diag_bf = P.sb("diag_bf", [128, 128], BF16)
        swaprev_bf = P.sb("swaprev_bf", [128, 128], BF16)
        negdiag = P.sb("negdiag", [128, 128], F32)
        P.ld("pool", ident_bf[:], dr["c_ident"], [], [Bc])
        P.ld("pool", ones_bf[:], dr["c_ones"], [], [Bc])
        P.ld("pool", pm64[:], dr["c_pm64"], [], [Bc])
        P.ld("pool", pmA[:], dr["c_pmA"], [], [Bc])
        P.ld("pool", diag_bf[:], dr["c_diag"], [], [Bc])
        P.ld("pool", swaprev_bf[:], dr["c_swaprev"], [], [Bc])
        P.ld("sp", ident_f[:], dr["c_ident"], [], [Bc])
        P.ld("sp", negdiag[:], dr["c_negdiag"], [], [Bc])
        P.guard_src = (ident_f[:, 0:1], Bc)

        def gsl(G):
            return slice(G * 512, (G + 1) * 512)

        def tsl(tt):
            return slice(tt * 128, (tt + 1) * 128)

        class Rot:
            def __init__(self, items):
                self.items = items
                self.i = 0

            def next(self):
                it = self.items[self.i % len(self.items)]
                self.i += 1
                return it

        def dump(name, ap_sb, rbufs):
            if name in dbg:
                final_events.append(P.st("sp", dbg[name], ap_sb, rbufs, [], key=NB("dbg_" + name)))

        def ln_inplace(t_ap, Bt, gB, bB, Bgb, st, Bst, junk, Bjunk):
            P.act(junk, t_ap, AF.Copy, [Bt], [Bjunk, Bst], accum=st[:, 0:1])
            P.act(junk, t_ap, AF.Square, [Bt], [Bjunk, Bst], accum=st[:, 1:2])
            P.ts("dve", st[:, 2:3], st[:, 0:1], 1.0 / D, None, ALU.mult, [Bst], [Bst])
            P.tt("dve", st[:, 3:4], st[:, 2:3], st[:, 2:3], ALU.mult, [Bst], [Bst])
            P.stt("dve", st[:, 4:5], st[:, 1:2], 1.0 / D, st[:, 3:4], ALU.mult, ALU.subtract, [Bst], [Bst])
            P.ts("dve", st[:, 4:5], st[:, 4:5], LN_EPS, None, ALU.add, [Bst], [Bst])
            P.act(st[:, 5:6], st[:, 4:5], AF.Sqrt, [Bst], [Bst])
            P.recip(st[:, 6:7], st[:, 5:6], [Bst], [Bst])
            P.stt("dve", st[:, 7:8], st[:, 2:3], -1.0, st[:, 6:7], ALU.mult, ALU.mult, [Bst], [Bst])
            P.act(t_ap, t_ap, AF.Identity, [Bt, Bst], [Bt], scale=st[:, 6:7], bias=st[:, 7:8])
            P.tt("dve", t_ap, t_ap, gB, ALU.mult, [Bt, Bgb], [Bt])
            P.tt("dve", t_ap, t_ap, bB, ALU.add, [Bt, Bgb], [Bt])

        def to_xT(t_ap, Bt, tt, xbf, Bxbf, bk):
            P.cp("act", xbf, t_ap, [Bt], [Bxbf])
            bv = banks[bk][:].bitcast(BF16)
            for kc in range(KC):
                P.tr(bv[:, kc * 128:(kc + 1) * 128], xbf[:, kc * 128:(kc + 1) * 128], ident_bf[:],
                     [Bxbf, Bc], [Bbank[bk]])
            P.cp("dve", xT[:, :, tsl(tt)], bv.rearrange("p (k t) -> p k t", t=128), [Bbank[bk]], [BxT[tt]])

        def ln_batch(items, gB, bB, Bgb, st, Bsum, Bsq, Bvec, junks, Bjunks):
            nb = len(items)
            for i, (t_ap, Bt) in enumerate(items):
                P.act(junks[i % 2], t_ap, AF.Copy, [Bt], [Bjunks[i % 2], Bsum[i]], accum=st[:, 0, i:i + 1])
            for i, (t_ap, Bt) in enumerate(items):
                P.act(junks[i % 2], t_ap, AF.Square, [Bt], [Bjunks[i % 2], Bsq[i]], accum=st[:, 1, i:i + 1])
            V = [Bvec]
            P.ts("dve", st[:, 2, 0:nb], st[:, 0, 0:nb], 1.0 / D, None, ALU.mult, Bsum[0:nb], V)
            P.tt("dve", st[:, 3, 0:nb], st[:, 2, 0:nb], st[:, 2, 0:nb], ALU.mult, V, V)
            P.stt("dve", st[:, 4, 0:nb], st[:, 1, 0:nb], 1.0 / D, st[:, 3, 0:nb], ALU.mult, ALU.subtract, Bsq[0:nb] + V, V)
            P.ts("dve", st[:, 4, 0:nb], st[:, 4, 0:nb], LN_EPS, None, ALU.add, V, V)
            P.act(st[:, 5, 0:nb], st[:, 4, 0:nb], AF.Sqrt, V, V)
            P.recip(st[:, 6, 0:nb], st[:, 5, 0:nb], V, V)
            P.stt("dve", st[:, 7, 0:nb], st[:, 2, 0:nb], -1.0, st[:, 6, 0:nb], ALU.mult, ALU.mult, V, V)
            for i, (t_ap, Bt) in enumerate(items):
                P.act(t_ap, t_ap, AF.Identity, [Bt, Bvec], [Bt], scale=st[:, 6, i:i + 1], bias=st[:, 7, i:i + 1])
            for i, (t_ap, Bt) in enumerate(items):
                P.tt("dve", t_ap, t_ap, gB, ALU.mult, [Bt, Bgb], [Bt])
            for i, (t_ap, Bt) in enumerate(items):
                P.tt("dve", t_ap, t_ap, bB, ALU.add, [Bt, Bgb], [Bt])

        def to_xT_batch(items, tts, xbfs, Bxbfs, bks):
            for i, (t_ap, Bt) in enumerate(items):
                P.cp("act", xbfs[i % len(xbfs)], t_ap, [Bt], [Bxbfs[i % len(xbfs)]])
                bk = bks[i % len(bks)]
                bv = banks[bk][:].bitcast(BF16)
                xb = xbfs[i % len(xbfs)]
                for kc in range(KC):
                    P.tr(bv[:, kc * 128:(kc + 1) * 128], xb[:, kc * 128:(kc + 1) * 128], ident_bf[:],
                         [Bxbfs[i % len(xbfs)], Bc], [Bbank[bk]])
                P.cp("dve", xT[:, :, tsl(tts[i])], bv.rearrange("p (k t) -> p k t", t=128), [Bbank[bk]], [BxT[tts[i]]])

        with Scope(P):
            gB = P.sb("ln0_g", [128, D], F32)
            bB = P.sb("ln0_b", [128, D], F32)
            Bgb = NB("ln0gb")
            if do_ln_in:
                P.ld("sp", gB[:], dr["ln_in_g"].partition_broadcast(128), [], [Bgb])
                P.ld("sp", bB[:], dr["ln_in_b"].partition_broadcast(128), [], [Bgb])
            xt = [P.sb("in_x%d" % i, [128, D], F32) for i in range(4)]
            Bxt = bufs("in_x", 4)
            stt_ = P.sb("in_st", [128, 8, 4], F32)
            Bsum = bufs("in_sum", 4)
            Bsq = bufs("in_sq", 4)
            Bvec = NB("in_vec")
            junks = [P.sb("in_junk%d" % i, [128, D], BF16)[:] for i in range(2)]
            Bjunks = bufs("in_junk", 2)
            xbf = [P.sb("in_xbf%d" % i, [128, D], BF16)[:] for i in range(2)]
            Bxbf = bufs("in_xbf", 2)
            for t0 in range(0, NT, 4):
                items = []
                for i in range(4):
                    P.ld("sp", xt[i][:], dr["x"][tsl(t0 + i), :], [], [Bxt[i]])
                    items.append((xt[i][:], Bxt[i]))
                if do_ln_in:
                    ln_batch(items, gB[:], bB[:], Bgb, stt_, Bsum, Bsq, Bvec, junks, Bjunks)
                for i in range(4):
                    P.st("sp", xres[tsl(t0 + i), :], xt[i][:], [Bxt[i]], [Bxres[t0 + i]], key=Bxt[i])
                to_xT_batch(items, [t0 + i for i in range(4)], xbf, Bxbf, [0, 1, 2, 3])

        def load_w(q, dst_ap, src_ap, Bw):
            P.ld(q, dst_ap, src_ap, [], [Bw])

        def proj_fm(lhsT_of_kc, M, G, bk, wr):
            for kc in range(KC):
                P.mm(banks[bk][0:M, :], lhsT_of_kc(kc), xT[:, kc, gsl(G)], kc == 0, kc == KC - 1,
                     [BxT[4 * G + j] for j in range(4)] + wr, [Bbank[bk]])

        def layer(l, last):
            w_in_v = dr["w_in"][l].rearrange("(kc p) c -> p kc c", p=128)
            logit = P.sb("logit", [128, NT, 16], F32)
            Blogit = bufs("logit", NT)
            with Scope(P):
                oT = [P.sb("oT%d" % i, [128, 4, T], BF16) for i in range(3)]
                BoT = [bufs("oT%d_" % i, NG) for i in range(3)]
                with Scope(P):
                    aqn = P.sb("aqn", [128, 3, T], BF16)
                    Baqn = bufs("aqn", NG)
                    akvn = P.sb("akvn", [128, 2, T], BF16)
                    Bakvn = bufs("akvn", NG)
                    kpe = P.sb("kpe", [96, T], BF16)
                    Bkpe = bufs("kpe", NG)
                    cosA = P.sb("cosA", [128, T], F32)
                    sinA = P.sb("sinA", [128, T], F32)
                    Btab = NB("tabA")
                    P.ld("sp", cosA[:], dr["c_cosA"], [], [Btab])
                    P.ld("sp", sinA[:], dr["c_sinA"], [], [Btab])
                    wuq = P.sb("wuq", [128, 3, 768], BF16)
                    wukv = P.sb("wukv", [128, 2, 1024], BF16)
                    Bwu = NB("wu")
                    P.ld("pool", wuq[:], dr["a_w_uq"][l].rearrange("(kc p) c -> p kc c", p=128), [], [Bwu])
                    P.ld("pool", wukv[:], dr["a_w_ukv"][l].rearrange("(kc p) c -> p kc c", p=128), [], [Bwu])
                    t1 = [P.sb("a_t1_%d" % i, [128, 512], F32) for i in range(2)]
                    t2 = [P.sb("a_t2_%d" % i, [128, 512], F32) for i in range(2)]
                    Bt1 = bufs("a_t1", 2)
                    Bt2 = bufs("a_t2", 2)
                    trot = Rot([0, 1])
                    with Scope(P):
                        Wa = P.sb("Wa", [128, KC, 672], BF16)
                        BWa = NB("Wa")
                        P.ld("pool", Wa[:], w_in_v[:, :, 0:672], [], [BWa])
                        gq = P.sb("gq", [128, 3], F32)
                        gkv = P.sb("gkv", [128, 2], F32)
                        Bg = NB("gqkv")
                        P.ld("sp", gq[:], dr["a_q_ln_g"][l], [], [Bg])
                        P.ld("sp", gkv[:], dr["a_kv_ln_g"][l], [], [Bg])
                        sq = P.sb("sq", [128, 3, 512], BF16)
                        Bsq = bufs("sq", 3)
                        rs = P.sb("rs", [128, 512], F32)
                        Brs = NB("rs")
                        kraw = P.sb("kraw", [96, 512], BF16)
                        Bkraw = NB("kraw")
                        P.memset("dve", kraw[:], 0.0, [Bkraw])
                        brot = Rot([0, 1, 2, 3])
                        for G in range(NG):
                            for (dst, Bdst, nch, col0, g_ap, nfeat) in ((aqn, Baqn, 3, 0, gq, 384.0), (akvn, Bakvn, 2, 384, gkv, 256.0)):
                                for c in range(nch):
                                    bk = brot.next()
                                    proj_fm(lambda kc, c=c, col0=col0: Wa[:, kc, col0 + c * 128: col0 + (c + 1) * 128], 128, G, bk, [BWa])
                                    P.cp("act", dst[:, c, gsl(G)], banks[bk][:], [Bbank[bk]], [Bdst[G]])
                                    P.act(sq[:, c, :], banks[bk][:], AF.Square, [Bbank[bk]], [Bsq[c]])
                                bk = brot.next()
                                for c in range(nch):
                                    P.mm(banks[bk][:], ones_bf[:], sq[:, c, :], c == 0, c == nch - 1, [Bc, Bsq[c]], [Bbank[bk]])
                                P.ts("dve", rs[:], banks[bk][:], 1.0 / nfeat, RMS_EPS, ALU.mult, [Bbank[bk]], [Brs], op1=ALU.add)
                                P.act(rs[:], rs[:], AF.Sqrt, [Brs], [Brs])
                                P.recip(rs[:], rs[:], [Brs], [Brs])
                                for c in range(nch):
                                    P.stt("dve", dst[:, c, gsl(G)], dst[:, c, gsl(G)], g_ap[:, c:c + 1], rs[:], ALU.mult, ALU.mult,
                                          [Bdst[G], Bg, Brs], [Bdst[G]])
                            bk = brot.next()
                            proj_fm(lambda kc: Wa[:, kc, 576:672], 96, G, bk, [BWa])
                            P.cp("act", kraw[64:96, :], banks[bk][64:96, :], [Bbank[bk]], [Bkraw])
                            bk2 = brot.next()
                            P.mm(banks[bk2][0:96, :], pmA[0:96, 0:96], kraw[0:96, :], True, True, [Bc, Bkraw], [Bbank[bk2]])
                            k = trot.next()
                            P.tt("dve", t1[k][64:96, :], kraw[64:96, :], cosA[64:96, gsl(G)], ALU.mult, [Bkraw, Btab], [Bt1[k]])
                            P.tt("dve", t2[k][64:96, :], banks[bk2][64:96, :], sinA[64:96, gsl(G)], ALU.mult, [Bbank[bk2], Btab], [Bt2[k]])
                            P.tt("dve", kpe[64:96, gsl(G)], t1[k][64:96, :], t2[k][64:96, :], ALU.add, [Bt1[k], Bt2[k]], [Bkpe[G]])
                    Va = P.sb("Va", [128, NT, 8, VP], BF16)
                    BVa = bufs("Va", NT)
                    BVa1 = NB("Va_ones")
                    P.memset("dve", Va[:, :, :, 64:VP], 1.0, [BVa1])
                    wukv_v = wukv[:].rearrange("p k (h d) -> p k h d", d=128)
                    brot = Rot([5, 6, 7])
                    for tt in range(NT):
                        bk = brot.next()
                        for kc in range(2):
                            P.mm(banks[bk][:].rearrange("p (h d) -> p h d", d=64), akvn[:, kc, tsl(tt)], wukv_v[:, kc, :, 64:128],
                                 kc == 0, kc == 1, [Bakvn[tt // 4], Bwu], [Bbank[bk]])
                        P.cp("act", Va[:, tt, :, 0:64], banks[bk][:].rearrange("p (h d) -> p h d", d=64), [Bbank[bk]], [BVa[tt]])
                    QT = [P.sb("QTa%d" % i, [96, T], BF16) for i in range(2)]
                    KT = [P.sb("KTa%d" % i, [96, T], BF16) for i in range(2)]
                    BQT = [bufs("QTa%d_" % i, NG) for i in range(2)]
                    BKT = [bufs("KTa%d_" % i, NG) for i in range(2)]
                    pt = [P.sb("a_pt%d" % i, [128, 512], BF16) for i in range(3)]
                    Bpt = bufs("a_pt", 3)
                    ptrot = Rot([0, 1, 2])
                    srot = Rot([0, 1, 2])
                    arot = Rot([3, 4])
                    otok = [P.sb("a_otok%d" % i, [128, NT, 128], BF16) for i in range(2)]
                    Botok = [bufs("a_otok%d_" % i, NT) for i in range(2)]
                    rec = [P.sb("a_rec%d" % i, [128, 4], F32) for i in range(2)]
                    Brec = bufs("a_rec", 2)
                    recrot = Rot([0, 1])
                    sc_a = 96.0 ** -0.5
                    def proj_head(h):
                        hp = h % 2
                        for G in range(NG):
                            bk = brot.next()
                            for kc in range(3):
                                P.mm(banks[bk][0:96, :], wuq[:, kc, h * 96:(h + 1) * 96], aqn[:, kc, gsl(G)], kc == 0, kc == 2,
                                     [Bwu, Baqn[G]], [Bbank[bk]])
                            P.cp("act", QT[hp][0:96, gsl(G)], banks[bk][0:96, :], [Bbank[bk]], [BQT[hp][G]])
                            bk2 = brot.next()
                            P.mm(banks[bk2][0:96, :], pmA[0:96, 0:96], QT[hp][0:96, gsl(G)], True, True, [Bc, BQT[hp][G]], [Bbank[bk2]])
                            k = trot.next()
                            P.tt("dve", t1[k][64:96, :], QT[hp][64:96, gsl(G)], cosA[64:96, gsl(G)], ALU.mult, [BQT[hp][G], Btab], [Bt1[k]])
                            P.tt("dve", t2[k][64:96, :], banks[bk2][64:96, :], sinA[64:96, gsl(G)], ALU.mult, [Bbank[bk2], Btab], [Bt2[k]])
                            P.tt("dve", QT[hp][64:96, gsl(G)], t1[k][64:96, :], t2[k][64:96, :], ALU.add, [Bt1[k], Bt2[k]], [BQT[hp][G]])
                            bk = brot.next()
                            for kc in range(2):
                                P.mm(banks[bk][0:64, :], wukv[:, kc, h * 128:h * 128 + 64], akvn[:, kc, gsl(G)], kc == 0, kc == 1,
                                     [Bwu, Bakvn[G]], [Bbank[bk]])
                            P.cp("act", KT[hp][0:64, gsl(G)], banks[bk][0:64, :], [Bbank[bk]], [BKT[hp][G]])
                            P.cp("dve", KT[hp][64:96, gsl(G)], kpe[64:96, gsl(G)], [Bkpe[G]], [BKT[hp][G]])

                    blocks = [(G, st_) for G in range(NG) for st_ in range(4 * G + 4)]
                    proj_head(0)
                    for h in range(8):
                        hp = h % 2
                        cpair = h // 2
                        if h + 1 < 8:
                            proj_head(h + 1)
                        info = {}

                        def qk(k):
                            G, st_ = blocks[k]
                            j0 = max(0, st_ - 4 * G)
                            ncol = (4 - j0) * 128
                            q0 = (4 * G + j0) * 128
                            sb_ = srot.next()
                            P.mm(banks[sb_][:, 0:ncol], KT[hp][0:96, tsl(st_)], QT[hp][0:96, q0:q0 + ncol], True, True,
                                 [BKT[hp][st_ // 4], BQT[hp][G]], [Bbank[sb_]])
                            info[k] = (sb_, j0, ncol)

                        qk(0)
                        ab = None
                        for k, (G, st_) in enumerate(blocks):
                            if k + 1 < len(blocks):
                                qk(k + 1)
                            sb_, j0, ncol = info[k]
                            if st_ == 0:
                                ab = arot.next()
                            accv = banks[ab][:, 0:4 * VP].rearrange("p (j d) -> p j d", d=VP)
                            pk = ptrot.next()
                            P.act(pt[pk][:, 0:ncol], banks[sb_][:, 0:ncol], AF.Exp, [Bbank[sb_]], [Bpt[pk]], scale=sc_a)
                            if st_ >= 4 * G:
                                P.tt("pool", pt[pk][:, 0:128], pt[pk][:, 0:128], diag_bf[:], ALU.mult, [Bpt[pk], Bc], [Bpt[pk]])
                            for j in range(j0, 4):
                                P.mm(accv[:, j, 0:65], pt[pk][:, (j - j0) * 128:(j - j0 + 1) * 128], Va[:, st_, h, 0:65],
                                     (st_ == 0 and j == 0), st_ == 4 * G + j, [Bpt[pk], BVa[st_], BVa1], [Bbank[ab]], sgc=True)
                            if st_ == 4 * G + 3:
                                rk = recrot.next()
                                P.recip(rec[rk][:], accv[:, :, 64], [Bbank[ab]], [Brec[rk]])
                                for j in range(4):
                                    tt = 4 * G + j
                                    P.ts("dve", otok[cpair % 2][:, tt, hp * 64:(hp + 1) * 64], accv[:, j, 0:64], rec[rk][:, j:j + 1], None, ALU.mult,
                                         [Bbank[ab], Brec[rk]], [Botok[cpair % 2][tt]])
                        if hp == 1:
                            for t0 in range(0, NT, 8):
                                bk = brot.next()
                                bv = banks[bk][:].bitcast(BF16)
                                for tt in range(t0, t0 + 8):
                                    P.tr(bv[:, (tt - t0) * 128:(tt - t0 + 1) * 128], otok[cpair % 2][:, tt, :], ident_bf[:],
                                         [Botok[cpair % 2][tt], Bc], [Bbank[bk]])
                                P.cp("act", oT[0][:, cpair, t0 * 128:(t0 + 8) * 128], bv[:], [Bbank[bk]], [BoT[0][t0 // 4], BoT[0][t0 // 4 + 1]])
                dump("d_oTa", oT[0][:], BoT[0])
                if STOP_AFTER == "mla":
                    return

                def hd64_proj(name, col_q, col_k, col_v, QTx, BQTx, KTx, BKTx, Vx, BVx, extra=None):
                    with Scope(P):
                        cos64 = P.sb(name + "cos", [128, T], F32)
                        sin64 = P.sb(name + "sin", [128, T], F32)
                        Btab = NB(name + "tab")
                        P.ld("sp", cos64[:], dr["c_cos64"], [], [Btab])
                        P.ld("sp", sin64[:], dr["c_sin64"], [], [Btab])
                        Wq = P.sb(name + "Wq", [128, KC, 512], BF16)
                        Wk2 = P.sb(name + "Wk2", [128, KC, 2, 128], BF16)
                        Wv = P.sb(name + "Wv", [128, KC, 128], BF16)
                        BW = NB(name + "W")
                        P.ld("pool", Wq[:], w_in_v[:, :, col_q:col_q + 512], [], [BW])
                        for g in range(2):
                            for i in range(2):
                                P.ld("pool", Wk2[:, :, g, i * 64:(i + 1) * 64], w_in_v[:, :, col_k + g * 64: col_k + (g + 1) * 64], [], [BW])
                        P.ld("pool", Wv[:], w_in_v[:, :, col_v:col_v + 128], [], [BW])
                        chunks = []
                        for c in range(4):
                            chunks.append((lambda kc, c=c: Wq[:, kc, c * 128:(c + 1) * 128], lambda G, c=c: QTx[:, c, gsl(G)], BQTx))
                        for g in range(2):
                            chunks.append((lambda kc, g=g: Wk2[:, kc, g, :], lambda G, g=g: KTx[:, g, gsl(G)], BKTx))
                        xw = None
                        if extra is not None:
                            xw = extra(BW, chunks)
                        raw = [P.sb(name + "raw%d" % i, [128, 512], BF16) for i in range(2)]
                        Braw = bufs(name + "raw", 2)
                        t1 = [P.sb(name + "t1_%d" % i, [128, 512], F32) for i in range(2)]
                        t2 = [P.sb(name + "t2_%d" % i, [128, 512], F32) for i in range(2)]
                        Bt1 = bufs(name + "t1", 2)
                        Bt2 = bufs(name + "t2", 2)
                        rrot = Rot([0, 1])
                        brot = Rot([0, 1, 2, 3, 4, 5, 6, 7])
                        for G in range(NG):
                            if STOP_AFTER == name + "w":
                                break
                            for (lf, df, Bd) in chunks:
                                bk = brot.next()
                                proj_fm(lf, 128, G, bk, [BW])
                                k = rrot.next()
                                P.cp("act", raw[k][:], banks[bk][:], [Bbank[bk]], [Braw[k]])
                                bk2 = brot.next()
                                P.mm(banks[bk2][:], pm64[:], raw[k][:], True, True, [Bc, Braw[k]], [Bbank[bk2]])
                                P.tt("dve", t1[k][:], raw[k][:], cos64[:, gsl(G)], ALU.mult, [Braw[k], Btab], [Bt1[k]])
                                P.tt("dve", t2[k][:], banks[bk2][:], sin64[:, gsl(G)], ALU.mult, [Bbank[bk2], Btab], [Bt2[k]])
                                P.tt("dve", df(G), t1[k][:], t2[k][:], ALU.add, [Bt1[k], Bt2[k]], [Bd[G]])
                        BV1 = NB(name + "V1")
                        if STOP_AFTER == name + "rope":
                            return BV1
                        P.memset("dve", Vx[:, :, :, 64:VP], 1.0, [BV1])
                        for tt in range(NT):
                            bk = brot.next()
                            for kc in range(KC):
                                P.mm(banks[bk][:, 0:128], xT[:, kc, tsl(tt)], Wv[:, kc, :], kc == 0, kc == KC - 1, [BxT[tt], BW], [Bbank[bk]])
                            if xw is None and EXPER == "A":
                                for kc in range(KC):
                                    P.mm(banks[bk][:, 128:132], xT[:, kc, tsl(tt)], Wv[:, kc, 0:4], kc == 0, kc == KC - 1, [BxT[tt], BW], [Bbank[bk]])
                            if xw is not None:
                                Wwi, widx, Bwidx = xw
                                for kc in range(KC):
                                    P.mm(banks[bk][:, 128:132], xT[:, kc, tsl(tt)], Wwi[:, kc, :], kc == 0, kc == KC - 1, [BxT[tt], BW], [Bbank[bk]])
                                P.act(widx[:, tt, :], banks[bk][:, 128:132], AF.Copy, [Bbank[bk]], [Bwidx[tt]], scale=1.0 / 16.0)
                            P.cp("act", Vx[:, tt, :, 0:64], banks[bk][:, 0:128].rearrange("p (g d) -> p g d", d=64), [Bbank[bk]], [BVx[tt]])
                        return BV1

                with Scope(P):
                    if SKIP_DSA:
                        raise_skip = True
                    QTb = P.sb("QTb", [128, 4, T], BF16)
                    BQTb = bufs("QTb", NG)
                    KTb = P.sb("KTb", [128, 2, T], BF16)
                    BKTb = bufs("KTb", NG)
                    Vb = P.sb("Vb", [128, NT, 2, VP], BF16)
                    BVb = bufs("Vb", NT)
                    QIT = P.sb("QIT", [128, 2, T], BF16)
                    BQIT = bufs("QIT", NG)
                    KIT = P.sb("KIT", [128, T], BF16)
                    BKIT = bufs("KIT", NG)
                    widx = P.sb("widx", [128, NT, 4], F32)
                    Bwidx = bufs("widx", NT)

                    def extra_b(BW, chunks):
                        Wqi = P.sb("Wqi", [128, KC, 256], BF16)
                        Wki2 = P.sb("Wki2", [128, KC, 128], BF16)
                        Wwi = P.sb("Wwi", [128, KC, 4], BF16)
                        P.ld("pool", Wqi[:], w_in_v[:, :, 1440:1696], [], [BW])
                        for i in range(2):
                            P.ld("pool", Wki2[:, :, i * 64:(i + 1) * 64], w_in_v[:, :, 1696:1760], [], [BW])
                        P.ld("pool", Wwi[:], w_in_v[:, :, 1760:1764], [], [BW])
                        for c in range(2):
                            chunks.append((lambda kc, c=c: Wqi[:, kc, c * 128:(c + 1) * 128], lambda G, c=c: QIT[:, c, gsl(G)], BQIT))
                        chunks.append((lambda kc: Wki2[:, kc, :], lambda G: KIT[:, gsl(G)], BKIT))
                        return (Wwi, widx, Bwidx)

                    if not SKIP_DSA:
                        BVb1 = hd64_proj("b_", 672, 1184, 1312, QTb, BQTb, KTb, BKTb, Vb, BVb, extra_b)
                    if not SKIP_DSA:
                        score32 = [P.sb("score32_%d" % i, [128, 512], F32) for i in range(2)]
                        Bs32 = bufs("score32_", 2)
                        s32rot = Rot([0, 1])
                        scorebf = [P.sb("scorebf%d" % i, [128, T], BF16) for i in range(4)]
                        Bsbf = bufs("scorebf", 4)
                        junkb = P.sb("junkb", [128, T], BF16)
                        Bjunkb = NB("junkb")
                        junka = P.sb("junka", [128, T], BF16)
                        Bjunka = NB("junka")
                        mbias = [P.sb("mbias%d" % i, [128, 4, T], BF16) for i in range(2)]
                        Bmb = [bufs("mbias%d_" % i, 4) for i in range(2)]
                        rr = [P.sb("rr%d" % i, [128, 512], F32) for i in range(2)]
                        Brr = bufs("rr", 2)
                        rrot = Rot([0, 1])
                        bis = P.sb("bis", [128, 16], F32)
                        Bmid = NB("bis_mid")
                        Bval = bufs("bis_val", 4)
                        Btmp = NB("bis_tmp")
                        Bthr = NB("bis_thr")
                        pt = [P.sb("b_pt%d" % i, [128, 512], BF16) for i in range(3)]
                        Bpt = bufs("b_pt", 3)
                        ptrot = Rot([0, 1, 2])
                        srot = Rot([0, 1, 2])
                        arot = Rot([3, 4])
                        irot = Rot([5, 6, 7])
                        otg = [P.sb("b_otg%d" % i, [128, 4, 512], BF16) for i in range(2)]
                        Botg = [bufs("b_otg%d_" % i, 4) for i in range(2)]
                        rec = [P.sb("b_rec%d" % i, [128, 4], F32) for i in range(2)]
                        Brec = bufs("b_rec", 2)
                        recrot = Rot([0, 1])

                        def topk_gen(G):
                            nSs = [(4 * G + j + 1) * 128 for j in range(4)]
                            for j in range(4):
                                qt = 4 * G + j
                                nS = nSs[j]
                                nsc = (nS + 511) // 512
                                for sc in range(nsc):
                                    ncol = min(512, nS - sc * 512)
                                    cols = slice(sc * 512, sc * 512 + ncol)
                                    sk = s32rot.next()
                                    s32 = score32[sk]
                                    for ih in range(4):
                                        c, i = ih // 2, ih % 2
                                        bk = irot.next()
                                        P.mm(banks[bk][:, 0:ncol], QIT[i * 64:(i + 1) * 64, c, tsl(qt)], KIT[i * 64:(i + 1) * 64, cols], True, True,
                                             [BQIT[G], BKIT[sc]], [Bbank[bk]])
                                        rk = rrot.next()
                                        r = rr[rk]
                                        P.act(r[:, 0:ncol], banks[bk][:, 0:ncol], AF.Relu, [Bbank[bk]], [Brr[rk]])
                                        if ih == 0:
                                            P.ts("dve", s32[:, 0:ncol], r[:, 0:ncol], widx[:, qt, 0:1], None, ALU.mult, [Brr[rk], Bwidx[qt]], [Bs32[sk]])
                                        elif ih < 3:
                                            P.stt("dve", s32[:, 0:ncol], r[:, 0:ncol], widx[:, qt, ih:ih + 1], s32[:, 0:ncol], ALU.mult, ALU.add,
                                                  [Brr[rk], Bwidx[qt], Bs32[sk]], [Bs32[sk]])
                                        else:
                                            if sc == nsc - 1:
                                                P.tt("dve", s32[:, ncol - 128:ncol], s32[:, ncol - 128:ncol], negdiag[:], ALU.add, [Bs32[sk], Bc], [Bs32[sk]])
                                            P.stt("dve", scorebf[j][:, cols], r[:, 0:ncol], widx[:, qt, 3:4], s32[:, 0:ncol], ALU.mult, ALU.add,
                                                  [Brr[rk], Bwidx[qt], Bs32[sk]], [Bsbf[j]])
                                    yield
                            P.memset("dve", bis[:, 0:4], 0.0, [Bmid])
                            w = W0
                            for it in range(NIT):
                                for j in (2, 3):
                                    P.act(junka[:, 0:nSs[j]], scorebf[j][:, 0:nSs[j]], AF.Sign, [Bsbf[j], Bmid], [Bjunka, Bval[j]],
                                          bias=bis[:, j:j + 1], scale=-1.0, accum=bis[:, 4 + j:5 + j])
                                for j in (0, 1):
                                    P.ts("dve", junkb[:, 0:nSs[j]], scorebf[j][:, 0:nSs[j]], bis[:, j:j + 1], None, ALU.is_ge, [Bsbf[j], Bmid], [Bjunkb, Bval[j]],
                                         op1=ALU.add, accum=bis[:, 4 + j:5 + j])
                                P.ts("dve", bis[:, 8:10], bis[:, 4:6], 256.0, w, ALU.is_ge, [Bval[0], Bval[1]], [Btmp], op1=ALU.mult)
                                for j in (2, 3):
                                    P.ts("dve", bis[:, 8 + j:9 + j], bis[:, 4 + j:5 + j], float(nSs[j] - 512), w, ALU.is_le, [Bval[j]], [Btmp], op1=ALU.mult)
                                P.stt("dve", bis[:, 0:4], bis[:, 8:12], -w / 2, bis[:, 0:4], ALU.add, ALU.add, [Btmp, Bmid], [Bmid])
                                w = w / 2
                                yield
                            P.ts("dve", bis[:, 12:16], bis[:, 0:4], -w, None, ALU.add, [Bmid], [Bthr])
                            for j in range(4):
                                nS = nSs[j]
                                P.ts("dve", mbias[G % 2][:, j, 0:nS], scorebf[j][:, 0:nS], bis[:, 12 + j:13 + j], -30000.0, ALU.is_lt,
                                     [Bsbf[j], Bthr], [Bmb[G % 2][j]], op1=ALU.mult)
                                yield

                        def attn_gen(G):
                            mb = mbias[G % 2]
                            Bm = Bmb[G % 2]
                            og = otg[G % 2]
                            Bog = Botg[G % 2]
                            blocks = [(h, st_) for h in range(8) for st_ in range(4 * G + 4)]
                            info = {}

                            def qk(k):
                                h, st_ = blocks[k]
                                c, i, g = h // 2, h % 2, h // 4
                                j0 = max(0, st_ - 4 * G)
                                ncol = (4 - j0) * 128
                                q0 = (4 * G + j0) * 128
                                sb_ = srot.next()
                                P.mm(banks[sb_][:, 0:ncol], KTb[i * 64:(i + 1) * 64, g, tsl(st_)], QTb[i * 64:(i + 1) * 64, c, q0:q0 + ncol], True, True,
                                     [BKTb[st_ // 4], BQTb[G]], [Bbank[sb_]])
                                for j in range(j0, 4):
                                    P.mm(banks[sb_][:, (j - j0) * 128:(j - j0 + 1) * 128], mb[:, j, tsl(st_)], ident_bf[:], False, j == 3,
                                         [Bm[j], Bc], [Bbank[sb_]], sgc=True)
                                info[k] = (sb_, j0, ncol)

                            qk(0)
                            ab = None
                            for k, (h, st_) in enumerate(blocks):
                                g = h // 4
                                if k + 1 < len(blocks):
                                    qk(k + 1)
                                sb_, j0, ncol = info[k]
                                if st_ == 0:
                                    ab = arot.next()
                                accv = banks[ab][:, 0:4 * VP].rearrange("p (j d) -> p j d", d=VP)
                                pk = ptrot.next()
                                P.act(pt[pk][:, 0:ncol], banks[sb_][:, 0:ncol], AF.Exp, [Bbank[sb_]], [Bpt[pk]], scale=0.125)
                                for j in range(j0, 4):
                                    P.mm(accv[:, j, 0:65], pt[pk][:, (j - j0) * 128:(j - j0 + 1) * 128], Vb[:, st_, g, 0:65],
                                         (st_ == 0 and j == 0), st_ == 4 * G + j, [Bpt[pk], BVb[st_], BVb1], [Bbank[ab]], sgc=True)
                                if st_ == 4 * G + 3:
                                    rk = recrot.next()
                                    P.recip(rec[rk][:], accv[:, :, 64], [Bbank[ab]], [Brec[rk]])
                                    for j in range(4):
                                        P.ts("dve", og[:, j, h * 64:(h + 1) * 64], accv[:, j, 0:64], rec[rk][:, j:j + 1], None, ALU.mult,
                                             [Bbank[ab], Brec[rk]], [Bog[j]])
                                yield
                            for j in range(4):
                                tt = 4 * G + j
                                bk = irot.next()
                                bv = banks[bk][:].bitcast(BF16)
                                for c in range(4):
                                    P.tr(bv[:, c * 128:(c + 1) * 128], og[:, j, c * 128:(c + 1) * 128], ident_bf[:], [Bog[j], Bc], [Bbank[bk]])
                                P.cp("act", oT[1][:, :, tsl(tt)], bv[:, 0:512].rearrange("p (c t) -> p c t", t=128), [Bbank[bk]], [BoT[1][G]])
                            yield

                        for _ in topk_gen(0):
                            pass
                        for G in range(NG):
                            ag = list_len = None
                            a_units = 8 * (4 * G + 4) + 1
                            if G + 1 < NG:
                                t_units = sum(((4 * (G + 1) + j + 1) * 128 + 511) // 512 for j in range(4)) + NIT + 4
                                tg = topk_gen(G + 1)
                            else:
                                t_units = 0
                                tg = None
                            done_t = 0
                            for ai, _ in enumerate(attn_gen(G)):
                                if tg is not None:
                                    want = ((ai + 1) * t_units) // a_units
                                    while done_t < want:
                                        try:
                                            next(tg)
                                        except StopIteration:
                                            tg = None
                                            break
                                        done_t += 1
                            if tg is not None:
                                for _ in tg:
                                    pass
                dump("d_oTb", oT[1][:], BoT[1])
                if STOP_AFTER == "dsa":
                    return
                with Scope(P):
                    QTc = P.sb("QTc", [128, 4, T], BF16)
                    BQTc = bufs("QTc", NG)
                    KTc = P.sb("KTc", [128, 2, T], BF16)
                    BKTc = bufs("KTc", NG)
                    Vc = P.sb("Vc", [128, NT, 2, VP], BF16)
                    BVc = bufs("Vc", NT)
                    BVc1 = hd64_proj("c_", 1764, 2276, 2404, QTc, BQTc, KTc, BKTc, Vc, BVc, None)
                    if STOP_AFTER in ("swa_proj", "c_rope", "c_w"):
                        return
                    esink = P.sb("esink", [128, 8], F32)
                    Besink = NB("esink")
                    P.ld("sp", esink[:], dr["c_sinks"][l].partition_broadcast(128), [], [Besink])
                    P.act(esink[:], esink[:], AF.Exp, [Besink], [Besink])
                    smask = P.sb("smask", [128, 2, 2, 128], BF16)
                    Bsm = NB("smask")
                    for i in range(2):
                        P.cp("dve", smask[:, i, 0, :], swaprev_bf[:], [Bc], [Bsm])
                        P.cp("dve", smask[:, i, 1, :], diag_bf[:], [Bc], [Bsm])
                    ptc = [P.sb("c_pt%d" % i, [128, 2, 2, 128], BF16) for i in range(3)]
                    Bptc = bufs("c_pt", 3)
                    ptrot = Rot([0, 1, 2])
                    srot = Rot([0, 1, 2])
                    arot = Rot([3, 4])
                    irot = Rot([5, 6, 7])
                    otc = [P.sb("c_ot%d" % i, [128, 512], BF16) for i in range(2)]
                    Botc = bufs("c_ot", 2)
                    den = [P.sb("c_den%d" % i, [128, 2], F32) for i in range(2)]
                    Bden = bufs("c_den", 2)
                    drot = Rot([0, 1])
                    srot = Rot([0, 1, 2, 5])
                    irot = Rot([6, 7])
                    blocks = [(qt, c) for qt in range(NT) for c in range(4)]
                    info = {}

                    def qk_c(k):
                        qt, c = blocks[k]
                        g = c // 2
                        u0 = 0 if qt > 0 else 1
                        sbs = []
                        for i in range(2):
                            sb_ = srot.next()
                            sv = banks[sb_][:, 0:256].rearrange("p (u t) -> p u t", u=2)
                            for u in range(u0, 2):
                                st_ = qt - 1 + u
                                P.mm(sv[:, u, :], KTc[i * 64:(i + 1) * 64, g, tsl(st_)], QTc[i * 64:(i + 1) * 64, c, tsl(qt)], True, True,
                                     [BKTc[st_ // 4], BQTc[qt // 4]], [Bbank[sb_]])
                            sbs.append((sb_, sv))
                        info[k] = sbs

                    qk_c(0)
                    for k, (qt, c) in enumerate(blocks):
                        if k + 1 < len(blocks):
                            qk_c(k + 1)
                        g = c // 2
                        u0 = 0 if qt > 0 else 1
                        ok = qt % 2
                        pk = ptrot.next()
                        for i in range(2):
                            sb_, sv = info[k][i]
                            P.act(ptc[pk][:, i, u0:2, :], sv[:, u0:2, :], AF.Exp, [Bbank[sb_]], [Bptc[pk]], scale=0.125)
                        P.tt("pool", ptc[pk][:, :, u0:2, :], ptc[pk][:, :, u0:2, :], smask[:, :, u0:2, :], ALU.mult, [Bptc[pk], Bsm], [Bptc[pk]])
                        ab = arot.next()
                        accv = banks[ab][:, 0:2 * VP].rearrange("p (i d) -> p i d", d=VP)
                        for i in range(2):
                            for u in range(u0, 2):
                                st_ = qt - 1 + u
                                P.mm(accv[:, i, 0:65], ptc[pk][:, i, u, :], Vc[:, st_, g, 0:65], (u == u0 and i == 0), u == 1, [Bptc[pk], BVc[st_], BVc1], [Bbank[ab]], sgc=True)
                        dk = drot.next()
                        P.tt("dve", den[dk][:], accv[:, :, 64], esink[:, 2 * c:2 * c + 2], ALU.add, [Bbank[ab], Besink], [Bden[dk]])
                        P.recip(den[dk][:], den[dk][:], [Bden[dk]], [Bden[dk]])
                        for i in range(2):
                            h = 2 * c + i
                            P.ts("dve", otc[ok][:, h * 64:(h + 1) * 64], accv[:, i, 0:64], den[dk][:, i:i + 1], None, ALU.mult,
                                 [Bbank[ab], Bden[dk]], [Botc[ok]])
                        if c == 3:
                            bk = irot.next()
                            bv = banks[bk][:].bitcast(BF16)
                            for c2 in range(4):
                                P.tr(bv[:, c2 * 128:(c2 + 1) * 128], otc[ok][:, c2 * 128:(c2 + 1) * 128], ident_bf[:], [Botc[ok], Bc], [Bbank[bk]])
                            P.cp("act", oT[2][:, :, tsl(qt)], bv[:, 0:512].rearrange("p (c t) -> p c t", t=128), [Bbank[bk]], [BoT[2][qt // 4]])
                dump("d_oTc", oT[2][:], BoT[2])
                if STOP_AFTER == "swa":
                    return
                with Scope(P):
                    wout = P.sb("wout", [128, KC, D], BF16)
                    Bwout = NB("wout")
                    P.ld("pool", wout[:], dr["w_out"][l].rearrange("(kc p) c -> p kc c", p=128), [], [Bwout])
                    g1B = P.sb("g1B", [128, D], F32)
                    b1B = P.sb("b1B", [128, D], F32)
                    Bg1 = NB("g1b1")
                    P.ld("sp", g1B[:], dr["ln1_g"][l].partition_broadcast(128), [], [Bg1])
                    P.ld("sp", b1B[:], dr["ln1_b"][l].partition_broadcast(128), [], [Bg1])
                    wr = P.sb("wr", [128, KC, 16], F32)
                    Bwr = NB("wr")
                    P.ld("sp", wr[:], dr["w_router"].rearrange("(kc p) e -> p kc e", p=128), [], [Bwr])
                    wbr = [P.sb("wbr%d" % i, [128, 3, 4, 128], BF16) for i in range(2)]
                    wgt = [P.sb("wgt%d" % i, [128, 3, KC, 128], BF16) for i in range(2)]
                    Bwm = bufs("wm", 2)
                    mg = P.sb("mg", [128, KC, T], BF16)
                    Bmg = [bufs("mg%d_" % i, NG) for i in range(KC)]
                    sg = [P.sb("sg%d" % i, [128, 512], F32) for i in range(2)]
                    Bsg = bufs("sg", 2)
                    sgrot = Rot([0, 1])
                    macc = P.sb("macc", [128, 512], F32)
                    Bmacc = NB("macc")
                    pre = [P.sb("m_pre%d" % i, [128, D], F32) for i in range(4)]
                    Bpre = bufs("m_pre", 4)
                    st1 = P.sb("m_st", [128, 8, 4], F32)
                    Bsum1 = bufs("m_sum", 4)
                    Bsq1 = bufs("m_sq", 4)
                    Bvec1 = NB("m_vec")
                    junks = [P.sb("m_junk%d" % i, [128, D], BF16)[:] for i in range(2)]
                    Bjunks = bufs("m_junk", 2)
                    xbf = [P.sb("m_xbf%d" % i, [128, D], BF16)[:] for i in range(2)]
                    Bxbf = bufs("m_xbf", 2)
                    x1Tf = P.sb("x1Tf", [128, KC, 128], F32)
                    Bx1Tf = NB("x1Tf")
                    brot = Rot([0, 1, 2, 3, 4, 5, 6, 7])
                    wbr_src = [dr[n][l].rearrange("(kc p) c -> p kc c", p=128) for n in ("w_br_a", "w_br_b", "w_br_c")]
                    def ld_merge(dc):
                        k = dc % 2
                        for i in range(3):
                            P.ld("pool", wbr[k][:, i, :, :], wbr_src[i][:, :, dc * 128:(dc + 1) * 128], [], [Bwm[k]])
                            P.ld("pool", wgt[k][:, i, :, :], w_in_v[:, :, 2532 + i * 1024 + dc * 128: 2532 + i * 1024 + (dc + 1) * 128], [], [Bwm[k]])

                    ld_merge(0)
                    for dc in range(KC):
                        k = dc % 2
                        if dc + 1 < KC:
                            ld_merge(dc + 1)
                        for G in range(NG):
                            for i in range(3):
                                yb = brot.next()
                                for kc in range(4):
                                    P.mm(banks[yb][:], wbr[k][:, i, kc, :], oT[i][:, kc, gsl(G)], kc == 0, kc == 3, [Bwm[k], BoT[i][G]], [Bbank[yb]])
                                gb = brot.next()
                                for kc in range(KC):
                                    P.mm(banks[gb][:], wgt[k][:, i, kc, :], xT[:, kc, gsl(G)], kc == 0, kc == KC - 1,
                                         [Bwm[k]] + [BxT[4 * G + j] for j in range(4)], [Bbank[gb]])
                                sk = sgrot.next()
                                P.act(sg[sk][:], banks[gb][:], AF.Sigmoid, [Bbank[gb]], [Bsg[sk]])
                                if i == 0:
                                    P.tt("dve", macc[:], sg[sk][:], banks[yb][:], ALU.mult, [Bsg[sk], Bbank[yb]], [Bmacc])
                                elif i == 1:
                                    P.tt("dve", sg[sk][:], sg[sk][:], banks[yb][:], ALU.mult, [Bsg[sk], Bbank[yb]], [Bsg[sk]])
                                    P.tt("dve", macc[:], macc[:], sg[sk][:], ALU.add, [Bmacc, Bsg[sk]], [Bmacc])
                                else:
                                    P.tt("dve", sg[sk][:], sg[sk][:], banks[yb][:], ALU.mult, [Bsg[sk], Bbank[yb]], [Bsg[sk]])
                                    P.tt("dve", mg[:, dc, gsl(G)], macc[:], sg[sk][:], ALU.add, [Bmacc, Bsg[sk]], [Bmg[dc][G]])
                    for G in range(NG):
                        items = []
                        for j in range(4):
                            tt = 4 * G + j
                            P.ld("sp", pre[j][:], xres[tsl(tt), :], [Bxres[tt]], [Bpre[j]])
                            items.append((pre[j][:], Bpre[j]))
                        for j in range(4):
                            tt = 4 * G + j
                            for half in range(2):
                                ob = brot.next()
                                for kc in range(KC):
                                    P.mm(banks[ob][:], mg[:, kc, tsl(tt)], wout[:, kc, half * 512:(half + 1) * 512], kc == 0, kc == KC - 1,
                                         [Bmg[kc][G], Bwout], [Bbank[ob]])
                                hs = slice(half * 512, (half + 1) * 512)
                                P.stt("dve", pre[j][:, hs], pre[j][:, hs], ALPHA, banks[ob][:], ALU.mult, ALU.add, [Bpre[j], Bbank[ob]], [Bpre[j]])
                        ln_batch(items, g1B[:], b1B[:], Bg1, st1, Bsum1, Bsq1, Bvec1, junks, Bjunks)
                        for j in range(4):
                            tt = 4 * G + j
                            P.st("sp", xres[tsl(tt), :], pre[j][:], [Bpre[j]], [Bxres[tt]], key=Bpre[j])
                        to_xT_batch(items, [4 * G + j for j in range(4)], xbf, Bxbf, [brot.next() for _ in range(4)])
                        for j in range(4):
                            tt = 4 * G + j
                            tb0 = brot.next()
                            tb1 = brot.next()
                            for kc in range(KC):
                                tb = tb0 if kc < 4 else tb1
                                P.tr(banks[tb][:, (kc % 4) * 128:(kc % 4 + 1) * 128], pre[j][:, kc * 128:(kc + 1) * 128], ident_f[:], [Bpre[j], Bc], [Bbank[tb]])
                            P.cp("act", x1Tf[:, 0:4, :], banks[tb0][:].rearrange("p (k t) -> p k t", t=128), [Bbank[tb0]], [Bx1Tf])
                            P.cp("act", x1Tf[:, 4:8, :], banks[tb1][:].rearrange("p (k t) -> p k t", t=128), [Bbank[tb1]], [Bx1Tf])
                            lb = brot.next()
                            for kc in range(KC):
                                P.mm(banks[lb][:, 0:16], x1Tf[:, kc, :], wr[:, kc, :], kc == 0, kc == KC - 1, [Bx1Tf, Bwr], [Bbank[lb]])
                            P.cp("dve", logit[:, tt, :], banks[lb][:, 0:16], [Bbank[lb]], [Blogit[tt]])
            if STOP_AFTER == "ln1":
                return
            with Scope(P):
                gate = P.sb("gate", [128, NT, 16], F32)
                Bgate = NB("gate")
                with Scope(P):
                    sco = P.sb("r_sco", [128, NT, 16], F32)
                    bia = P.sb("r_bia", [128, NT, 16], F32)
                    mb = P.sb("r_mb", [128, NT, 16], F32)
                    rbias = P.sb("r_bias", [128, 16], F32)
                    ps6 = P.sb("r_ps6", [128, 6, NT, 4], F32)
                    gs = P.sb("r_gs", [128, NT, 4], F32)
                    gmax = P.sb("r_gmax", [128, NT], F32)
                    ing = P.sb("r_ing", [128, NT, 4], F32)
                    red = P.sb("r_red", [128, NT, 8], F32)
                    tmax = P.sb("r_tmax", [128, NT], F32)
                    e1 = P.sb("r_e1", [128, NT, 16], F32)
                    e2 = P.sb("r_e2", [128, NT, 16], F32)
                    Br = NB("routing")
                    R = [Br] + Blogit
                    P.ld("sp", rbias[:], dr["router_bias"].partition_broadcast(128), [], [Br])
                    P.act(sco[:], logit[:], AF.Sigmoid, R, [Br])
                    for tt in range(NT):
                        P.tt("dve", bia[:, tt, :], sco[:, tt, :], rbias[:], ALU.add, [Br], [Br])
                    bg = bia[:].rearrange("p t (g e) -> p t g e", e=4)
                    pairs = [(0, 1), (0, 2), (0, 3), (1, 2), (1, 3), (2, 3)]
                    for pi, (a, b_) in enumerate(pairs):
                        P.tt("dve", ps6[:, pi, :, :], bg[:, :, :, a], bg[:, :, :, b_], ALU.add, [Br], [Br])
                    P.tt("dve", gs[:], ps6[:, 0, :, :], ps6[:, 1, :, :], ALU.max, [Br], [Br])
                    for pi in range(2, 6):
                        P.tt("dve", gs[:], gs[:], ps6[:, pi, :, :], ALU.max, [Br], [Br])
                    P.tt("dve", gmax[:], gs[:, :, 0], gs[:, :, 1], ALU.max, [Br], [Br])
                    P.tt("dve", gmax[:], gmax[:], gs[:, :, 2], ALU.max, [Br], [Br])
                    P.tt("dve", gmax[:], gmax[:], gs[:, :, 3], ALU.max, [Br], [Br])
                    for g in range(4):
                        P.tt("dve", ing[:, :, g], gs[:, :, g], gmax[:], ALU.is_equal, [Br], [Br])
                    P.ts("dve", ing[:], ing[:], -1.0, 1e30, ALU.add, [Br], [Br], op1=ALU.mult)
                    mbg = mb[:].rearrange("p t (g e) -> p t g e", e=4)
                    for e_ in range(4):
                        P.tt("dve", mbg[:, :, :, e_], bg[:, :, :, e_], ing[:], ALU.add, [Br], [Br])

                    def max16(dst, src):
                        P.tt("dve", red[:, :, 0:8], src[:, :, 0:8], src[:, :, 8:16], ALU.max, [Br], [Br])
                        P.tt("dve", red[:, :, 0:4], red[:, :, 0:4], red[:, :, 4:8], ALU.max, [Br], [Br])
                        P.tt("dve", red[:, :, 0:2], red[:, :, 0:2], red[:, :, 2:4], ALU.max, [Br], [Br])
                        P.tt("dve", dst, red[:, :, 0], red[:, :, 1], ALU.max, [Br], [Br])

                    max16(tmax[:], mb)
                    for tt in range(NT):
                        P.ts("dve", e1[:, tt, :], mb[:, tt, :], tmax[:, tt:tt + 1], None, ALU.is_equal, [Br], [Br])
                    P.stt("dve", mb[:], e1[:], -1e30, mb[:], ALU.mult, ALU.add, [Br], [Br])
                    max16(tmax[:], mb)
                    for tt in range(NT):
                        P.ts("dve", e2[:, tt, :], mb[:, tt, :], tmax[:, tt:tt + 1], None, ALU.is_equal, [Br], [Br])
                    P.tt("dve", e1[:], e1[:], e2[:], ALU.add, [Br], [Br])
                    P.tt("dve", e1[:], e1[:], sco[:], ALU.mult, [Br], [Br])
                    P.tt("dve", red[:, :, 0:8], e1[:, :, 0:8], e1[:, :, 8:16], ALU.add, [Br], [Br])
                    P.tt("dve", red[:, :, 0:4], red[:, :, 0:4], red[:, :, 4:8], ALU.add, [Br], [Br])
                    P.tt("dve", red[:, :, 0:2], red[:, :, 0:2], red[:, :, 2:4], ALU.add, [Br], [Br])
                    P.tt("dve", tmax[:], red[:, :, 0], red[:, :, 1], ALU.add, [Br], [Br])
                    P.recip(tmax[:], tmax[:], [Br], [Br])
                    for tt in range(NT):
                        P.ts("dve", gate[:, tt, :], e1[:, tt, :], tmax[:, tt:tt + 1], None, ALU.mult, [Br], [Bgate])
                dump("d_gate", gate[:], [Bgate])
                if STOP_AFTER == "route":
                    return
                acc = P.sb("acc", [128, NT, D], F32)
                Bacc = bufs("acc", NT)
                with Scope(P):
                    Wg = [P.sb("Wg%d" % i, [128, KC, 512], BF16) for i in range(2)]
                    Wu = [P.sb("Wu%d" % i, [128, KC, 512], BF16) for i in range(2)]
                    Wd = [P.sb("Wd%d" % i, [128, 4, D], BF16) for i in range(2)]
                    BWe = bufs("We", 2)
                    actT = [P.sb("actT%d" % i, [128, 4, 512], BF16) for i in range(2)]
                    BactT = [bufs("actT%d_" % i, 4) for i in range(2)]
                    sgm = [P.sb("sgm%d" % i, [128, 512], BF16) for i in range(2)]
                    Bsgm = bufs("sgm", 2)
                    sgrot = Rot([0, 1])
                    hrot = Rot([0, 1, 2, 3])
                    orot = Rot([4, 5, 6, 7])
                    ai = 0
                    for e_ in range(16):
                        k = e_ % 2
                        P.ld("pool", Wg[k][:], dr["w_exp_gate"][l, e_].rearrange("(kc p) f -> p kc f", p=128), [], [BWe[k]])
                        P.ld("pool", Wu[k][:], dr["w_exp_up"][l, e_].rearrange("(kc p) f -> p kc f", p=128), [], [BWe[k]])
                        P.ld("pool", Wd[k][:], dr["w_exp_down"][l, e_].rearrange("(kc p) f -> p kc f", p=128), [], [BWe[k]])
                        for G in range(NG):
                            a = ai % 2
                            ai += 1
                            xr_ = [BxT[4 * G + j] for j in range(4)]
                            for fc in range(4):
                                hb = hrot.next()
                                for kc in range(KC):
                                    P.mm(banks[hb][:], Wg[k][:, kc, fc * 128:(fc + 1) * 128], xT[:, kc, gsl(G)], kc == 0, kc == KC - 1, [BWe[k]] + xr_, [Bbank[hb]])
                                ub = hrot.next()
                                for kc in range(KC):
                                    P.mm(banks[ub][:], Wu[k][:, kc, fc * 128:(fc + 1) * 128], xT[:, kc, gsl(G)], kc == 0, kc == KC - 1, [BWe[k]] + xr_, [Bbank[ub]])
                                sk = sgrot.next()
                                P.act(sgm[sk][:], banks[hb][:], AF.Silu, [Bbank[hb]], [Bsgm[sk]])
                                P.tt("dve", actT[a][:, fc, :], sgm[sk][:], banks[ub][:], ALU.mult, [Bsgm[sk], Bbank[ub]], [BactT[a][fc]])
                            for j in range(4):
                                tt = 4 * G + j
                                for half in range(2):
                                    ob = orot.next()
                                    for fc in range(4):
                                        P.mm(banks[ob][:], actT[a][:, fc, j * 128:(j + 1) * 128], Wd[k][:, fc, half * 512:(half + 1) * 512], fc == 0, fc == 3,
                                             [BactT[a][fc], BWe[k]], [Bbank[ob]])
                                    dst = acc[:, tt, half * 512:(half + 1) * 512]
                                    if e_ == 0:
                                        P.ts("dve", dst, banks[ob][:], gate[:, tt, e_:e_ + 1], None, ALU.mult, [Bbank[ob], Bgate], [Bacc[tt]])
                                    else:
                                        P.stt("dve", dst, banks[ob][:], gate[:, tt, e_:e_ + 1], dst, ALU.mult, ALU.add, [Bbank[ob], Bgate, Bacc[tt]], [Bacc[tt]])
                with Scope(P):
                    g2B = P.sb("g2B", [128, D], F32)
                    b2B = P.sb("b2B", [128, D], F32)
                    Bg2 = NB("g2b2")
                    P.ld("sp", g2B[:], dr["ln2_g"][l].partition_broadcast(128), [], [Bg2])
                    P.ld("sp", b2B[:], dr["ln2_b"][l].partition_broadcast(128), [], [Bg2])
                    xr = [P.sb("f_xr%d" % i, [128, D], F32) for i in range(4)]
                    Bxr = bufs("f_xr", 4)
                    st2 = P.sb("f_st", [128, 8, 4], F32)
                    Bsum2 = bufs("f_sum", 4)
                    Bsq2 = bufs("f_sq", 4)
                    Bvec2 = NB("f_vec")
                    junks = [P.sb("f_junk%d" % i, [128, D], BF16)[:] for i in range(2)]
                    Bjunks = bufs("f_junk", 2)
                    xbf = [P.sb("f_xbf%d" % i, [128, D], BF16)[:] for i in range(2)]
                    Bxbf = bufs("f_xbf", 2)
                    Bstk = bufs("f_stkey", 4)
                    for t0 in range(0, NT, 4):
                        items = []
                        for i in range(4):
                            tt = t0 + i
                            P.ld("sp", xr[i][:], xres[tsl(tt), :], [Bxres[tt]], [Bxr[i]])
                        for i in range(4):
                            tt = t0 + i
                            P.stt("dve", acc[:, tt, :], xr[i][:], ALPHA, acc[:, tt, :], ALU.mult, ALU.add, [Bxr[i], Bacc[tt]], [Bacc[tt]])
                            items.append((acc[:, tt, :], Bacc[tt]))
                        ln_batch(items, g2B[:], b2B[:], Bg2, st2, Bsum2, Bsq2, Bvec2, junks, Bjunks)
                        for i in range(4):
                            tt = t0 + i
                            kb = Bstk[i]
                            if last:
                                final_events.append(P.st("sp", y[tsl(tt), :], acc[:, tt, :], [Bacc[tt]], [kb], key=kb))
                            else:
                                P.st("sp", xres[tsl(tt), :], acc[:, tt, :], [Bacc[tt]], [Bxres[tt], kb], key=kb)
                        if not last:
                            to_xT_batch(items, [t0 + i for i in range(4)], xbf, Bxbf, [0, 1, 2, 3])

        for li, l in enumerate(layers):
            layer(l, li == len(layers) - 1)

        dump("d_xT", xT[:], BxT)
        P.finish(final_events)
        print("build: sems", P.nsem, "ops", {e: len(P.ops[e]) for e in P.ENG})
    return nc


STOP_AFTER = None
EXPER = None
SKIP_DSA = False
SKIP_MLA = False
_NB = {}


def NB(name):
    if name not in _NB:
        _NB[name] = Buf(name)
    return _NB[name]


def prep_inputs(inputs):
    f32 = np.float32
    common = {}
    for k in WEIGHT_SHAPES:
        a = np.ascontiguousarray(np.asarray(inputs[k], dtype=f32))
        if k == "a_q_ln_g":
            a = np.ascontiguousarray(a.reshape(2, 3, 128).transpose(0, 2, 1))
        if k == "a_kv_ln_g":
            a = np.ascontiguousarray(a.reshape(2, 2, 128).transpose(0, 2, 1))
        common[k] = a
    common.update(make_consts())
    return common


_PROG_CACHE = {}


def get_prog(key, *a, **kw):
    if key not in _PROG_CACHE:
        _NB.clear()
        _PROG_CACHE[key] = build_program(*a, **kw)
    return _PROG_CACHE[key]


FUSED = True


def kernel(**inputs):
    x = np.ascontiguousarray(np.asarray(inputs["x"], dtype=np.float32))
    B = x.shape[0]
    common = prep_inputs(inputs)
    cores = list(range(B))
    if FUSED:
        nc = get_prog("fused", [0, 1], True, True)
        maps = [dict(common, x=x[b]) for b in cores]
        res = run_bass_kernel_spmd(nc, maps, core_ids=cores)
        return np.stack([res.results[b]["y"] for b in cores], axis=0).astype(np.float32)
    nc0 = get_prog("l0", [0], True, True)
    maps = [dict(common, x=x[b]) for b in cores]
    res = run_bass_kernel_spmd(nc0, maps, core_ids=cores)
    mid = [res.results[b]["y"] for b in cores]
    nc1 = get_prog("l1", [1], False, True)
    maps = [dict(common, x=np.ascontiguousarray(mid[b])) for b in cores]
    res = run_bass_kernel_spmd(nc1, maps, core_ids=cores)
    return np.stack([res.results[b]["y"] for b in cores], axis=0).astype(np.float32)
```

```python
import contextlib
import numpy as np
import concourse.bass as bass
import concourse.mybir as mybir
from concourse.bass_utils import run_bass_kernel_spmd

F32 = mybir.dt.float32
BF16 = mybir.dt.bfloat16
AF = mybir.ActivationFunctionType
ALU = mybir.AluOpType
AX = mybir.AxisListType


GUARD = True


class Buf:
    __slots__ = ("name", "w", "r", "sem_in", "cnt_in", "sem_out", "cnt_out", "psum")

    def __init__(self, name):
        self.name = name
        self.w = None
        self.r = []
        self.sem_in = None
        self.cnt_in = 0
        self.sem_out = None
        self.cnt_out = 0
        self.psum = False


class Prog:
    ENG = ("pe", "act", "dve", "pool", "sp")

    def __init__(self, nc, es):
        self.nc = nc
        self.es = es
        self.root = es
        self.eng = {"pe": nc.tensor, "act": nc.scalar, "dve": nc.vector,
                    "pool": nc.gpsimd, "sp": nc.sync}
        self.ops = {e: [] for e in self.ENG}
        self.idx = {e: 0 for e in self.ENG}
        self.sem = {e: es.enter_context(nc.semaphore("s_" + e)) for e in self.ENG if e != "sp"}
        self.waited = {e: {} for e in self.ENG}
        self.nsem = 0
        self.uid = 0
        self.last_out_events = []
        self.pending = {e: {} for e in self.ENG}
        self.dma_since = {}
        self.guard = {}
        self.guard_hist = {"act": [], "dve": []}
        self.guard_src = None

    def new_sem(self, name):
        self.nsem += 1
        return self.root.enter_context(self.nc.semaphore("%s_%d" % (name, self.nsem)))

    def sb(self, name, shape, dt):
        self.uid += 1
        return self.es.enter_context(self.nc.sbuf_tensor("%s_%d" % (name, self.uid), list(shape), dt))

    def ps(self, name, shape, dt):
        self.uid += 1
        return self.es.enter_context(self.nc.psum_tensor("%s_%d" % (name, self.uid), list(shape), dt))

    def _collect(self, e, reads, writes, skip_sem=None):
        waits = {}

        def need(ev, raw, waw=False):
            if ev is None:
                return
            sem, val, ee, ii = ev
            if waw and skip_sem is not None and sem is skip_sem:
                return
            if ee == e and ii is not None:
                if e == "pe":
                    return
            k = id(sem)
            if self.waited[e].get(k, 0) >= val:
                return
            if k not in waits or waits[k][1] < val:
                waits[k] = (sem, val)

        for b in reads:
            need(b.w, True)
            if b.psum and e in ("act", "dve"):
                for r in b.r:
                    if r[2] != e:
                        need(r, True)
        for b in writes:
            need(b.w, True, True)
            for r in b.r:
                if GUARD and b.psum and e == "pe" and r[3] is not None and r[2] in ("act", "dve"):
                    r = self._guarded(r)
                need(r, False)
        if self.pending[e]:
            for k, (sem, val) in self.pending[e].items():
                if self.waited[e].get(k, 0) >= val:
                    continue
                if k not in waits or waits[k][1] < val:
                    waits[k] = (sem, val)
            self.pending[e] = {}
        for k, (sem, val) in waits.items():
            self.waited[e][k] = val
        return list(waits.values())

    def _commit(self, ev, reads, writes):
        for b in reads:
            b.r.append(ev)
        for b in writes:
            b.w = ev
            b.r = []

    def _guarded(self, r):
        sem, val, E, ii = r
        if self.idx[E] > ii + 1:
            return (sem, ii + 2, E, ii + 1)
        hist = self.guard_hist[E]
        n = len(hist)
        g = self.guard[E][:, (n % 8):(n % 8) + 1]
        gw = []
        if n >= 8:
            k = id(self.sem[E])
            need = hist[n - 8] + 1
            if self.waited[E].get(k, 0) < need:
                gw.append((self.sem[E], need))
                self.waited[E][k] = need
        if E == "act":
            src, bsrc = self.guard_src
            sv = bsrc.w
            if sv is not None and self.waited[E].get(id(sv[0]), 0) < sv[1]:
                gw.append((sv[0], sv[1]))
                self.waited[E][id(sv[0])] = sv[1]
        i2 = self.idx[E]
        self.idx[E] = i2 + 1
        hist.append(i2)
        if E == "act":
            self.ops[E].append((gw, (lambda en, g=g, src=src: en.activation(out=g, in_=src, func=AF.Copy)), (self.sem[E], 1)))
        else:
            self.ops[E].append((gw, (lambda en, g=g: en.memset(g, 0.0)), (self.sem[E], 1)))
        return (self.sem[E], i2 + 1, E, i2)

    def op(self, e, fn, reads=(), writes=()):
        waits = self._collect(e, reads, writes)
        i = self.idx[e]
        self.idx[e] = i + 1
        ev = (self.sem[e], i + 1, e, i)
        self.ops[e].append((waits, fn, (self.sem[e], 1)))
        self._commit(ev, reads, writes)
        return ev

    def dma(self, e, fn, reads=(), writes=(), key=None):
        if key is None:
            key = writes[0] if writes else reads[0]
        if key.sem_in is None:
            key.sem_in = {}
        if e not in key.sem_in:
            key.sem_in[e] = [self.new_sem("d%s_%s" % (e, key.name)), 0]
        ent = key.sem_in[e]
        sem = ent[0]
        waits = self._collect(e, reads, writes, skip_sem=sem)
        ent[1] += 16
        val = ent[1]
        ev = (sem, val, e, None)
        self.dma_since[id(sem)] = (sem, val)
        self.ops[e].append((waits, fn, (sem, 16)))
        self._commit(ev, reads, writes)
        return ev

    def mm(self, out, lhsT, rhs, start, stop, reads, writes, sgc=False):
        if sgc:
            return self.op("pe", lambda e: e.matmul(out, lhsT=lhsT, rhs=rhs, start=start, stop=stop, skip_group_check=True), reads, writes)
        return self.op("pe", lambda e: e.matmul(out, lhsT=lhsT, rhs=rhs, start=start, stop=stop), reads, writes)

    def tr(self, out, in_, ident, reads, writes):
        return self.op("pe", lambda e: e.transpose(out, in_, ident), reads, writes)

    def act(self, out, in_, func, reads, writes, bias=None, scale=None, accum=None):
        kw = {}
        if bias is not None:
            kw["bias"] = bias
        if scale is not None:
            kw["scale"] = scale
        if accum is not None:
            kw["accum_out"] = accum
        return self.op("act", lambda e: e.activation(out=out, in_=in_, func=func, **kw), reads, writes)

    def ts(self, eng, out, in0, s1, s2, op0, reads, writes, op1=None, accum=None):
        kw = {}
        if op1 is not None:
            kw["op1"] = op1
        if accum is not None:
            kw["accum_out"] = accum
        return self.op(eng, lambda e: e.tensor_scalar(out=out, in0=in0, scalar1=s1, scalar2=s2, op0=op0, **kw), reads, writes)

    def tt(self, eng, out, in0, in1, op, reads, writes):
        return self.op(eng, lambda e: e.tensor_tensor(out=out, in0=in0, in1=in1, op=op), reads, writes)

    def stt(self, eng, out, in0, scalar, in1, op0, op1, reads, writes):
        return self.op(eng, lambda e: e.scalar_tensor_tensor(out=out, in0=in0, scalar=scalar, in1=in1, op0=op0, op1=op1), reads, writes)

    def cp(self, eng, out, in_, reads, writes):
        if eng == "act":
            return self.op("act", lambda e: e.activation(out=out, in_=in_, func=AF.Copy), reads, writes)
        return self.op(eng, lambda e: e.tensor_copy(out=out, in_=in_), reads, writes)

    def memset(self, eng, ap, val, writes):
        return self.op(eng, lambda e: e.memset(ap, val), (), writes)

    def recip(self, out, in_, reads, writes):
        return self.op("dve", lambda e: e.reciprocal(out=out, in_=in_), reads, writes)

    def ld(self, q, out, in_, reads, writes):
        return self.dma(q, lambda e: e.dma_start(out=out, in_=in_), reads, writes)

    def st(self, q, out, in_, reads, writes, key):
        return self.dma(q, lambda e: e.dma_start(out=out, in_=in_), reads, writes, key=key)

    def fence(self):
        for e in self.ENG:
            pend = self.pending[e]
            for f in self.ENG:
                if f == "sp" or f == e or self.idx[f] == 0:
                    continue
                k = id(self.sem[f])
                pend[k] = (self.sem[f], self.idx[f])
            for k, sv in self.dma_since.items():
                if k not in pend or pend[k][1] < sv[1]:
                    pend[k] = sv
        self.dma_since = {}

    def finish(self, final_events):
        nc = self.nc
        ops = self.ops
        with nc.Block() as block:
            def emit(engname, eng):
                for waits, fn, (sem, inc) in ops[engname]:
                    for (s, v) in waits:
                        eng.wait_ge(s, v)
                    ins = fn(eng)
                    ins.then_inc(sem, inc)

            @block.tensor
            def _(eng):
                emit("pe", eng)

            @block.scalar
            def _(eng):
                emit("act", eng)

            @block.vector
            def _(eng):
                emit("dve", eng)

            @block.gpsimd
            def _(eng):
                emit("pool", eng)

            @block.sync
            def _(eng):
                emit("sp", eng)
                for (s, v, _e, _i) in final_events:
                    eng.wait_ge(s, v)

T = 2048
D = 1024
NT = 16
NG = 4
KC = 8
LN_EPS = 1e-5
RMS_EPS = 1e-6
DEPTH = 2
ALPHA = (2.0 * DEPTH) ** 0.25
IN_COLS = 5604
VP = 68
NIT = 18
W0 = 64.0


def make_consts():
    f32 = np.float32
    c = {}
    c["c_ident"] = np.eye(128, dtype=f32)
    c["c_ones"] = np.ones((128, 128), f32)
    t = np.arange(T, dtype=f32)
    p = np.arange(128)
    i64 = ((p % 64) % 32).astype(f32)
    inv64 = np.power(f32(10000.0), -(i64 / f32(32.0))).astype(f32)
    ang = (t[None, :] * inv64[:, None]).astype(f32).astype(np.float64)
    c["c_cos64"] = np.cos(ang).astype(f32)
    c["c_sin64"] = np.sin(ang).astype(f32)
    iA = ((p - 64) % 16).astype(f32)
    invA = np.power(f32(10000.0), -(iA / f32(16.0))).astype(f32)
    angA = (t[None, :] * invA[:, None]).astype(f32).astype(np.float64)
    c["c_cosA"] = np.cos(angA).astype(f32)
    c["c_sinA"] = np.sin(angA).astype(f32)
    pm = np.zeros((128, 128), f32)
    for fp in range(128):
        d = fp % 64
        if d < 32:
            pm[fp + 32, fp] = -1.0
        else:
            pm[fp - 32, fp] = 1.0
    c["c_pm64"] = pm
    pa = np.zeros((128, 128), f32)
    for fp in range(64, 96):
        d = fp - 64
        if d < 16:
            pa[fp + 16, fp] = -1.0
        else:
            pa[fp - 16, fp] = 1.0
    c["c_pmA"] = pa
    s = np.arange(128)[:, None]
    q = np.arange(128)[None, :]
    c["c_diag"] = ((s < 64) | (q >= 64)).astype(f32)
    c["c_swaprev"] = (~((s < 64) & (q >= 64))).astype(f32)
    tt_ = np.arange(128)[:, None]
    ss_ = np.arange(128)[None, :]
    c["c_negdiag"] = np.where((tt_ < 64) & (ss_ >= 64), f32(-1e30), f32(0.0)).astype(f32)
    c["c_negprev"] = np.where((tt_ >= 64) & (ss_ < 64), f32(-1e30), f32(0.0)).astype(f32)
    return c


CONST_SHAPES = {"c_ident": [128, 128], "c_ones": [128, 128], "c_cos64": [128, T], "c_sin64": [128, T],
                "c_cosA": [128, T], "c_sinA": [128, T], "c_pm64": [128, 128], "c_pmA": [128, 128],
                "c_diag": [128, 128], "c_swaprev": [128, 128], "c_negdiag": [128, 128], "c_negprev": [128, 128]}

WEIGHT_SHAPES = {
    "ln_in_g": [D], "ln_in_b": [D], "w_in": [2, D, IN_COLS],
    "a_q_ln_g": [2, 128, 3], "a_kv_ln_g": [2, 128, 2],
    "a_w_uq": [2, 384, 768], "a_w_ukv": [2, 256, 1024], "c_sinks": [2, 8],
    "w_br_a": [2, 512, D], "w_br_b": [2, 512, D], "w_br_c": [2, 512, D], "w_out": [2, D, D],
    "ln1_g": [2, D], "ln1_b": [2, D], "w_router": [D, 16], "router_bias": [16],
    "w_exp_gate": [2, 16, D, 512], "w_exp_up": [2, 16, D, 512], "w_exp_down": [2, 16, 512, D],
    "ln2_g": [2, D], "ln2_b": [2, D],
}


class Scope:
    def __init__(self, P):
        self.P = P

    def __enter__(self):
        self.prev = self.P.es
        self.stack = contextlib.ExitStack()
        self.stack.__enter__()
        self.P.es = self.stack
        return self

    def __exit__(self, *a):
        self.P.es = self.prev
        self.P.fence()
        return self.stack.__exit__(*a)


def bufs(name, n):
    return [NB("%s%d" % (name, i)) for i in range(n)]


def build_program(layers, do_ln_in, final, dbg_names=()):
    nc = bass.Bass("TRN2", target_bir_lowering=False)
    dr = {}
    dr["x"] = nc.dram_tensor("x", [T, D], F32, kind="ExternalInput").ap()
    for k, shp in WEIGHT_SHAPES.items():
        dr[k] = nc.dram_tensor(k, shp, F32, kind="ExternalInput").ap()
    for k, shp in CONST_SHAPES.items():
        dr[k] = nc.dram_tensor(k, shp, F32, kind="ExternalInput").ap()
    y = nc.dram_tensor("y", [T, D], F32, kind="ExternalOutput").ap()
    xres = nc.dram_tensor("xres", [T, D], F32).ap()
    dbg = {}
    DBG_SHAPES = {"d_xT": ([128, KC, T], BF16), "d_oTa": ([128, 4, T], BF16), "d_oTb": ([128, 4, T], BF16),
                  "d_oTc": ([128, 4, T], BF16), "d_gate": ([128, NT, 16], F32)}
    for k in dbg_names:
        shp, dt_ = DBG_SHAPES[k]
        dbg[k] = nc.dram_tensor(k, shp, dt_, kind="ExternalOutput").ap()

    es = contextlib.ExitStack()
    with es:
        P = Prog(nc, es)
        final_events = []
        banks = [P.ps("bank%d" % i, [128, 512], F32) for i in range(8)]
        Bbank = bufs("bank", 8)
        for b_ in Bbank:
            b_.psum = True
        P.guard["act"] = P.sb("guard_act", [128, 8], F32)[:]
        P.guard["dve"] = P.sb("guard_dve", [128, 8], F32)[:]
        xT = P.sb("xT", [128, KC, T], BF16)
        BxT = bufs("xT", NT)
        Bxres = bufs("xres", NT)
        Bc = NB("consts")
        ident_bf = P.sb("ident_bf", [128, 128], BF16)
        ident_f = P.sb("ident_f", [128, 128], F32)
        ones_bf = P.sb("ones_bf", [128, 128], BF16)
        pm64 = P.sb("pm64", [128, 128], BF16)
        pmA = P.sb("pmA", [128, 128], BF16)
        diag_bf = P.sb("diag_bf", [128, 128], BF16)
        swaprev_bf = P.sb("swaprev_bf", [128, 128], BF16)
        negdiag = P.sb("negdiag", [128, 128], F32)
        negdiag_bf = P.sb("negdiag_bf", [128, 128], BF16)
        negprev_bf = P.sb("negprev_bf", [128, 128], BF16)
        P.ld("pool", ident_bf[:], dr["c_ident"], [], [Bc])
        P.ld("pool", ones_bf[:], dr["c_ones"], [], [Bc])
        P.ld("pool", pm64[:], dr["c_pm64"], [], [Bc])
        P.ld("pool", pmA[:], dr["c_pmA"], [], [Bc])
        P.ld("pool", diag_bf[:], dr["c_diag"], [], [Bc])
        P.ld("pool", swaprev_bf[:], dr["c_swaprev"], [], [Bc])
        P.ld("pool", negdiag_bf[:], dr["c_negdiag"], [], [Bc])
        P.ld("pool", negprev_bf[:], dr["c_negprev"], [], [Bc])
        P.ld("sp", ident_f[:], dr["c_ident"], [], [Bc])
        P.ld("sp", negdiag[:], dr["c_negdiag"], [], [Bc])
        P.guard_src = (ident_f[:, 0:1], Bc)

        def gsl(G):
            return slice(G * 512, (G + 1) * 512)

        def tsl(tt):
            return slice(tt * 128, (tt + 1) * 128)

        class Rot:
            def __init__(self, items):
                self.items = items
                self.i = 0

            def next(self):
                it = self.items[self.i % len(self.items)]
                self.i += 1
                return it

        def dump(name, ap_sb, rbufs):
            if name in dbg:
                final_events.append(P.st("sp", dbg[name], ap_sb, rbufs, [], key=NB("dbg_" + name)))

        def ln_inplace(t_ap, Bt, gB, bB, Bgb, st, Bst, junk, Bjunk):
            P.act(junk, t_ap, AF.Copy, [Bt], [Bjunk, Bst], accum=st[:, 0:1])
            P.act(junk, t_ap, AF.Square, [Bt], [Bjunk, Bst], accum=st[:, 1:2])
            P.ts("dve", st[:, 2:3], st[:, 0:1], 1.0 / D, None, ALU.mult, [Bst], [Bst])
            P.tt("dve", st[:, 3:4], st[:, 2:3], st[:, 2:3], ALU.mult, [Bst], [Bst])
            P.stt("dve", st[:, 4:5], st[:, 1:2], 1.0 / D, st[:, 3:4], ALU.mult, ALU.subtract, [Bst], [Bst])
            P.ts("dve", st[:, 4:5], st[:, 4:5], LN_EPS, None, ALU.add, [Bst], [Bst])
            P.act(st[:, 5:6], st[:, 4:5], AF.Sqrt, [Bst], [Bst])
            P.recip(st[:, 6:7], st[:, 5:6], [Bst], [Bst])
            P.stt("dve", st[:, 7:8], st[:, 2:3], -1.0, st[:, 6:7], ALU.mult, ALU.mult, [Bst], [Bst])
            P.act(t_ap, t_ap, AF.Identity, [Bt, Bst], [Bt], scale=st[:, 6:7], bias=st[:, 7:8])
            P.tt("dve", t_ap, t_ap, gB, ALU.mult, [Bt, Bgb], [Bt])
            P.tt("dve", t_ap, t_ap, bB, ALU.add, [Bt, Bgb], [Bt])

        def to_xT(t_ap, Bt, tt, xbf, Bxbf, bk):
            P.cp("act", xbf, t_ap, [Bt], [Bxbf])
            bv = banks[bk][:].bitcast(BF16)
            for kc in range(KC):
                P.tr(bv[:, kc * 128:(kc + 1) * 128], xbf[:, kc * 128:(kc + 1) * 128], ident_bf[:],
                     [Bxbf, Bc], [Bbank[bk]])
            P.cp("dve", xT[:, :, tsl(tt)], bv.rearrange("p (k t) -> p k t", t=128), [Bbank[bk]], [BxT[tt]])

        def ln_batch(items, gB, bB, Bgb, st, Bsum, Bsq, Bvec, junks, Bjunks):
            nb = len(items)
            for i, (t_ap, Bt) in enumerate(items):
                P.act(junks[i % 2], t_ap, AF.Copy, [Bt], [Bjunks[i % 2], Bsum[i]], accum=st[:, 0, i:i + 1])
            for i, (t_ap, Bt) in enumerate(items):
                P.act(junks[i % 2], t_ap, AF.Square, [Bt], [Bjunks[i % 2], Bsq[i]], accum=st[:, 1, i:i + 1])
            V = [Bvec]
            P.ts("dve", st[:, 2, 0:nb], st[:, 0, 0:nb], 1.0 / D, None, ALU.mult, Bsum[0:nb], V)
            P.tt("dve", st[:, 3, 0:nb], st[:, 2, 0:nb], st[:, 2, 0:nb], ALU.mult, V, V)
            P.stt("dve", st[:, 4, 0:nb], st[:, 1, 0:nb], 1.0 / D, st[:, 3, 0:nb], ALU.mult, ALU.subtract, Bsq[0:nb] + V, V)
            P.ts("dve", st[:, 4, 0:nb], st[:, 4, 0:nb], LN_EPS, None, ALU.add, V, V)
            P.act(st[:, 5, 0:nb], st[:, 4, 0:nb], AF.Sqrt, V, V)
            P.recip(st[:, 6, 0:nb], st[:, 5, 0:nb], V, V)
            P.stt("dve", st[:, 7, 0:nb], st[:, 2, 0:nb], -1.0, st[:, 6, 0:nb], ALU.mult, ALU.mult, V, V)
            for i, (t_ap, Bt) in enumerate(items):
                P.act(t_ap, t_ap, AF.Identity, [Bt, Bvec], [Bt], scale=st[:, 6, i:i + 1], bias=st[:, 7, i:i + 1])
            for i, (t_ap, Bt) in enumerate(items):
                P.tt("dve", t_ap, t_ap, gB, ALU.mult, [Bt, Bgb], [Bt])
            for i, (t_ap, Bt) in enumerate(items):
                P.tt("dve", t_ap, t_ap, bB, ALU.add, [Bt, Bgb], [Bt])

        def to_xT_batch(items, tts, xbfs, Bxbfs, bks):
            for i, (t_ap, Bt) in enumerate(items):
                P.cp("act", xbfs[i % len(xbfs)], t_ap, [Bt], [Bxbfs[i % len(xbfs)]])
                bk = bks[i % len(bks)]
                bv = banks[bk][:].bitcast(BF16)
                xb = xbfs[i % len(xbfs)]
                for kc in range(KC):
                    P.tr(bv[:, kc * 128:(kc + 1) * 128], xb[:, kc * 128:(kc + 1) * 128], ident_bf[:],
                         [Bxbfs[i % len(xbfs)], Bc], [Bbank[bk]])
                P.cp("dve", xT[:, :, tsl(tts[i])], bv.rearrange("p (k t) -> p k t", t=128), [Bbank[bk]], [BxT[tts[i]]])

        with Scope(P):
            gB = P.sb("ln0_g", [128, D], F32)
            bB = P.sb("ln0_b", [128, D], F32)
            Bgb = NB("ln0gb")
            if do_ln_in:
                P.ld("sp", gB[:], dr["ln_in_g"].partition_broadcast(128), [], [Bgb])
                P.ld("sp", bB[:], dr["ln_in_b"].partition_broadcast(128), [], [Bgb])
            xt = [P.sb("in_x%d" % i, [128, D], F32) for i in range(4)]
            Bxt = bufs("in_x", 4)
            stt_ = P.sb("in_st", [128, 8, 4], F32)
            Bsum = bufs("in_sum", 4)
            Bsq = bufs("in_sq", 4)
            Bvec = NB("in_vec")
            junks = [P.sb("in_junk%d" % i, [128, D], BF16)[:] for i in range(2)]
            Bjunks = bufs("in_junk", 2)
            xbf = [P.sb("in_xbf%d" % i, [128, D], BF16)[:] for i in range(2)]
            Bxbf = bufs("in_xbf", 2)
            for t0 in range(0, NT, 4):
                items = []
                for i in range(4):
                    P.ld("sp", xt[i][:], dr["x"][tsl(t0 + i), :], [], [Bxt[i]])
                    items.append((xt[i][:], Bxt[i]))
                if do_ln_in:
                    ln_batch(items, gB[:], bB[:], Bgb, stt_, Bsum, Bsq, Bvec, junks, Bjunks)
                for i in range(4):
                    P.st("sp", xres[tsl(t0 + i), :], xt[i][:], [Bxt[i]], [Bxres[t0 + i]], key=Bxt[i])
                to_xT_batch(items, [t0 + i for i in range(4)], xbf, Bxbf, [0, 1, 2, 3])

        def load_w(q, dst_ap, src_ap, Bw):
            P.ld(q, dst_ap, src_ap, [], [Bw])

        def proj_fm(lhsT_of_kc, M, G, bk, wr):
            for kc in range(KC):
                P.mm(banks[bk][0:M, :], lhsT_of_kc(kc), xT[:, kc, gsl(G)], kc == 0, kc == KC - 1,
                     [BxT[4 * G + j] for j in range(4)] + wr, [Bbank[bk]])

        def layer(l, last):
            w_in_v = dr["w_in"][l].rearrange("(kc p) c -> p kc c", p=128)
            logit = P.sb("logit", [128, NT, 16], F32)
            Blogit = bufs("logit", NT)
            with Scope(P):
                oT = [P.sb("oT%d" % i, [128, 4, T], BF16) for i in range(3)]
                BoT = [bufs("oT%d_" % i, NG) for i in range(3)]
                with Scope(P):
                    aqn = P.sb("aqn", [128, 3, T], BF16)
                    Baqn = bufs("aqn", NG)
                    akvn = P.sb("akvn", [128, 2, T], BF16)
                    Bakvn = bufs("akvn", NG)
                    kpe = P.sb("kpe", [96, T], BF16)
                    Bkpe = bufs("kpe", NG)
                    cosA = P.sb("cosA", [128, T], F32)
                    sinA = P.sb("sinA", [128, T], F32)
                    Btab = NB("tabA")
                    P.ld("sp", cosA[:], dr["c_cosA"], [], [Btab])
                    P.ld("sp", sinA[:], dr["c_sinA"], [], [Btab])
                    wuq = P.sb("wuq", [128, 3, 768], BF16)
                    wukv = P.sb("wukv", [128, 2, 1024], BF16)
                    Bwu = NB("wu")
                    P.ld("pool", wuq[:], dr["a_w_uq"][l].rearrange("(kc p) c -> p kc c", p=128), [], [Bwu])
                    P.ld("pool", wukv[:], dr["a_w_ukv"][l].rearrange("(kc p) c -> p kc c", p=128), [], [Bwu])
                    t1 = [P.sb("a_t1_%d" % i, [128, 512], F32) for i in range(2)]
                    t2 = [P.sb("a_t2_%d" % i, [128, 512], F32) for i in range(2)]
                    Bt1 = bufs("a_t1", 2)
                    Bt2 = bufs("a_t2", 2)
                    trot = Rot([0, 1])
                    with Scope(P):
                        Wa = P.sb("Wa", [128, KC, 672], BF16)
                        BWa = NB("Wa")
                        P.ld("pool", Wa[:], w_in_v[:, :, 0:672], [], [BWa])
                        gq = P.sb("gq", [128, 3], F32)
                        gkv = P.sb("gkv", [128, 2], F32)
                        Bg = NB("gqkv")
                        P.ld("sp", gq[:], dr["a_q_ln_g"][l], [], [Bg])
                        P.ld("sp", gkv[:], dr["a_kv_ln_g"][l], [], [Bg])
                        sq = P.sb("sq", [128, 3, 512], BF16)
                        Bsq = bufs("sq", 3)
                        rs = P.sb("rs", [128, 512], F32)
                        Brs = NB("rs")
                        kraw = P.sb("kraw", [96, 512], BF16)
                        Bkraw = NB("kraw")
                        P.memset("dve", kraw[:], 0.0, [Bkraw])
                        brot = Rot([0, 1, 2, 3])
                        for G in range(NG):
                            for (dst, Bdst, nch, col0, g_ap, nfeat) in ((aqn, Baqn, 3, 0, gq, 384.0), (akvn, Bakvn, 2, 384, gkv, 256.0)):
                                for c in range(nch):
                                    bk = brot.next()
                                    proj_fm(lambda kc, c=c, col0=col0: Wa[:, kc, col0 + c * 128: col0 + (c + 1) * 128], 128, G, bk, [BWa])
                                    P.cp("act", dst[:, c, gsl(G)], banks[bk][:], [Bbank[bk]], [Bdst[G]])
                                    P.act(sq[:, c, :], banks[bk][:], AF.Square, [Bbank[bk]], [Bsq[c]])
                                bk = brot.next()
                                for c in range(nch):
                                    P.mm(banks[bk][:], ones_bf[:], sq[:, c, :], c == 0, c == nch - 1, [Bc, Bsq[c]], [Bbank[bk]])
                                P.ts("dve", rs[:], banks[bk][:], 1.0 / nfeat, RMS_EPS, ALU.mult, [Bbank[bk]], [Brs], op1=ALU.add)
                                P.act(rs[:], rs[:], AF.Sqrt, [Brs], [Brs])
                                P.recip(rs[:], rs[:], [Brs], [Brs])
                                for c in range(nch):
                                    P.stt("dve", dst[:, c, gsl(G)], dst[:, c, gsl(G)], g_ap[:, c:c + 1], rs[:], ALU.mult, ALU.mult,
                                          [Bdst[G], Bg, Brs], [Bdst[G]])
                            bk = brot.next()
                            proj_fm(lambda kc: Wa[:, kc, 576:672], 96, G, bk, [BWa])
                            P.cp("act", kraw[64:96, :], banks[bk][64:96, :], [Bbank[bk]], [Bkraw])
                            bk2 = brot.next()
                            P.mm(banks[bk2][0:96, :], pmA[0:96, 0:96], kraw[0:96, :], True, True, [Bc, Bkraw], [Bbank[bk2]])
                            k = trot.next()
                            P.tt("dve", t1[k][64:96, :], kraw[64:96, :], cosA[64:96, gsl(G)], ALU.mult, [Bkraw, Btab], [Bt1[k]])
                            P.tt("dve", t2[k][64:96, :], banks[bk2][64:96, :], sinA[64:96, gsl(G)], ALU.mult, [Bbank[bk2], Btab], [Bt2[k]])
                            P.tt("dve", kpe[64:96, gsl(G)], t1[k][64:96, :], t2[k][64:96, :], ALU.add, [Bt1[k], Bt2[k]], [Bkpe[G]])
                    Va = P.sb("Va", [128, NT, 8, VP], BF16)
                    BVa = bufs("Va", NT)
                    BVa1 = NB("Va_ones")
                    P.memset("dve", Va[:, :, :, 64:VP], 1.0, [BVa1])
                    wukv_v = wukv[:].rearrange("p k (h d) -> p k h d", d=128)
                    brot = Rot([5, 6, 7])
                    for tt in range(NT):
                        bk = brot.next()
                        for kc in range(2):
                            P.mm(banks[bk][:].rearrange("p (h d) -> p h d", d=64), akvn[:, kc, tsl(tt)], wukv_v[:, kc, :, 64:128],
                                 kc == 0, kc == 1, [Bakvn[tt // 4], Bwu], [Bbank[bk]])
                        P.cp("act", Va[:, tt, :, 0:64], banks[bk][:].rearrange("p (h d) -> p h d", d=64), [Bbank[bk]], [BVa[tt]])
                    QT = [P.sb("QTa%d" % i, [96, T], BF16) for i in range(2)]
                    KT = [P.sb("KTa%d" % i, [96, T], BF16) for i in range(2)]
                    BQT = [bufs("QTa%d_" % i, NG) for i in range(2)]
                    BKT = [bufs("KTa%d_" % i, NG) for i in range(2)]
                    pt = [P.sb("a_pt%d" % i, [128, 512], BF16) for i in range(3)]
                    Bpt = bufs("a_pt", 3)
                    ptrot = Rot([0, 1, 2])
                    srot = Rot([0, 1, 2])
                    arot = Rot([3, 4])
                    otok = [P.sb("a_otok%d" % i, [128, NT, 128], BF16) for i in range(2)]
                    Botok = [bufs("a_otok%d_" % i, NT) for i in range(2)]
                    rec = [P.sb("a_rec%d" % i, [128, 4], F32) for i in range(2)]
                    Brec = bufs("a_rec", 2)
                    recrot = Rot([0, 1])
                    sc_a = 96.0 ** -0.5
                    def proj_head(h):
                        hp = h % 2
                        for G in range(NG):
                            bk = brot.next()
                            for kc in range(3):
                                P.mm(banks[bk][0:96, :], wuq[:, kc, h * 96:(h + 1) * 96], aqn[:, kc, gsl(G)], kc == 0, kc == 2,
                                     [Bwu, Baqn[G]], [Bbank[bk]])
                            P.cp("act", QT[hp][0:96, gsl(G)], banks[bk][0:96, :], [Bbank[bk]], [BQT[hp][G]])
                            bk2 = brot.next()
                            P.mm(banks[bk2][0:96, :], pmA[0:96, 0:96], QT[hp][0:96, gsl(G)], True, True, [Bc, BQT[hp][G]], [Bbank[bk2]])
                            k = trot.next()
                            P.tt("dve", t1[k][64:96, :], QT[hp][64:96, gsl(G)], cosA[64:96, gsl(G)], ALU.mult, [BQT[hp][G], Btab], [Bt1[k]])
                            P.tt("dve", t2[k][64:96, :], banks[bk2][64:96, :], sinA[64:96, gsl(G)], ALU.mult, [Bbank[bk2], Btab], [Bt2[k]])
                            P.tt("dve", QT[hp][64:96, gsl(G)], t1[k][64:96, :], t2[k][64:96, :], ALU.add, [Bt1[k], Bt2[k]], [BQT[hp][G]])
                            bk = brot.next()
                            for kc in range(2):
                                P.mm(banks[bk][0:64, :], wukv[:, kc, h * 128:h * 128 + 64], akvn[:, kc, gsl(G)], kc == 0, kc == 1,
                                     [Bwu, Bakvn[G]], [Bbank[bk]])
                            P.cp("act", KT[hp][0:64, gsl(G)], banks[bk][0:64, :], [Bbank[bk]], [BKT[hp][G]])
                            P.cp("dve", KT[hp][64:96, gsl(G)], kpe[64:96, gsl(G)], [Bkpe[G]], [BKT[hp][G]])

                    blocks = [(G, st_) for G in range(NG) for st_ in range(4 * G + 4)]
                    proj_head(0)
                    for h in range(8):
                        hp = h % 2
                        cpair = h // 2
                        if h + 1 < 8:
                            proj_head(h + 1)
                        info = {}

                        def qk(k):
                            G, st_ = blocks[k]
                            j0 = max(0, st_ - 4 * G)
                            ncol = (4 - j0) * 128
                            q0 = (4 * G + j0) * 128
                            sb_ = srot.next()
                            P.mm(banks[sb_][:, 0:ncol], KT[hp][0:96, tsl(st_)], QT[hp][0:96, q0:q0 + ncol], True, True,
                                 [BKT[hp][st_ // 4], BQT[hp][G]], [Bbank[sb_]])
                            if st_ >= 4 * G:
                                P.mm(banks[sb_][:, 0:128], negdiag_bf[:], ident_bf[:], False, True, [Bc], [Bbank[sb_]], sgc=True)
                            info[k] = (sb_, j0, ncol)

                        qk(0)
                        ab = None
                        for k, (G, st_) in enumerate(blocks):
                            if k + 1 < len(blocks):
                                qk(k + 1)
                            sb_, j0, ncol = info[k]
                            if st_ == 0:
                                ab = arot.next()
                            accv = banks[ab][:, 0:4 * VP].rearrange("p (j d) -> p j d", d=VP)
                            pk = ptrot.next()
                            P.act(pt[pk][:, 0:ncol], banks[sb_][:, 0:ncol], AF.Exp, [Bbank[sb_]], [Bpt[pk]], scale=sc_a)
                            for j in range(j0, 4):
                                P.mm(accv[:, j, 0:65], pt[pk][:, (j - j0) * 128:(j - j0 + 1) * 128], Va[:, st_, h, 0:65],
                                     (st_ == 0 and j == 0), st_ == 4 * G + j, [Bpt[pk], BVa[st_], BVa1], [Bbank[ab]], sgc=True)
                            if st_ == 4 * G + 3:
                                rk = recrot.next()
                                P.recip(rec[rk][:], accv[:, :, 64], [Bbank[ab]], [Brec[rk]])
                                for j in range(4):
                                    tt = 4 * G + j
                                    P.ts("dve", otok[cpair % 2][:, tt, hp * 64:(hp + 1) * 64], accv[:, j, 0:64], rec[rk][:, j:j + 1], None, ALU.mult,
                                         [Bbank[ab], Brec[rk]], [Botok[cpair % 2][tt]])
                        if hp == 1:
                            for t0 in range(0, NT, 8):
                                bk = brot.next()
                                bv = banks[bk][:].bitcast(BF16)
                                for tt in range(t0, t0 + 8):
                                    P.tr(bv[:, (tt - t0) * 128:(tt - t0 + 1) * 128], otok[cpair % 2][:, tt, :], ident_bf[:],
                                         [Botok[cpair % 2][tt], Bc], [Bbank[bk]])
                                P.cp("act", oT[0][:, cpair, t0 * 128:(t0 + 8) * 128], bv[:], [Bbank[bk]], [BoT[0][t0 // 4], BoT[0][t0 // 4 + 1]])
                dump("d_oTa", oT[0][:], BoT[0])
                if STOP_AFTER == "mla":
                    return

                def hd64_proj(name, col_q, col_k, col_v, QTx, BQTx, KTx, BKTx, Vx, BVx, extra=None):
                    with Scope(P):
                        cos64 = P.sb(name + "cos", [128, T], F32)
                        sin64 = P.sb(name + "sin", [128, T], F32)
                        Btab = NB(name + "tab")
                        P.ld("sp", cos64[:], dr["c_cos64"], [], [Btab])
                        P.ld("sp", sin64[:], dr["c_sin64"], [], [Btab])
                        Wq = P.sb(name + "Wq", [128, KC, 512], BF16)
                        Wk2 = P.sb(name + "Wk2", [128, KC, 2, 128], BF16)
                        Wv = P.sb(name + "Wv", [128, KC, 128], BF16)
                        BW = NB(name + "W")
                        P.ld("pool", Wq[:], w_in_v[:, :, col_q:col_q + 512], [], [BW])
                        for g in range(2):
                            for i in range(2):
                                P.ld("pool", Wk2[:, :, g, i * 64:(i + 1) * 64], w_in_v[:, :, col_k + g * 64: col_k + (g + 1) * 64], [], [BW])
                        P.ld("pool", Wv[:], w_in_v[:, :, col_v:col_v + 128], [], [BW])
                        chunks = []
                        for c in range(4):
                            chunks.append((lambda kc, c=c: Wq[:, kc, c * 128:(c + 1) * 128], lambda G, c=c: QTx[:, c, gsl(G)], BQTx))
                        for g in range(2):
                            chunks.append((lambda kc, g=g: Wk2[:, kc, g, :], lambda G, g=g: KTx[:, g, gsl(G)], BKTx))
                        xw = None
                        if extra is not None:
                            xw = extra(BW, chunks)
                        raw = [P.sb(name + "raw%d" % i, [128, 512], BF16) for i in range(2)]
                        Braw = bufs(name + "raw", 2)
                        t1 = [P.sb(name + "t1_%d" % i, [128, 512], F32) for i in range(2)]
                        t2 = [P.sb(name + "t2_%d" % i, [128, 512], F32) for i in range(2)]
                        Bt1 = bufs(name + "t1", 2)
                        Bt2 = bufs(name + "t2", 2)
                        rrot = Rot([0, 1])
                        brot = Rot([0, 1, 2, 3, 4, 5, 6, 7])
                        for G in range(NG):
                            if STOP_AFTER == name + "w":
                                break
                            for (lf, df, Bd) in chunks:
                                bk = brot.next()
                                proj_fm(lf, 128, G, bk, [BW])
                                k = rrot.next()
                                P.cp("act", raw[k][:], banks[bk][:], [Bbank[bk]], [Braw[k]])
                                bk2 = brot.next()
                                P.mm(banks[bk2][:], pm64[:], raw[k][:], True, True, [Bc, Braw[k]], [Bbank[bk2]])
                                P.tt("dve", t1[k][:], raw[k][:], cos64[:, gsl(G)], ALU.mult, [Braw[k], Btab], [Bt1[k]])
                                P.tt("dve", t2[k][:], banks[bk2][:], sin64[:, gsl(G)], ALU.mult, [Bbank[bk2], Btab], [Bt2[k]])
                                P.tt("dve", df(G), t1[k][:], t2[k][:], ALU.add, [Bt1[k], Bt2[k]], [Bd[G]])
                        BV1 = NB(name + "V1")
                        if STOP_AFTER == name + "rope":
                            return BV1
                        P.memset("dve", Vx[:, :, :, 64:VP], 1.0, [BV1])
                        for tt in range(NT):
                            bk = brot.next()
                            for kc in range(KC):
                                P.mm(banks[bk][:, 0:128], xT[:, kc, tsl(tt)], Wv[:, kc, :], kc == 0, kc == KC - 1, [BxT[tt], BW], [Bbank[bk]])
                            if xw is None and EXPER == "A":
                                for kc in range(KC):
                                    P.mm(banks[bk][:, 128:132], xT[:, kc, tsl(tt)], Wv[:, kc, 0:4], kc == 0, kc == KC - 1, [BxT[tt], BW], [Bbank[bk]])
                            if xw is not None:
                                Wwi, widx, Bwidx = xw
                                for kc in range(KC):
                                    P.mm(banks[bk][:, 128:132], xT[:, kc, tsl(tt)], Wwi[:, kc, :], kc == 0, kc == KC - 1, [BxT[tt], BW], [Bbank[bk]])
                                P.act(widx[:, tt, :], banks[bk][:, 128:132], AF.Copy, [Bbank[bk]], [Bwidx[tt]], scale=1.0 / 16.0)
                            P.cp("act", Vx[:, tt, :, 0:64], banks[bk][:, 0:128].rearrange("p (g d) -> p g d", d=64), [Bbank[bk]], [BVx[tt]])
                        return BV1

                with Scope(P):
                    if SKIP_DSA:
                        raise_skip = True
                    QTb = P.sb("QTb", [128, 4, T], BF16)
                    BQTb = bufs("QTb", NG)
                    KTb = P.sb("KTb", [128, 2, T], BF16)
                    BKTb = bufs("KTb", NG)
                    Vb = P.sb("Vb", [128, NT, 2, VP], BF16)
                    BVb = bufs("Vb", NT)
                    QIT = P.sb("QIT", [128, 2, T], BF16)
                    BQIT = bufs("QIT", NG)
                    KIT = P.sb("KIT", [128, T], BF16)
                    BKIT = bufs("KIT", NG)
                    widx = P.sb("widx", [128, NT, 4], F32)
                    Bwidx = bufs("widx", NT)

                    def extra_b(BW, chunks):
                        Wqi = P.sb("Wqi", [128, KC, 256], BF16)
                        Wki2 = P.sb("Wki2", [128, KC, 128], BF16)
                        Wwi = P.sb("Wwi", [128, KC, 4], BF16)
                        P.ld("pool", Wqi[:], w_in_v[:, :, 1440:1696], [], [BW])
                        for i in range(2):
                            P.ld("pool", Wki2[:, :, i * 64:(i + 1) * 64], w_in_v[:, :, 1696:1760], [], [BW])
                        P.ld("pool", Wwi[:], w_in_v[:, :, 1760:1764], [], [BW])
                        for c in range(2):
                            chunks.append((lambda kc, c=c: Wqi[:, kc, c * 128:(c + 1) * 128], lambda G, c=c: QIT[:, c, gsl(G)], BQIT))
                        chunks.append((lambda kc: Wki2[:, kc, :], lambda G: KIT[:, gsl(G)], BKIT))
                        return (Wwi, widx, Bwidx)

                    if not SKIP_DSA:
                        BVb1 = hd64_proj("b_", 672, 1184, 1312, QTb, BQTb, KTb, BKTb, Vb, BVb, extra_b)
                    if not SKIP_DSA:
                        score32 = [P.sb("score32_%d" % i, [128, 512], F32) for i in range(2)]
                        Bs32 = bufs("score32_", 2)
                        s32rot = Rot([0, 1])
                        scorebf = [P.sb("scorebf%d" % i, [128, T], BF16) for i in range(4)]
                        Bsbf = bufs("scorebf", 4)
                        junkb = P.sb("junkb", [128, T], BF16)
                        Bjunkb = NB("junkb")
                        junka = P.sb("junka", [128, T], BF16)
                        Bjunka = NB("junka")
                        mbias = [P.sb("mbias%d" % i, [128, 4, T], BF16) for i in range(2)]
                        Bmb = [bufs("mbias%d_" % i, 4) for i in range(2)]
                        rr = [P.sb("rr%d" % i, [128, 512], F32) for i in range(2)]
                        Brr = bufs("rr", 2)
                        rrot = Rot([0, 1])
                        bis = P.sb("bis", [128, 16], F32)
                        Bmid = NB("bis_mid")
                        Bval = bufs("bis_val", 4)
                        Btmp = NB("bis_tmp")
                        Bthr = NB("bis_thr")
                        pt = [P.sb("b_pt%d" % i, [128, 512], BF16) for i in range(3)]
                        Bpt = bufs("b_pt", 3)
                        ptrot = Rot([0, 1, 2])
                        srot = Rot([0, 1, 2])
                        arot = Rot([3, 4])
                        irot = Rot([5, 6, 7])
                        otg = [P.sb("b_otg%d" % i, [128, 4, 512], BF16) for i in range(2)]
                        Botg = [bufs("b_otg%d_" % i, 4) for i in range(2)]
                        rec = [P.sb("b_rec%d" % i, [128, 4], F32) for i in range(2)]
                        Brec = bufs("b_rec", 2)
                        recrot = Rot([0, 1])

                        def topk_gen(G):
                            nSs = [(4 * G + j + 1) * 128 for j in range(4)]
                            for j in range(4):
                                qt = 4 * G + j
                                nS = nSs[j]
                                nsc = (nS + 511) // 512
                                for sc in range(nsc):
                                    ncol = min(512, nS - sc * 512)
                                    cols = slice(sc * 512, sc * 512 + ncol)
                                    sk = s32rot.next()
                                    s32 = score32[sk]
                                    for ih in range(4):
                                        c, i = ih // 2, ih % 2
                                        bk = irot.next()
                                        P.mm(banks[bk][:, 0:ncol], QIT[i * 64:(i + 1) * 64, c, tsl(qt)], KIT[i * 64:(i + 1) * 64, cols], True, True,
                                             [BQIT[G], BKIT[sc]], [Bbank[bk]])
                                        rk = rrot.next()
                                        r = rr[rk]
                                        P.act(r[:, 0:ncol], banks[bk][:, 0:ncol], AF.Relu, [Bbank[bk]], [Brr[rk]])
                                        if ih == 0:
                                            P.ts("dve", s32[:, 0:ncol], r[:, 0:ncol], widx[:, qt, 0:1], None, ALU.mult, [Brr[rk], Bwidx[qt]], [Bs32[sk]])
                                        elif ih < 3:
                                            P.stt("dve", s32[:, 0:ncol], r[:, 0:ncol], widx[:, qt, ih:ih + 1], s32[:, 0:ncol], ALU.mult, ALU.add,
                                                  [Brr[rk], Bwidx[qt], Bs32[sk]], [Bs32[sk]])
                                        else:
                                            if sc == nsc - 1:
                                                P.tt("dve", s32[:, ncol - 128:ncol], s32[:, ncol - 128:ncol], negdiag[:], ALU.add, [Bs32[sk], Bc], [Bs32[sk]])
                                            P.stt("dve", scorebf[j][:, cols], r[:, 0:ncol], widx[:, qt, 3:4], s32[:, 0:ncol], ALU.mult, ALU.add,
                                                  [Brr[rk], Bwidx[qt], Bs32[sk]], [Bsbf[j]])
                                    yield
                            P.memset("dve", bis[:, 0:4], 0.0, [Bmid])
                            w = W0
                            for it in range(NIT):
                                for j in (2, 3):
                                    P.act(junka[:, 0:nSs[j]], scorebf[j][:, 0:nSs[j]], AF.Sign, [Bsbf[j], Bmid], [Bjunka, Bval[j]],
                                          bias=bis[:, j:j + 1], scale=-1.0, accum=bis[:, 4 + j:5 + j])
                                for j in (0, 1):
                                    P.ts("dve", junkb[:, 0:nSs[j]], scorebf[j][:, 0:nSs[j]], bis[:, j:j + 1], None, ALU.is_ge, [Bsbf[j], Bmid], [Bjunkb, Bval[j]],
                                         op1=ALU.add, accum=bis[:, 4 + j:5 + j])
                                P.ts("dve", bis[:, 8:10], bis[:, 4:6], 256.0, w, ALU.is_ge, [Bval[0], Bval[1]], [Btmp], op1=ALU.mult)
                                for j in (2, 3):
                                    P.ts("dve", bis[:, 8 + j:9 + j], bis[:, 4 + j:5 + j], float(nSs[j] - 512), w, ALU.is_le, [Bval[j]], [Btmp], op1=ALU.mult)
                                P.stt("dve", bis[:, 0:4], bis[:, 8:12], -w / 2, bis[:, 0:4], ALU.add, ALU.add, [Btmp, Bmid], [Bmid])
                                w = w / 2
                                yield
                            P.ts("dve", bis[:, 12:16], bis[:, 0:4], -w, None, ALU.add, [Bmid], [Bthr])
                            for j in range(4):
                                nS = nSs[j]
                                P.ts("dve", mbias[G % 2][:, j, 0:nS], scorebf[j][:, 0:nS], bis[:, 12 + j:13 + j], -30000.0, ALU.is_lt,
                                     [Bsbf[j], Bthr], [Bmb[G % 2][j]], op1=ALU.mult)
                                yield

                        def attn_gen(G):
                            mb = mbias[G % 2]
                            Bm = Bmb[G % 2]
                            og = otg[G % 2]
                            Bog = Botg[G % 2]
                            blocks = [(h, st_) for h in range(8) for st_ in range(4 * G + 4)]
                            info = {}

                            def qk(k):
                                h, st_ = blocks[k]
                                c, i, g = h // 2, h % 2, h // 4
                                j0 = max(0, st_ - 4 * G)
                                ncol = (4 - j0) * 128
                                q0 = (4 * G + j0) * 128
                                sb_ = srot.next()
                                P.mm(banks[sb_][:, 0:ncol], KTb[i * 64:(i + 1) * 64, g, tsl(st_)], QTb[i * 64:(i + 1) * 64, c, q0:q0 + ncol], True, True,
                                     [BKTb[st_ // 4], BQTb[G]], [Bbank[sb_]])
                                for j in range(j0, 4):
                                    P.mm(banks[sb_][:, (j - j0) * 128:(j - j0 + 1) * 128], mb[:, j, tsl(st_)], ident_bf[:], False, j == 3,
                                         [Bm[j], Bc], [Bbank[sb_]], sgc=True)
                                info[k] = (sb_, j0, ncol)

                            qk(0)
                            ab = None
                            for k, (h, st_) in enumerate(blocks):
                                g = h // 4
                                if k + 1 < len(blocks):
                                    qk(k + 1)
                                sb_, j0, ncol = info[k]
                                if st_ == 0:
                                    ab = arot.next()
                                accv = banks[ab][:, 0:4 * VP].rearrange("p (j d) -> p j d", d=VP)
                                pk = ptrot.next()
                                P.act(pt[pk][:, 0:ncol], banks[sb_][:, 0:ncol], AF.Exp, [Bbank[sb_]], [Bpt[pk]], scale=0.125)
                                for j in range(j0, 4):
                                    P.mm(accv[:, j, 0:65], pt[pk][:, (j - j0) * 128:(j - j0 + 1) * 128], Vb[:, st_, g, 0:65],
                                         (st_ == 0 and j == 0), st_ == 4 * G + j, [Bpt[pk], BVb[st_], BVb1], [Bbank[ab]], sgc=True)
                                if st_ == 4 * G + 3:
                                    rk = recrot.next()
                                    P.recip(rec[rk][:], accv[:, :, 64], [Bbank[ab]], [Brec[rk]])
                                    for j in range(4):
                                        P.ts("dve", og[:, j, h * 64:(h + 1) * 64], accv[:, j, 0:64], rec[rk][:, j:j + 1], None, ALU.mult,
                                             [Bbank[ab], Brec[rk]], [Bog[j]])
                                yield
                            for j in range(4):
                                tt = 4 * G + j
                                bk = irot.next()
                                bv = banks[bk][:].bitcast(BF16)
                                for c in range(4):
                                    P.tr(bv[:, c * 128:(c + 1) * 128], og[:, j, c * 128:(c + 1) * 128], ident_bf[:], [Bog[j], Bc], [Bbank[bk]])
                                P.cp("act", oT[1][:, :, tsl(tt)], bv[:, 0:512].rearrange("p (c t) -> p c t", t=128), [Bbank[bk]], [BoT[1][G]])
                            yield

                        for _ in topk_gen(0):
                            pass
                        for G in range(NG):
                            ag = list_len = None
                            a_units = 8 * (4 * G + 4) + 1
                            if G + 1 < NG:
                                t_units = sum(((4 * (G + 1) + j + 1) * 128 + 511) // 512 for j in range(4)) + NIT + 4
                                tg = topk_gen(G + 1)
                            else:
                                t_units = 0
                                tg = None
                            done_t = 0
                            for ai, _ in enumerate(attn_gen(G)):
                                if tg is not None:
                                    want = ((ai + 1) * t_units) // a_units
                                    while done_t < want:
                                        try:
                                            next(tg)
                                        except StopIteration:
                                            tg = None
                                            break
                                        done_t += 1
                            if tg is not None:
                                for _ in tg:
                                    pass
                dump("d_oTb", oT[1][:], BoT[1])
                if STOP_AFTER == "dsa":
                    return
                with Scope(P):
                    QTc = P.sb("QTc", [128, 4, T], BF16)
                    BQTc = bufs("QTc", NG)
                    KTc = P.sb("KTc", [128, 2, T], BF16)
                    BKTc = bufs("KTc", NG)
                    Vc = P.sb("Vc", [128, NT, 2, VP], BF16)
                    BVc = bufs("Vc", NT)
                    BVc1 = hd64_proj("c_", 1764, 2276, 2404, QTc, BQTc, KTc, BKTc, Vc, BVc, None)
                    if STOP_AFTER in ("swa_proj", "c_rope", "c_w"):
                        return
                    esink = P.sb("esink", [128, 8], F32)
                    Besink = NB("esink")
                    P.ld("sp", esink[:], dr["c_sinks"][l].partition_broadcast(128), [], [Besink])
                    P.act(esink[:], esink[:], AF.Exp, [Besink], [Besink])
                    smask = P.sb("smask", [128, 2, 2, 128], BF16)
                    Bsm = NB("smask")
                    for i in range(2):
                        P.cp("dve", smask[:, i, 0, :], swaprev_bf[:], [Bc], [Bsm])
                        P.cp("dve", smask[:, i, 1, :], diag_bf[:], [Bc], [Bsm])
                    ptc = [P.sb("c_pt%d" % i, [128, 2, 2, 128], BF16) for i in range(3)]
                    Bptc = bufs("c_pt", 3)
                    ptrot = Rot([0, 1, 2])
                    srot = Rot([0, 1, 2])
                    arot = Rot([3, 4])
                    irot = Rot([5, 6, 7])
                    otc = [P.sb("c_ot%d" % i, [128, 512], BF16) for i in range(2)]
                    Botc = bufs("c_ot", 2)
                    den = [P.sb("c_den%d" % i, [128, 2], F32) for i in range(2)]
                    Bden = bufs("c_den", 2)
                    drot = Rot([0, 1])
                    srot = Rot([0, 1, 2, 5])
                    irot = Rot([6, 7])
                    blocks = [(qt, c) for qt in range(NT) for c in range(4)]
                    info = {}

                    def qk_c(k):
                        qt, c = blocks[k]
                        g = c // 2
                        u0 = 0 if qt > 0 else 1
                        sbs = []
                        for i in range(2):
                            sb_ = srot.next()
                            sv = banks[sb_][:, 0:256].rearrange("p (u t) -> p u t", u=2)
                            for u in range(u0, 2):
                                st_ = qt - 1 + u
                                P.mm(sv[:, u, :], KTc[i * 64:(i + 1) * 64, g, tsl(st_)], QTc[i * 64:(i + 1) * 64, c, tsl(qt)], True, True,
                                     [BKTc[st_ // 4], BQTc[qt // 4]], [Bbank[sb_]])
                                P.mm(sv[:, u, :], (negprev_bf if u == 0 else negdiag_bf)[:], ident_bf[:], False, True, [Bc], [Bbank[sb_]], sgc=True)
                            sbs.append((sb_, sv))
                        info[k] = sbs

                    qk_c(0)
                    for k, (qt, c) in enumerate(blocks):
                        if k + 1 < len(blocks):
                            qk_c(k + 1)
                        g = c // 2
                        u0 = 0 if qt > 0 else 1
                        ok = qt % 2
                        pk = ptrot.next()
                        for i in range(2):
                            sb_, sv = info[k][i]
                            P.act(ptc[pk][:, i, u0:2, :], sv[:, u0:2, :], AF.Exp, [Bbank[sb_]], [Bptc[pk]], scale=0.125)
                        ab = arot.next()
                        accv = banks[ab][:, 0:2 * VP].rearrange("p (i d) -> p i d", d=VP)
                        for i in range(2):
                            for u in range(u0, 2):
                                st_ = qt - 1 + u
                                P.mm(accv[:, i, 0:65], ptc[pk][:, i, u, :], Vc[:, st_, g, 0:65], (u == u0 and i == 0), u == 1, [Bptc[pk], BVc[st_], BVc1], [Bbank[ab]], sgc=True)
                        dk = drot.next()
                        P.tt("dve", den[dk][:], accv[:, :, 64], esink[:, 2 * c:2 * c + 2], ALU.add, [Bbank[ab], Besink], [Bden[dk]])
                        P.recip(den[dk][:], den[dk][:], [Bden[dk]], [Bden[dk]])
                        for i in range(2):
                            h = 2 * c + i
                            P.ts("dve", otc[ok][:, h * 64:(h + 1) * 64], accv[:, i, 0:64], den[dk][:, i:i + 1], None, ALU.mult,
                                 [Bbank[ab], Bden[dk]], [Botc[ok]])
                        if c == 3:
                            bk = irot.next()
                            bv = banks[bk][:].bitcast(BF16)
                            for c2 in range(4):
                                P.tr(bv[:, c2 * 128:(c2 + 1) * 128], otc[ok][:, c2 * 128:(c2 + 1) * 128], ident_bf[:], [Botc[ok], Bc], [Bbank[bk]])
                            P.cp("act", oT[2][:, :, tsl(qt)], bv[:, 0:512].rearrange("p (c t) -> p c t", t=128), [Bbank[bk]], [BoT[2][qt // 4]])
                dump("d_oTc", oT[2][:], BoT[2])
                if STOP_AFTER == "swa":
                    return
                with Scope(P):
                    wout = P.sb("wout", [128, KC, D], BF16)
                    Bwout = NB("wout")
                    P.ld("pool", wout[:], dr["w_out"][l].rearrange("(kc p) c -> p kc c", p=128), [], [Bwout])
                    g1B = P.sb("g1B", [128, D], F32)
                    b1B = P.sb("b1B", [128, D], F32)
                    Bg1 = NB("g1b1")
                    P.ld("sp", g1B[:], dr["ln1_g"][l].partition_broadcast(128), [], [Bg1])
                    P.ld("sp", b1B[:], dr["ln1_b"][l].partition_broadcast(128), [], [Bg1])
                    wr = P.sb("wr", [128, KC, 16], F32)
                    Bwr = NB("wr")
                    P.ld("sp", wr[:], dr["w_router"].rearrange("(kc p) e -> p kc e", p=128), [], [Bwr])
                    wbr = [P.sb("wbr%d" % i, [128, 3, 4, 128], BF16) for i in range(2)]
                    wgt = [P.sb("wgt%d" % i, [128, 3, KC, 128], BF16) for i in range(2)]
                    Bwm = bufs("wm", 2)
                    mg = P.sb("mg", [128, KC, T], BF16)
                    Bmg = [bufs("mg%d_" % i, NG) for i in range(KC)]
                    sg = [P.sb("sg%d" % i, [128, 512], F32) for i in range(2)]
                    Bsg = bufs("sg", 2)
                    sgrot = Rot([0, 1])
                    macc = P.sb("macc", [128, 512], F32)
                    Bmacc = NB("macc")
                    pre = [P.sb("m_pre%d" % i, [128, D], F32) for i in range(4)]
                    Bpre = bufs("m_pre", 4)
                    st1 = P.sb("m_st", [128, 8, 4], F32)
                    Bsum1 = bufs("m_sum", 4)
                    Bsq1 = bufs("m_sq", 4)
                    Bvec1 = NB("m_vec")
                    junks = [P.sb("m_junk%d" % i, [128, D], BF16)[:] for i in range(2)]
                    Bjunks = bufs("m_junk", 2)
                    xbf = [P.sb("m_xbf%d" % i, [128, D], BF16)[:] for i in range(2)]
                    Bxbf = bufs("m_xbf", 2)
                    x1Tf = P.sb("x1Tf", [128, KC, 128], F32)
                    Bx1Tf = NB("x1Tf")
                    brot = Rot([0, 1, 2, 3, 4, 5, 6, 7])
                    wbr_src = [dr[n][l].rearrange("(kc p) c -> p kc c", p=128) for n in ("w_br_a", "w_br_b", "w_br_c")]
                    def ld_merge(dc):
                        k = dc % 2
                        for i in range(3):
                            P.ld("pool", wbr[k][:, i, :, :], wbr_src[i][:, :, dc * 128:(dc + 1) * 128], [], [Bwm[k]])
                            P.ld("pool", wgt[k][:, i, :, :], w_in_v[:, :, 2532 + i * 1024 + dc * 128: 2532 + i * 1024 + (dc + 1) * 128], [], [Bwm[k]])

                    ld_merge(0)
                    for dc in range(KC):
                        k = dc % 2
                        if dc + 1 < KC:
                            ld_merge(dc + 1)
                        for G in range(NG):
                            for i in range(3):
                                yb = brot.next()
                                for kc in range(4):
                                    P.mm(banks[yb][:], wbr[k][:, i, kc, :], oT[i][:, kc, gsl(G)], kc == 0, kc == 3, [Bwm[k], BoT[i][G]], [Bbank[yb]])
                                gb = brot.next()
                                for kc in range(KC):
                                    P.mm(banks[gb][:], wgt[k][:, i, kc, :], xT[:, kc, gsl(G)], kc == 0, kc == KC - 1,
                                         [Bwm[k]] + [BxT[4 * G + j] for j in range(4)], [Bbank[gb]])
                                sk = sgrot.next()
                                P.act(sg[sk][:], banks[gb][:], AF.Sigmoid, [Bbank[gb]], [Bsg[sk]])
                                if i == 0:
                                    P.tt("dve", macc[:], sg[sk][:], banks[yb][:], ALU.mult, [Bsg[sk], Bbank[yb]], [Bmacc])
                                elif i == 1:
                                    P.tt("dve", sg[sk][:], sg[sk][:], banks[yb][:], ALU.mult, [Bsg[sk], Bbank[yb]], [Bsg[sk]])
                                    P.tt("dve", macc[:], macc[:], sg[sk][:], ALU.add, [Bmacc, Bsg[sk]], [Bmacc])
                                else:
                                    P.tt("dve", sg[sk][:], sg[sk][:], banks[yb][:], ALU.mult, [Bsg[sk], Bbank[yb]], [Bsg[sk]])
                                    P.tt("dve", mg[:, dc, gsl(G)], macc[:], sg[sk][:], ALU.add, [Bmacc, Bsg[sk]], [Bmg[dc][G]])
                    for G in range(NG):
                        items = []
                        for j in range(4):
                            tt = 4 * G + j
                            P.ld("sp", pre[j][:], xres[tsl(tt), :], [Bxres[tt]], [Bpre[j]])
                            items.append((pre[j][:], Bpre[j]))
                        for j in range(4):
                            tt = 4 * G + j
                            for half in range(2):
                                ob = brot.next()
                                for kc in range(KC):
                                    P.mm(banks[ob][:], mg[:, kc, tsl(tt)], wout[:, kc, half * 512:(half + 1) * 512], kc == 0, kc == KC - 1,
                                         [Bmg[kc][G], Bwout], [Bbank[ob]])
                                hs = slice(half * 512, (half + 1) * 512)
                                P.stt("dve", pre[j][:, hs], pre[j][:, hs], ALPHA, banks[ob][:], ALU.mult, ALU.add, [Bpre[j], Bbank[ob]], [Bpre[j]])
                        ln_batch(items, g1B[:], b1B[:], Bg1, st1, Bsum1, Bsq1, Bvec1, junks, Bjunks)
                        for j in range(4):
                            tt = 4 * G + j
                            P.st("sp", xres[tsl(tt), :], pre[j][:], [Bpre[j]], [Bxres[tt]], key=Bpre[j])
                        to_xT_batch(items, [4 * G + j for j in range(4)], xbf, Bxbf, [brot.next() for _ in range(4)])
                        for j in range(4):
                            tt = 4 * G + j
                            tb0 = brot.next()
                            tb1 = brot.next()
                            for kc in range(KC):
                                tb = tb0 if kc < 4 else tb1
                                P.tr(banks[tb][:, (kc % 4) * 128:(kc % 4 + 1) * 128], pre[j][:, kc * 128:(kc + 1) * 128], ident_f[:], [Bpre[j], Bc], [Bbank[tb]])
                            P.cp("act", x1Tf[:, 0:4, :], banks[tb0][:].rearrange("p (k t) -> p k t", t=128), [Bbank[tb0]], [Bx1Tf])
                            P.cp("act", x1Tf[:, 4:8, :], banks[tb1][:].rearrange("p (k t) -> p k t", t=128), [Bbank[tb1]], [Bx1Tf])
                            lb = brot.next()
                            for kc in range(KC):
                                P.mm(banks[lb][:, 0:16], x1Tf[:, kc, :], wr[:, kc, :], kc == 0, kc == KC - 1, [Bx1Tf, Bwr], [Bbank[lb]])
                            P.cp("dve", logit[:, tt, :], banks[lb][:, 0:16], [Bbank[lb]], [Blogit[tt]])
            if STOP_AFTER == "ln1":
                return
            with Scope(P):
                gate = P.sb("gate", [128, NT, 16], F32)
                Bgate = NB("gate")
                with Scope(P):
                    sco = P.sb("r_sco", [128, NT, 16], F32)
                    bia = P.sb("r_bia", [128, NT, 16], F32)
                    mb = P.sb("r_mb", [128, NT, 16], F32)
                    rbias = P.sb("r_bias", [128, 16], F32)
                    ps6 = P.sb("r_ps6", [128, 6, NT, 4], F32)
                    gs = P.sb("r_gs", [128, NT, 4], F32)
                    gmax = P.sb("r_gmax", [128, NT], F32)
                    ing = P.sb("r_ing", [128, NT, 4], F32)
                    red = P.sb("r_red", [128, NT, 8], F32)
                    tmax = P.sb("r_tmax", [128, NT], F32)
                    e1 = P.sb("r_e1", [128, NT, 16], F32)
                    e2 = P.sb("r_e2", [128, NT, 16], F32)
                    Br = NB("routing")
                    R = [Br] + Blogit
                    P.ld("sp", rbias[:], dr["router_bias"].partition_broadcast(128), [], [Br])
                    P.act(sco[:], logit[:], AF.Sigmoid, R, [Br])
                    for tt in range(NT):
                        P.tt("dve", bia[:, tt, :], sco[:, tt, :], rbias[:], ALU.add, [Br], [Br])
                    bg = bia[:].rearrange("p t (g e) -> p t g e", e=4)
                    pairs = [(0, 1), (0, 2), (0, 3), (1, 2), (1, 3), (2, 3)]
                    for pi, (a, b_) in enumerate(pairs):
                        P.tt("dve", ps6[:, pi, :, :], bg[:, :, :, a], bg[:, :, :, b_], ALU.add, [Br], [Br])
                    P.tt("dve", gs[:], ps6[:, 0, :, :], ps6[:, 1, :, :], ALU.max, [Br], [Br])
                    for pi in range(2, 6):
                        P.tt("dve", gs[:], gs[:], ps6[:, pi, :, :], ALU.max, [Br], [Br])
                    P.tt("dve", gmax[:], gs[:, :, 0], gs[:, :, 1], ALU.max, [Br], [Br])
                    P.tt("dve", gmax[:], gmax[:], gs[:, :, 2], ALU.max, [Br], [Br])
                    P.tt("dve", gmax[:], gmax[:], gs[:, :, 3], ALU.max, [Br], [Br])
                    for g in range(4):
                        P.tt("dve", ing[:, :, g], gs[:, :, g], gmax[:], ALU.is_equal, [Br], [Br])
                    P.ts("dve", ing[:], ing[:], -1.0, 1e30, ALU.add, [Br], [Br], op1=ALU.mult)
                    mbg = mb[:].rearrange("p t (g e) -> p t g e", e=4)
                    for e_ in range(4):
                        P.tt("dve", mbg[:, :, :, e_], bg[:, :, :, e_], ing[:], ALU.add, [Br], [Br])

                    def max16(dst, src):
                        P.tt("dve", red[:, :, 0:8], src[:, :, 0:8], src[:, :, 8:16], ALU.max, [Br], [Br])
                        P.tt("dve", red[:, :, 0:4], red[:, :, 0:4], red[:, :, 4:8], ALU.max, [Br], [Br])
                        P.tt("dve", red[:, :, 0:2], red[:, :, 0:2], red[:, :, 2:4], ALU.max, [Br], [Br])
                        P.tt("dve", dst, red[:, :, 0], red[:, :, 1], ALU.max, [Br], [Br])

                    max16(tmax[:], mb)
                    for tt in range(NT):
                        P.ts("dve", e1[:, tt, :], mb[:, tt, :], tmax[:, tt:tt + 1], None, ALU.is_equal, [Br], [Br])
                    P.stt("dve", mb[:], e1[:], -1e30, mb[:], ALU.mult, ALU.add, [Br], [Br])
                    max16(tmax[:], mb)
                    for tt in range(NT):
                        P.ts("dve", e2[:, tt, :], mb[:, tt, :], tmax[:, tt:tt + 1], None, ALU.is_equal, [Br], [Br])
                    P.tt("dve", e1[:], e1[:], e2[:], ALU.add, [Br], [Br])
                    P.tt("dve", e1[:], e1[:], sco[:], ALU.mult, [Br], [Br])
                    P.tt("dve", red[:, :, 0:8], e1[:, :, 0:8], e1[:, :, 8:16], ALU.add, [Br], [Br])
                    P.tt("dve", red[:, :, 0:4], red[:, :, 0:4], red[:, :, 4:8], ALU.add, [Br], [Br])
                    P.tt("dve", red[:, :, 0:2], red[:, :, 0:2], red[:, :, 2:4], ALU.add, [Br], [Br])
                    P.tt("dve", tmax[:], red[:, :, 0], red[:, :, 1], ALU.add, [Br], [Br])
                    P.recip(tmax[:], tmax[:], [Br], [Br])
                    for tt in range(NT):
                        P.ts("dve", gate[:, tt, :], e1[:, tt, :], tmax[:, tt:tt + 1], None, ALU.mult, [Br], [Bgate])
                dump("d_gate", gate[:], [Bgate])
                if STOP_AFTER == "route":
                    return
                acc = P.sb("acc", [128, NT, D], F32)
                Bacc = bufs("acc", NT)
                with Scope(P):
                    Wg = [P.sb("Wg%d" % i, [128, KC, 512], BF16) for i in range(2)]
                    Wu = [P.sb("Wu%d" % i, [128, KC, 512], BF16) for i in range(2)]
                    Wd = [P.sb("Wd%d" % i, [128, 4, D], BF16) for i in range(2)]
                    BWe = bufs("We", 2)
                    actT = [P.sb("actT%d" % i, [128, 4, 512], BF16) for i in range(2)]
                    BactT = [bufs("actT%d_" % i, 4) for i in range(2)]
                    sgm = [P.sb("sgm%d" % i, [128, 512], BF16) for i in range(2)]
                    Bsgm = bufs("sgm", 2)
                    sgrot = Rot([0, 1])
                    hrot = Rot([0, 1, 2, 3])
                    orot = Rot([4, 5, 6, 7])
                    ai = 0
                    for e_ in range(16):
                        k = e_ % 2
                        P.ld("pool", Wg[k][:], dr["w_exp_gate"][l, e_].rearrange("(kc p) f -> p kc f", p=128), [], [BWe[k]])
                        P.ld("pool", Wu[k][:], dr["w_exp_up"][l, e_].rearrange("(kc p) f -> p kc f", p=128), [], [BWe[k]])
                        P.ld("pool", Wd[k][:], dr["w_exp_down"][l, e_].rearrange("(kc p) f -> p kc f", p=128), [], [BWe[k]])
                        for G in range(NG):
                            a = ai % 2
                            ai += 1
                            xr_ = [BxT[4 * G + j] for j in range(4)]
                            for fc in range(4):
                                hb = hrot.next()
                                for kc in range(KC):
                                    P.mm(banks[hb][:], Wg[k][:, kc, fc * 128:(fc + 1) * 128], xT[:, kc, gsl(G)], kc == 0, kc == KC - 1, [BWe[k]] + xr_, [Bbank[hb]])
                                ub = hrot.next()
                                for kc in range(KC):
                                    P.mm(banks[ub][:], Wu[k][:, kc, fc * 128:(fc + 1) * 128], xT[:, kc, gsl(G)], kc == 0, kc == KC - 1, [BWe[k]] + xr_, [Bbank[ub]])
                                sk = sgrot.next()
                                P.act(sgm[sk][:], banks[hb][:], AF.Silu, [Bbank[hb]], [Bsgm[sk]])
                                P.tt("dve", actT[a][:, fc, :], sgm[sk][:], banks[ub][:], ALU.mult, [Bsgm[sk], Bbank[ub]], [BactT[a][fc]])
                            for j in range(4):
                                tt = 4 * G + j
                                for half in range(2):
                                    ob = orot.next()
                                    for fc in range(4):
                                        P.mm(banks[ob][:], actT[a][:, fc, j * 128:(j + 1) * 128], Wd[k][:, fc, half * 512:(half + 1) * 512], fc == 0, fc == 3,
                                             [BactT[a][fc], BWe[k]], [Bbank[ob]])
                                    dst = acc[:, tt, half * 512:(half + 1) * 512]
                                    if e_ == 0:
                                        P.ts("dve", dst, banks[ob][:], gate[:, tt, e_:e_ + 1], None, ALU.mult, [Bbank[ob], Bgate], [Bacc[tt]])
                                    else:
                                        P.stt("dve", dst, banks[ob][:], gate[:, tt, e_:e_ + 1], dst, ALU.mult, ALU.add, [Bbank[ob], Bgate, Bacc[tt]], [Bacc[tt]])
                with Scope(P):
                    g2B = P.sb("g2B", [128, D], F32)
                    b2B = P.sb("b2B", [128, D], F32)
                    Bg2 = NB("g2b2")
                    P.ld("sp", g2B[:], dr["ln2_g"][l].partition_broadcast(128), [], [Bg2])
                    P.ld("sp", b2B[:], dr["ln2_b"][l].partition_broadcast(128), [], [Bg2])
                    xr = [P.sb("f_xr%d" % i, [128, D], F32) for i in range(4)]
                    Bxr = bufs("f_xr", 4)
                    st2 = P.sb("f_st", [128, 8, 4], F32)
                    Bsum2 = bufs("f_sum", 4)
                    Bsq2 = bufs("f_sq", 4)
                    Bvec2 = NB("f_vec")
                    junks = [P.sb("f_junk%d" % i, [128, D], BF16)[:] for i in range(2)]
                    Bjunks = bufs("f_junk", 2)
                    xbf = [P.sb("f_xbf%d" % i, [128, D], BF16)[:] for i in range(2)]
                    Bxbf = bufs("f_xbf", 2)
                    Bstk = bufs("f_stkey", 4)
                    for t0 in range(0, NT, 4):
                        items = []
                        for i in range(4):
                            tt = t0 + i
                            P.ld("sp", xr[i][:], xres[tsl(tt), :], [Bxres[tt]], [Bxr[i]])
                        for i in range(4):
                            tt = t0 + i
                            P.stt("dve", acc[:, tt, :], xr[i][:], ALPHA, acc[:, tt, :], ALU.mult, ALU.add, [Bxr[i], Bacc[tt]], [Bacc[tt]])
                            items.append((acc[:, tt, :], Bacc[tt]))
                        ln_batch(items, g2B[:], b2B[:], Bg2, st2, Bsum2, Bsq2, Bvec2, junks, Bjunks)
                        for i in range(4):
                            tt = t0 + i
                            kb = Bstk[i]
                            if last:
                                final_events.append(P.st("sp", y[tsl(tt), :], acc[:, tt, :], [Bacc[tt]], [kb], key=kb))
                            else:
                                P.st("sp", xres[tsl(tt), :], acc[:, tt, :], [Bacc[tt]], [Bxres[tt], kb], key=kb)
                        if not last:
                            to_xT_batch(items, [t0 + i for i in range(4)], xbf, Bxbf, [0, 1, 2, 3])

        for li, l in enumerate(layers):
            layer(l, li == len(layers) - 1)

        dump("d_xT", xT[:], BxT)
        P.finish(final_events)
        print("build: sems", P.nsem, "ops", {e: len(P.ops[e]) for e in P.ENG})
    return nc


STOP_AFTER = None
EXPER = None
SKIP_DSA = False
SKIP_MLA = False
_NB = {}


def NB(name):
    if name not in _NB:
        _NB[name] = Buf(name)
    return _NB[name]


def prep_inputs(inputs):
    f32 = np.float32
    common = {}
    for k in WEIGHT_SHAPES:
        a = np.ascontiguousarray(np.asarray(inputs[k], dtype=f32))
        if k == "a_q_ln_g":
            a = np.ascontiguousarray(a.reshape(2, 3, 128).transpose(0, 2, 1))
        if k == "a_kv_ln_g":
            a = np.ascontiguousarray(a.reshape(2, 2, 128).transpose(0, 2, 1))
        common[k] = a
    common.update(make_consts())
    return common


_PROG_CACHE = {}


def get_prog(key, *a, **kw):
    if key not in _PROG_CACHE:
        _NB.clear()
        _PROG_CACHE[key] = build_program(*a, **kw)
    return _PROG_CACHE[key]


FUSED = True


def kernel(**inputs):
    x = np.ascontiguousarray(np.asarray(inputs["x"], dtype=np.float32))
    B = x.shape[0]
    common = prep_inputs(inputs)
    cores = list(range(B))
    if FUSED:
        nc = get_prog("fused", [0, 1], True, True)
        maps = [dict(common, x=x[b]) for b in cores]
        res = run_bass_kernel_spmd(nc, maps, core_ids=cores)
        return np.stack([res.results[b]["y"] for b in cores], axis=0).astype(np.float32)
    nc0 = get_prog("l0", [0], True, True)
    maps = [dict(common, x=x[b]) for b in cores]
    res = run_bass_kernel_spmd(nc0, maps, core_ids=cores)
    mid = [res.results[b]["y"] for b in cores]
    nc1 = get_prog("l1", [1], False, True)
    maps = [dict(common, x=np.ascontiguousarray(mid[b])) for b in cores]
    res = run_bass_kernel_spmd(nc1, maps, core_ids=cores)
    return np.stack([res.results[b]["y"] for b in cores], axis=0).astype(np.float32)
```

```python
import contextlib
import numpy as np
import concourse.bass as bass
import concourse.mybir as mybir
from concourse.bass_utils import run_bass_kernel_spmd

F32 = mybir.dt.float32
BF16 = mybir.dt.bfloat16
AF = mybir.ActivationFunctionType
ALU = mybir.AluOpType
AX = mybir.AxisListType


GUARD = True


class Buf:
    __slots__ = ("name", "w", "r", "sem_in", "cnt_in", "sem_out", "cnt_out", "psum")

    def __init__(self, name):
        self.name = name
        self.w = None
        self.r = []
        self.sem_in = None
        self.cnt_in = 0
        self.sem_out = None
        self.cnt_out = 0
        self.psum = False


class Prog:
    ENG = ("pe", "act", "dve", "pool", "sp")

    def __init__(self, nc, es):
        self.nc = nc
        self.es = es
        self.root = es
        self.eng = {"pe": nc.tensor, "act": nc.scalar, "dve": nc.vector,
                    "pool": nc.gpsimd, "sp": nc.sync}
        self.ops = {e: [] for e in self.ENG}
        self.idx = {e: 0 for e in self.ENG}
        self.sem = {e: es.enter_context(nc.semaphore("s_" + e)) for e in self.ENG if e != "sp"}
        self.waited = {e: {} for e in self.ENG}
        self.nsem = 0
        self.uid = 0
        self.last_out_events = []
        self.pending = {e: {} for e in self.ENG}
        self.dma_since = {}
        self.guard = {}
        self.guard_hist = {"act": [], "dve": []}
        self.guard_src = None

    def new_sem(self, name):
        self.nsem += 1
        return self.root.enter_context(self.nc.semaphore("%s_%d" % (name, self.nsem)))

    def sb(self, name, shape, dt):
        self.uid += 1
        return self.es.enter_context(self.nc.sbuf_tensor("%s_%d" % (name, self.uid), list(shape), dt))

    def ps(self, name, shape, dt):
        self.uid += 1
        return self.es.enter_context(self.nc.psum_tensor("%s_%d" % (name, self.uid), list(shape), dt))

    def _collect(self, e, reads, writes, skip_sem=None):
        waits = {}

        def need(ev, raw, waw=False):
            if ev is None:
                return
            sem, val, ee, ii = ev
            if waw and skip_sem is not None and sem is skip_sem:
                return
            if ee == e and ii is not None:
                if e == "pe":
                    return
            k = id(sem)
            if self.waited[e].get(k, 0) >= val:
                return
            if k not in waits or waits[k][1] < val:
                waits[k] = (sem, val)

        for b in reads:
            need(b.w, True)
            if b.psum and e in ("act", "dve"):
                for r in b.r:
                    if r[2] != e:
                        need(r, True)
        for b in writes:
            need(b.w, True, True)
            for r in b.r:
                if GUARD and b.psum and e == "pe" and r[3] is not None and r[2] in ("act", "dve"):
                    r = self._guarded(r)
                need(r, False)
        if self.pending[e]:
            for k, (sem, val) in self.pending[e].items():
                if self.waited[e].get(k, 0) >= val:
                    continue
                if k not in waits or waits[k][1] < val:
                    waits[k] = (sem, val)
            self.pending[e] = {}
        for k, (sem, val) in waits.items():
            self.waited[e][k] = val
        return list(waits.values())

    def _commit(self, ev, reads, writes):
        for b in reads:
            b.r.append(ev)
        for b in writes:
            b.w = ev
            b.r = []

    def _guarded(self, r):
        sem, val, E, ii = r
        if self.idx[E] > ii + 1:
            return (sem, ii + 2, E, ii + 1)
        hist = self.guard_hist[E]
        n = len(hist)
        g = self.guard[E][:, (n % 8):(n % 8) + 1]
        gw = []
        if n >= 8:
            k = id(self.sem[E])
            need = hist[n - 8] + 1
            if self.waited[E].get(k, 0) < need:
                gw.append((self.sem[E], need))
                self.waited[E][k] = need
        if E == "act":
            src, bsrc = self.guard_src
            sv = bsrc.w
            if sv is not None and self.waited[E].get(id(sv[0]), 0) < sv[1]:
                gw.append((sv[0], sv[1]))
                self.waited[E][id(sv[0])] = sv[1]
        i2 = self.idx[E]
        self.idx[E] = i2 + 1
        hist.append(i2)
        if E == "act":
            self.ops[E].append((gw, (lambda en, g=g, src=src: en.activation(out=g, in_=src, func=AF.Copy)), (self.sem[E], 1)))
        else:
            self.ops[E].append((gw, (lambda en, g=g: en.memset(g, 0.0)), (self.sem[E], 1)))
        return (self.sem[E], i2 + 1, E, i2)

    def op(self, e, fn, reads=(), writes=()):
        waits = self._collect(e, reads, writes)
        i = self.idx[e]
        self.idx[e] = i + 1
        ev = (self.sem[e], i + 1, e, i)
        self.ops[e].append((waits, fn, (self.sem[e], 1)))
        self._commit(ev, reads, writes)
        return ev

    def dma(self, e, fn, reads=(), writes=(), key=None):
        if key is None:
            key = writes[0] if writes else reads[0]
        if key.sem_in is None:
            key.sem_in = {}
        if e not in key.sem_in:
            key.sem_in[e] = [self.new_sem("d%s_%s" % (e, key.name)), 0]
        ent = key.sem_in[e]
        sem = ent[0]
        waits = self._collect(e, reads, writes, skip_sem=sem)
        ent[1] += 16
        val = ent[1]
        ev = (sem, val, e, None)
        self.dma_since[id(sem)] = (sem, val)
        self.ops[e].append((waits, fn, (sem, 16)))
        self._commit(ev, reads, writes)
        return ev

    def mm(self, out, lhsT, rhs, start, stop, reads, writes, sgc=False):
        if sgc:
            return self.op("pe", lambda e: e.matmul(out, lhsT=lhsT, rhs=rhs, start=start, stop=stop, skip_group_check=True), reads, writes)
        return self.op("pe", lambda e: e.matmul(out, lhsT=lhsT, rhs=rhs, start=start, stop=stop), reads, writes)

    def tr(self, out, in_, ident, reads, writes):
        return self.op("pe", lambda e: e.transpose(out, in_, ident), reads, writes)

    def act(self, out, in_, func, reads, writes, bias=None, scale=None, accum=None):
        kw = {}
        if bias is not None:
            kw["bias"] = bias
        if scale is not None:
            kw["scale"] = scale
        if accum is not None:
            kw["accum_out"] = accum
        return self.op("act", lambda e: e.activation(out=out, in_=in_, func=func, **kw), reads, writes)

    def ts(self, eng, out, in0, s1, s2, op0, reads, writes, op1=None, accum=None):
        kw = {}
        if op1 is not None:
            kw["op1"] = op1
        if accum is not None:
            kw["accum_out"] = accum
        return self.op(eng, lambda e: e.tensor_scalar(out=out, in0=in0, scalar1=s1, scalar2=s2, op0=op0, **kw), reads, writes)

    def tt(self, eng, out, in0, in1, op, reads, writes):
        return self.op(eng, lambda e: e.tensor_tensor(out=out, in0=in0, in1=in1, op=op), reads, writes)

    def stt(self, eng, out, in0, scalar, in1, op0, op1, reads, writes):
        return self.op(eng, lambda e: e.scalar_tensor_tensor(out=out, in0=in0, scalar=scalar, in1=in1, op0=op0, op1=op1), reads, writes)

    def cp(self, eng, out, in_, reads, writes):
        if eng == "act":
            return self.op("act", lambda e: e.activation(out=out, in_=in_, func=AF.Copy), reads, writes)
        return self.op(eng, lambda e: e.tensor_copy(out=out, in_=in_), reads, writes)

    def memset(self, eng, ap, val, writes):
        return self.op(eng, lambda e: e.memset(ap, val), (), writes)

    def recip(self, out, in_, reads, writes):
        return self.op("dve", lambda e: e.reciprocal(out=out, in_=in_), reads, writes)

    def ld(self, q, out, in_, reads, writes):
        return self.dma(q, lambda e: e.dma_start(out=out, in_=in_), reads, writes)

    def st(self, q, out, in_, reads, writes, key):
        return self.dma(q, lambda e: e.dma_start(out=out, in_=in_), reads, writes, key=key)

    def fence(self):
        for e in self.ENG:
            pend = self.pending[e]
            for f in self.ENG:
                if f == "sp" or f == e or self.idx[f] == 0:
                    continue
                k = id(self.sem[f])
                pend[k] = (self.sem[f], self.idx[f])
            for k, sv in self.dma_since.items():
                if k not in pend or pend[k][1] < sv[1]:
                    pend[k] = sv
        self.dma_since = {}

    def finish(self, final_events):
        nc = self.nc
        ops = self.ops
        with nc.Block() as block:
            def emit(engname, eng):
                for waits, fn, (sem, inc) in ops[engname]:
                    for (s, v) in waits:
                        eng.wait_ge(s, v)
                    ins = fn(eng)
                    ins.then_inc(sem, inc)

            @block.tensor
            def _(eng):
                emit("pe", eng)

            @block.scalar
            def _(eng):
                emit("act", eng)

            @block.vector
            def _(eng):
                emit("dve", eng)

            @block.gpsimd
            def _(eng):
                emit("pool", eng)

            @block.sync
            def _(eng):
                emit("sp", eng)
                for (s, v, _e, _i) in final_events:
                    eng.wait_ge(s, v)

T = 2048
D = 1024
NT = 16
NG = 4
KC = 8
LN_EPS = 1e-5
RMS_EPS = 1e-6
DEPTH = 2
ALPHA = (2.0 * DEPTH) ** 0.25
IN_COLS = 5604
VP = 68
NIT = 18
W0 = 64.0


def make_consts():
    f32 = np.float32
    c = {}
    c["c_ident"] = np.eye(128, dtype=f32)
    c["c_ones"] = np.ones((128, 128), f32)
    t = np.arange(T, dtype=f32)
    p = np.arange(128)
    i64 = ((p % 64) % 32).astype(f32)
    inv64 = np.power(f32(10000.0), -(i64 / f32(32.0))).astype(f32)
    ang = (t[None, :] * inv64[:, None]).astype(f32).astype(np.float64)
    c["c_cos64"] = np.cos(ang).astype(f32)
    c["c_sin64"] = np.sin(ang).astype(f32)
    iA = ((p - 64) % 16).astype(f32)
    invA = np.power(f32(10000.0), -(iA / f32(16.0))).astype(f32)
    angA = (t[None, :] * invA[:, None]).astype(f32).astype(np.float64)
    c["c_cosA"] = np.cos(angA).astype(f32)
    c["c_sinA"] = np.sin(angA).astype(f32)
    pm = np.zeros((128, 128), f32)
    for fp in range(128):
        d = fp % 64
        if d < 32:
            pm[fp + 32, fp] = -1.0
        else:
            pm[fp - 32, fp] = 1.0
    c["c_pm64"] = pm
    pa = np.zeros((128, 128), f32)
    for fp in range(64, 96):
        d = fp - 64
        if d < 16:
            pa[fp + 16, fp] = -1.0
        else:
            pa[fp - 16, fp] = 1.0
    c["c_pmA"] = pa
    s = np.arange(128)[:, None]
    q = np.arange(128)[None, :]
    c["c_diag"] = ((s < 64) | (q >= 64)).astype(f32)
    c["c_swaprev"] = (~((s < 64) & (q >= 64))).astype(f32)
    tt_ = np.arange(128)[:, None]
    ss_ = np.arange(128)[None, :]
    c["c_negdiag"] = np.where((tt_ < 64) & (ss_ >= 64), f32(-1e30), f32(0.0)).astype(f32)
    c["c_negprev"] = np.where((tt_ >= 64) & (ss_ < 64), f32(-1e30), f32(0.0)).astype(f32)
    return c


CONST_SHAPES = {"c_ident": [128, 128], "c_ones": [128, 128], "c_cos64": [128, T], "c_sin64": [128, T],
                "c_cosA": [128, T], "c_sinA": [128, T], "c_pm64": [128, 128], "c_pmA": [128, 128],
                "c_diag": [128, 128], "c_swaprev": [128, 128], "c_negdiag": [128, 128], "c_negprev": [128, 128]}

WEIGHT_SHAPES = {
    "ln_in_g": [D], "ln_in_b": [D], "w_in": [2, D, IN_COLS],
    "a_q_ln_g": [2, 128, 3], "a_kv_ln_g": [2, 128, 2],
    "a_w_uq": [2, 384, 768], "a_w_ukv": [2, 256, 1024], "c_sinks": [2, 8],
    "w_br_a": [2, 512, D], "w_br_b": [2, 512, D], "w_br_c": [2, 512, D], "w_out": [2, D, D],
    "ln1_g": [2, D], "ln1_b": [2, D], "w_router": [D, 16], "router_bias": [16],
    "w_exp_gate": [2, 16, D, 512], "w_exp_up": [2, 16, D, 512], "w_exp_down": [2, 16, 512, D],
    "ln2_g": [2, D], "ln2_b": [2, D],
}


class Scope:
    def __init__(self, P):
        self.P = P

    def __enter__(self):
        self.prev = self.P.es
        self.stack = contextlib.ExitStack()
        self.stack.__enter__()
        self.P.es = self.stack
        return self

    def __exit__(self, *a):
        self.P.es = self.prev
        self.P.fence()
        return self.stack.__exit__(*a)


def bufs(name, n):
    return [NB("%s%d" % (name, i)) for i in range(n)]


def build_program(layers, do_ln_in, final, dbg_names=()):
    nc = bass.Bass("TRN2", target_bir_lowering=False)
    dr = {}
    dr["x"] = nc.dram_tensor("x", [T, D], F32, kind="ExternalInput").ap()
    for k, shp in WEIGHT_SHAPES.items():
        dr[k] = nc.dram_tensor(k, shp, F32, kind="ExternalInput").ap()
    for k, shp in CONST_SHAPES.items():
        dr[k] = nc.dram_tensor(k, shp, F32, kind="ExternalInput").ap()
    y = nc.dram_tensor("y", [T, D], F32, kind="ExternalOutput").ap()
    xres = nc.dram_tensor("xres", [T, D], F32).ap()
    dbg = {}
    DBG_SHAPES = {"d_xT": ([128, KC, T], BF16), "d_oTa": ([128, 4, T], BF16), "d_oTb": ([128, 4, T], BF16),
                  "d_oTc": ([128, 4, T], BF16), "d_gate": ([128, NT, 16], F32)}
    for k in dbg_names:
        shp, dt_ = DBG_SHAPES[k]
        dbg[k] = nc.dram_tensor(k, shp, dt_, kind="ExternalOutput").ap()

    es = contextlib.ExitStack()
    with es:
        P = Prog(nc, es)
        final_events = []
        banks = [P.ps("bank%d" % i, [128, 512], F32) for i in range(8)]
        Bbank = bufs("bank", 8)
        for b_ in Bbank:
            b_.psum = True
        P.guard["act"] = P.sb("guard_act", [128, 8], F32)[:]
        P.guard["dve"] = P.sb("guard_dve", [128, 8], F32)[:]
        xT = P.sb("xT", [128, KC, T], BF16)
        BxT = bufs("xT", NT)
        Bxres = bufs("xres", NT)
        Bc = NB("consts")
        ident_bf = P.sb("ident_bf", [128, 128], BF16)
        ident_f = P.sb("ident_f", [128, 128], F32)
        ones_bf = P.sb("ones_bf", [128, 128], BF16)
        pm64 = P.sb("pm64", [128, 128], BF16)
        pmA = P.sb("pmA", [128, 128], BF16)
        diag_bf = P.sb("diag_bf", [128, 128], BF16)
        swaprev_bf = P.sb("swaprev_bf", [128, 128], BF16)
        negdiag = P.sb("negdiag", [128, 128], F32)
        negdiag_bf = P.sb("negdiag_bf", [128, 128], BF16)
        negprev_bf = P.sb("negprev_bf", [128, 128], BF16)
        P.ld("pool", ident_bf[:], dr["c_ident"], [], [Bc])
        P.ld("pool", ones_bf[:], dr["c_ones"], [], [Bc])
        P.ld("pool", pm64[:], dr["c_pm64"], [], [Bc])
        P.ld("pool", pmA[:], dr["c_pmA"], [], [Bc])
        P.ld("pool", diag_bf[:], dr["c_diag"], [], [Bc])
        P.ld("pool", swaprev_bf[:], dr["c_swaprev"], [], [Bc])
        P.ld("pool", negdiag_bf[:], dr["c_negdiag"], [], [Bc])
        P.ld("pool", negprev_bf[:], dr["c_negprev"], [], [Bc])
        P.ld("sp", ident_f[:], dr["c_ident"], [], [Bc])
        P.ld("sp", negdiag[:], dr["c_negdiag"], [], [Bc])
        P.guard_src = (ident_f[:, 0:1], Bc)

        def gsl(G):
            return slice(G * 512, (G + 1) * 512)

        def tsl(tt):
            return slice(tt * 128, (tt + 1) * 128)

        class Rot:
            def __init__(self, items):
                self.items = items
                self.i = 0

            def next(self):
                it = self.items[self.i % len(self.items)]
                self.i += 1
                return it

        def dump(name, ap_sb, rbufs):
            if name in dbg:
                final_events.append(P.st("sp", dbg[name], ap_sb, rbufs, [], key=NB("dbg_" + name)))

        def ln_inplace(t_ap, Bt, gB, bB, Bgb, st, Bst, junk, Bjunk):
            P.act(junk, t_ap, AF.Copy, [Bt], [Bjunk, Bst], accum=st[:, 0:1])
            P.act(junk, t_ap, AF.Square, [Bt], [Bjunk, Bst], accum=st[:, 1:2])
            P.ts("dve", st[:, 2:3], st[:, 0:1], 1.0 / D, None, ALU.mult, [Bst], [Bst])
            P.tt("dve", st[:, 3:4], st[:, 2:3], st[:, 2:3], ALU.mult, [Bst], [Bst])
            P.stt("dve", st[:, 4:5], st[:, 1:2], 1.0 / D, st[:, 3:4], ALU.mult, ALU.subtract, [Bst], [Bst])
            P.ts("dve", st[:, 4:5], st[:, 4:5], LN_EPS, None, ALU.add, [Bst], [Bst])
            P.act(st[:, 5:6], st[:, 4:5], AF.Sqrt, [Bst], [Bst])
            P.recip(st[:, 6:7], st[:, 5:6], [Bst], [Bst])
            P.stt("dve", st[:, 7:8], st[:, 2:3], -1.0, st[:, 6:7], ALU.mult, ALU.mult, [Bst], [Bst])
            P.act(t_ap, t_ap, AF.Identity, [Bt, Bst], [Bt], scale=st[:, 6:7], bias=st[:, 7:8])
            P.tt("dve", t_ap, t_ap, gB, ALU.mult, [Bt, Bgb], [Bt])
            P.tt("dve", t_ap, t_ap, bB, ALU.add, [Bt, Bgb], [Bt])

        def to_xT(t_ap, Bt, tt, xbf, Bxbf, bk):
            P.cp("act", xbf, t_ap, [Bt], [Bxbf])
            bv = banks[bk][:].bitcast(BF16)
            for kc in range(KC):
                P.tr(bv[:, kc * 128:(kc + 1) * 128], xbf[:, kc * 128:(kc + 1) * 128], ident_bf[:],
                     [Bxbf, Bc], [Bbank[bk]])
            P.cp("dve", xT[:, :, tsl(tt)], bv.rearrange("p (k t) -> p k t", t=128), [Bbank[bk]], [BxT[tt]])

        def ln_batch(items, gB, bB, Bgb, st, Bsum, Bsq, Bvec, junks, Bjunks):
            nb = len(items)
            for i, (t_ap, Bt) in enumerate(items):
                P.act(junks[i % 2], t_ap, AF.Copy, [Bt], [Bjunks[i % 2], Bsum[i]], accum=st[:, 0, i:i + 1])
            for i, (t_ap, Bt) in enumerate(items):
                P.act(junks[i % 2], t_ap, AF.Square, [Bt], [Bjunks[i % 2], Bsq[i]], accum=st[:, 1, i:i + 1])
            V = [Bvec]
            P.ts("dve", st[:, 2, 0:nb], st[:, 0, 0:nb], 1.0 / D, None, ALU.mult, Bsum[0:nb], V)
            P.tt("dve", st[:, 3, 0:nb], st[:, 2, 0:nb], st[:, 2, 0:nb], ALU.mult, V, V)
            P.stt("dve", st[:, 4, 0:nb], st[:, 1, 0:nb], 1.0 / D, st[:, 3, 0:nb], ALU.mult, ALU.subtract, Bsq[0:nb] + V, V)
            P.ts("dve", st[:, 4, 0:nb], st[:, 4, 0:nb], LN_EPS, None, ALU.add, V, V)
            P.act(st[:, 5, 0:nb], st[:, 4, 0:nb], AF.Sqrt, V, V)
            P.recip(st[:, 6, 0:nb], st[:, 5, 0:nb], V, V)
            P.stt("dve", st[:, 7, 0:nb], st[:, 2, 0:nb], -1.0, st[:, 6, 0:nb], ALU.mult, ALU.mult, V, V)
            for i, (t_ap, Bt) in enumerate(items):
                P.act(t_ap, t_ap, AF.Identity, [Bt, Bvec], [Bt], scale=st[:, 6, i:i + 1], bias=st[:, 7, i:i + 1])
            for i, (t_ap, Bt) in enumerate(items):
                P.tt("dve", t_ap, t_ap, gB, ALU.mult, [Bt, Bgb], [Bt])
            for i, (t_ap, Bt) in enumerate(items):
                P.tt("dve", t_ap, t_ap, bB, ALU.add, [Bt, Bgb], [Bt])

        def to_xT_batch(items, tts, xbfs, Bxbfs, bks):
            for i, (t_ap, Bt) in enumerate(items):
                P.cp("act", xbfs[i % len(xbfs)], t_ap, [Bt], [Bxbfs[i % len(xbfs)]])
                bk = bks[i % len(bks)]
                bv = banks[bk][:].bitcast(BF16)
                xb = xbfs[i % len(xbfs)]
                for kc in range(KC):
                    P.tr(bv[:, kc * 128:(kc + 1) * 128], xb[:, kc * 128:(kc + 1) * 128], ident_bf[:],
                         [Bxbfs[i % len(xbfs)], Bc], [Bbank[bk]])
                P.cp("dve", xT[:, :, tsl(tts[i])], bv.rearrange("p (k t) -> p k t", t=128), [Bbank[bk]], [BxT[tts[i]]])

        with Scope(P):
            gB = P.sb("ln0_g", [128, D], F32)
            bB = P.sb("ln0_b", [128, D], F32)
            Bgb = NB("ln0gb")
            if do_ln_in:
                P.ld("sp", gB[:], dr["ln_in_g"].partition_broadcast(128), [], [Bgb])
                P.ld("sp", bB[:], dr["ln_in_b"].partition_broadcast(128), [], [Bgb])
            xt = [P.sb("in_x%d" % i, [128, D], F32) for i in range(4)]
            Bxt = bufs("in_x", 4)
            stt_ = P.sb("in_st", [128, 8, 4], F32)
            Bsum = bufs("in_sum", 4)
            Bsq = bufs("in_sq", 4)
            Bvec = NB("in_vec")
            junks = [P.sb("in_junk%d" % i, [128, D], BF16)[:] for i in range(2)]
            Bjunks = bufs("in_junk", 2)
            xbf = [P.sb("in_xbf%d" % i, [128, D], BF16)[:] for i in range(2)]
            Bxbf = bufs("in_xbf", 2)
            for t0 in range(0, NT, 4):
                items = []
                for i in range(4):
                    P.ld("sp", xt[i][:], dr["x"][tsl(t0 + i), :], [], [Bxt[i]])
                    items.append((xt[i][:], Bxt[i]))
                if do_ln_in:
                    ln_batch(items, gB[:], bB[:], Bgb, stt_, Bsum, Bsq, Bvec, junks, Bjunks)
                for i in range(4):
                    P.st("sp", xres[tsl(t0 + i), :], xt[i][:], [Bxt[i]], [Bxres[t0 + i]], key=Bxt[i])
                to_xT_batch(items, [t0 + i for i in range(4)], xbf, Bxbf, [0, 1, 2, 3])

        def load_w(q, dst_ap, src_ap, Bw):
            P.ld(q, dst_ap, src_ap, [], [Bw])

        def proj_fm(lhsT_of_kc, M, G, bk, wr):
            for kc in range(KC):
                P.mm(banks[bk][0:M, :], lhsT_of_kc(kc), xT[:, kc, gsl(G)], kc == 0, kc == KC - 1,
                     [BxT[4 * G + j] for j in range(4)] + wr, [Bbank[bk]])

        def layer(l, last):
            w_in_v = dr["w_in"][l].rearrange("(kc p) c -> p kc c", p=128)
            logit = P.sb("logit", [128, NT, 16], F32)
            Blogit = bufs("logit", NT)
            with Scope(P):
                oT = [P.sb("oT%d" % i, [128, 4, T], BF16) for i in range(3)]
                BoT = [bufs("oT%d_" % i, NG) for i in range(3)]
                with Scope(P):
                    aqn = P.sb("aqn", [128, 3, T], BF16)
                    Baqn = bufs("aqn", NG)
                    akvn = P.sb("akvn", [128, 2, T], BF16)
                    Bakvn = bufs("akvn", NG)
                    kpe = P.sb("kpe", [96, T], BF16)
                    Bkpe = bufs("kpe", NG)
                    cosA = P.sb("cosA", [128, T], F32)
                    sinA = P.sb("sinA", [128, T], F32)
                    Btab = NB("tabA")
                    P.ld("sp", cosA[:], dr["c_cosA"], [], [Btab])
                    P.ld("sp", sinA[:], dr["c_sinA"], [], [Btab])
                    wuq = P.sb("wuq", [128, 3, 768], BF16)
                    wukv = P.sb("wukv", [128, 2, 1024], BF16)
                    Bwu = NB("wu")
                    P.ld("pool", wuq[:], dr["a_w_uq"][l].rearrange("(kc p) c -> p kc c", p=128), [], [Bwu])
                    P.ld("pool", wukv[:], dr["a_w_ukv"][l].rearrange("(kc p) c -> p kc c", p=128), [], [Bwu])
                    t1 = [P.sb("a_t1_%d" % i, [128, 512], F32) for i in range(2)]
                    t2 = [P.sb("a_t2_%d" % i, [128, 512], F32) for i in range(2)]
                    Bt1 = bufs("a_t1", 2)
                    Bt2 = bufs("a_t2", 2)
                    trot = Rot([0, 1])
                    with Scope(P):
                        Wa = P.sb("Wa", [128, KC, 672], BF16)
                        BWa = NB("Wa")
                        P.ld("pool", Wa[:], w_in_v[:, :, 0:672], [], [BWa])
                        gq = P.sb("gq", [128, 3], F32)
                        gkv = P.sb("gkv", [128, 2], F32)
                        Bg = NB("gqkv")
                        P.ld("sp", gq[:], dr["a_q_ln_g"][l], [], [Bg])
                        P.ld("sp", gkv[:], dr["a_kv_ln_g"][l], [], [Bg])
                        sq = P.sb("sq", [128, 3, 512], BF16)
                        Bsq = bufs("sq", 3)
                        rs = P.sb("rs", [128, 512], F32)
                        Brs = NB("rs")
                        kraw = P.sb("kraw", [96, 512], BF16)
                        Bkraw = NB("kraw")
                        P.memset("dve", kraw[:], 0.0, [Bkraw])
                        brot = Rot([0, 1, 2, 3])
                        for G in range(NG):
                            for (dst, Bdst, nch, col0, g_ap, nfeat) in ((aqn, Baqn, 3, 0, gq, 384.0), (akvn, Bakvn, 2, 384, gkv, 256.0)):
                                for c in range(nch):
                                    bk = brot.next()
                                    proj_fm(lambda kc, c=c, col0=col0: Wa[:, kc, col0 + c * 128: col0 + (c + 1) * 128], 128, G, bk, [BWa])
                                    P.cp("act", dst[:, c, gsl(G)], banks[bk][:], [Bbank[bk]], [Bdst[G]])
                                    P.act(sq[:, c, :], banks[bk][:], AF.Square, [Bbank[bk]], [Bsq[c]])
                                bk = brot.next()
                                for c in range(nch):
                                    P.mm(banks[bk][:], ones_bf[:], sq[:, c, :], c == 0, c == nch - 1, [Bc, Bsq[c]], [Bbank[bk]])
                                P.ts("dve", rs[:], banks[bk][:], 1.0 / nfeat, RMS_EPS, ALU.mult, [Bbank[bk]], [Brs], op1=ALU.add)
                                P.act(rs[:], rs[:], AF.Sqrt, [Brs], [Brs])
                                P.recip(rs[:], rs[:], [Brs], [Brs])
                                for c in range(nch):
                                    P.stt("dve", dst[:, c, gsl(G)], dst[:, c, gsl(G)], g_ap[:, c:c + 1], rs[:], ALU.mult, ALU.mult,
                                          [Bdst[G], Bg, Brs], [Bdst[G]])
                            bk = brot.next()
                            proj_fm(lambda kc: Wa[:, kc, 576:672], 96, G, bk, [BWa])
                            P.cp("act", kraw[64:96, :], banks[bk][64:96, :], [Bbank[bk]], [Bkraw])
                            bk2 = brot.next()
                            P.mm(banks[bk2][0:96, :], pmA[0:96, 0:96], kraw[0:96, :], True, True, [Bc, Bkraw], [Bbank[bk2]])
                            k = trot.next()
                            P.tt("dve", t1[k][64:96, :], kraw[64:96, :], cosA[64:96, gsl(G)], ALU.mult, [Bkraw, Btab], [Bt1[k]])
                            P.tt("dve", t2[k][64:96, :], banks[bk2][64:96, :], sinA[64:96, gsl(G)], ALU.mult, [Bbank[bk2], Btab], [Bt2[k]])
                            P.tt("dve", kpe[64:96, gsl(G)], t1[k][64:96, :], t2[k][64:96, :], ALU.add, [Bt1[k], Bt2[k]], [Bkpe[G]])
                    Va = P.sb("Va", [128, NT, 8, VP], BF16)
                    BVa = bufs("Va", NT)
                    BVa1 = NB("Va_ones")
                    P.memset("dve", Va[:, :, :, 64:VP], 1.0, [BVa1])
                    wukv_v = wukv[:].rearrange("p k (h d) -> p k h d", d=128)
                    brot = Rot([5, 6, 7])
                    for tt in range(NT):
                        bk = brot.next()
                        for kc in range(2):
                            P.mm(banks[bk][:].rearrange("p (h d) -> p h d", d=64), akvn[:, kc, tsl(tt)], wukv_v[:, kc, :, 64:128],
                                 kc == 0, kc == 1, [Bakvn[tt // 4], Bwu], [Bbank[bk]])
                        P.cp("act", Va[:, tt, :, 0:64], banks[bk][:].rearrange("p (h d) -> p h d", d=64), [Bbank[bk]], [BVa[tt]])
                    QT = [P.sb("QTa%d" % i, [128, T], BF16) for i in range(2)]
                    KT = [P.sb("KTa%d" % i, [128, T], BF16) for i in range(2)]
                    BQT = [bufs("QTa%d_" % i, NG) for i in range(2)]
                    BKT = [bufs("KTa%d_" % i, NG) for i in range(2)]
                    Bzp = NB("a_zpad")
                    for i in range(2):
                        P.memset("dve", QT[i][96:128, :], 0.0, [Bzp])
                        P.memset("dve", KT[i][96:128, :], 0.0, [Bzp])
                    pt = [P.sb("a_pt%d" % i, [128, 512], BF16) for i in range(3)]
                    Bpt = bufs("a_pt", 3)
                    ptrot = Rot([0, 1, 2])
                    srot = Rot([0, 1, 2])
                    arot = Rot([3, 4])
                    otok = [P.sb("a_otok%d" % i, [128, NT, 128], BF16) for i in range(2)]
                    Botok = [bufs("a_otok%d_" % i, NT) for i in range(2)]
                    rec = [P.sb("a_rec%d" % i, [128, 4], F32) for i in range(2)]
                    Brec = bufs("a_rec", 2)
                    recrot = Rot([0, 1])
                    sc_a = 96.0 ** -0.5
                    def proj_head(h):
                        hp = h % 2
                        for G in range(NG):
                            bk = brot.next()
                            for kc in range(3):
                                P.mm(banks[bk][0:96, :], wuq[:, kc, h * 96:(h + 1) * 96], aqn[:, kc, gsl(G)], kc == 0, kc == 2,
                                     [Bwu, Baqn[G]], [Bbank[bk]])
                            P.cp("act", QT[hp][0:96, gsl(G)], banks[bk][0:96, :], [Bbank[bk]], [BQT[hp][G]])
                            bk2 = brot.next()
                            P.mm(banks[bk2][0:96, :], pmA[0:96, 0:96], QT[hp][0:96, gsl(G)], True, True, [Bc, BQT[hp][G]], [Bbank[bk2]])
                            k = trot.next()
                            P.tt("dve", t1[k][64:96, :], QT[hp][64:96, gsl(G)], cosA[64:96, gsl(G)], ALU.mult, [BQT[hp][G], Btab], [Bt1[k]])
                            P.tt("dve", t2[k][64:96, :], banks[bk2][64:96, :], sinA[64:96, gsl(G)], ALU.mult, [Bbank[bk2], Btab], [Bt2[k]])
                            P.tt("dve", QT[hp][64:96, gsl(G)], t1[k][64:96, :], t2[k][64:96, :], ALU.add, [Bt1[k], Bt2[k]], [BQT[hp][G]])
                            bk = brot.next()
                            for kc in range(2):
                                P.mm(banks[bk][0:64, :], wukv[:, kc, h * 128:h * 128 + 64], akvn[:, kc, gsl(G)], kc == 0, kc == 1,
                                     [Bwu, Bakvn[G]], [Bbank[bk]])
                            P.cp("act", KT[hp][0:64, gsl(G)], banks[bk][0:64, :], [Bbank[bk]], [BKT[hp][G]])
                            P.cp("dve", KT[hp][64:96, gsl(G)], kpe[64:96, gsl(G)], [Bkpe[G]], [BKT[hp][G]])

                    blocks = [(G, st_) for G in range(NG) for st_ in range(4 * G + 4)]
                    proj_head(0)
                    for h in range(8):
                        hp = h % 2
                        cpair = h // 2
                        if h + 1 < 8:
                            proj_head(h + 1)
                        info = {}

                        def qk(k):
                            G, st_ = blocks[k]
                            j0 = max(0, st_ - 4 * G)
                            ncol = (4 - j0) * 128
                            q0 = (4 * G + j0) * 128
                            sb_ = srot.next()
                            P.mm(banks[sb_][:, 0:ncol], KT[hp][:, tsl(st_)], QT[hp][:, q0:q0 + ncol], True, True,
                                 [BKT[hp][st_ // 4], BQT[hp][G], Bzp], [Bbank[sb_]])
                            if st_ >= 4 * G:
                                P.mm(banks[sb_][:, 0:128], negdiag_bf[:], ident_bf[:], False, True, [Bc], [Bbank[sb_]], sgc=True)
                            info[k] = (sb_, j0, ncol)

                        qk(0)
                        ab = None
                        for k, (G, st_) in enumerate(blocks):
                            if k + 1 < len(blocks):
                                qk(k + 1)
                            sb_, j0, ncol = info[k]
                            if st_ == 0:
                                ab = arot.next()
                            accv = banks[ab][:, 0:4 * VP].rearrange("p (j d) -> p j d", d=VP)
                            pk = ptrot.next()
                            P.act(pt[pk][:, 0:ncol], banks[sb_][:, 0:ncol], AF.Exp, [Bbank[sb_]], [Bpt[pk]], scale=sc_a)
                            for j in range(j0, 4):
                                P.mm(accv[:, j, 0:65], pt[pk][:, (j - j0) * 128:(j - j0 + 1) * 128], Va[:, st_, h, 0:65],
                                     (st_ == 0 and j == 0), st_ == 4 * G + j, [Bpt[pk], BVa[st_], BVa1], [Bbank[ab]], sgc=True)
                            if st_ == 4 * G + 3:
                                rk = recrot.next()
                                P.recip(rec[rk][:], accv[:, :, 64], [Bbank[ab]], [Brec[rk]])
                                for j in range(4):
                                    tt = 4 * G + j
                                    P.ts("dve", otok[cpair % 2][:, tt, hp * 64:(hp + 1) * 64], accv[:, j, 0:64], rec[rk][:, j:j + 1], None, ALU.mult,
                                         [Bbank[ab], Brec[rk]], [Botok[cpair % 2][tt]])
                        if hp == 1:
                            for t0 in range(0, NT, 8):
                                bk = brot.next()
                                bv = banks[bk][:].bitcast(BF16)
                                for tt in range(t0, t0 + 8):
                                    P.tr(bv[:, (tt - t0) * 128:(tt - t0 + 1) * 128], otok[cpair % 2][:, tt, :], ident_bf[:],
                                         [Botok[cpair % 2][tt], Bc], [Bbank[bk]])
                                P.cp("act", oT[0][:, cpair, t0 * 128:(t0 + 8) * 128], bv[:], [Bbank[bk]], [BoT[0][t0 // 4], BoT[0][t0 // 4 + 1]])
                dump("d_oTa", oT[0][:], BoT[0])
                if STOP_AFTER == "mla":
                    return

                def hd64_proj(name, col_q, col_k, col_v, QTx, BQTx, KTx, BKTx, Vx, BVx, extra=None, kz=None):
                    with Scope(P):
                        cos64 = P.sb(name + "cos", [128, T], F32)
                        sin64 = P.sb(name + "sin", [128, T], F32)
                        Btab = NB(name + "tab")
                        P.ld("sp", cos64[:], dr["c_cos64"], [], [Btab])
                        P.ld("sp", sin64[:], dr["c_sin64"], [], [Btab])
                        Wq = P.sb(name + "Wq", [128, KC, 512], BF16)
                        Wk2 = P.sb(name + "Wk2", [128, KC, 2, 128], BF16)
                        Wv = P.sb(name + "Wv", [128, KC, 128], BF16)
                        BW = NB(name + "W")
                        P.ld("pool", Wq[:], w_in_v[:, :, col_q:col_q + 512], [], [BW])
                        for g in range(2):
                            for i in range(2):
                                P.ld("pool", Wk2[:, :, g, i * 64:(i + 1) * 64], w_in_v[:, :, col_k + g * 64: col_k + (g + 1) * 64], [], [BW])
                        P.ld("pool", Wv[:], w_in_v[:, :, col_v:col_v + 128], [], [BW])
                        chunks = []
                        for c in range(4):
                            chunks.append((lambda kc, c=c: Wq[:, kc, c * 128:(c + 1) * 128], lambda G, c=c: QTx[:, c, gsl(G)], BQTx))
                        for g in range(2):
                            chunks.append((lambda kc, g=g: Wk2[:, kc, g, :], (lambda G, g=g: KTx[:, g, gsl(G)]) if kz is None else ("kz", g), BKTx))
                        xw = None
                        if extra is not None:
                            xw = extra(BW, chunks)
                        raw = [P.sb(name + "raw%d" % i, [128, 512], BF16) for i in range(2)]
                        Braw = bufs(name + "raw", 2)
                        t1 = [P.sb(name + "t1_%d" % i, [128, 512], F32) for i in range(2)]
                        t2 = [P.sb(name + "t2_%d" % i, [128, 512], F32) for i in range(2)]
                        Bt1 = bufs(name + "t1", 2)
                        Bt2 = bufs(name + "t2", 2)
                        rrot = Rot([0, 1])
                        brot = Rot([0, 1, 2, 3, 4, 5, 6, 7])
                        for G in range(NG):
                            if STOP_AFTER == name + "w":
                                break
                            for (lf, df, Bd) in chunks:
                                bk = brot.next()
                                proj_fm(lf, 128, G, bk, [BW])
                                k = rrot.next()
                                P.cp("act", raw[k][:], banks[bk][:], [Bbank[bk]], [Braw[k]])
                                bk2 = brot.next()
                                P.mm(banks[bk2][:], pm64[:], raw[k][:], True, True, [Bc, Braw[k]], [Bbank[bk2]])
                                P.tt("dve", t1[k][:], raw[k][:], cos64[:, gsl(G)], ALU.mult, [Braw[k], Btab], [Bt1[k]])
                                P.tt("dve", t2[k][:], banks[bk2][:], sin64[:, gsl(G)], ALU.mult, [Bbank[bk2], Btab], [Bt2[k]])
                                if isinstance(df, tuple):
                                    g_ = df[1]
                                    P.tt("dve", kz[0:64, g_, 0, gsl(G)], t1[k][0:64, :], t2[k][0:64, :], ALU.add, [Bt1[k], Bt2[k]], [Bd[G]])
                                    P.tt("dve", kz[64:128, g_, 1, gsl(G)], t1[k][64:128, :], t2[k][64:128, :], ALU.add, [Bt1[k], Bt2[k]], [Bd[G]])
                                else:
                                    P.tt("dve", df(G), t1[k][:], t2[k][:], ALU.add, [Bt1[k], Bt2[k]], [Bd[G]])
                        BV1 = NB(name + "V1")
                        if STOP_AFTER == name + "rope":
                            return BV1
                        P.memset("dve", Vx[:, :, :, 64:VP], 1.0, [BV1])
                        for tt in range(NT):
                            bk = brot.next()
                            for kc in range(KC):
                                P.mm(banks[bk][:, 0:128], xT[:, kc, tsl(tt)], Wv[:, kc, :], kc == 0, kc == KC - 1, [BxT[tt], BW], [Bbank[bk]])
                            if xw is None and EXPER == "A":
                                for kc in range(KC):
                                    P.mm(banks[bk][:, 128:132], xT[:, kc, tsl(tt)], Wv[:, kc, 0:4], kc == 0, kc == KC - 1, [BxT[tt], BW], [Bbank[bk]])
                            if xw is not None:
                                Wwi, widx, Bwidx = xw
                                for kc in range(KC):
                                    P.mm(banks[bk][:, 128:132], xT[:, kc, tsl(tt)], Wwi[:, kc, :], kc == 0, kc == KC - 1, [BxT[tt], BW], [Bbank[bk]])
                                P.act(widx[:, tt, :], banks[bk][:, 128:132], AF.Copy, [Bbank[bk]], [Bwidx[tt]], scale=1.0 / 16.0)
                            P.cp("act", Vx[:, tt, :, 0:64], banks[bk][:, 0:128].rearrange("p (g d) -> p g d", d=64), [Bbank[bk]], [BVx[tt]])
                        return BV1

                with Scope(P):
                    if SKIP_DSA:
                        raise_skip = True
                    QTb = P.sb("QTb", [128, 4, T], BF16)
                    BQTb = bufs("QTb", NG)
                    KTb = P.sb("KTbz", [128, 2, 2, T], BF16)
                    BKTb = bufs("KTb", NG)
                    Bkz = NB("b_kz")
                    P.memset("dve", KTb[64:128, :, 0, :], 0.0, [Bkz])
                    P.memset("dve", KTb[0:64, :, 1, :], 0.0, [Bkz])
                    Vb = P.sb("Vb", [128, NT, 2, VP], BF16)
                    BVb = bufs("Vb", NT)
                    QIT = P.sb("QIT", [128, 2, T], BF16)
                    BQIT = bufs("QIT", NG)
                    KIT = P.sb("KIT", [128, T], BF16)
                    BKIT = bufs("KIT", NG)
                    widx = P.sb("widx", [128, NT, 4], F32)
                    Bwidx = bufs("widx", NT)

                    def extra_b(BW, chunks):
                        Wqi = P.sb("Wqi", [128, KC, 256], BF16)
                        Wki2 = P.sb("Wki2", [128, KC, 128], BF16)
                        Wwi = P.sb("Wwi", [128, KC, 4], BF16)
                        P.ld("pool", Wqi[:], w_in_v[:, :, 1440:1696], [], [BW])
                        for i in range(2):
                            P.ld("pool", Wki2[:, :, i * 64:(i + 1) * 64], w_in_v[:, :, 1696:1760], [], [BW])
                        P.ld("pool", Wwi[:], w_in_v[:, :, 1760:1764], [], [BW])
                        for c in range(2):
                            chunks.append((lambda kc, c=c: Wqi[:, kc, c * 128:(c + 1) * 128], lambda G, c=c: QIT[:, c, gsl(G)], BQIT))
                        chunks.append((lambda kc: Wki2[:, kc, :], lambda G: KIT[:, gsl(G)], BKIT))
                        return (Wwi, widx, Bwidx)

                    if not SKIP_DSA:
                        BVb1 = hd64_proj("b_", 672, 1184, 1312, QTb, BQTb, None, BKTb, Vb, BVb, extra_b, kz=KTb)
                    if not SKIP_DSA:
                        score32 = [P.sb("score32_%d" % i, [128, 512], F32) for i in range(2)]
                        Bs32 = bufs("score32_", 2)
                        s32rot = Rot([0, 1])
                        scorebf = [P.sb("scorebf%d" % i, [128, T], BF16) for i in range(4)]
                        Bsbf = bufs("scorebf", 4)
                        mbias = [P.sb("mbias%d" % i, [128, 4, T], BF16) for i in range(2)]
                        Bmb = [bufs("mbias%d_" % i, 4) for i in range(2)]
                        rr = [P.sb("rr%d" % i, [128, 512], F32) for i in range(2)]
                        Brr = bufs("rr", 2)
                        rrot = Rot([0, 1])
                        bis = P.sb("bis", [128, 16], F32)
                        Bmid = NB("bis_mid")
                        Bval = bufs("bis_val", 4)
                        Btmp = NB("bis_tmp")
                        Bthr = NB("bis_thr")
                        pt = [P.sb("b_pt%d" % i, [128, 512], BF16) for i in range(3)]
                        Bpt = bufs("b_pt", 3)
                        ptrot = Rot([0, 1, 2])
                        srot = Rot([0, 1, 2])
                        arot = Rot([3, 4])
                        irot = Rot([5, 6, 7])
                        otg = [P.sb("b_otg%d" % i, [128, 4, 512], BF16) for i in range(2)]
                        Botg = [bufs("b_otg%d_" % i, 4) for i in range(2)]
                        rec = [P.sb("b_rec%d" % i, [128, 4], F32) for i in range(2)]
                        Brec = bufs("b_rec", 2)
                        recrot = Rot([0, 1])

                        def topk_gen(G):
                            nSs = [(4 * G + j + 1) * 128 for j in range(4)]
                            for j in range(4):
                                qt = 4 * G + j
                                nS = nSs[j]
                                nsc = (nS + 511) // 512
                                for sc in range(nsc):
                                    ncol = min(512, nS - sc * 512)
                                    cols = slice(sc * 512, sc * 512 + ncol)
                                    sk = s32rot.next()
                                    s32 = score32[sk]
                                    for ih in range(4):
                                        c, i = ih // 2, ih % 2
                                        bk = irot.next()
                                        P.mm(banks[bk][:, 0:ncol], QIT[i * 64:(i + 1) * 64, c, tsl(qt)], KIT[i * 64:(i + 1) * 64, cols], True, True,
                                             [BQIT[G], BKIT[sc]], [Bbank[bk]])
                                        rk = rrot.next()
                                        r = rr[rk]
                                        P.act(r[:, 0:ncol], banks[bk][:, 0:ncol], AF.Relu, [Bbank[bk]], [Brr[rk]])
                                        if ih == 0:
                                            P.ts("dve", s32[:, 0:ncol], r[:, 0:ncol], widx[:, qt, 0:1], None, ALU.mult, [Brr[rk], Bwidx[qt]], [Bs32[sk]])
                                        elif ih < 3:
                                            P.stt("dve", s32[:, 0:ncol], r[:, 0:ncol], widx[:, qt, ih:ih + 1], s32[:, 0:ncol], ALU.mult, ALU.add,
                                                  [Brr[rk], Bwidx[qt], Bs32[sk]], [Bs32[sk]])
                                        else:
                                            if sc == nsc - 1:
                                                P.tt("dve", s32[:, ncol - 128:ncol], s32[:, ncol - 128:ncol], negdiag[:], ALU.add, [Bs32[sk], Bc], [Bs32[sk]])
                                            P.stt("dve", scorebf[j][:, cols], r[:, 0:ncol], widx[:, qt, 3:4], s32[:, 0:ncol], ALU.mult, ALU.add,
                                                  [Brr[rk], Bwidx[qt], Bs32[sk]], [Bsbf[j]])
                                    yield
                            P.memset("dve", bis[:, 0:4], 0.0, [Bmid])
                            w = W0
                            for it in range(NIT):
                                for j in (2, 3):
                                    P.act(mbias[G % 2][:, j, 0:nSs[j]], scorebf[j][:, 0:nSs[j]], AF.Sign, [Bsbf[j], Bmid], [Bmb[G % 2][j], Bval[j]],
                                          bias=bis[:, j:j + 1], scale=-1.0, accum=bis[:, 4 + j:5 + j])
                                for j in (0, 1):
                                    P.ts("dve", mbias[G % 2][:, j, 0:nSs[j]], scorebf[j][:, 0:nSs[j]], bis[:, j:j + 1], None, ALU.is_ge, [Bsbf[j], Bmid], [Bmb[G % 2][j], Bval[j]],
                                         op1=ALU.add, accum=bis[:, 4 + j:5 + j])
                                P.ts("dve", bis[:, 8:10], bis[:, 4:6], 256.0, w, ALU.is_ge, [Bval[0], Bval[1]], [Btmp], op1=ALU.mult)
                                for j in (2, 3):
                                    P.ts("dve", bis[:, 8 + j:9 + j], bis[:, 4 + j:5 + j], float(nSs[j] - 512), w, ALU.is_le, [Bval[j]], [Btmp], op1=ALU.mult)
                                P.stt("dve", bis[:, 0:4], bis[:, 8:12], -w / 2, bis[:, 0:4], ALU.add, ALU.add, [Btmp, Bmid], [Bmid])
                                w = w / 2
                                yield
                            P.ts("dve", bis[:, 12:16], bis[:, 0:4], -w, None, ALU.add, [Bmid], [Bthr])
                            for j in range(4):
                                nS = nSs[j]
                                P.ts("dve", mbias[G % 2][:, j, 0:nS], scorebf[j][:, 0:nS], bis[:, 12 + j:13 + j], -30000.0, ALU.is_lt,
                                     [Bsbf[j], Bthr], [Bmb[G % 2][j]], op1=ALU.mult)
                                yield

                        def attn_gen(G):
                            mb = mbias[G % 2]
                            Bm = Bmb[G % 2]
                            og = otg[G % 2]
                            Bog = Botg[G % 2]
                            blocks = [(h, st_) for h in range(8) for st_ in range(4 * G + 4)]
                            info = {}

                            def qk(k):
                                h, st_ = blocks[k]
                                c, i, g = h // 2, h % 2, h // 4
                                j0 = max(0, st_ - 4 * G)
                                ncol = (4 - j0) * 128
                                q0 = (4 * G + j0) * 128
                                sb_ = srot.next()
                                P.mm(banks[sb_][:, 0:ncol], KTb[:, g, i, tsl(st_)], QTb[:, c, q0:q0 + ncol], True, True,
                                     [BKTb[st_ // 4], BQTb[G], Bkz], [Bbank[sb_]])
                                for j in range(j0, 4):
                                    P.mm(banks[sb_][:, (j - j0) * 128:(j - j0 + 1) * 128], mb[:, j, tsl(st_)], ident_bf[:], False, j == 3,
                                         [Bm[j], Bc], [Bbank[sb_]], sgc=True)
                                info[k] = (sb_, j0, ncol)

                            qk(0)
                            ab = None
                            for k, (h, st_) in enumerate(blocks):
                                g = h // 4
                                if k + 1 < len(blocks):
                                    qk(k + 1)
                                sb_, j0, ncol = info[k]
                                if st_ == 0:
                                    ab = arot.next()
                                accv = banks[ab][:, 0:4 * VP].rearrange("p (j d) -> p j d", d=VP)
                                pk = ptrot.next()
                                P.act(pt[pk][:, 0:ncol], banks[sb_][:, 0:ncol], AF.Exp, [Bbank[sb_]], [Bpt[pk]], scale=0.125)
                                for j in range(j0, 4):
                                    P.mm(accv[:, j, 0:65], pt[pk][:, (j - j0) * 128:(j - j0 + 1) * 128], Vb[:, st_, g, 0:65],
                                         (st_ == 0 and j == 0), st_ == 4 * G + j, [Bpt[pk], BVb[st_], BVb1], [Bbank[ab]], sgc=True)
                                if st_ == 4 * G + 3:
                                    rk = recrot.next()
                                    P.recip(rec[rk][:], accv[:, :, 64], [Bbank[ab]], [Brec[rk]])
                                    for j in range(4):
                                        P.ts("dve", og[:, j, h * 64:(h + 1) * 64], accv[:, j, 0:64], rec[rk][:, j:j + 1], None, ALU.mult,
                                             [Bbank[ab], Brec[rk]], [Bog[j]])
                                yield
                            for j in range(4):
                                tt = 4 * G + j
                                bk = irot.next()
                                bv = banks[bk][:].bitcast(BF16)
                                for c in range(4):
                                    P.tr(bv[:, c * 128:(c + 1) * 128], og[:, j, c * 128:(c + 1) * 128], ident_bf[:], [Bog[j], Bc], [Bbank[bk]])
                                P.cp("act", oT[1][:, :, tsl(tt)], bv[:, 0:512].rearrange("p (c t) -> p c t", t=128), [Bbank[bk]], [BoT[1][G]])
                            yield

                        for _ in topk_gen(0):
                            pass
                        for G in range(NG):
                            ag = list_len = None
                            a_units = 8 * (4 * G + 4) + 1
                            if G + 1 < NG:
                                t_units = sum(((4 * (G + 1) + j + 1) * 128 + 511) // 512 for j in range(4)) + NIT + 4
                                tg = topk_gen(G + 1)
                            else:
                                t_units = 0
                                tg = None
                            done_t = 0
                            for ai, _ in enumerate(attn_gen(G)):
                                if tg is not None:
                                    want = ((ai + 1) * t_units) // a_units
                                    while done_t < want:
                                        try:
                                            next(tg)
                                        except StopIteration:
                                            tg = None
                                            break
                                        done_t += 1
                            if tg is not None:
                                for _ in tg:
                                    pass
                dump("d_oTb", oT[1][:], BoT[1])
                if STOP_AFTER == "dsa":
                    return
                with Scope(P):
                    QTc = P.sb("QTc", [128, 4, T], BF16)
                    BQTc = bufs("QTc", NG)
                    KTc = P.sb("KTc", [128, 2, T], BF16)
                    BKTc = bufs("KTc", NG)
                    Vc = P.sb("Vc", [128, NT, 2, VP], BF16)
                    BVc = bufs("Vc", NT)
                    BVc1 = hd64_proj("c_", 1764, 2276, 2404, QTc, BQTc, KTc, BKTc, Vc, BVc, None)
                    if STOP_AFTER in ("swa_proj", "c_rope", "c_w"):
                        return
                    esink = P.sb("esink", [128, 8], F32)
                    Besink = NB("esink")
                    P.ld("sp", esink[:], dr["c_sinks"][l].partition_broadcast(128), [], [Besink])
                    P.act(esink[:], esink[:], AF.Exp, [Besink], [Besink])
                    smask = P.sb("smask", [128, 2, 2, 128], BF16)
                    Bsm = NB("smask")
                    for i in range(2):
                        P.cp("dve", smask[:, i, 0, :], swaprev_bf[:], [Bc], [Bsm])
                        P.cp("dve", smask[:, i, 1, :], diag_bf[:], [Bc], [Bsm])
                    ptc = [P.sb("c_pt%d" % i, [128, 2, 2, 128], BF16) for i in range(3)]
                    Bptc = bufs("c_pt", 3)
                    ptrot = Rot([0, 1, 2])
                    srot = Rot([0, 1, 2])
                    arot = Rot([3, 4])
                    irot = Rot([5, 6, 7])
                    otc = [P.sb("c_ot%d" % i, [128, 512], BF16) for i in range(2)]
                    Botc = bufs("c_ot", 2)
                    den = [P.sb("c_den%d" % i, [128, 2], F32) for i in range(2)]
                    Bden = bufs("c_den", 2)
                    drot = Rot([0, 1])
                    srot = Rot([0, 1, 2, 5])
                    irot = Rot([6, 7])
                    blocks = [(qt, c) for qt in range(NT) for c in range(4)]
                    info = {}

                    def qk_c(k):
                        qt, c = blocks[k]
                        g = c // 2
                        u0 = 0 if qt > 0 else 1
                        sbs = []
                        for i in range(2):
                            sb_ = srot.next()
                            sv = banks[sb_][:, 0:256].rearrange("p (u t) -> p u t", u=2)
                            for u in range(u0, 2):
                                st_ = qt - 1 + u
                                P.mm(sv[:, u, :], KTc[i * 64:(i + 1) * 64, g, tsl(st_)], QTc[i * 64:(i + 1) * 64, c, tsl(qt)], True, True,
                                     [BKTc[st_ // 4], BQTc[qt // 4]], [Bbank[sb_]])
                                P.mm(sv[:, u, :], (negprev_bf if u == 0 else negdiag_bf)[:], ident_bf[:], False, True, [Bc], [Bbank[sb_]], sgc=True)
                            sbs.append((sb_, sv))
                        info[k] = sbs

                    qk_c(0)
                    for k, (qt, c) in enumerate(blocks):
                        if k + 1 < len(blocks):
                            qk_c(k + 1)
                        g = c // 2
                        u0 = 0 if qt > 0 else 1
                        ok = qt % 2
                        pk = ptrot.next()
                        for i in range(2):
                            sb_, sv = info[k][i]
                            P.act(ptc[pk][:, i, u0:2, :], sv[:, u0:2, :], AF.Exp, [Bbank[sb_]], [Bptc[pk]], scale=0.125)
                        ab = arot.next()
                        accv = banks[ab][:, 0:2 * VP].rearrange("p (i d) -> p i d", d=VP)
                        for i in range(2):
                            for u in range(u0, 2):
                                st_ = qt - 1 + u
                                P.mm(accv[:, i, 0:65], ptc[pk][:, i, u, :], Vc[:, st_, g, 0:65], (u == u0 and i == 0), u == 1, [Bptc[pk], BVc[st_], BVc1], [Bbank[ab]], sgc=True)
                        dk = drot.next()
                        P.tt("dve", den[dk][:], accv[:, :, 64], esink[:, 2 * c:2 * c + 2], ALU.add, [Bbank[ab], Besink], [Bden[dk]])
                        P.recip(den[dk][:], den[dk][:], [Bden[dk]], [Bden[dk]])
                        for i in range(2):
                            h = 2 * c + i
                            P.ts("dve", otc[ok][:, h * 64:(h + 1) * 64], accv[:, i, 0:64], den[dk][:, i:i + 1], None, ALU.mult,
                                 [Bbank[ab], Bden[dk]], [Botc[ok]])
                        if c == 3:
                            bk = irot.next()
                            bv = banks[bk][:].bitcast(BF16)
                            for c2 in range(4):
                                P.tr(bv[:, c2 * 128:(c2 + 1) * 128], otc[ok][:, c2 * 128:(c2 + 1) * 128], ident_bf[:], [Botc[ok], Bc], [Bbank[bk]])
                            P.cp("act", oT[2][:, :, tsl(qt)], bv[:, 0:512].rearrange("p (c t) -> p c t", t=128), [Bbank[bk]], [BoT[2][qt // 4]])
                dump("d_oTc", oT[2][:], BoT[2])
                if STOP_AFTER == "swa":
                    return
                with Scope(P):
                    wout = P.sb("wout", [128, KC, D], BF16)
                    Bwout = NB("wout")
                    P.ld("pool", wout[:], dr["w_out"][l].rearrange("(kc p) c -> p kc c", p=128), [], [Bwout])
                    g1B = P.sb("g1B", [128, D], F32)
                    b1B = P.sb("b1B", [128, D], F32)
                    Bg1 = NB("g1b1")
                    P.ld("sp", g1B[:], dr["ln1_g"][l].partition_broadcast(128), [], [Bg1])
                    P.ld("sp", b1B[:], dr["ln1_b"][l].partition_broadcast(128), [], [Bg1])
                    wr = P.sb("wr", [128, KC, 16], F32)
                    Bwr = NB("wr")
                    P.ld("sp", wr[:], dr["w_router"].rearrange("(kc p) e -> p kc e", p=128), [], [Bwr])
                    wbr = [P.sb("wbr%d" % i, [128, 3, 4, 128], BF16) for i in range(2)]
                    wgt = [P.sb("wgt%d" % i, [128, 3, KC, 128], BF16) for i in range(2)]
                    Bwm = bufs("wm", 2)
                    mg = P.sb("mg", [128, KC, T], BF16)
                    Bmg = [bufs("mg%d_" % i, NG) for i in range(KC)]
                    sg = [P.sb("sg%d" % i, [128, 512], F32) for i in range(2)]
                    Bsg = bufs("sg", 2)
                    sgrot = Rot([0, 1])
                    macc = P.sb("macc", [128, 512], F32)
                    Bmacc = NB("macc")
                    pre = [P.sb("m_pre%d" % i, [128, D], F32) for i in range(4)]
                    Bpre = bufs("m_pre", 4)
                    st1 = P.sb("m_st", [128, 8, 4], F32)
                    Bsum1 = bufs("m_sum", 4)
                    Bsq1 = bufs("m_sq", 4)
                    Bvec1 = NB("m_vec")
                    junks = [P.sb("m_junk%d" % i, [128, D], BF16)[:] for i in range(2)]
                    Bjunks = bufs("m_junk", 2)
                    xbf = [P.sb("m_xbf%d" % i, [128, D], BF16)[:] for i in range(2)]
                    Bxbf = bufs("m_xbf", 2)
                    x1Tf = P.sb("x1Tf", [128, KC, 128], F32)
                    Bx1Tf = NB("x1Tf")
                    brot = Rot([0, 1, 2, 3, 4, 5, 6, 7])
                    wbr_src = [dr[n][l].rearrange("(kc p) c -> p kc c", p=128) for n in ("w_br_a", "w_br_b", "w_br_c")]
                    def ld_merge(dc):
                        k = dc % 2
                        for i in range(3):
                            P.ld("pool", wbr[k][:, i, :, :], wbr_src[i][:, :, dc * 128:(dc + 1) * 128], [], [Bwm[k]])
                            P.ld("pool", wgt[k][:, i, :, :], w_in_v[:, :, 2532 + i * 1024 + dc * 128: 2532 + i * 1024 + (dc + 1) * 128], [], [Bwm[k]])

                    ld_merge(0)
                    for dc in range(KC):
                        k = dc % 2
                        if dc + 1 < KC:
                            ld_merge(dc + 1)
                        for G in range(NG):
                            for i in range(3):
                                yb = brot.next()
                                for kc in range(4):
                                    P.mm(banks[yb][:], wbr[k][:, i, kc, :], oT[i][:, kc, gsl(G)], kc == 0, kc == 3, [Bwm[k], BoT[i][G]], [Bbank[yb]])
                                gb = brot.next()
                                for kc in range(KC):
                                    P.mm(banks[gb][:], wgt[k][:, i, kc, :], xT[:, kc, gsl(G)], kc == 0, kc == KC - 1,
                                         [Bwm[k]] + [BxT[4 * G + j] for j in range(4)], [Bbank[gb]])
                                sk = sgrot.next()
                                P.act(sg[sk][:], banks[gb][:], AF.Sigmoid, [Bbank[gb]], [Bsg[sk]])
                                if i == 0:
                                    P.tt("dve", macc[:], sg[sk][:], banks[yb][:], ALU.mult, [Bsg[sk], Bbank[yb]], [Bmacc])
                                elif i == 1:
                                    P.tt("dve", sg[sk][:], sg[sk][:], banks[yb][:], ALU.mult, [Bsg[sk], Bbank[yb]], [Bsg[sk]])
                                    P.tt("dve", macc[:], macc[:], sg[sk][:], ALU.add, [Bmacc, Bsg[sk]], [Bmacc])
                                else:
                                    P.tt("dve", sg[sk][:], sg[sk][:], banks[yb][:], ALU.mult, [Bsg[sk], Bbank[yb]], [Bsg[sk]])
                                    P.tt("dve", mg[:, dc, gsl(G)], macc[:], sg[sk][:], ALU.add, [Bmacc, Bsg[sk]], [Bmg[dc][G]])
                    for G in range(NG):
                        items = []
                        for j in range(4):
                            tt = 4 * G + j
                            P.ld("sp", pre[j][:], xres[tsl(tt), :], [Bxres[tt]], [Bpre[j]])
                            items.append((pre[j][:], Bpre[j]))
                        for j in range(4):
                            tt = 4 * G + j
                            for half in range(2):
                                ob = brot.next()
                                for kc in range(KC):
                                    P.mm(banks[ob][:], mg[:, kc, tsl(tt)], wout[:, kc, half * 512:(half + 1) * 512], kc == 0, kc == KC - 1,
                                         [Bmg[kc][G], Bwout], [Bbank[ob]])
                                hs = slice(half * 512, (half + 1) * 512)
                                P.stt("dve", pre[j][:, hs], pre[j][:, hs], ALPHA, banks[ob][:], ALU.mult, ALU.add, [Bpre[j], Bbank[ob]], [Bpre[j]])
                        ln_batch(items, g1B[:], b1B[:], Bg1, st1, Bsum1, Bsq1, Bvec1, junks, Bjunks)
                        for j in range(4):
                            tt = 4 * G + j
                            P.st("sp", xres[tsl(tt), :], pre[j][:], [Bpre[j]], [Bxres[tt]], key=Bpre[j])
                        to_xT_batch(items, [4 * G + j for j in range(4)], xbf, Bxbf, [brot.next() for _ in range(4)])
                        for j in range(4):
                            tt = 4 * G + j
                            tb0 = brot.next()
                            tb1 = brot.next()
                            for kc in range(KC):
                                tb = tb0 if kc < 4 else tb1
                                P.tr(banks[tb][:, (kc % 4) * 128:(kc % 4 + 1) * 128], pre[j][:, kc * 128:(kc + 1) * 128], ident_f[:], [Bpre[j], Bc], [Bbank[tb]])
                            P.cp("act", x1Tf[:, 0:4, :], banks[tb0][:].rearrange("p (k t) -> p k t", t=128), [Bbank[tb0]], [Bx1Tf])
                            P.cp("act", x1Tf[:, 4:8, :], banks[tb1][:].rearrange("p (k t) -> p k t", t=128), [Bbank[tb1]], [Bx1Tf])
                            lb = brot.next()
                            for kc in range(KC):
                                P.mm(banks[lb][:, 0:16], x1Tf[:, kc, :], wr[:, kc, :], kc == 0, kc == KC - 1, [Bx1Tf, Bwr], [Bbank[lb]])
                            P.cp("dve", logit[:, tt, :], banks[lb][:, 0:16], [Bbank[lb]], [Blogit[tt]])
            if STOP_AFTER == "ln1":
                return
            with Scope(P):
                gate = P.sb("gate", [128, NT, 16], F32)
                Bgate = NB("gate")
                with Scope(P):
                    sco = P.sb("r_sco", [128, NT, 16], F32)
                    bia = P.sb("r_bia", [128, NT, 16], F32)
                    mb = P.sb("r_mb", [128, NT, 16], F32)
                    rbias = P.sb("r_bias", [128, 16], F32)
                    ps6 = P.sb("r_ps6", [128, 6, NT, 4], F32)
                    gs = P.sb("r_gs", [128, NT, 4], F32)
                    gmax = P.sb("r_gmax", [128, NT], F32)
                    ing = P.sb("r_ing", [128, NT, 4], F32)
                    red = P.sb("r_red", [128, NT, 8], F32)
                    tmax = P.sb("r_tmax", [128, NT], F32)
                    e1 = P.sb("r_e1", [128, NT, 16], F32)
                    e2 = P.sb("r_e2", [128, NT, 16], F32)
                    Br = NB("routing")
                    R = [Br] + Blogit
                    P.ld("sp", rbias[:], dr["router_bias"].partition_broadcast(128), [], [Br])
                    P.act(sco[:], logit[:], AF.Sigmoid, R, [Br])
                    for tt in range(NT):
                        P.tt("dve", bia[:, tt, :], sco[:, tt, :], rbias[:], ALU.add, [Br], [Br])
                    bg = bia[:].rearrange("p t (g e) -> p t g e", e=4)
                    pairs = [(0, 1), (0, 2), (0, 3), (1, 2), (1, 3), (2, 3)]
                    for pi, (a, b_) in enumerate(pairs):
                        P.tt("dve", ps6[:, pi, :, :], bg[:, :, :, a], bg[:, :, :, b_], ALU.add, [Br], [Br])
                    P.tt("dve", gs[:], ps6[:, 0, :, :], ps6[:, 1, :, :], ALU.max, [Br], [Br])
                    for pi in range(2, 6):
                        P.tt("dve", gs[:], gs[:], ps6[:, pi, :, :], ALU.max, [Br], [Br])
                    P.tt("dve", gmax[:], gs[:, :, 0], gs[:, :, 1], ALU.max, [Br], [Br])
                    P.tt("dve", gmax[:], gmax[:], gs[:, :, 2], ALU.max, [Br], [Br])
                    P.tt("dve", gmax[:], gmax[:], gs[:, :, 3], ALU.max, [Br], [Br])
                    for g in range(4):
                        P.tt("dve", ing[:, :, g], gs[:, :, g], gmax[:], ALU.is_equal, [Br], [Br])
                    P.ts("dve", ing[:], ing[:], -1.0, 1e30, ALU.add, [Br], [Br], op1=ALU.mult)
                    mbg = mb[:].rearrange("p t (g e) -> p t g e", e=4)
                    for e_ in range(4):
                        P.tt("dve", mbg[:, :, :, e_], bg[:, :, :, e_], ing[:], ALU.add, [Br], [Br])

                    def max16(dst, src):
                        P.tt("dve", red[:, :, 0:8], src[:, :, 0:8], src[:, :, 8:16], ALU.max, [Br], [Br])
                        P.tt("dve", red[:, :, 0:4], red[:, :, 0:4], red[:, :, 4:8], ALU.max, [Br], [Br])
                        P.tt("dve", red[:, :, 0:2], red[:, :, 0:2], red[:, :, 2:4], ALU.max, [Br], [Br])
                        P.tt("dve", dst, red[:, :, 0], red[:, :, 1], ALU.max, [Br], [Br])

                    max16(tmax[:], mb)
                    for tt in range(NT):
                        P.ts("dve", e1[:, tt, :], mb[:, tt, :], tmax[:, tt:tt + 1], None, ALU.is_equal, [Br], [Br])
                    P.stt("dve", mb[:], e1[:], -1e30, mb[:], ALU.mult, ALU.add, [Br], [Br])
                    max16(tmax[:], mb)
                    for tt in range(NT):
                        P.ts("dve", e2[:, tt, :], mb[:, tt, :], tmax[:, tt:tt + 1], None, ALU.is_equal, [Br], [Br])
                    P.tt("dve", e1[:], e1[:], e2[:], ALU.add, [Br], [Br])
                    P.tt("dve", e1[:], e1[:], sco[:], ALU.mult, [Br], [Br])
                    P.tt("dve", red[:, :, 0:8], e1[:, :, 0:8], e1[:, :, 8:16], ALU.add, [Br], [Br])
                    P.tt("dve", red[:, :, 0:4], red[:, :, 0:4], red[:, :, 4:8], ALU.add, [Br], [Br])
                    P.tt("dve", red[:, :, 0:2], red[:, :, 0:2], red[:, :, 2:4], ALU.add, [Br], [Br])
                    P.tt("dve", tmax[:], red[:, :, 0], red[:, :, 1], ALU.add, [Br], [Br])
                    P.recip(tmax[:], tmax[:], [Br], [Br])
                    for tt in range(NT):
                        P.ts("dve", gate[:, tt, :], e1[:, tt, :], tmax[:, tt:tt + 1], None, ALU.mult, [Br], [Bgate])
                dump("d_gate", gate[:], [Bgate])
                if STOP_AFTER == "route":
                    return
                acc = P.sb("acc", [128, NT, D], F32)
                Bacc = bufs("acc", NT)
                with Scope(P):
                    Wg = [P.sb("Wg%d" % i, [128, KC, 512], BF16) for i in range(2)]
                    Wu = [P.sb("Wu%d" % i, [128, KC, 512], BF16) for i in range(2)]
                    Wd = [P.sb("Wd%d" % i, [128, 4, D], BF16) for i in range(2)]
                    BWe = bufs("We", 2)
                    actT = [P.sb("actT%d" % i, [128, 4, 512], BF16) for i in range(2)]
                    BactT = [bufs("actT%d_" % i, 4) for i in range(2)]
                    sgm = [P.sb("sgm%d" % i, [128, 512], BF16) for i in range(2)]
                    Bsgm = bufs("sgm", 2)
                    sgrot = Rot([0, 1])
                    hrot = Rot([0, 1, 2, 3])
                    orot = Rot([4, 5, 6, 7])
                    ai = 0
                    for e_ in range(16):
                        k = e_ % 2
                        P.ld("pool", Wg[k][:], dr["w_exp_gate"][l, e_].rearrange("(kc p) f -> p kc f", p=128), [], [BWe[k]])
                        P.ld("pool", Wu[k][:], dr["w_exp_up"][l, e_].rearrange("(kc p) f -> p kc f", p=128), [], [BWe[k]])
                        P.ld("pool", Wd[k][:], dr["w_exp_down"][l, e_].rearrange("(kc p) f -> p kc f", p=128), [], [BWe[k]])
                        for G in range(NG):
                            a = ai % 2
                            ai += 1
                            xr_ = [BxT[4 * G + j] for j in range(4)]
                            for fc in range(4):
                                hb = hrot.next()
                                for kc in range(KC):
                                    P.mm(banks[hb][:], Wg[k][:, kc, fc * 128:(fc + 1) * 128], xT[:, kc, gsl(G)], kc == 0, kc == KC - 1, [BWe[k]] + xr_, [Bbank[hb]])
                                ub = hrot.next()
                                for kc in range(KC):
                                    P.mm(banks[ub][:], Wu[k][:, kc, fc * 128:(fc + 1) * 128], xT[:, kc, gsl(G)], kc == 0, kc == KC - 1, [BWe[k]] + xr_, [Bbank[ub]])
                                sk = sgrot.next()
                                P.act(sgm[sk][:], banks[hb][:], AF.Silu, [Bbank[hb]], [Bsgm[sk]])
                                P.tt("dve", actT[a][:, fc, :], sgm[sk][:], banks[ub][:], ALU.mult, [Bsgm[sk], Bbank[ub]], [BactT[a][fc]])
                            for j in range(4):
                                tt = 4 * G + j
                                for half in range(2):
                                    ob = orot.next()
                                    for fc in range(4):
                                        P.mm(banks[ob][:], actT[a][:, fc, j * 128:(j + 1) * 128], Wd[k][:, fc, half * 512:(half + 1) * 512], fc == 0, fc == 3,
                                             [BactT[a][fc], BWe[k]], [Bbank[ob]])
                                    dst = acc[:, tt, half * 512:(half + 1) * 512]
                                    if e_ == 0:
                                        P.ts("dve", dst, banks[ob][:], gate[:, tt, e_:e_ + 1], None, ALU.mult, [Bbank[ob], Bgate], [Bacc[tt]])
                                    else:
                                        P.stt("dve", dst, banks[ob][:], gate[:, tt, e_:e_ + 1], dst, ALU.mult, ALU.add, [Bbank[ob], Bgate, Bacc[tt]], [Bacc[tt]])
                with Scope(P):
                    g2B = P.sb("g2B", [128, D], F32)
                    b2B = P.sb("b2B", [128, D], F32)
                    Bg2 = NB("g2b2")
                    P.ld("sp", g2B[:], dr["ln2_g"][l].partition_broadcast(128), [], [Bg2])
                    P.ld("sp", b2B[:], dr["ln2_b"][l].partition_broadcast(128), [], [Bg2])
                    xr = [P.sb("f_xr%d" % i, [128, D], F32) for i in range(4)]
                    Bxr = bufs("f_xr", 4)
                    st2 = P.sb("f_st", [128, 8, 4], F32)
                    Bsum2 = bufs("f_sum", 4)
                    Bsq2 = bufs("f_sq", 4)
                    Bvec2 = NB("f_vec")
                    junks = [P.sb("f_junk%d" % i, [128, D], BF16)[:] for i in range(2)]
                    Bjunks = bufs("f_junk", 2)
                    xbf = [P.sb("f_xbf%d" % i, [128, D], BF16)[:] for i in range(2)]
                    Bxbf = bufs("f_xbf", 2)
                    Bstk = bufs("f_stkey", 4)
                    for t0 in range(0, NT, 4):
                        items = []
                        for i in range(4):
                            tt = t0 + i
                            P.ld("sp", xr[i][:], xres[tsl(tt), :], [Bxres[tt]], [Bxr[i]])
                        for i in range(4):
                            tt = t0 + i
                            P.stt("dve", acc[:, tt, :], xr[i][:], ALPHA, acc[:, tt, :], ALU.mult, ALU.add, [Bxr[i], Bacc[tt]], [Bacc[tt]])
                            items.append((acc[:, tt, :], Bacc[tt]))
                        ln_batch(items, g2B[:], b2B[:], Bg2, st2, Bsum2, Bsq2, Bvec2, junks, Bjunks)
                        for i in range(4):
                            tt = t0 + i
                            kb = Bstk[i]
                            if last:
                                final_events.append(P.st("sp", y[tsl(tt), :], acc[:, tt, :], [Bacc[tt]], [kb], key=kb))
                            else:
                                P.st("sp", xres[tsl(tt), :], acc[:, tt, :], [Bacc[tt]], [Bxres[tt], kb], key=kb)
                        if not last:
                            to_xT_batch(items, [t0 + i for i in range(4)], xbf, Bxbf, [0, 1, 2, 3])

        for li, l in enumerate(layers):
            layer(l, li == len(layers) - 1)

        dump("d_xT", xT[:], BxT)
        P.finish(final_events)
        print("build: sems", P.nsem, "ops", {e: len(P.ops[e]) for e in P.ENG})
    return nc


STOP_AFTER = None
EXPER = None
SKIP_DSA = False
SKIP_MLA = False
_NB = {}


def NB(name):
    if name not in _NB:
        _NB[name] = Buf(name)
    return _NB[name]


def prep_inputs(inputs):
    f32 = np.float32
    common = {}
    for k in WEIGHT_SHAPES:
        a = np.ascontiguousarray(np.asarray(inputs[k], dtype=f32))
        if k == "a_q_ln_g":
            a = np.ascontiguousarray(a.reshape(2, 3, 128).transpose(0, 2, 1))
        if k == "a_kv_ln_g":
            a = np.ascontiguousarray(a.reshape(2, 2, 128).transpose(0, 2, 1))
        common[k] = a
    common.update(make_consts())
    return common


_PROG_CACHE = {}


def get_prog(key, *a, **kw):
    if key not in _PROG_CACHE:
        _NB.clear()
        _PROG_CACHE[key] = build_program(*a, **kw)
    return _PROG_CACHE[key]


FUSED = True


def kernel(**inputs):
    x = np.ascontiguousarray(np.asarray(inputs["x"], dtype=np.float32))
    B = x.shape[0]
    common = prep_inputs(inputs)
    cores = list(range(B))
    if FUSED:
        nc = get_prog("fused", [0, 1], True, True)
        maps = [dict(common, x=x[b]) for b in cores]
        res = run_bass_kernel_spmd(nc, maps, core_ids=cores)
        return np.stack([res.results[b]["y"] for b in cores], axis=0).astype(np.float32)
    nc0 = get_prog("l0", [0], True, True)
    maps = [dict(common, x=x[b]) for b in cores]
    res = run_bass_kernel_spmd(nc0, maps, core_ids=cores)
    mid = [res.results[b]["y"] for b in cores]
    nc1 = get_prog("l1", [1], False, True)
    maps = [dict(common, x=np.ascontiguousarray(mid[b])) for b in cores]
    res = run_bass_kernel_spmd(nc1, maps, core_ids=cores)
    return np.stack([res.results[b]["y"] for b in cores], axis=0).astype(np.float32)
```

```python
import contextlib
import numpy as np
import concourse.bass as bass
import concourse.mybir as mybir
from concourse.bass_utils import run_bass_kernel_spmd

F32 = mybir.dt.float32
BF16 = mybir.dt.bfloat16
AF = mybir.ActivationFunctionType
ALU = mybir.AluOpType
AX = mybir.AxisListType


GUARD = True


class Buf:
    __slots__ = ("name", "w", "r", "sem_in", "cnt_in", "sem_out", "cnt_out", "psum")

    def __init__(self, name):
        self.name = name
        self.w = None
        self.r = []
        self.sem_in = None
        self.cnt_in = 0
        self.sem_out = None
        self.cnt_out = 0
        self.psum = False


class Prog:
    ENG = ("pe", "act", "dve", "pool", "sp")

    def __init__(self, nc, es):
        self.nc = nc
        self.es = es
        self.root = es
        self.eng = {"pe": nc.tensor, "act": nc.scalar, "dve": nc.vector,
                    "pool": nc.gpsimd, "sp": nc.sync}
        self.ops = {e: [] for e in self.ENG}
        self.idx = {e: 0 for e in self.ENG}
        self.sem = {e: es.enter_context(nc.semaphore("s_" + e)) for e in self.ENG if e != "sp"}
        self.waited = {e: {} for e in self.ENG}
        self.nsem = 0
        self.uid = 0
        self.last_out_events = []
        self.pending = {e: {} for e in self.ENG}
        self.dma_since = {}
        self.guard = {}
        self.guard_hist = {"act": [], "dve": []}
        self.guard_src = None

    def new_sem(self, name):
        self.nsem += 1
        return self.root.enter_context(self.nc.semaphore("%s_%d" % (name, self.nsem)))

    def sb(self, name, shape, dt):
        self.uid += 1
        return self.es.enter_context(self.nc.sbuf_tensor("%s_%d" % (name, self.uid), list(shape), dt))

    def ps(self, name, shape, dt):
        self.uid += 1
        return self.es.enter_context(self.nc.psum_tensor("%s_%d" % (name, self.uid), list(shape), dt))

    def _collect(self, e, reads, writes, skip_sem=None):
        waits = {}

        def need(ev, raw, waw=False):
            if ev is None:
                return
            sem, val, ee, ii = ev
            if waw and skip_sem is not None and sem is skip_sem:
                return
            if ee == e and ii is not None:
                if e == "pe":
                    return
            k = id(sem)
            if self.waited[e].get(k, 0) >= val:
                return
            if k not in waits or waits[k][1] < val:
                waits[k] = (sem, val)

        for b in reads:
            need(b.w, True)
            if b.psum and e in ("act", "dve"):
                for r in b.r:
                    if r[2] != e:
                        need(r, True)
        for b in writes:
            need(b.w, True, True)
            for r in b.r:
                if GUARD and b.psum and e == "pe" and r[3] is not None and r[2] in ("act", "dve"):
                    r = self._guarded(r)
                need(r, False)
        if self.pending[e]:
            for k, (sem, val) in self.pending[e].items():
                if self.waited[e].get(k, 0) >= val:
                    continue
                if k not in waits or waits[k][1] < val:
                    waits[k] = (sem, val)
            self.pending[e] = {}
        for k, (sem, val) in waits.items():
            self.waited[e][k] = val
        return list(waits.values())

    def _commit(self, ev, reads, writes):
        for b in reads:
            b.r.append(ev)
        for b in writes:
            b.w = ev
            b.r = []

    def _guarded(self, r):
        sem, val, E, ii = r
        if self.idx[E] > ii + 1:
            return (sem, ii + 2, E, ii + 1)
        hist = self.guard_hist[E]
        n = len(hist)
        g = self.guard[E][:, (n % 8):(n % 8) + 1]
        gw = []
        if n >= 8:
            k = id(self.sem[E])
            need = hist[n - 8] + 1
            if self.waited[E].get(k, 0) < need:
                gw.append((self.sem[E], need))
                self.waited[E][k] = need
        if E == "act":
            src, bsrc = self.guard_src
            sv = bsrc.w
            if sv is not None and self.waited[E].get(id(sv[0]), 0) < sv[1]:
                gw.append((sv[0], sv[1]))
                self.waited[E][id(sv[0])] = sv[1]
        i2 = self.idx[E]
        self.idx[E] = i2 + 1
        hist.append(i2)
        if E == "act":
            self.ops[E].append((gw, (lambda en, g=g, src=src: en.activation(out=g, in_=src, func=AF.Copy)), (self.sem[E], 1)))
        else:
            self.ops[E].append((gw, (lambda en, g=g: en.memset(g, 0.0)), (self.sem[E], 1)))
        return (self.sem[E], i2 + 1, E, i2)

    def op(self, e, fn, reads=(), writes=()):
        waits = self._collect(e, reads, writes)
        i = self.idx[e]
        self.idx[e] = i + 1
        ev = (self.sem[e], i + 1, e, i)
        self.ops[e].append((waits, fn, (self.sem[e], 1)))
        self._commit(ev, reads, writes)
        return ev

    def dma(self, e, fn, reads=(), writes=(), key=None):
        if key is None:
            key = writes[0] if writes else reads[0]
        if key.sem_in is None:
            key.sem_in = {}
        if e not in key.sem_in:
            key.sem_in[e] = [self.new_sem("d%s_%s" % (e, key.name)), 0]
        ent = key.sem_in[e]
        sem = ent[0]
        waits = self._collect(e, reads, writes, skip_sem=sem)
        ent[1] += 16
        val = ent[1]
        ev = (sem, val, e, None)
        self.dma_since[id(sem)] = (sem, val)
        self.ops[e].append((waits, fn, (sem, 16)))
        self._commit(ev, reads, writes)
        return ev

    def mm(self, out, lhsT, rhs, start, stop, reads, writes, sgc=False):
        if sgc:
            return self.op("pe", lambda e: e.matmul(out, lhsT=lhsT, rhs=rhs, start=start, stop=stop, skip_group_check=True), reads, writes)
        return self.op("pe", lambda e: e.matmul(out, lhsT=lhsT, rhs=rhs, start=start, stop=stop), reads, writes)

    def tr(self, out, in_, ident, reads, writes):
        return self.op("pe", lambda e: e.transpose(out, in_, ident), reads, writes)

    def act(self, out, in_, func, reads, writes, bias=None, scale=None, accum=None):
        kw = {}
        if bias is not None:
            kw["bias"] = bias
        if scale is not None:
            kw["scale"] = scale
        if accum is not None:
            kw["accum_out"] = accum
        return self.op("act", lambda e: e.activation(out=out, in_=in_, func=func, **kw), reads, writes)

    def ts(self, eng, out, in0, s1, s2, op0, reads, writes, op1=None, accum=None):
        kw = {}
        if op1 is not None:
            kw["op1"] = op1
        if accum is not None:
            kw["accum_out"] = accum
        return self.op(eng, lambda e: e.tensor_scalar(out=out, in0=in0, scalar1=s1, scalar2=s2, op0=op0, **kw), reads, writes)

    def tt(self, eng, out, in0, in1, op, reads, writes):
        return self.op(eng, lambda e: e.tensor_tensor(out=out, in0=in0, in1=in1, op=op), reads, writes)

    def stt(self, eng, out, in0, scalar, in1, op0, op1, reads, writes):
        return self.op(eng, lambda e: e.scalar_tensor_tensor(out=out, in0=in0, scalar=scalar, in1=in1, op0=op0, op1=op1), reads, writes)

    def cp(self, eng, out, in_, reads, writes):
        if eng == "act":
            return self.op("act", lambda e: e.activation(out=out, in_=in_, func=AF.Copy), reads, writes)
        return self.op(eng, lambda e: e.tensor_copy(out=out, in_=in_), reads, writes)

    def memset(self, eng, ap, val, writes):
        return self.op(eng, lambda e: e.memset(ap, val), (), writes)

    def recip(self, out, in_, reads, writes):
        return self.op("dve", lambda e: e.reciprocal(out=out, in_=in_), reads, writes)

    def ld(self, q, out, in_, reads, writes):
        return self.dma(q, lambda e: e.dma_start(out=out, in_=in_), reads, writes)

    def st(self, q, out, in_, reads, writes, key):
        return self.dma(q, lambda e: e.dma_start(out=out, in_=in_), reads, writes, key=key)

    def fence(self):
        for e in self.ENG:
            pend = self.pending[e]
            for f in self.ENG:
                if f == "sp" or f == e or self.idx[f] == 0:
                    continue
                k = id(self.sem[f])
                pend[k] = (self.sem[f], self.idx[f])
            for k, sv in self.dma_since.items():
                if k not in pend or pend[k][1] < sv[1]:
                    pend[k] = sv
        self.dma_since = {}

    def finish(self, final_events):
        nc = self.nc
        ops = self.ops
        with nc.Block() as block:
            def emit(engname, eng):
                for waits, fn, (sem, inc) in ops[engname]:
                    for (s, v) in waits:
                        eng.wait_ge(s, v)
                    ins = fn(eng)
                    ins.then_inc(sem, inc)

            @block.tensor
            def _(eng):
                emit("pe", eng)

            @block.scalar
            def _(eng):
                emit("act", eng)

            @block.vector
            def _(eng):
                emit("dve", eng)

            @block.gpsimd
            def _(eng):
                emit("pool", eng)

            @block.sync
            def _(eng):
                emit("sp", eng)
                for (s, v, _e, _i) in final_events:
                    eng.wait_ge(s, v)

T = 2048
D = 1024
NT = 16
NG = 4
KC = 8
LN_EPS = 1e-5
RMS_EPS = 1e-6
DEPTH = 2
ALPHA = (2.0 * DEPTH) ** 0.25
IN_COLS = 5604
VP = 68
NIT = 16
W0 = 16.0


def make_consts():
    f32 = np.float32
    c = {}
    c["c_ident"] = np.eye(128, dtype=f32)
    c["c_ones"] = np.ones((128, 128), f32)
    t = np.arange(T, dtype=f32)
    p = np.arange(128)
    i64 = ((p % 64) % 32).astype(f32)
    inv64 = np.power(f32(10000.0), -(i64 / f32(32.0))).astype(f32)
    ang = (t[None, :] * inv64[:, None]).astype(f32).astype(np.float64)
    c["c_cos64"] = np.cos(ang).astype(f32)
    c["c_sin64"] = np.sin(ang).astype(f32)
    iA = ((p - 64) % 16).astype(f32)
    invA = np.power(f32(10000.0), -(iA / f32(16.0))).astype(f32)
    angA = (t[None, :] * invA[:, None]).astype(f32).astype(np.float64)
    c["c_cosA"] = np.cos(angA).astype(f32)
    c["c_sinA"] = np.sin(angA).astype(f32)
    pm = np.zeros((128, 128), f32)
    for fp in range(128):
        d = fp % 64
        if d < 32:
            pm[fp + 32, fp] = -1.0
        else:
            pm[fp - 32, fp] = 1.0
    c["c_pm64"] = pm
    pa = np.zeros((128, 128), f32)
    for fp in range(64, 96):
        d = fp - 64
        if d < 16:
            pa[fp + 16, fp] = -1.0
        else:
            pa[fp - 16, fp] = 1.0
    c["c_pmA"] = pa
    s = np.arange(128)[:, None]
    q = np.arange(128)[None, :]
    c["c_diag"] = ((s < 64) | (q >= 64)).astype(f32)
    c["c_swaprev"] = (~((s < 64) & (q >= 64))).astype(f32)
    tt_ = np.arange(128)[:, None]
    ss_ = np.arange(128)[None, :]
    c["c_negdiag"] = np.where((tt_ < 64) & (ss_ >= 64), f32(-1e30), f32(0.0)).astype(f32)
    c["c_negprev"] = np.where((tt_ >= 64) & (ss_ < 64), f32(-1e30), f32(0.0)).astype(f32)
    return c


CONST_SHAPES = {"c_ident": [128, 128], "c_ones": [128, 128], "c_cos64": [128, T], "c_sin64": [128, T],
                "c_cosA": [128, T], "c_sinA": [128, T], "c_pm64": [128, 128], "c_pmA": [128, 128],
                "c_diag": [128, 128], "c_swaprev": [128, 128], "c_negdiag": [128, 128], "c_negprev": [128, 128]}

WEIGHT_SHAPES = {
    "ln_in_g": [D], "ln_in_b": [D], "w_in": [2, D, IN_COLS],
    "a_q_ln_g": [2, 128, 3], "a_kv_ln_g": [2, 128, 2],
    "a_w_uq": [2, 384, 768], "a_w_ukv": [2, 256, 1024], "c_sinks": [2, 8],
    "w_br_a": [2, 512, D], "w_br_b": [2, 512, D], "w_br_c": [2, 512, D], "w_out": [2, D, D],
    "ln1_g": [2, D], "ln1_b": [2, D], "w_router": [D, 16], "router_bias": [16],
    "w_exp_gate": [2, 16, D, 512], "w_exp_up": [2, 16, D, 512], "w_exp_down": [2, 16, 512, D],
    "ln2_g": [2, D], "ln2_b": [2, D],
}


class Scope:
    def __init__(self, P):
        self.P = P

    def __enter__(self):
        self.prev = self.P.es
        self.stack = contextlib.ExitStack()
        self.stack.__enter__()
        self.P.es = self.stack
        return self

    def __exit__(self, *a):
        self.P.es = self.prev
        self.P.fence()
        return self.stack.__exit__(*a)


def bufs(name, n):
    return [NB("%s%d" % (name, i)) for i in range(n)]


def build_program(layers, do_ln_in, final, dbg_names=()):
    nc = bass.Bass("TRN2", target_bir_lowering=False)
    dr = {}
    dr["x"] = nc.dram_tensor("x", [T, D], F32, kind="ExternalInput").ap()
    for k, shp in WEIGHT_SHAPES.items():
        dr[k] = nc.dram_tensor(k, shp, F32, kind="ExternalInput").ap()
    for k, shp in CONST_SHAPES.items():
        dr[k] = nc.dram_tensor(k, shp, F32, kind="ExternalInput").ap()
    y = nc.dram_tensor("y", [T, D], F32, kind="ExternalOutput").ap()
    xres = nc.dram_tensor("xres", [T, D], F32).ap()
    dbg = {}
    DBG_SHAPES = {"d_xT": ([128, KC, T], BF16), "d_oTa": ([128, 4, T], BF16), "d_oTb": ([128, 4, T], BF16),
                  "d_oTc": ([128, 4, T], BF16), "d_gate": ([128, NT, 16], F32)}
    for k in dbg_names:
        shp, dt_ = DBG_SHAPES[k]
        dbg[k] = nc.dram_tensor(k, shp, dt_, kind="ExternalOutput").ap()

    es = contextlib.ExitStack()
    with es:
        P = Prog(nc, es)
        final_events = []
        banks = [P.ps("bank%d" % i, [128, 512], F32) for i in range(8)]
        Bbank = bufs("bank", 8)
        for b_ in Bbank:
            b_.psum = True
        P.guard["act"] = P.sb("guard_act", [128, 8], F32)[:]
        P.guard["dve"] = P.sb("guard_dve", [128, 8], F32)[:]
        xT = P.sb("xT", [128, KC, T], BF16)
        BxT = bufs("xT", NT)
        Bxres = bufs("xres", NT)
        Bc = NB("consts")
        ident_bf = P.sb("ident_bf", [128, 128], BF16)
        ident_f = P.sb("ident_f", [128, 128], F32)
        ones_bf = P.sb("ones_bf", [128, 128], BF16)
        pm64 = P.sb("pm64", [128, 128], BF16)
        pmA = P.sb("pmA", [128, 128], BF16)
        diag_bf = P.sb("diag_bf", [128, 128], BF16)
        swaprev_bf = P.sb("swaprev_bf", [128, 128], BF16)
        negdiag = P.sb("negdiag", [128, 128], F32)
        negdiag_bf = P.sb("negdiag_bf", [128, 128], BF16)
        negprev_bf = P.sb("negprev_bf", [128, 128], BF16)
        P.ld("pool", ident_bf[:], dr["c_ident"], [], [Bc])
        P.ld("pool", ones_bf[:], dr["c_ones"], [], [Bc])
        P.ld("pool", pm64[:], dr["c_pm64"], [], [Bc])
        P.ld("pool", pmA[:], dr["c_pmA"], [], [Bc])
        P.ld("pool", diag_bf[:], dr["c_diag"], [], [Bc])
        P.ld("pool", swaprev_bf[:], dr["c_swaprev"], [], [Bc])
        P.ld("pool", negdiag_bf[:], dr["c_negdiag"], [], [Bc])
        P.ld("pool", negprev_bf[:], dr["c_negprev"], [], [Bc])
        P.ld("sp", ident_f[:], dr["c_ident"], [], [Bc])
        P.ld("sp", negdiag[:], dr["c_negdiag"], [], [Bc])
        P.guard_src = (ident_f[:, 0:1], Bc)

        def gsl(G):
            return slice(G * 512, (G + 1) * 512)

        def tsl(tt):
            return slice(tt * 128, (tt + 1) * 128)

        class Rot:
            def __init__(self, items):
                self.items = items
                self.i = 0

            def next(self):
                it = self.items[self.i % len(self.items)]
                self.i += 1
                return it

        def dump(name, ap_sb, rbufs):
            if name in dbg:
                final_events.append(P.st("sp", dbg[name], ap_sb, rbufs, [], key=NB("dbg_" + name)))

        def ln_inplace(t_ap, Bt, gB, bB, Bgb, st, Bst, junk, Bjunk):
            P.act(junk, t_ap, AF.Copy, [Bt], [Bjunk, Bst], accum=st[:, 0:1])
            P.act(junk, t_ap, AF.Square, [Bt], [Bjunk, Bst], accum=st[:, 1:2])
            P.ts("dve", st[:, 2:3], st[:, 0:1], 1.0 / D, None, ALU.mult, [Bst], [Bst])
            P.tt("dve", st[:, 3:4], st[:, 2:3], st[:, 2:3], ALU.mult, [Bst], [Bst])
            P.stt("dve", st[:, 4:5], st[:, 1:2], 1.0 / D, st[:, 3:4], ALU.mult, ALU.subtract, [Bst], [Bst])
            P.ts("dve", st[:, 4:5], st[:, 4:5], LN_EPS, None, ALU.add, [Bst], [Bst])
            P.act(st[:, 5:6], st[:, 4:5], AF.Sqrt, [Bst], [Bst])
            P.recip(st[:, 6:7], st[:, 5:6], [Bst], [Bst])
            P.stt("dve", st[:, 7:8], st[:, 2:3], -1.0, st[:, 6:7], ALU.mult, ALU.mult, [Bst], [Bst])
            P.act(t_ap, t_ap, AF.Identity, [Bt, Bst], [Bt], scale=st[:, 6:7], bias=st[:, 7:8])
            P.tt("dve", t_ap, t_ap, gB, ALU.mult, [Bt, Bgb], [Bt])
            P.tt("dve", t_ap, t_ap, bB, ALU.add, [Bt, Bgb], [Bt])

        def to_xT(t_ap, Bt, tt, xbf, Bxbf, bk):
            P.cp("act", xbf, t_ap, [Bt], [Bxbf])
            bv = banks[bk][:].bitcast(BF16)
            for kc in range(KC):
                P.tr(bv[:, kc * 128:(kc + 1) * 128], xbf[:, kc * 128:(kc + 1) * 128], ident_bf[:],
                     [Bxbf, Bc], [Bbank[bk]])
            P.cp("dve", xT[:, :, tsl(tt)], bv.rearrange("p (k t) -> p k t", t=128), [Bbank[bk]], [BxT[tt]])

        def ln_batch(items, gB, bB, Bgb, st, Bsum, Bsq, Bvec, junks, Bjunks):
            nb = len(items)
            for i, (t_ap, Bt) in enumerate(items):
                P.act(junks[i % 2], t_ap, AF.Copy, [Bt], [Bjunks[i % 2], Bsum[i]], accum=st[:, 0, i:i + 1])
            for i, (t_ap, Bt) in enumerate(items):
                P.act(junks[i % 2], t_ap, AF.Square, [Bt], [Bjunks[i % 2], Bsq[i]], accum=st[:, 1, i:i + 1])
            V = [Bvec]
            P.ts("dve", st[:, 2, 0:nb], st[:, 0, 0:nb], 1.0 / D, None, ALU.mult, Bsum[0:nb], V)
            P.tt("dve", st[:, 3, 0:nb], st[:, 2, 0:nb], st[:, 2, 0:nb], ALU.mult, V, V)
            P.stt("dve", st[:, 4, 0:nb], st[:, 1, 0:nb], 1.0 / D, st[:, 3, 0:nb], ALU.mult, ALU.subtract, Bsq[0:nb] + V, V)
            P.ts("dve", st[:, 4, 0:nb], st[:, 4, 0:nb], LN_EPS, None, ALU.add, V, V)
            P.act(st[:, 5, 0:nb], st[:, 4, 0:nb], AF.Sqrt, V, V)
            P.recip(st[:, 6, 0:nb], st[:, 5, 0:nb], V, V)
            P.stt("dve", st[:, 7, 0:nb], st[:, 2, 0:nb], -1.0, st[:, 6, 0:nb], ALU.mult, ALU.mult, V, V)
            for i, (t_ap, Bt) in enumerate(items):
                P.act(t_ap, t_ap, AF.Identity, [Bt, Bvec], [Bt], scale=st[:, 6, i:i + 1], bias=st[:, 7, i:i + 1])
            for i, (t_ap, Bt) in enumerate(items):
                P.tt("dve", t_ap, t_ap, gB, ALU.mult, [Bt, Bgb], [Bt])
            for i, (t_ap, Bt) in enumerate(items):
                P.tt("dve", t_ap, t_ap, bB, ALU.add, [Bt, Bgb], [Bt])

        def to_xT_batch(items, tts, xbfs, Bxbfs, bks):
            for i, (t_ap, Bt) in enumerate(items):
                P.cp("act", xbfs[i % len(xbfs)], t_ap, [Bt], [Bxbfs[i % len(xbfs)]])
                bk = bks[i % len(bks)]
                bv = banks[bk][:].bitcast(BF16)
                xb = xbfs[i % len(xbfs)]
                for kc in range(KC):
                    P.tr(bv[:, kc * 128:(kc + 1) * 128], xb[:, kc * 128:(kc + 1) * 128], ident_bf[:],
                         [Bxbfs[i % len(xbfs)], Bc], [Bbank[bk]])
                P.cp("dve", xT[:, :, tsl(tts[i])], bv.rearrange("p (k t) -> p k t", t=128), [Bbank[bk]], [BxT[tts[i]]])

        with Scope(P):
            gB = P.sb("ln0_g", [128, D], F32)
            bB = P.sb("ln0_b", [128, D], F32)
            Bgb = NB("ln0gb")
            if do_ln_in:
                P.ld("sp", gB[:], dr["ln_in_g"].partition_broadcast(128), [], [Bgb])
                P.ld("sp", bB[:], dr["ln_in_b"].partition_broadcast(128), [], [Bgb])
            xt = [P.sb("in_x%d" % i, [128, D], F32) for i in range(4)]
            Bxt = bufs("in_x", 4)
            stt_ = P.sb("in_st", [128, 8, 4], F32)
            Bsum = bufs("in_sum", 4)
            Bsq = bufs("in_sq", 4)
            Bvec = NB("in_vec")
            junks = [P.sb("in_junk%d" % i, [128, D], BF16)[:] for i in range(2)]
            Bjunks = bufs("in_junk", 2)
            xbf = [P.sb("in_xbf%d" % i, [128, D], BF16)[:] for i in range(2)]
            Bxbf = bufs("in_xbf", 2)
            for t0 in range(0, NT, 4):
                items = []
                for i in range(4):
                    P.ld("sp", xt[i][:], dr["x"][tsl(t0 + i), :], [], [Bxt[i]])
                    items.append((xt[i][:], Bxt[i]))
                if do_ln_in:
                    ln_batch(items, gB[:], bB[:], Bgb, stt_, Bsum, Bsq, Bvec, junks, Bjunks)
                for i in range(4):
                    P.st("sp", xres[tsl(t0 + i), :], xt[i][:], [Bxt[i]], [Bxres[t0 + i]], key=Bxt[i])
                to_xT_batch(items, [t0 + i for i in range(4)], xbf, Bxbf, [0, 1, 2, 3])

        def load_w(q, dst_ap, src_ap, Bw):
            P.ld(q, dst_ap, src_ap, [], [Bw])

        def proj_fm(lhsT_of_kc, M, G, bk, wr):
            for kc in range(KC):
                P.mm(banks[bk][0:M, :], lhsT_of_kc(kc), xT[:, kc, gsl(G)], kc == 0, kc == KC - 1,
                     [BxT[4 * G + j] for j in range(4)] + wr, [Bbank[bk]])

        def layer(l, last):
            w_in_v = dr["w_in"][l].rearrange("(kc p) c -> p kc c", p=128)
            logit = P.sb("logit", [128, NT, 16], F32)
            Blogit = bufs("logit", NT)
            gate = P.sb("gate", [128, NT, 16], F32)
            Bgate = NB("gate")
            with Scope(P):
                oT = [P.sb("oT%d" % i, [128, 4, T], BF16) for i in range(3)]
                BoT = [bufs("oT%d_" % i, NG) for i in range(3)]
                with Scope(P):
                    aqn = P.sb("aqn", [128, 3, T], BF16)
                    Baqn = bufs("aqn", NG)
                    akvn = P.sb("akvn", [128, 2, T], BF16)
                    Bakvn = bufs("akvn", NG)
                    kpe = P.sb("kpe", [96, T], BF16)
                    Bkpe = bufs("kpe", NG)
                    cosA = P.sb("cosA", [128, T], F32)
                    sinA = P.sb("sinA", [128, T], F32)
                    Btab = NB("tabA")
                    P.ld("sp", cosA[:], dr["c_cosA"], [], [Btab])
                    P.ld("sp", sinA[:], dr["c_sinA"], [], [Btab])
                    wuq = P.sb("wuq", [128, 3, 768], BF16)
                    wukv = P.sb("wukv", [128, 2, 1024], BF16)
                    Bwu = NB("wu")
                    P.ld("pool", wuq[:], dr["a_w_uq"][l].rearrange("(kc p) c -> p kc c", p=128), [], [Bwu])
                    P.ld("pool", wukv[:], dr["a_w_ukv"][l].rearrange("(kc p) c -> p kc c", p=128), [], [Bwu])
                    t1 = [P.sb("a_t1_%d" % i, [128, 512], F32) for i in range(2)]
                    t2 = [P.sb("a_t2_%d" % i, [128, 512], F32) for i in range(2)]
                    Bt1 = bufs("a_t1", 2)
                    Bt2 = bufs("a_t2", 2)
                    trot = Rot([0, 1])
                    with Scope(P):
                        Wa = P.sb("Wa", [128, KC, 672], BF16)
                        BWa = NB("Wa")
                        P.ld("pool", Wa[:], w_in_v[:, :, 0:672], [], [BWa])
                        gq = P.sb("gq", [128, 3], F32)
                        gkv = P.sb("gkv", [128, 2], F32)
                        Bg = NB("gqkv")
                        P.ld("sp", gq[:], dr["a_q_ln_g"][l], [], [Bg])
                        P.ld("sp", gkv[:], dr["a_kv_ln_g"][l], [], [Bg])
                        sq = P.sb("sq", [128, 3, 512], BF16)
                        Bsq = bufs("sq", 3)
                        rs = P.sb("rs", [128, 512], F32)
                        Brs = NB("rs")
                        kraw = P.sb("kraw", [96, 512], BF16)
                        Bkraw = NB("kraw")
                        P.memset("dve", kraw[:], 0.0, [Bkraw])
                        brot = Rot([0, 1, 2, 3])
                        for G in range(NG):
                            for (dst, Bdst, nch, col0, g_ap, nfeat) in ((aqn, Baqn, 3, 0, gq, 384.0), (akvn, Bakvn, 2, 384, gkv, 256.0)):
                                for c in range(nch):
                                    bk = brot.next()
                                    proj_fm(lambda kc, c=c, col0=col0: Wa[:, kc, col0 + c * 128: col0 + (c + 1) * 128], 128, G, bk, [BWa])
                                    P.cp("act", dst[:, c, gsl(G)], banks[bk][:], [Bbank[bk]], [Bdst[G]])
                                    P.act(sq[:, c, :], banks[bk][:], AF.Square, [Bbank[bk]], [Bsq[c]])
                                bk = brot.next()
                                for c in range(nch):
                                    P.mm(banks[bk][:], ones_bf[:], sq[:, c, :], c == 0, c == nch - 1, [Bc, Bsq[c]], [Bbank[bk]])
                                P.ts("dve", rs[:], banks[bk][:], 1.0 / nfeat, RMS_EPS, ALU.mult, [Bbank[bk]], [Brs], op1=ALU.add)
                                P.act(rs[:], rs[:], AF.Sqrt, [Brs], [Brs])
                                P.recip(rs[:], rs[:], [Brs], [Brs])
                                for c in range(nch):
                                    P.stt("dve", dst[:, c, gsl(G)], dst[:, c, gsl(G)], g_ap[:, c:c + 1], rs[:], ALU.mult, ALU.mult,
                                          [Bdst[G], Bg, Brs], [Bdst[G]])
                            bk = brot.next()
                            proj_fm(lambda kc: Wa[:, kc, 576:672], 96, G, bk, [BWa])
                            P.cp("act", kraw[64:96, :], banks[bk][64:96, :], [Bbank[bk]], [Bkraw])
                            bk2 = brot.next()
                            P.mm(banks[bk2][0:96, :], pmA[0:96, 0:96], kraw[0:96, :], True, True, [Bc, Bkraw], [Bbank[bk2]])
                            k = trot.next()
                            P.tt("dve", t1[k][64:96, :], kraw[64:96, :], cosA[64:96, gsl(G)], ALU.mult, [Bkraw, Btab], [Bt1[k]])
                            P.tt("dve", t2[k][64:96, :], banks[bk2][64:96, :], sinA[64:96, gsl(G)], ALU.mult, [Bbank[bk2], Btab], [Bt2[k]])
                            P.tt("dve", kpe[64:96, gsl(G)], t1[k][64:96, :], t2[k][64:96, :], ALU.add, [Bt1[k], Bt2[k]], [Bkpe[G]])
                    Va = P.sb("Va", [128, NT, 8, VP], BF16)
                    BVa = bufs("Va", NT)
                    BVa1 = NB("Va_ones")
                    P.memset("dve", Va[:, :, :, 64:VP], 1.0, [BVa1])
                    wukv_v = wukv[:].rearrange("p k (h d) -> p k h d", d=128)
                    brot = Rot([5, 6, 7])
                    for tt in range(NT):
                        bk = brot.next()
                        for kc in range(2):
                            P.mm(banks[bk][:].rearrange("p (h d) -> p h d", d=64), akvn[:, kc, tsl(tt)], wukv_v[:, kc, :, 64:128],
                                 kc == 0, kc == 1, [Bakvn[tt // 4], Bwu], [Bbank[bk]])
                        P.cp("act", Va[:, tt, :, 0:64], banks[bk][:].rearrange("p (h d) -> p h d", d=64), [Bbank[bk]], [BVa[tt]])
                    QT = [P.sb("QTa%d" % i, [128, T], BF16) for i in range(2)]
                    KT = [P.sb("KTa%d" % i, [128, T], BF16) for i in range(2)]
                    BQT = [bufs("QTa%d_" % i, NG) for i in range(2)]
                    BKT = [bufs("KTa%d_" % i, NG) for i in range(2)]
                    Bzp = NB("a_zpad")
                    for i in range(2):
                        P.memset("dve", QT[i][96:128, :], 0.0, [Bzp])
                        P.memset("dve", KT[i][96:128, :], 0.0, [Bzp])
                    pt = [P.sb("a_pt%d" % i, [128, 512], BF16) for i in range(3)]
                    Bpt = bufs("a_pt", 3)
                    ptrot = Rot([0, 1, 2])
                    srot = Rot([0, 1, 2])
                    arot = Rot([3, 4])
                    otok = [P.sb("a_otok%d" % i, [128, NT, 128], BF16) for i in range(2)]
                    Botok = [bufs("a_otok%d_" % i, NT) for i in range(2)]
                    rec = [P.sb("a_rec%d" % i, [128, 4], F32) for i in range(2)]
                    Brec = bufs("a_rec", 2)
                    recrot = Rot([0, 1])
                    sc_a = 96.0 ** -0.5
                    def proj_head(h):
                        hp = h % 2
                        for G in range(NG):
                            bk = brot.next()
                            for kc in range(3):
                                P.mm(banks[bk][0:96, :], wuq[:, kc, h * 96:(h + 1) * 96], aqn[:, kc, gsl(G)], kc == 0, kc == 2,
                                     [Bwu, Baqn[G]], [Bbank[bk]])
                            P.cp("act", QT[hp][0:96, gsl(G)], banks[bk][0:96, :], [Bbank[bk]], [BQT[hp][G]])
                            bk2 = brot.next()
                            P.mm(banks[bk2][0:96, :], pmA[0:96, 0:96], QT[hp][0:96, gsl(G)], True, True, [Bc, BQT[hp][G]], [Bbank[bk2]])
                            k = trot.next()
                            P.tt("dve", t1[k][64:96, :], QT[hp][64:96, gsl(G)], cosA[64:96, gsl(G)], ALU.mult, [BQT[hp][G], Btab], [Bt1[k]])
                            P.tt("dve", t2[k][64:96, :], banks[bk2][64:96, :], sinA[64:96, gsl(G)], ALU.mult, [Bbank[bk2], Btab], [Bt2[k]])
                            P.tt("dve", QT[hp][64:96, gsl(G)], t1[k][64:96, :], t2[k][64:96, :], ALU.add, [Bt1[k], Bt2[k]], [BQT[hp][G]])
                            bk = brot.next()
                            for kc in range(2):
                                P.mm(banks[bk][0:64, :], wukv[:, kc, h * 128:h * 128 + 64], akvn[:, kc, gsl(G)], kc == 0, kc == 1,
                                     [Bwu, Bakvn[G]], [Bbank[bk]])
                            P.cp("act", KT[hp][0:64, gsl(G)], banks[bk][0:64, :], [Bbank[bk]], [BKT[hp][G]])
                            P.cp("dve", KT[hp][64:96, gsl(G)], kpe[64:96, gsl(G)], [Bkpe[G]], [BKT[hp][G]])

                    blocks = [(G, st_) for G in range(NG) for st_ in range(4 * G + 4)]
                    proj_head(0)
                    for h in range(8):
                        hp = h % 2
                        cpair = h // 2
                        if h + 1 < 8:
                            proj_head(h + 1)
                        info = {}

                        def qk(k):
                            G, st_ = blocks[k]
                            j0 = max(0, st_ - 4 * G)
                            ncol = (4 - j0) * 128
                            q0 = (4 * G + j0) * 128
                            sb_ = srot.next()
                            P.mm(banks[sb_][:, 0:ncol], KT[hp][:, tsl(st_)], QT[hp][:, q0:q0 + ncol], True, True,
                                 [BKT[hp][st_ // 4], BQT[hp][G], Bzp], [Bbank[sb_]])
                            if st_ >= 4 * G:
                                P.mm(banks[sb_][:, 0:128], negdiag_bf[:], ident_bf[:], False, True, [Bc], [Bbank[sb_]], sgc=True)
                            info[k] = (sb_, j0, ncol)

                        qk(0)
                        ab = None
                        for k, (G, st_) in enumerate(blocks):
                            if k + 1 < len(blocks):
                                qk(k + 1)
                            sb_, j0, ncol = info[k]
                            if st_ == 0:
                                ab = arot.next()
                            accv = banks[ab][:, 0:4 * VP].rearrange("p (j d) -> p j d", d=VP)
                            pk = ptrot.next()
                            P.act(pt[pk][:, 0:ncol], banks[sb_][:, 0:ncol], AF.Exp, [Bbank[sb_]], [Bpt[pk]], scale=sc_a)
                            for j in range(j0, 4):
                                P.mm(accv[:, j, 0:65], pt[pk][:, (j - j0) * 128:(j - j0 + 1) * 128], Va[:, st_, h, 0:65],
                                     (st_ == 0 and j == 0), st_ == 4 * G + j, [Bpt[pk], BVa[st_], BVa1], [Bbank[ab]], sgc=True)
                            if st_ == 4 * G + 3:
                                rk = recrot.next()
                                P.recip(rec[rk][:], accv[:, :, 64], [Bbank[ab]], [Brec[rk]])
                                for j in range(4):
                                    tt = 4 * G + j
                                    P.ts("dve", otok[cpair % 2][:, tt, hp * 64:(hp + 1) * 64], accv[:, j, 0:64], rec[rk][:, j:j + 1], None, ALU.mult,
                                         [Bbank[ab], Brec[rk]], [Botok[cpair % 2][tt]])
                        if hp == 1:
                            for t0 in range(0, NT, 8):
                                bk = brot.next()
                                bv = banks[bk][:].bitcast(BF16)
                                for tt in range(t0, t0 + 8):
                                    P.tr(bv[:, (tt - t0) * 128:(tt - t0 + 1) * 128], otok[cpair % 2][:, tt, :], ident_bf[:],
                                         [Botok[cpair % 2][tt], Bc], [Bbank[bk]])
                                P.cp("act", oT[0][:, cpair, t0 * 128:(t0 + 8) * 128], bv[:], [Bbank[bk]], [BoT[0][t0 // 4], BoT[0][t0 // 4 + 1]])
                dump("d_oTa", oT[0][:], BoT[0])
                if STOP_AFTER == "mla":
                    return

                def hd64_proj(name, col_q, col_k, col_v, QTx, BQTx, KTx, BKTx, Vx, BVx, extra=None, kz=None):
                    with Scope(P):
                        cos64 = P.sb(name + "cos", [128, T], F32)
                        sin64 = P.sb(name + "sin", [128, T], F32)
                        Btab = NB(name + "tab")
                        P.ld("sp", cos64[:], dr["c_cos64"], [], [Btab])
                        P.ld("sp", sin64[:], dr["c_sin64"], [], [Btab])
                        Wq = P.sb(name + "Wq", [128, KC, 512], BF16)
                        Wk2 = P.sb(name + "Wk2", [128, KC, 2, 128], BF16)
                        Wv = P.sb(name + "Wv", [128, KC, 128], BF16)
                        BW = NB(name + "W")
                        P.ld("pool", Wq[:], w_in_v[:, :, col_q:col_q + 512], [], [BW])
                        for g in range(2):
                            for i in range(2):
                                P.ld("pool", Wk2[:, :, g, i * 64:(i + 1) * 64], w_in_v[:, :, col_k + g * 64: col_k + (g + 1) * 64], [], [BW])
                        P.ld("pool", Wv[:], w_in_v[:, :, col_v:col_v + 128], [], [BW])
                        chunks = []
                        for c in range(4):
                            chunks.append((lambda kc, c=c: Wq[:, kc, c * 128:(c + 1) * 128], lambda G, c=c: QTx[:, c, gsl(G)], BQTx))
                        for g in range(2):
                            chunks.append((lambda kc, g=g: Wk2[:, kc, g, :], (lambda G, g=g: KTx[:, g, gsl(G)]) if kz is None else ("kz", g), BKTx))
                        xw = None
                        if extra is not None:
                            xw = extra(BW, chunks)
                        raw = [P.sb(name + "raw%d" % i, [128, 512], BF16) for i in range(2)]
                        Braw = bufs(name + "raw", 2)
                        t1 = [P.sb(name + "t1_%d" % i, [128, 512], F32) for i in range(2)]
                        t2 = [P.sb(name + "t2_%d" % i, [128, 512], F32) for i in range(2)]
                        Bt1 = bufs(name + "t1", 2)
                        Bt2 = bufs(name + "t2", 2)
                        rrot = Rot([0, 1])
                        brot = Rot([0, 1, 2, 3, 4, 5, 6, 7])
                        for G in range(NG):
                            if STOP_AFTER == name + "w":
                                break
                            for (lf, df, Bd) in chunks:
                                bk = brot.next()
                                proj_fm(lf, 128, G, bk, [BW])
                                k = rrot.next()
                                P.cp("act", raw[k][:], banks[bk][:], [Bbank[bk]], [Braw[k]])
                                bk2 = brot.next()
                                P.mm(banks[bk2][:], pm64[:], raw[k][:], True, True, [Bc, Braw[k]], [Bbank[bk2]])
                                P.tt("dve", t1[k][:], raw[k][:], cos64[:, gsl(G)], ALU.mult, [Braw[k], Btab], [Bt1[k]])
                                P.tt("dve", t2[k][:], banks[bk2][:], sin64[:, gsl(G)], ALU.mult, [Bbank[bk2], Btab], [Bt2[k]])
                                if isinstance(df, tuple) and df[0] == "kiz":
                                    P.tt("dve", KIT[0:64, 0, gsl(G)], t1[k][0:64, :], t2[k][0:64, :], ALU.add, [Bt1[k], Bt2[k]], [Bd[G]])
                                    P.tt("dve", KIT[64:128, 1, gsl(G)], t1[k][64:128, :], t2[k][64:128, :], ALU.add, [Bt1[k], Bt2[k]], [Bd[G]])
                                elif isinstance(df, tuple):
                                    g_ = df[1]
                                    P.tt("dve", kz[0:64, g_, 0, gsl(G)], t1[k][0:64, :], t2[k][0:64, :], ALU.add, [Bt1[k], Bt2[k]], [Bd[G]])
                                    P.tt("dve", kz[64:128, g_, 1, gsl(G)], t1[k][64:128, :], t2[k][64:128, :], ALU.add, [Bt1[k], Bt2[k]], [Bd[G]])
                                else:
                                    P.tt("dve", df(G), t1[k][:], t2[k][:], ALU.add, [Bt1[k], Bt2[k]], [Bd[G]])
                        BV1 = NB(name + "V1")
                        if STOP_AFTER == name + "rope":
                            return BV1
                        P.memset("dve", Vx[:, :, :, 64:VP], 1.0, [BV1])
                        for tt in range(NT):
                            bk = brot.next()
                            for kc in range(KC):
                                P.mm(banks[bk][:, 0:128], xT[:, kc, tsl(tt)], Wv[:, kc, :], kc == 0, kc == KC - 1, [BxT[tt], BW], [Bbank[bk]])
                            if xw is None and EXPER == "A":
                                for kc in range(KC):
                                    P.mm(banks[bk][:, 128:132], xT[:, kc, tsl(tt)], Wv[:, kc, 0:4], kc == 0, kc == KC - 1, [BxT[tt], BW], [Bbank[bk]])
                            if xw is not None:
                                Wwi, widx, Bwidx = xw
                                for kc in range(KC):
                                    P.mm(banks[bk][:, 128:132], xT[:, kc, tsl(tt)], Wwi[:, kc, :], kc == 0, kc == KC - 1, [BxT[tt], BW], [Bbank[bk]])
                                P.act(widx[:, tt, :], banks[bk][:, 128:132], AF.Copy, [Bbank[bk]], [Bwidx[tt]], scale=1.0 / 16.0)
                            P.cp("act", Vx[:, tt, :, 0:64], banks[bk][:, 0:128].rearrange("p (g d) -> p g d", d=64), [Bbank[bk]], [BVx[tt]])
                        return BV1

                with Scope(P):
                    if SKIP_DSA:
                        raise_skip = True
                    QTb = P.sb("QTb", [128, 4, T], BF16)
                    BQTb = bufs("QTb", NG)
                    KTb = P.sb("KTbz", [128, 2, 2, T], BF16)
                    BKTb = bufs("KTb", NG)
                    Bkz = NB("b_kz")
                    P.memset("dve", KTb[64:128, :, 0, :], 0.0, [Bkz])
                    P.memset("dve", KTb[0:64, :, 1, :], 0.0, [Bkz])
                    Vb = P.sb("Vb", [128, NT, 2, VP], BF16)
                    BVb = bufs("Vb", NT)
                    QIT = P.sb("QIT", [128, 2, T], BF16)
                    BQIT = bufs("QIT", NG)
                    KIT = P.sb("KITz", [128, 2, T], BF16)
                    BKIT = bufs("KIT", NG)
                    Bkiz = NB("b_kiz")
                    P.memset("dve", KIT[64:128, 0, :], 0.0, [Bkiz])
                    P.memset("dve", KIT[0:64, 1, :], 0.0, [Bkiz])
                    widx = P.sb("widx", [128, NT, 4], F32)
                    Bwidx = bufs("widx", NT)

                    def extra_b(BW, chunks):
                        Wqi = P.sb("Wqi", [128, KC, 256], BF16)
                        Wki2 = P.sb("Wki2", [128, KC, 128], BF16)
                        Wwi = P.sb("Wwi", [128, KC, 4], BF16)
                        P.ld("pool", Wqi[:], w_in_v[:, :, 1440:1696], [], [BW])
                        for i in range(2):
                            P.ld("pool", Wki2[:, :, i * 64:(i + 1) * 64], w_in_v[:, :, 1696:1760], [], [BW])
                        P.ld("pool", Wwi[:], w_in_v[:, :, 1760:1764], [], [BW])
                        for c in range(2):
                            chunks.append((lambda kc, c=c: Wqi[:, kc, c * 128:(c + 1) * 128], lambda G, c=c: QIT[:, c, gsl(G)], BQIT))
                        chunks.append((lambda kc: Wki2[:, kc, :], ("kiz",), BKIT))
                        return (Wwi, widx, Bwidx)

                    if not SKIP_DSA:
                        BVb1 = hd64_proj("b_", 672, 1184, 1312, QTb, BQTb, None, BKTb, Vb, BVb, extra_b, kz=KTb)
                    if not SKIP_DSA:
                        score32 = [P.sb("score32_%d" % i, [128, 512], F32) for i in range(2)]
                        Bs32 = bufs("score32_", 2)
                        s32rot = Rot([0, 1])
                        scorebf = [P.sb("scorebf%d" % i, [128, T], BF16) for i in range(4)]
                        Bsbf = bufs("scorebf", 4)
                        mbias = [P.sb("mbias%d" % i, [128, 4, T], BF16) for i in range(2)]
                        Bmb = [bufs("mbias%d_" % i, 4) for i in range(2)]
                        rr = [P.sb("rr%d" % i, [128, 512], F32) for i in range(2)]
                        Brr = bufs("rr", 2)
                        rrot = Rot([0, 1])
                        bis = P.sb("bis", [128, 16], F32)
                        Bmid = NB("bis_mid")
                        Bval = bufs("bis_val", 4)
                        Btmp = NB("bis_tmp")
                        Bthr = NB("bis_thr")
                        pt = [P.sb("b_pt%d" % i, [128, 512], BF16) for i in range(3)]
                        Bpt = bufs("b_pt", 3)
                        ptrot = Rot([0, 1, 2])
                        srot = Rot([0, 1, 2])
                        arot = Rot([3, 4])
                        irot = Rot([5, 6, 7])
                        otg = [P.sb("b_otg%d" % i, [128, 4, 512], BF16) for i in range(2)]
                        Botg = [bufs("b_otg%d_" % i, 4) for i in range(2)]
                        rec = [P.sb("b_rec%d" % i, [128, 4], F32) for i in range(2)]
                        Brec = bufs("b_rec", 2)
                        recrot = Rot([0, 1])

                        def topk_gen(G):
                            nSs = [(4 * G + j + 1) * 128 for j in range(4)]
                            for j in range(4):
                                qt = 4 * G + j
                                nS = nSs[j]
                                nsc = (nS + 511) // 512
                                for sc in range(nsc):
                                    ncol = min(512, nS - sc * 512)
                                    cols = slice(sc * 512, sc * 512 + ncol)
                                    sk = s32rot.next()
                                    s32 = score32[sk]
                                    for ih in range(4):
                                        c, i = ih // 2, ih % 2
                                        bk = irot.next()
                                        P.mm(banks[bk][:, 0:ncol], QIT[:, c, tsl(qt)], KIT[:, i, cols], True, True,
                                             [BQIT[G], BKIT[sc], Bkiz], [Bbank[bk]])
                                        rk = rrot.next()
                                        r = rr[rk]
                                        P.act(r[:, 0:ncol], banks[bk][:, 0:ncol], AF.Relu, [Bbank[bk]], [Brr[rk]])
                                        if ih == 0:
                                            P.ts("dve", s32[:, 0:ncol], r[:, 0:ncol], widx[:, qt, 0:1], None, ALU.mult, [Brr[rk], Bwidx[qt]], [Bs32[sk]])
                                        elif ih < 3:
                                            P.stt("dve", s32[:, 0:ncol], r[:, 0:ncol], widx[:, qt, ih:ih + 1], s32[:, 0:ncol], ALU.mult, ALU.add,
                                                  [Brr[rk], Bwidx[qt], Bs32[sk]], [Bs32[sk]])
                                        else:
                                            if sc == nsc - 1:
                                                P.tt("dve", s32[:, ncol - 128:ncol], s32[:, ncol - 128:ncol], negdiag[:], ALU.add, [Bs32[sk], Bc], [Bs32[sk]])
                                            P.stt("dve", scorebf[j][:, cols], r[:, 0:ncol], widx[:, qt, 3:4], s32[:, 0:ncol], ALU.mult, ALU.add,
                                                  [Brr[rk], Bwidx[qt], Bs32[sk]], [Bsbf[j]])
                                    yield
                            P.memset("dve", bis[:, 0:4], 0.0, [Bmid])
                            w = W0
                            for it in range(NIT):
                                for j in (2, 3):
                                    P.act(mbias[G % 2][:, j, 0:nSs[j]], scorebf[j][:, 0:nSs[j]], AF.Sign, [Bsbf[j], Bmid], [Bmb[G % 2][j], Bval[j]],
                                          bias=bis[:, j:j + 1], scale=-1.0, accum=bis[:, 4 + j:5 + j])
                                for j in (0, 1):
                                    P.ts("dve", mbias[G % 2][:, j, 0:nSs[j]], scorebf[j][:, 0:nSs[j]], bis[:, j:j + 1], None, ALU.is_ge, [Bsbf[j], Bmid], [Bmb[G % 2][j], Bval[j]],
                                         op1=ALU.add, accum=bis[:, 4 + j:5 + j])
                                P.ts("dve", bis[:, 8:10], bis[:, 4:6], 256.0, w, ALU.is_ge, [Bval[0], Bval[1]], [Btmp], op1=ALU.mult)
                                for j in (2, 3):
                                    P.ts("dve", bis[:, 8 + j:9 + j], bis[:, 4 + j:5 + j], float(nSs[j] - 512), w, ALU.is_le, [Bval[j]], [Btmp], op1=ALU.mult)
                                P.stt("dve", bis[:, 0:4], bis[:, 8:12], -w / 2, bis[:, 0:4], ALU.add, ALU.add, [Btmp, Bmid], [Bmid])
                                w = w / 2
                                yield
                            P.ts("dve", bis[:, 12:16], bis[:, 0:4], -w, None, ALU.add, [Bmid], [Bthr])
                            for j in range(4):
                                nS = nSs[j]
                                P.ts("dve", mbias[G % 2][:, j, 0:nS], scorebf[j][:, 0:nS], bis[:, 12 + j:13 + j], -30000.0, ALU.is_lt,
                                     [Bsbf[j], Bthr], [Bmb[G % 2][j]], op1=ALU.mult)
                                yield

                        def attn_gen(G):
                            mb = mbias[G % 2]
                            Bm = Bmb[G % 2]
                            og = otg[G % 2]
                            Bog = Botg[G % 2]
                            blocks = [(h, st_) for h in range(8) for st_ in range(4 * G + 4)]
                            info = {}

                            def qk(k):
                                h, st_ = blocks[k]
                                c, i, g = h // 2, h % 2, h // 4
                                j0 = max(0, st_ - 4 * G)
                                ncol = (4 - j0) * 128
                                q0 = (4 * G + j0) * 128
                                sb_ = srot.next()
                                P.mm(banks[sb_][:, 0:ncol], KTb[:, g, i, tsl(st_)], QTb[:, c, q0:q0 + ncol], True, True,
                                     [BKTb[st_ // 4], BQTb[G], Bkz], [Bbank[sb_]])
                                for j in range(j0, 4):
                                    P.mm(banks[sb_][:, (j - j0) * 128:(j - j0 + 1) * 128], mb[:, j, tsl(st_)], ident_bf[:], False, j == 3,
                                         [Bm[j], Bc], [Bbank[sb_]], sgc=True)
                                info[k] = (sb_, j0, ncol)

                            qk(0)
                            ab = None
                            for k, (h, st_) in enumerate(blocks):
                                g = h // 4
                                if k + 1 < len(blocks):
                                    qk(k + 1)
                                sb_, j0, ncol = info[k]
                                if st_ == 0:
                                    ab = arot.next()
                                accv = banks[ab][:, 0:4 * VP].rearrange("p (j d) -> p j d", d=VP)
                                pk = ptrot.next()
                                P.act(pt[pk][:, 0:ncol], banks[sb_][:, 0:ncol], AF.Exp, [Bbank[sb_]], [Bpt[pk]], scale=0.125)
                                for j in range(j0, 4):
                                    P.mm(accv[:, j, 0:65], pt[pk][:, (j - j0) * 128:(j - j0 + 1) * 128], Vb[:, st_, g, 0:65],
                                         (st_ == 0 and j == 0), st_ == 4 * G + j, [Bpt[pk], BVb[st_], BVb1], [Bbank[ab]], sgc=True)
                                if st_ == 4 * G + 3:
                                    rk = recrot.next()
                                    P.recip(rec[rk][:], accv[:, :, 64], [Bbank[ab]], [Brec[rk]])
                                    for j in range(4):
                                        P.ts("dve", og[:, j, h * 64:(h + 1) * 64], accv[:, j, 0:64], rec[rk][:, j:j + 1], None, ALU.mult,
                                             [Bbank[ab], Brec[rk]], [Bog[j]])
                                yield
                            for j in range(4):
                                tt = 4 * G + j
                                bk = irot.next()
                                bv = banks[bk][:].bitcast(BF16)
                                for c in range(4):
                                    P.tr(bv[:, c * 128:(c + 1) * 128], og[:, j, c * 128:(c + 1) * 128], ident_bf[:], [Bog[j], Bc], [Bbank[bk]])
                                P.cp("act", oT[1][:, :, tsl(tt)], bv[:, 0:512].rearrange("p (c t) -> p c t", t=128), [Bbank[bk]], [BoT[1][G]])
                            yield

                        for _ in topk_gen(0):
                            pass
                        for G in range(NG):
                            ag = list_len = None
                            a_units = 8 * (4 * G + 4) + 1
                            if G + 1 < NG:
                                t_units = sum(((4 * (G + 1) + j + 1) * 128 + 511) // 512 for j in range(4)) + NIT + 4
                                tg = topk_gen(G + 1)
                            else:
                                t_units = 0
                                tg = None
                            done_t = 0
                            for ai, _ in enumerate(attn_gen(G)):
                                if tg is not None:
                                    want = ((ai + 1) * t_units) // a_units
                                    while done_t < want:
                                        try:
                                            next(tg)
                                        except StopIteration:
                                            tg = None
                                            break
                                        done_t += 1
                            if tg is not None:
                                for _ in tg:
                                    pass
                dump("d_oTb", oT[1][:], BoT[1])
                if STOP_AFTER == "dsa":
                    return
                with Scope(P):
                    QTc = P.sb("QTc", [128, 4, T], BF16)
                    BQTc = bufs("QTc", NG)
                    KTc = P.sb("KTc", [128, 2, T], BF16)
                    BKTc = bufs("KTc", NG)
                    Vc = P.sb("Vc", [128, NT, 2, VP], BF16)
                    BVc = bufs("Vc", NT)
                    BVc1 = hd64_proj("c_", 1764, 2276, 2404, QTc, BQTc, KTc, BKTc, Vc, BVc, None)
                    if STOP_AFTER in ("swa_proj", "c_rope", "c_w"):
                        return
                    esink = P.sb("esink", [128, 8], F32)
                    Besink = NB("esink")
                    P.ld("sp", esink[:], dr["c_sinks"][l].partition_broadcast(128), [], [Besink])
                    P.act(esink[:], esink[:], AF.Exp, [Besink], [Besink])
                    smask = P.sb("smask", [128, 2, 2, 128], BF16)
                    Bsm = NB("smask")
                    for i in range(2):
                        P.cp("dve", smask[:, i, 0, :], swaprev_bf[:], [Bc], [Bsm])
                        P.cp("dve", smask[:, i, 1, :], diag_bf[:], [Bc], [Bsm])
                    ptc = [P.sb("c_pt%d" % i, [128, 2, 2, 128], BF16) for i in range(3)]
                    Bptc = bufs("c_pt", 3)
                    ptrot = Rot([0, 1, 2])
                    srot = Rot([0, 1, 2])
                    arot = Rot([3, 4])
                    irot = Rot([5, 6, 7])
                    otc = [P.sb("c_ot%d" % i, [128, 512], BF16) for i in range(2)]
                    Botc = bufs("c_ot", 2)
                    den = [P.sb("c_den%d" % i, [128, 2], F32) for i in range(2)]
                    Bden = bufs("c_den", 2)
                    drot = Rot([0, 1])
                    srot = Rot([0, 1, 2, 5])
                    irot = Rot([6, 7])
                    blocks = [(qt, c) for qt in range(NT) for c in range(4)]
                    info = {}

                    def qk_c(k):
                        qt, c = blocks[k]
                        g = c // 2
                        u0 = 0 if qt > 0 else 1
                        sbs = []
                        for i in range(2):
                            sb_ = srot.next()
                            sv = banks[sb_][:, 0:256].rearrange("p (u t) -> p u t", u=2)
                            for u in range(u0, 2):
                                st_ = qt - 1 + u
                                P.mm(sv[:, u, :], KTc[i * 64:(i + 1) * 64, g, tsl(st_)], QTc[i * 64:(i + 1) * 64, c, tsl(qt)], True, True,
                                     [BKTc[st_ // 4], BQTc[qt // 4]], [Bbank[sb_]])
                                P.mm(sv[:, u, :], (negprev_bf if u == 0 else negdiag_bf)[:], ident_bf[:], False, True, [Bc], [Bbank[sb_]], sgc=True)
                            sbs.append((sb_, sv))
                        info[k] = sbs

                    qk_c(0)
                    for k, (qt, c) in enumerate(blocks):
                        if k + 1 < len(blocks):
                            qk_c(k + 1)
                        g = c // 2
                        u0 = 0 if qt > 0 else 1
                        ok = qt % 2
                        pk = ptrot.next()
                        for i in range(2):
                            sb_, sv = info[k][i]
                            P.act(ptc[pk][:, i, u0:2, :], sv[:, u0:2, :], AF.Exp, [Bbank[sb_]], [Bptc[pk]], scale=0.125)
                        ab = arot.next()
                        accv = banks[ab][:, 0:2 * VP].rearrange("p (i d) -> p i d", d=VP)
                        for i in range(2):
                            for u in range(u0, 2):
                                st_ = qt - 1 + u
                                P.mm(accv[:, i, 0:65], ptc[pk][:, i, u, :], Vc[:, st_, g, 0:65], (u == u0 and i == 0), u == 1, [Bptc[pk], BVc[st_], BVc1], [Bbank[ab]], sgc=True)
                        dk = drot.next()
                        P.tt("dve", den[dk][:], accv[:, :, 64], esink[:, 2 * c:2 * c + 2], ALU.add, [Bbank[ab], Besink], [Bden[dk]])
                        P.recip(den[dk][:], den[dk][:], [Bden[dk]], [Bden[dk]])
                        for i in range(2):
                            h = 2 * c + i
                            P.ts("dve", otc[ok][:, h * 64:(h + 1) * 64], accv[:, i, 0:64], den[dk][:, i:i + 1], None, ALU.mult,
                                 [Bbank[ab], Bden[dk]], [Botc[ok]])
                        if c == 3:
                            bk = irot.next()
                            bv = banks[bk][:].bitcast(BF16)
                            for c2 in range(4):
                                P.tr(bv[:, c2 * 128:(c2 + 1) * 128], otc[ok][:, c2 * 128:(c2 + 1) * 128], ident_bf[:], [Botc[ok], Bc], [Bbank[bk]])
                            P.cp("act", oT[2][:, :, tsl(qt)], bv[:, 0:512].rearrange("p (c t) -> p c t", t=128), [Bbank[bk]], [BoT[2][qt // 4]])
                dump("d_oTc", oT[2][:], BoT[2])
                if STOP_AFTER == "swa":
                    return
                with Scope(P):
                    wout = P.sb("wout", [128, KC, D], BF16)
                    Bwout = NB("wout")
                    P.ld("pool", wout[:], dr["w_out"][l].rearrange("(kc p) c -> p kc c", p=128), [], [Bwout])
                    g1B = P.sb("g1B", [128, D], F32)
                    b1B = P.sb("b1B", [128, D], F32)
                    Bg1 = NB("g1b1")
                    P.ld("sp", g1B[:], dr["ln1_g"][l].partition_broadcast(128), [], [Bg1])
                    P.ld("sp", b1B[:], dr["ln1_b"][l].partition_broadcast(128), [], [Bg1])
                    wr = P.sb("wr", [128, KC, 16], F32)
                    Bwr = NB("wr")
                    P.ld("sp", wr[:], dr["w_router"].rearrange("(kc p) e -> p kc e", p=128), [], [Bwr])
                    wbr = [P.sb("wbr%d" % i, [128, 3, 4, 128], BF16) for i in range(2)]
                    wgt = [P.sb("wgt%d" % i, [128, 3, KC, 128], BF16) for i in range(2)]
                    Bwm = bufs("wm", 2)
                    mg = P.sb("mg", [128, KC, T], BF16)
                    Bmg = [bufs("mg%d_" % i, NG) for i in range(KC)]
                    sg = [P.sb("sg%d" % i, [128, 512], F32) for i in range(2)]
                    Bsg = bufs("sg", 2)
                    sgrot = Rot([0, 1])
                    macc = P.sb("macc", [128, 512], F32)
                    Bmacc = NB("macc")
                    pre = [P.sb("m_pre%d" % i, [128, D], F32) for i in range(4)]
                    Bpre = bufs("m_pre", 4)
                    st1 = P.sb("m_st", [128, 8, 4], F32)
                    Bsum1 = bufs("m_sum", 4)
                    Bsq1 = bufs("m_sq", 4)
                    Bvec1 = NB("m_vec")
                    junks = [P.sb("m_junk%d" % i, [128, D], BF16)[:] for i in range(2)]
                    Bjunks = bufs("m_junk", 2)
                    xbf = [P.sb("m_xbf%d" % i, [128, D], BF16)[:] for i in range(2)]
                    Bxbf = bufs("m_xbf", 2)
                    x1Tf = P.sb("x1Tf", [128, KC, 128], F32)
                    Bx1Tf = NB("x1Tf")
                    brot = Rot([0, 1, 2, 3, 4, 5, 6, 7])
                    wbr_src = [dr[n][l].rearrange("(kc p) c -> p kc c", p=128) for n in ("w_br_a", "w_br_b", "w_br_c")]
                    def ld_merge(dc):
                        k = dc % 2
                        for i in range(3):
                            P.ld("pool", wbr[k][:, i, :, :], wbr_src[i][:, :, dc * 128:(dc + 1) * 128], [], [Bwm[k]])
                            P.ld("pool", wgt[k][:, i, :, :], w_in_v[:, :, 2532 + i * 1024 + dc * 128: 2532 + i * 1024 + (dc + 1) * 128], [], [Bwm[k]])

                    ld_merge(0)
                    for dc in range(KC):
                        k = dc % 2
                        if dc + 1 < KC:
                            ld_merge(dc + 1)
                        for G in range(NG):
                            for i in range(3):
                                yb = brot.next()
                                for kc in range(4):
                                    P.mm(banks[yb][:], wbr[k][:, i, kc, :], oT[i][:, kc, gsl(G)], kc == 0, kc == 3, [Bwm[k], BoT[i][G]], [Bbank[yb]])
                                gb = brot.next()
                                for kc in range(KC):
                                    P.mm(banks[gb][:], wgt[k][:, i, kc, :], xT[:, kc, gsl(G)], kc == 0, kc == KC - 1,
                                         [Bwm[k]] + [BxT[4 * G + j] for j in range(4)], [Bbank[gb]])
                                sk = sgrot.next()
                                P.act(sg[sk][:], banks[gb][:], AF.Sigmoid, [Bbank[gb]], [Bsg[sk]])
                                if i == 0:
                                    P.tt("dve", macc[:], sg[sk][:], banks[yb][:], ALU.mult, [Bsg[sk], Bbank[yb]], [Bmacc])
                                elif i == 1:
                                    P.tt("dve", sg[sk][:], sg[sk][:], banks[yb][:], ALU.mult, [Bsg[sk], Bbank[yb]], [Bsg[sk]])
                                    P.tt("dve", macc[:], macc[:], sg[sk][:], ALU.add, [Bmacc, Bsg[sk]], [Bmacc])
                                else:
                                    P.tt("dve", sg[sk][:], sg[sk][:], banks[yb][:], ALU.mult, [Bsg[sk], Bbank[yb]], [Bsg[sk]])
                                    P.tt("dve", mg[:, dc, gsl(G)], macc[:], sg[sk][:], ALU.add, [Bmacc, Bsg[sk]], [Bmg[dc][G]])
                    NR = 4
                    sco = P.sb("r_sco", [128, NR, 16], F32)
                    bia = P.sb("r_bia", [128, NR, 16], F32)
                    mb = P.sb("r_mb", [128, NR, 16], F32)
                    rbias = P.sb("r_bias", [128, 16], F32)
                    ps6 = P.sb("r_ps6", [128, 6, NR, 4], F32)
                    gs = P.sb("r_gs", [128, NR, 4], F32)
                    gmax = P.sb("r_gmax", [128, NR], F32)
                    ing = P.sb("r_ing", [128, NR, 4], F32)
                    red = P.sb("r_red", [128, NR, 8], F32)
                    tmax = P.sb("r_tmax", [128, NR], F32)
                    e1 = P.sb("r_e1", [128, NR, 16], F32)
                    e2 = P.sb("r_e2", [128, NR, 16], F32)
                    Br = NB("routing")
                    Brb = NB("rbias")
                    P.ld("sp", rbias[:], dr["router_bias"].partition_broadcast(128), [], [Brb])

                    def route(t0):
                        R = [Br, Brb] + Blogit[t0:t0 + NR]
                        P.act(sco[:], logit[:, t0:t0 + NR, :], AF.Sigmoid, R, [Br])
                        for tt in range(NR):
                            P.tt("dve", bia[:, tt, :], sco[:, tt, :], rbias[:], ALU.add, [Br, Brb], [Br])
                        bg = bia[:].rearrange("p t (g e) -> p t g e", e=4)
                        pairs = [(0, 1), (0, 2), (0, 3), (1, 2), (1, 3), (2, 3)]
                        for pi, (a, b_) in enumerate(pairs):
                            P.tt("dve", ps6[:, pi, :, :], bg[:, :, :, a], bg[:, :, :, b_], ALU.add, [Br], [Br])
                        P.tt("dve", gs[:], ps6[:, 0, :, :], ps6[:, 1, :, :], ALU.max, [Br], [Br])
                        for pi in range(2, 6):
                            P.tt("dve", gs[:], gs[:], ps6[:, pi, :, :], ALU.max, [Br], [Br])
                        P.tt("dve", gmax[:], gs[:, :, 0], gs[:, :, 1], ALU.max, [Br], [Br])
                        P.tt("dve", gmax[:], gmax[:], gs[:, :, 2], ALU.max, [Br], [Br])
                        P.tt("dve", gmax[:], gmax[:], gs[:, :, 3], ALU.max, [Br], [Br])
                        for g in range(4):
                            P.tt("dve", ing[:, :, g], gs[:, :, g], gmax[:], ALU.is_equal, [Br], [Br])
                        P.ts("dve", ing[:], ing[:], -1.0, 1e30, ALU.add, [Br], [Br], op1=ALU.mult)
                        mbg = mb[:].rearrange("p t (g e) -> p t g e", e=4)
                        for e_ in range(4):
                            P.tt("dve", mbg[:, :, :, e_], bg[:, :, :, e_], ing[:], ALU.add, [Br], [Br])

                        def max16(dst, src):
                            P.tt("dve", red[:, :, 0:8], src[:, :, 0:8], src[:, :, 8:16], ALU.max, [Br], [Br])
                            P.tt("dve", red[:, :, 0:4], red[:, :, 0:4], red[:, :, 4:8], ALU.max, [Br], [Br])
                            P.tt("dve", red[:, :, 0:2], red[:, :, 0:2], red[:, :, 2:4], ALU.max, [Br], [Br])
                            P.tt("dve", dst, red[:, :, 0], red[:, :, 1], ALU.max, [Br], [Br])

                        max16(tmax[:], mb)
                        for tt in range(NR):
                            P.ts("dve", e1[:, tt, :], mb[:, tt, :], tmax[:, tt:tt + 1], None, ALU.is_equal, [Br], [Br])
                        P.stt("dve", mb[:], e1[:], -1e30, mb[:], ALU.mult, ALU.add, [Br], [Br])
                        max16(tmax[:], mb)
                        for tt in range(NR):
                            P.ts("dve", e2[:, tt, :], mb[:, tt, :], tmax[:, tt:tt + 1], None, ALU.is_equal, [Br], [Br])
                        P.tt("dve", e1[:], e1[:], e2[:], ALU.add, [Br], [Br])
                        P.tt("dve", e1[:], e1[:], sco[:], ALU.mult, [Br], [Br])
                        P.tt("dve", red[:, :, 0:8], e1[:, :, 0:8], e1[:, :, 8:16], ALU.add, [Br], [Br])
                        P.tt("dve", red[:, :, 0:4], red[:, :, 0:4], red[:, :, 4:8], ALU.add, [Br], [Br])
                        P.tt("dve", red[:, :, 0:2], red[:, :, 0:2], red[:, :, 2:4], ALU.add, [Br], [Br])
                        P.tt("dve", tmax[:], red[:, :, 0], red[:, :, 1], ALU.add, [Br], [Br])
                        P.recip(tmax[:], tmax[:], [Br], [Br])
                        for tt in range(NR):
                            P.ts("dve", gate[:, t0 + tt, :], e1[:, tt, :], tmax[:, tt:tt + 1], None, ALU.mult, [Br], [Bgate])

                    for G in range(NG):
                        items = []
                        for j in range(4):
                            tt = 4 * G + j
                            P.ld("sp", pre[j][:], xres[tsl(tt), :], [Bxres[tt]], [Bpre[j]])
                            items.append((pre[j][:], Bpre[j]))
                        for j in range(4):
                            tt = 4 * G + j
                            for half in range(2):
                                ob = brot.next()
                                for kc in range(KC):
                                    P.mm(banks[ob][:], mg[:, kc, tsl(tt)], wout[:, kc, half * 512:(half + 1) * 512], kc == 0, kc == KC - 1,
                                         [Bmg[kc][G], Bwout], [Bbank[ob]])
                                hs = slice(half * 512, (half + 1) * 512)
                                P.stt("dve", pre[j][:, hs], pre[j][:, hs], ALPHA, banks[ob][:], ALU.mult, ALU.add, [Bpre[j], Bbank[ob]], [Bpre[j]])
                        ln_batch(items, g1B[:], b1B[:], Bg1, st1, Bsum1, Bsq1, Bvec1, junks, Bjunks)
                        for j in range(4):
                            tt = 4 * G + j
                            P.st("sp", xres[tsl(tt), :], pre[j][:], [Bpre[j]], [Bxres[tt]], key=Bpre[j])
                        to_xT_batch(items, [4 * G + j for j in range(4)], xbf, Bxbf, [brot.next() for _ in range(4)])
                        for j in range(4):
                            tt = 4 * G + j
                            tb0 = brot.next()
                            tb1 = brot.next()
                            for kc in range(KC):
                                tb = tb0 if kc < 4 else tb1
                                P.tr(banks[tb][:, (kc % 4) * 128:(kc % 4 + 1) * 128], pre[j][:, kc * 128:(kc + 1) * 128], ident_f[:], [Bpre[j], Bc], [Bbank[tb]])
                            P.cp("act", x1Tf[:, 0:4, :], banks[tb0][:].rearrange("p (k t) -> p k t", t=128), [Bbank[tb0]], [Bx1Tf])
                            P.cp("act", x1Tf[:, 4:8, :], banks[tb1][:].rearrange("p (k t) -> p k t", t=128), [Bbank[tb1]], [Bx1Tf])
                            lb = brot.next()
                            for kc in range(KC):
                                P.mm(banks[lb][:, 0:16], x1Tf[:, kc, :], wr[:, kc, :], kc == 0, kc == KC - 1, [Bx1Tf, Bwr], [Bbank[lb]])
                            P.cp("dve", logit[:, tt, :], banks[lb][:, 0:16], [Bbank[lb]], [Blogit[tt]])
                        if G >= 1:
                            route(4 * (G - 1))
                    route(4 * (NG - 1))
            if STOP_AFTER == "ln1":
                return
            with Scope(P):
                dump("d_gate", gate[:], [Bgate])
                if STOP_AFTER == "route":
                    return
                acc = P.sb("acc", [128, NT, D], F32)
                Bacc = bufs("acc", NT)
                with Scope(P):
                    Wg = [P.sb("Wg%d" % i, [128, KC, 512], BF16) for i in range(2)]
                    Wu = [P.sb("Wu%d" % i, [128, KC, 512], BF16) for i in range(2)]
                    Wd = [P.sb("Wd%d" % i, [128, 4, D], BF16) for i in range(2)]
                    BWe = bufs("We", 2)
                    actT = [P.sb("actT%d" % i, [128, 4, 512], BF16) for i in range(2)]
                    BactT = [bufs("actT%d_" % i, 4) for i in range(2)]
                    sgm = [P.sb("sgm%d" % i, [128, 512], BF16) for i in range(2)]
                    Bsgm = bufs("sgm", 2)
                    sgrot = Rot([0, 1])
                    hrot = Rot([0, 1, 2, 3])
                    orot = Rot([4, 5, 6, 7])
                    ai = 0
                    for e_ in range(16):
                        k = e_ % 2
                        P.ld("pool", Wg[k][:], dr["w_exp_gate"][l, e_].rearrange("(kc p) f -> p kc f", p=128), [], [BWe[k]])
                        P.ld("pool", Wu[k][:], dr["w_exp_up"][l, e_].rearrange("(kc p) f -> p kc f", p=128), [], [BWe[k]])
                        P.ld("pool", Wd[k][:], dr["w_exp_down"][l, e_].rearrange("(kc p) f -> p kc f", p=128), [], [BWe[k]])
                        for G in range(NG):
                            a = ai % 2
                            ai += 1
                            xr_ = [BxT[4 * G + j] for j in range(4)]
                            for fc in range(4):
                                hb = hrot.next()
                                for kc in range(KC):
                                    P.mm(banks[hb][:], Wg[k][:, kc, fc * 128:(fc + 1) * 128], xT[:, kc, gsl(G)], kc == 0, kc == KC - 1, [BWe[k]] + xr_, [Bbank[hb]])
                                ub = hrot.next()
                                for kc in range(KC):
                                    P.mm(banks[ub][:], Wu[k][:, kc, fc * 128:(fc + 1) * 128], xT[:, kc, gsl(G)], kc == 0, kc == KC - 1, [BWe[k]] + xr_, [Bbank[ub]])
                                sk = sgrot.next()
                                P.act(sgm[sk][:], banks[hb][:], AF.Silu, [Bbank[hb]], [Bsgm[sk]])
                                P.tt("dve", actT[a][:, fc, :], sgm[sk][:], banks[ub][:], ALU.mult, [Bsgm[sk], Bbank[ub]], [BactT[a][fc]])
                            for j in range(4):
                                tt = 4 * G + j
                                for half in range(2):
                                    ob = orot.next()
                                    for fc in range(4):
                                        P.mm(banks[ob][:], actT[a][:, fc, j * 128:(j + 1) * 128], Wd[k][:, fc, half * 512:(half + 1) * 512], fc == 0, fc == 3,
                                             [BactT[a][fc], BWe[k]], [Bbank[ob]])
                                    dst = acc[:, tt, half * 512:(half + 1) * 512]
                                    if e_ == 0:
                                        P.ts("dve", dst, banks[ob][:], gate[:, tt, e_:e_ + 1], None, ALU.mult, [Bbank[ob], Bgate], [Bacc[tt]])
                                    else:
                                        P.stt("dve", dst, banks[ob][:], gate[:, tt, e_:e_ + 1], dst, ALU.mult, ALU.add, [Bbank[ob], Bgate, Bacc[tt]], [Bacc[tt]])
                with Scope(P):
                    g2B = P.sb("g2B", [128, D], F32)
                    b2B = P.sb("b2B", [128, D], F32)
                    Bg2 = NB("g2b2")
                    P.ld("sp", g2B[:], dr["ln2_g"][l].partition_broadcast(128), [], [Bg2])
                    P.ld("sp", b2B[:], dr["ln2_b"][l].partition_broadcast(128), [], [Bg2])
                    xr = [P.sb("f_xr%d" % i, [128, D], F32) for i in range(4)]
                    Bxr = bufs("f_xr", 4)
                    st2 = P.sb("f_st", [128, 8, 4], F32)
                    Bsum2 = bufs("f_sum", 4)
                    Bsq2 = bufs("f_sq", 4)
                    Bvec2 = NB("f_vec")
                    junks = [P.sb("f_junk%d" % i, [128, D], BF16)[:] for i in range(2)]
                    Bjunks = bufs("f_junk", 2)
                    xbf = [P.sb("f_xbf%d" % i, [128, D], BF16)[:] for i in range(2)]
                    Bxbf = bufs("f_xbf", 2)
                    Bstk = bufs("f_stkey", 4)
                    for t0 in range(0, NT, 4):
                        items = []
                        for i in range(4):
                            tt = t0 + i
                            P.ld("sp", xr[i][:], xres[tsl(tt), :], [Bxres[tt]], [Bxr[i]])
                        for i in range(4):
                            tt = t0 + i
                            P.stt("dve", acc[:, tt, :], xr[i][:], ALPHA, acc[:, tt, :], ALU.mult, ALU.add, [Bxr[i], Bacc[tt]], [Bacc[tt]])
                            items.append((acc[:, tt, :], Bacc[tt]))
                        ln_batch(items, g2B[:], b2B[:], Bg2, st2, Bsum2, Bsq2, Bvec2, junks, Bjunks)
                        for i in range(4):
                            tt = t0 + i
                            kb = Bstk[i]
                            if last:
                                final_events.append(P.st("sp", y[tsl(tt), :], acc[:, tt, :], [Bacc[tt]], [kb], key=kb))
                            else:
                                P.st("sp", xres[tsl(tt), :], acc[:, tt, :], [Bacc[tt]], [Bxres[tt], kb], key=kb)
                        if not last:
                            to_xT_batch(items, [t0 + i for i in range(4)], xbf, Bxbf, [0, 1, 2, 3])

        for li, l in enumerate(layers):
            layer(l, li == len(layers) - 1)

        dump("d_xT", xT[:], BxT)
        P.finish(final_events)
        print("build: sems", P.nsem, "ops", {e: len(P.ops[e]) for e in P.ENG})
    return nc


STOP_AFTER = None
EXPER = None
SKIP_DSA = False
SKIP_MLA = False
_NB = {}


def NB(name):
    if name not in _NB:
        _NB[name] = Buf(name)
    return _NB[name]


def prep_inputs(inputs):
    f32 = np.float32
    common = {}
    for k in WEIGHT_SHAPES:
        a = np.ascontiguousarray(np.asarray(inputs[k], dtype=f32))
        if k == "a_q_ln_g":
            a = np.ascontiguousarray(a.reshape(2, 3, 128).transpose(0, 2, 1))
        if k == "a_kv_ln_g":
            a = np.ascontiguousarray(a.reshape(2, 2, 128).transpose(0, 2, 1))
        common[k] = a
    common.update(make_consts())
    return common


_PROG_CACHE = {}


def get_prog(key, *a, **kw):
    if key not in _PROG_CACHE:
        _NB.clear()
        _PROG_CACHE[key] = build_program(*a, **kw)
    return _PROG_CACHE[key]


FUSED = True


def kernel(**inputs):
    x = np.ascontiguousarray(np.asarray(inputs["x"], dtype=np.float32))
    B = x.shape[0]
    common = prep_inputs(inputs)
    cores = list(range(B))
    if FUSED:
        nc = get_prog("fused", [0, 1], True, True)
        maps = [dict(common, x=x[b]) for b in cores]
        res = run_bass_kernel_spmd(nc, maps, core_ids=cores)
        return np.stack([res.results[b]["y"] for b in cores], axis=0).astype(np.float32)
    nc0 = get_prog("l0", [0], True, True)
    maps = [dict(common, x=x[b]) for b in cores]
    res = run_bass_kernel_spmd(nc0, maps, core_ids=cores)
    mid = [res.results[b]["y"] for b in cores]
    nc1 = get_prog("l1", [1], False, True)
    maps = [dict(common, x=np.ascontiguousarray(mid[b])) for b in cores]
    res = run_bass_kernel_spmd(nc1, maps, core_ids=cores)
    return np.stack([res.results[b]["y"] for b in cores], axis=0).astype(np.float32)
```
